# Optimizing a Trainium2 kernel written in Bass

```python
import jax, jax.numpy as jnp
from jax import lax
import numpy as np

D_MODEL = 1024
BATCH = 8
SEQ = 4096
DEPTH = 2

CTX_LEN = 256
GRID_W = 64
HEAD_DIM = 64
N_HEADS = 8
N_KV_HEADS = 2
GQA = N_HEADS // N_KV_HEADS
ATTN_WIDTH = N_HEADS * HEAD_DIM
KV_WIDTH = N_KV_HEADS * HEAD_DIM
WINDOW = 128
ATTN_BLOCK = 128
ATTN_SCALE = HEAD_DIM ** -0.5
ROPE_BASE = 10000.0
CONV_WIDTH = D_MODEL // 4
GM_WIDTH = D_MODEL // 4
GM_GROUPS = 4
GM_HEAD = GM_WIDTH // GM_GROUPS
CHUNK = 128
MIX_WIDTH = ATTN_WIDTH + CONV_WIDTH + GM_WIDTH
IN_WIDTH = ATTN_WIDTH + 2 * KV_WIDTH + 3 * CONV_WIDTH + 2 * GM_WIDTH
SPLIT_POINTS = (ATTN_WIDTH,
                ATTN_WIDTH + KV_WIDTH,
                ATTN_WIDTH + 2 * KV_WIDTH,
                ATTN_WIDTH + 2 * KV_WIDTH + CONV_WIDTH,
                ATTN_WIDTH + 2 * KV_WIDTH + 2 * CONV_WIDTH,
                ATTN_WIDTH + 2 * KV_WIDTH + 3 * CONV_WIDTH,
                ATTN_WIDTH + 2 * KV_WIDTH + 3 * CONV_WIDTH + GM_WIDTH)
N_GROUPS = 4
EXP_PER_GROUP = 8
N_EXPERTS = N_GROUPS * EXP_PER_GROUP
TOP_K = 2
D_EXPERT = D_MODEL // 2
MOE_BLOCK = 128
ALPHA = (2 * DEPTH) ** 0.25
BETA = (8 * DEPTH) ** -0.25
LN_EPS = 1e-6
NEG_INF = -1e30

kernel_name = 'hybrid_dit_conv_swa_gmlp_hmoe'


def layer_norm(x, g=None, b=None):
    xf = x.astype(jnp.float32)
    mu = jnp.mean(xf, axis=-1, keepdims=True)
    var = jnp.mean(jnp.square(xf - mu), axis=-1, keepdims=True)
    y = (xf - mu) * lax.rsqrt(var + LN_EPS)
    if g is not None:
        y = y * g.astype(jnp.float32) + b.astype(jnp.float32)
    return y.astype(x.dtype)


def _rope(x, pos):
    m = x.shape[-1] // 2
    freqs = ROPE_BASE ** (-jnp.arange(m, dtype=jnp.float32) / m)
    ang = pos.astype(jnp.float32)[:, None] * freqs[None, :]
    cos = jnp.cos(ang)[None, :, None, :]
    sin = jnp.sin(ang)[None, :, None, :]
    xf = x.astype(jnp.float32)
    a, b = xf[..., :m], xf[..., m:]
    return jnp.concatenate([a * cos - b * sin, b * cos + a * sin], axis=-1).astype(x.dtype)


def axial_rope(x, row, col):
    half = x.shape[-1] // 2
    return jnp.concatenate([_rope(x[..., :half], row), _rope(x[..., half:], col)], axis=-1)


def short_conv(u, w):
    up = jnp.pad(u, ((0, 0), (1, 1), (0, 0)))
    return up[:, :-2] * w[0] + up[:, 1:-1] * w[1] + up[:, 2:] * w[2]


def local_mixers(cb, cc, cx, gu, gv, conv_w, gm_ws, gm_bs):
    y_conv = cb * short_conv(cc * cx, conv_w)
    b_, l_ = gu.shape[:2]
    u = jax.nn.gelu(gu)
    v = layer_norm(jax.nn.gelu(gv)).reshape(b_, l_ // CHUNK, CHUNK, GM_GROUPS, GM_HEAD)
    s = jnp.einsum('gpq,bnqgc->bnpgc', gm_ws, v) + gm_bs.T[None, None, :, :, None]
    y_gm = u * s.reshape(b_, l_, GM_WIDTH).astype(u.dtype)
    return jnp.concatenate([y_conv, y_gm], axis=-1)


def sink_softmax(s, sink):
    col = jnp.broadcast_to(sink.astype(jnp.float32).reshape(N_KV_HEADS, GQA)[None, :, :, None, None],
                           s.shape[:-1] + (1,))
    return jax.nn.softmax(jnp.concatenate([col, s], axis=-1), axis=-1)[..., 1:]


def window_attention(q, k, v, k_ctx, v_ctx, sink):
    b_, s_ = q.shape[:2]
    nb = s_ // ATTN_BLOCK
    pad = ((0, 0), (ATTN_BLOCK, ATTN_BLOCK), (0, 0), (0, 0))
    kp, vp = jnp.pad(k, pad), jnp.pad(v, pad)
    span = 3 * ATTN_BLOCK
    rel = (jnp.arange(span)[None, :] - ATTN_BLOCK) - jnp.arange(ATTN_BLOCK)[:, None]
    band = jnp.abs(rel) <= WINDOW

    def one_block(n):
        start = n * ATTN_BLOCK
        qn = lax.dynamic_slice_in_dim(q, start, ATTN_BLOCK, axis=1).reshape(
            b_, ATTN_BLOCK, N_KV_HEADS, GQA, HEAD_DIM)
        kn = lax.dynamic_slice_in_dim(kp, start, span, axis=1)
        vn = lax.dynamic_slice_in_dim(vp, start, span, axis=1)
        kpos = start - ATTN_BLOCK + jnp.arange(span)
        valid = band & ((kpos >= 0) & (kpos < s_))[None, :]
        s_loc = jnp.einsum('bqkgd,bskd->bkgqs', qn, kn, preferred_element_type=jnp.float32) * ATTN_SCALE
        s_loc = jnp.where(valid, s_loc, NEG_INF)
        s_ctx = jnp.einsum('bqkgd,bckd->bkgqc', qn, k_ctx, preferred_element_type=jnp.float32) * ATTN_SCALE
        p = sink_softmax(jnp.concatenate([s_loc, s_ctx], axis=-1), sink).astype(v.dtype)
        o = (jnp.einsum('bkgqs,bskd->bqkgd', p[..., :span], vn)
             + jnp.einsum('bkgqc,bckd->bqkgd', p[..., span:], v_ctx))
        return o.reshape(b_, ATTN_BLOCK, ATTN_WIDTH)

    out = lax.map(one_block, jnp.arange(nb))
    return out.transpose(1, 0, 2, 3).reshape(b_, s_, ATTN_WIDTH)


def context_attention(q, k, v, sink):
    b_, c_ = q.shape[:2]
    qg = q.reshape(b_, c_, N_KV_HEADS, GQA, HEAD_DIM)
    s = jnp.einsum('bqkgd,bckd->bkgqc', qg, k, preferred_element_type=jnp.float32) * ATTN_SCALE
    p = sink_softmax(s, sink).astype(v.dtype)
    o = jnp.einsum('bkgqc,bckd->bqkgd', p, v)
    return o.reshape(b_, c_, ATTN_WIDTH)


def hier_moe(h, w_rg, b_rg, w_re, b_re, w1, w3, w2):
    n, d_ = h.shape
    hf = h.astype(jnp.float32)
    g_logit = hf @ w_rg.astype(jnp.float32) + b_rg.astype(jnp.float32)
    g_val, g_idx = lax.top_k(g_logit, 1)
    p_group = jnp.exp(g_val[:, 0] - jax.nn.logsumexp(g_logit, axis=-1))
    e_logit = (hf @ w_re.astype(jnp.float32)).reshape(n, N_GROUPS, EXP_PER_GROUP) \
        + b_re.astype(jnp.float32).reshape(N_GROUPS, EXP_PER_GROUP)
    e_logit = jnp.take_along_axis(e_logit, g_idx[:, :, None], axis=1)[:, 0]
    e_val, e_idx = lax.top_k(e_logit, TOP_K)
    gates = p_group[:, None] * jax.nn.softmax(e_val, axis=-1)
    expert = g_idx * EXP_PER_GROUP + e_idx

    a = n * TOP_K
    flat_e = expert.reshape(-1)
    flat_g = gates.reshape(-1)
    order = jnp.argsort(flat_e)
    se = flat_e[order]
    counts = jnp.bincount(flat_e, length=N_EXPERTS)
    starts = jnp.cumsum(counts) - counts
    padded = (counts + MOE_BLOCK - 1) // MOE_BLOCK * MOE_BLOCK
    pends = jnp.cumsum(padded)
    pstarts = pends - padded
    dest = pstarts[se] + jnp.arange(a) - starts[se]
    n_blocks = -(-a // MOE_BLOCK) + N_EXPERTS
    slot_tok = jnp.zeros(n_blocks * MOE_BLOCK, jnp.int32).at[dest].set((order // TOP_K).astype(jnp.int32))
    slot_gate = jnp.zeros(n_blocks * MOE_BLOCK, h.dtype).at[dest].set(flat_g[order].astype(h.dtype))
    block_e = jnp.minimum(jnp.searchsorted(pends, jnp.arange(n_blocks) * MOE_BLOCK, side='right'),
                          N_EXPERTS - 1)
    xs = h[slot_tok].reshape(n_blocks, MOE_BLOCK, d_)

    def run(args):
        xb, e = args
        return (jax.nn.silu(xb @ w1[e]) * (xb @ w3[e])) @ w2[e]

    ys = lax.map(run, (xs, block_e)).reshape(-1, d_)
    return jnp.zeros_like(h).at[slot_tok].add(ys * slot_gate[:, None])


def setup_inputs(seed: int = 0) -> dict:
    key = jax.random.key(seed)
    ks = jax.random.split(key, 23)

    def nrm(k, shape, s):
        return jax.random.normal(k, shape, jnp.float32) * s

    d = D_MODEL
    return {
        'x': nrm(ks[0], (BATCH, SEQ, d), 1.0),
        'c': nrm(ks[1], (BATCH, d), 1.0),
        'ctx': nrm(ks[2], (BATCH, CTX_LEN, d), 1.0),
        'c_ctx': nrm(ks[3], (d,), 1.0),
        'w_ada': nrm(ks[4], (DEPTH, d, 6 * d), d ** -0.5),
        'b_ada': nrm(ks[5], (DEPTH, 6 * d), 0.02),
        'w_in': nrm(ks[6], (DEPTH, d, IN_WIDTH), d ** -0.5),
        'conv_w': nrm(ks[7], (DEPTH, 3, CONV_WIDTH), 3 ** -0.5),
        'attn_sink': nrm(ks[8], (DEPTH, N_HEADS), 0.5),
        'gm_ws': nrm(ks[9], (DEPTH, GM_GROUPS, CHUNK, CHUNK), CHUNK ** -0.5),
        'gm_bs': 1.0 + nrm(ks[10], (DEPTH, GM_GROUPS, CHUNK), 0.02),
        'w_out': nrm(ks[11], (DEPTH, MIX_WIDTH, d), BETA * MIX_WIDTH ** -0.5),
        'ln1_g': 1.0 + nrm(ks[12], (DEPTH, d), 0.02),
        'ln1_b': nrm(ks[13], (DEPTH, d), 0.02),
        'w_rg': nrm(ks[14], (DEPTH, d, N_GROUPS), d ** -0.5),
        'b_rg': nrm(ks[15], (DEPTH, N_GROUPS), 0.01),
        'w_re': nrm(ks[16], (DEPTH, d, N_EXPERTS), d ** -0.5),
        'b_re': nrm(ks[17], (DEPTH, N_EXPERTS), 0.01),
        'w1': nrm(ks[18], (DEPTH, N_EXPERTS, d, D_EXPERT), d ** -0.5),
        'w3': nrm(ks[19], (DEPTH, N_EXPERTS, d, D_EXPERT), d ** -0.5),
        'w2': nrm(ks[20], (DEPTH, N_EXPERTS, D_EXPERT, d), BETA * D_EXPERT ** -0.5),
        'ln2_g': 1.0 + nrm(ks[21], (DEPTH, d), 0.02),
        'ln2_b': nrm(ks[22], (DEPTH, d), 0.02),
    }


def reference(x, c, ctx, c_ctx, w_ada, b_ada, w_in, conv_w, attn_sink, gm_ws, gm_bs, w_out,
              ln1_g, ln1_b, w_rg, b_rg, w_re, b_re, w1, w3, w2, ln2_g, ln2_b):
    b_, s_, d_ = x.shape
    c_len = ctx.shape[1]
    rows = s_ // GRID_W
    row = jnp.repeat(jnp.arange(rows), GRID_W)
    col = jnp.tile(jnp.arange(GRID_W), rows)
    c_act = jax.nn.silu(c)
    cc_act = jax.nn.silu(c_ctx)

    for l in range(DEPTH):
        last = l == DEPTH - 1
        mod_x = c_act @ w_ada[l] + b_ada[l]
        sh1, sc1, g1, sh2, sc2, g2 = jnp.split(mod_x[:, None, :], 6, axis=-1)
        n_mod = 2 if last else 6
        mod_c = (cc_act @ w_ada[l][:, :n_mod * d_] + b_ada[l][:n_mod * d_]).reshape(n_mod, d_)

        hx = x * (1 + sc1) + sh1
        hc = ctx * (1 + mod_c[1]) + mod_c[0]
        qx, kx, vx, cbx, ccx, cvx, gux, gvx = jnp.split(hx @ w_in[l], SPLIT_POINTS, axis=-1)
        qx = axial_rope(qx.reshape(b_, s_, N_HEADS, HEAD_DIM), row, col)
        kx = axial_rope(kx.reshape(b_, s_, N_KV_HEADS, HEAD_DIM), row, col)
        vx = vx.reshape(b_, s_, N_KV_HEADS, HEAD_DIM)
        if last:
            kc, vc = jnp.split(hc @ w_in[l][:, ATTN_WIDTH:ATTN_WIDTH + 2 * KV_WIDTH], 2, axis=-1)
        else:
            qc, kc, vc, cbc, ccc, cvc, guc, gvc = jnp.split(hc @ w_in[l], SPLIT_POINTS, axis=-1)
        kc = kc.reshape(b_, c_len, N_KV_HEADS, HEAD_DIM)
        vc = vc.reshape(b_, c_len, N_KV_HEADS, HEAD_DIM)

        att_x = window_attention(qx, kx, vx, kc, vc, attn_sink[l])
        o_x = jnp.concatenate(
            [att_x, local_mixers(cbx, ccx, cvx, gux, gvx, conv_w[l], gm_ws[l], gm_bs[l])], axis=-1) @ w_out[l]
        if not last:
            att_c = context_attention(qc, kc, vc, attn_sink[l])
            o_c = jnp.concatenate(
                [att_c, local_mixers(cbc, ccc, cvc, guc, gvc, conv_w[l], gm_ws[l], gm_bs[l])], axis=-1) @ w_out[l]
            ctx = layer_norm(ALPHA * ctx + mod_c[2] * o_c, ln1_g[l], ln1_b[l])
        x = layer_norm(ALPHA * x + g1 * o_x, ln1_g[l], ln1_b[l])

        hx = (x * (1 + sc2) + sh2).reshape(b_ * s_, d_)
        if last:
            tokens = hx
        else:
            hc = (ctx * (1 + mod_c[4]) + mod_c[3]).reshape(b_ * c_len, d_)
            tokens = jnp.concatenate([hx, hc], axis=0)
        y = hier_moe(tokens, w_rg[l], b_rg[l], w_re[l], b_re[l], w1[l], w3[l], w2[l])
        if not last:
            ctx = layer_norm(ALPHA * ctx + mod_c[5] * y[b_ * s_:].reshape(b_, c_len, d_), ln2_g[l], ln2_b[l])
        x = layer_norm(ALPHA * x + g2 * y[:b_ * s_].reshape(b_, s_, d_), ln2_g[l], ln2_b[l])

    return x
```

```python
from contextlib import ExitStack
import numpy as np
import ml_dtypes
import concourse.bass as bass
import concourse.mybir as mybir
from concourse.bass_utils import run_bass_kernel_spmd

F32 = mybir.dt.float32
BF16 = mybir.dt.bfloat16
AF = mybir.ActivationFunctionType
ALU = mybir.AluOpType

D = 1024
KC = 8
CTX = 256
NCB = CTX // 128
DEPTH = 2
NEXP = 32
DEXP = 512
ALPHA = (2 * DEPTH) ** 0.25
LN_EPS = 1e-6
GRID_W = 64
NEG = -30000.0
DBG = {}


class Prog:
    NDMA = 8

    def __init__(self, nc):
        self.nc = nc
        self.names = dict(pe="tensor", act="scalar", dve="vector", pool="gpsimd", sp="sync")
        self.sems = []
        self.semidx = {}
        for k in self.names:
            self.semidx[k] = len(self.sems)
            self.sems.append(nc.alloc_semaphore("s_" + k))
        self.cnt = {k: 0 for k in self.names}
        self.dq = {}
        for q in ("sp", "pool"):
            idx = []
            for i in range(self.NDMA):
                idx.append(len(self.sems))
                self.sems.append(nc.alloc_semaphore("d_%s%d" % (q, i)))
            self.dq[q] = dict(idx=idx, cnt=[0] * self.NDMA, nxt=0)
        self.seen = {k: {} for k in self.names}
        self.stream = {k: [] for k in self.names}
        self.res = {}
        self._cap = None

    def record(self, fn, *a, **kw):
        self._cap = []
        fn(*a, **kw)
        cap, self._cap = self._cap, None
        return cap

    def play(self, *lists):
        lists = [l for l in lists if l]
        pos = [0] * len(lists)
        while True:
            best, bf = None, None
            for i, l in enumerate(lists):
                if pos[i] < len(l):
                    f = pos[i] / len(l)
                    if bf is None or f < bf:
                        best, bf = i, f
            if best is None:
                break
            kind, a, kw = lists[best][pos[best]]
            pos[best] += 1
            getattr(self, kind)(*a, **kw)

    def _deps(self, reads, writes):
        deps = []
        for r in reads:
            e = self.res.get(r)
            if e and e["w"] is not None:
                deps.append(e["w"])
        for w in writes:
            e = self.res.get(w)
            if e:
                if e["w"] is not None:
                    deps.append(e["w"])
                deps.extend((si, v, src) for (si, src), v in e["r"].items())
        return deps

    def _waits(self, ek, deps):
        waits = {}
        for (si, val, src) in deps:
            if src == "pe" and ek == "pe":
                continue
            if self.seen[ek].get(si, 0) >= val:
                continue
            waits[si] = max(waits.get(si, 0), val)
        for si, v in waits.items():
            self.seen[ek][si] = v
        return list(waits.items())

    def _record(self, token, reads, writes):
        si, val, src = token
        for r in reads:
            e = self.res.setdefault(r, {"w": None, "r": {}})
            e["r"][(si, src)] = max(e["r"].get((si, src), 0), val)
        for w in writes:
            self.res[w] = {"w": token, "r": {}}

    def op(self, ek, fn, reads=(), writes=()):
        if self._cap is not None:
            self._cap.append(("op", (ek, fn), dict(reads=reads, writes=writes)))
            return None
        waits = self._waits(ek, self._deps(reads, writes))
        self.cnt[ek] += 1
        si = self.semidx[ek]
        token = (si, self.cnt[ek], ek)
        sems = self.sems

        def emit(eng):
            for (wsi, v) in waits:
                eng.wait_ge(sems[wsi], v)
            ins = fn(eng)
            ins.then_inc(sems[si], 1)

        self.stream[ek].append(emit)
        self._record(token, reads, writes)
        return token

    def dma(self, q, out, in_, reads=(), writes=()):
        if self._cap is not None:
            self._cap.append(("dma", (q, out, in_), dict(reads=reads, writes=writes)))
            return None
        d = self.dq[q]
        slot = d["nxt"]
        d["nxt"] = (slot + 1) % self.NDMA
        si = d["idx"][slot]
        deps = self._deps(reads, writes)
        if d["cnt"][slot] > 0:
            deps.append((si, 16 * d["cnt"][slot], "dma"))
        waits = self._waits(q, deps)
        d["cnt"][slot] += 1
        token = (si, 16 * d["cnt"][slot], "dma")
        sems = self.sems

        def emit(eng):
            for (wsi, v) in waits:
                eng.wait_ge(sems[wsi], v)
            eng.dma_start(out=out, in_=in_).then_inc(sems[si], 16)

        self.stream[q].append(emit)
        self._record(token, reads, writes)
        return token

    def idma(self, out, out_off, in_, in_off, reads=(), writes=()):
        if self._cap is not None:
            self._cap.append(("idma", (out, out_off, in_, in_off), dict(reads=reads, writes=writes)))
            return None
        q = "pool"
        d = self.dq[q]
        slot = d["nxt"]
        d["nxt"] = (slot + 1) % self.NDMA
        si = d["idx"][slot]
        deps = self._deps(reads, writes)
        if d["cnt"][slot] > 0:
            deps.append((si, 16 * d["cnt"][slot], "dma"))
        waits = self._waits(q, deps)
        d["cnt"][slot] += 1
        token = (si, 16 * d["cnt"][slot], "dma")
        sems = self.sems

        def emit(eng):
            for (wsi, v) in waits:
                eng.wait_ge(sems[wsi], v)
            eng.indirect_dma_start(out=out, out_offset=out_off, in_=in_, in_offset=in_off).then_inc(sems[si], 16)

        self.stream[q].append(emit)
        self._record(token, reads, writes)
        return token

    def flush(self):
        finals = []
        for k in self.names:
            if self.cnt[k] > 0:
                finals.append((self.semidx[k], self.cnt[k]))
        for q, d in self.dq.items():
            for i, si in enumerate(d["idx"]):
                if d["cnt"][i] > 0:
                    finals.append((si, 16 * d["cnt"][i]))
        with self.nc.Block() as block:
            for ek, nm in self.names.items():
                stream = self.stream[ek]
                seen = self.seen[ek]
                sems = self.sems

                def body(eng, stream=stream, seen=seen):
                    for emit in stream:
                        emit(eng)
                    for si, v in finals:
                        if seen.get(si, 0) < v:
                            eng.wait_ge(sems[si], v)
                            seen[si] = v

                getattr(block, nm)(body)
        self.stream = {k: [] for k in self.names}
        self.res = {}


def bc_mid(ap, n):
    p, f = ap.shape
    return ap.unsqueeze(1).to_broadcast([p, n, f])


def bc_last(ap, n):
    p, g = ap.shape
    return ap.unsqueeze(2).to_broadcast([p, g, n])


def build_program(NB, depth=DEPTH, tps=9, stop=None, nexp=NEXP, do_router=True, do_epi=True):
    S = NB * 128
    NT = NB + NCB
    T = NT * 128
    nc = bass.Bass("TRN2", target_bir_lowering=False)

    def din(name, shape, dt=F32):
        return nc.dram_tensor(name, list(shape), dt, kind="ExternalInput").ap()

    xin = din("xin", [S, D])
    ctxin = din("ctxin", [CTX, D])
    cvec = din("cvec", [128, KC, 2])
    w_ada = din("w_ada", [depth, D, 6 * D])
    bada_p = din("bada_p", [depth, 128, 48])
    b_ada = din("b_ada", [depth, 6 * D])
    w_in = din("w_in", [depth, D, 2048])
    convw = din("convw", [depth, 128, 2, 3])
    sink = din("sink", [depth, 8])
    gm_wsT = din("gm_wsT", [depth, 4, 128, 128])
    gm_bsp = din("gm_bsp", [depth, 128, 4])
    w_out = din("w_out", [depth, D, D])
    ln1_g = din("ln1_g", [depth, D])
    ln1_b = din("ln1_b", [depth, D])
    ln2_g = din("ln2_g", [depth, D])
    ln2_b = din("ln2_b", [depth, D])
    w_r = din("w_r", [depth, D, 36])
    b_r = din("b_r", [depth, 36])
    w1 = din("w1", [depth, NEXP, 128, KC, DEXP])
    w3 = din("w3", [depth, NEXP, 128, KC, DEXP])
    w2 = din("w2", [depth, NEXP, 128, 4, D])
    cos_t = din("cos_t", [NT, 128, 128])
    sin_t = din("sin_t", [NT, 128, 128])
    ident_d = din("ident", [128, 128])
    maskl_d = din("maskl", [128, 128])
    maskr_d = din("maskr", [128, 128])
    out_d = nc.dram_tensor("out", [S, D], F32, kind="ExternalOutput").ap()
    x1buf = nc.dram_tensor("x1buf", [T, D], F32).ap()
    x2buf = nc.dram_tensor("x2buf", [T, D], F32).ap()
    BS = 512
    NBLK_MAX = (2 * T) // BS + NEXP
    h2buf = nc.dram_tensor("h2buf", [T, D], BF16).ap()
    xs_dram = nc.dram_tensor("xs_dram", [NBLK_MAX * BS, D], BF16).ap()
    ys_dram = nc.dram_tensor("ys_dram", [NBLK_MAX * BS, D], F32).ap()
    modrows = nc.dram_tensor("modrows", [depth, 2, 4, D], F32).ap()
    lt_d = din("lt_d", [128, 128])
    jvec_d = din("jvec_d", [128, NBLK_MAX])
    base1_d = din("base1_d", [128, 8])
    base2_d = din("base2_d", [128, 4])

    P = Prog(nc)

    def tile_src(l, t):
        if l == 0:
            if t < NB:
                return xin[t * 128:(t + 1) * 128, :]
            return ctxin[(t - NB) * 128:(t - NB + 1) * 128, :]
        return x2buf[t * 128:(t + 1) * 128, :]

    glob = ExitStack()

    uid = [0]

    def alloc(st, name, shape, dt):
        uid[0] += 1
        return st.enter_context(nc.sbuf_tensor("sb%d_%s" % (uid[0], name), list(shape), dt))

    def palloc(st, name, shape, dt=F32):
        uid[0] += 1
        return st.enter_context(nc.psum_tensor("ps%d_%s" % (uid[0], name), list(shape), dt))

    ident_f = alloc(glob, "ident_f", [128, 128], F32)
    ident_b = alloc(glob, "ident_b", [128, 128], BF16)
    maskl = alloc(glob, "maskl", [128, 128], BF16)
    maskr = alloc(glob, "maskr", [128, 128], BF16)
    cact = alloc(glob, "cact", [128, KC, 2], F32)
    cact_rep = alloc(glob, "cact_rep", [128, KC, 2, 128], F32)
    ones_f = alloc(glob, "ones_f", [1, 128], F32)
    modT = alloc(glob, "modT", [128, 48, 2], F32)

    P.dma("sp", ident_f[:], ident_d[:, :], writes=["ident_f"])
    P.dma("pool", ident_b[:], ident_d[:, :], writes=["ident_b"])
    P.dma("pool", maskl[:], maskl_d[:, :], writes=["maskl"])
    P.dma("pool", maskr[:], maskr_d[:, :], writes=["maskr"])
    P.dma("sp", cact[:], cvec[:, :, :], writes=["cact"])
    P.op("act", lambda e: e.activation(out=cact[:], in_=cact[:], func=AF.Silu), reads=["cact"], writes=["cact"])
    P.op("dve", lambda e: e.tensor_copy(cact_rep[:].rearrange("p k c m -> p (k c) m"),
                                        bc_last(cact[:].rearrange("p k c -> p (k c)"), 128)),
         reads=["cact"], writes=["cact_rep"])
    P.op("dve", lambda e: e.memset(ones_f[:], 1.0), writes=["ones_f"])
    P.flush()
    if stop == "const":
        return nc

    for l in range(depth):
        last = (l == depth - 1)
        with ExitStack() as st:
            wa = [alloc(st, "wa%d" % i, [128, KC, D], F32) for i in range(2)]
            bada = alloc(st, "bada", [128, 48], F32)
            brow = [alloc(st, "brow%d" % i, [128, D], F32) for i in range(2)]
            grow = [alloc(st, "grow%d" % i, [128, D], F32) for i in range(2)]
            ps_mod = palloc(st, "ps_mod", [128, 96])
            ps_g = [palloc(st, "ps_g%d" % i, [128, 512]) for i in range(4)]
            P.dma("sp", bada[:], bada_p[l], writes=["bada"])
            for i in range(6):
                s = i % 2
                P.dma("sp", wa[s][:], w_ada[l][:, i * D:(i + 1) * D].rearrange("(k p) n -> p k n", p=128),
                      writes=[("wa", s)])

                def mm_mod(e, i=i, s=s):
                    ins = None
                    for j in range(KC):
                        for k in range(KC):
                            ins = e.matmul(ps_mod[:, (i * 8 + j) * 2:(i * 8 + j) * 2 + 2],
                                           lhsT=wa[s][:, k, j * 128:(j + 1) * 128], rhs=cact[:, k, :],
                                           start=(k == 0), stop=(k == KC - 1))
                    return ins
                P.op("pe", mm_mod, reads=[("wa", s), "cact"], writes=[("ps_mod", i)])
                if i >= 2:
                    gi = i - 2
                    P.dma("sp", brow[gi % 2][:], b_ada[l:l + 1, i * D:(i + 1) * D].to_broadcast([128, D]),
                          writes=[("brow", gi % 2)])
                    for c in range(2):
                        for h in range(2):
                            def mm_g(e, s=s, c=c, h=h):
                                ins = None
                                for k in range(KC):
                                    ins = e.matmul(ps_g[c * 2 + h][:, :], lhsT=cact_rep[:, k, c, :],
                                                   rhs=wa[s][:, k, h * 512:(h + 1) * 512],
                                                   start=(k == 0), stop=(k == KC - 1))
                                return ins
                            P.op("pe", mm_g, reads=[("wa", s), "cact_rep"], writes=[("ps_g", c * 2 + h)])
                            P.op("dve", lambda e, gi=gi, c=c, h=h: e.tensor_tensor(
                                grow[c][:, h * 512:(h + 1) * 512], ps_g[c * 2 + h][:, :],
                                brow[gi % 2][:, h * 512:(h + 1) * 512], ALU.add),
                                reads=[("ps_g", c * 2 + h), ("brow", gi % 2)], writes=[("grow", c, h)])
                        if i == 4:
                            P.op("dve", lambda e, c=c: e.tensor_scalar(grow[c][0:1, :], grow[c][0:1, :], 1.0, None, ALU.add),
                                 reads=[("grow", c, 0), ("grow", c, 1)], writes=[("grow", c, 0), ("grow", c, 1)])
                        P.dma("sp", modrows[l, c, gi:gi + 1, :], grow[c][0:1, :],
                              reads=[("grow", c, 0), ("grow", c, 1)], writes=[("modrows", c, gi)])
            P.op("dve", lambda e: e.tensor_tensor(modT[:], ps_mod[:].rearrange("p (j c) -> p j c", c=2),
                                                  bc_last(bada[:], 2), ALU.add),
                 reads=[("ps_mod", i) for i in range(6)] + ["bada"], writes=["modT"])
            P.op("dve", lambda e: e.tensor_scalar(modT[:, 8:16, :], modT[:, 8:16, :], 1.0, None, ALU.add),
                 reads=["modT"], writes=["modT"])
            P.op("dve", lambda e: e.tensor_scalar(modT[:, 32:40, :], modT[:, 32:40, :], 1.0, None, ALU.add),
                 reads=["modT"], writes=["modT"])
            P.flush()
        if stop == "p0":
            return nc

        with ExitStack() as st:
            NCOL = 18 * 128 + 640
            win = alloc(st, "win", [128, KC, NCOL], BF16)
            wout = alloc(st, "wout", [128, KC, D], BF16)
            kT = alloc(st, "kT", [128, 2, T], BF16)
            V = alloc(st, "V", [128, NT, 2, 65], BF16)
            convw_s = alloc(st, "convw_s", [128, 2, 3], F32)
            wsT = alloc(st, "wsT", [128, 4, 128], BF16)
            gmb = alloc(st, "gmb", [128, 4], F32)
            esink = alloc(st, "esink", [128, 8], F32)
            lng = alloc(st, "lng", [128, D], F32)
            lnb = alloc(st, "lnb", [128, D], F32)
            g1b = [alloc(st, "g1b%d" % c, [128, D], F32) for c in range(2)]
            for c in range(2):
                P.dma("sp", g1b[c][:], modrows[l, c, 0:1, :].to_broadcast([128, D]), writes=[("g1b", c)])
            R = 4
            xres = [alloc(st, "xres%d" % i, [128, D], F32) for i in range(3)]
            hT = [alloc(st, "hT%d" % i, [128, KC, 128], BF16) for i in range(2)]
            qrot = [alloc(st, "qrot%d" % i, [128, 4, 128], BF16) for i in range(R)]
            u = [alloc(st, "u%d" % i, [128, 2, 130], F32) for i in range(R)]
            cb = [alloc(st, "cb%d" % i, [128, 2, 128], F32) for i in range(R)]
            gut = [alloc(st, "gut%d" % i, [128, 256], F32) for i in range(R)]
            vln = [alloc(st, "vln%d" % i, [128, 256], BF16) for i in range(R)]
            cosb = [alloc(st, "cosb%d" % i, [128, 128], F32) for i in range(2)]
            sinb = [alloc(st, "sinb%d" % i, [128, 128], F32) for i in range(2)]
            t1 = alloc(st, "t1", [128, 4, 128], F32)
            t2 = alloc(st, "t2", [128, 4, 128], F32)
            cxs = alloc(st, "cxs", [128, 2, 128], F32)
            gvt = alloc(st, "gvt", [128, 256], F32)
            st6 = alloc(st, "st6", [128, 6], F32)
            mv = alloc(st, "mv", [128, 2], F32)
            rstd = alloc(st, "rstd", [128, 1], F32)
            nb_ = alloc(st, "nb_", [128, 1], F32)
            PT = [alloc(st, "PT%d" % i, [128, 5, 128], BF16) for i in range(4)]
            den = alloc(st, "den", [128, 4], F32)
            mixtok = [alloc(st, "mixtok%d" % i, [128, 768], BF16) for i in range(2)]
            mixT = [alloc(st, "mixT%d" % i, [128, KC, 128], BF16) for i in range(2)]
            ct = alloc(st, "ct", [128, 2, 128], F32)
            gtmp = alloc(st, "gtmp", [128, 256], F32)
            bufA = alloc(st, "bufA", [128, D], F32)
            bufB = alloc(st, "bufB", [128, D], F32)
            st12 = alloc(st, "st12", [128, 12], F32)
            mv2 = alloc(st, "mv2", [128, 2], F32)
            rstd2 = alloc(st, "rstd2", [128, 1], F32)
            nb2 = alloc(st, "nb2", [128, 1], F32)
            B = [palloc(st, "B%d" % i, [128, 512]) for i in range(7)]
            Bbf = palloc(st, "Bbf", [128, 1024], BF16)
            Bo = B[4]

            wv = w_in[l].rearrange("(k p) n -> p k n", p=128)
            P.dma("pool", win[:, :, 0:512], wv[:, :, 0:512], writes=["win"])
            for kv in range(2):
                for dup in range(2):
                    c0 = 1024 + kv * 128 + dup * 64
                    P.dma("pool", win[:, :, c0:c0 + 64], wv[:, :, 512 + kv * 64:512 + (kv + 1) * 64], writes=["win"])
            P.dma("pool", win[:, :, 1536:2304], wv[:, :, 768:1536], writes=["win"])
            P.dma("pool", win[:, :, 2304:2432], wv[:, :, 640:768], writes=["win"])
            P.dma("pool", win[:, :, 2432:2688], wv[:, :, 1792:2048], writes=["win"])
            P.dma("pool", win[:, :, 2688:2944], wv[:, :, 1536:1792], writes=["win"])
            P.dma("pool", wout[:], w_out[l].rearrange("(k p) n -> p k n", p=128), writes=["wout"])
            for (src0, dst0, nblk) in ((0, 512, 16), (1024, 1280, 8)):
                for a in range(2):
                    def swp(e, src0=src0, dst0=dst0, nblk=nblk, a=a):
                        srcv = win[:, :, src0:src0 + nblk * 32].rearrange("p k (b a f) -> p k b a f", a=2, f=16)
                        dstv = win[:, :, dst0:dst0 + nblk * 32].rearrange("p k (b a f) -> p k b a f", a=2, f=16)
                        return e.tensor_copy(dstv[:, :, :, a, :], srcv[:, :, :, 1 - a, :])
                    P.op("dve", swp, reads=["win"], writes=["win"])
            P.dma("sp", convw_s[:], convw[l], writes=["convw_s"])
            P.dma("pool", wsT[:], gm_wsT[l].rearrange("g q p -> q g p"), writes=["wsT"])
            P.dma("sp", gmb[:], gm_bsp[l], writes=["gmb"])
            P.dma("sp", esink[:], sink[l:l + 1, :].to_broadcast([128, 8]), writes=["esink"])
            P.op("act", lambda e: e.activation(out=esink[:], in_=esink[:], func=AF.Exp), reads=["esink"], writes=["esink"])
            P.dma("sp", lng[:], ln1_g[l:l + 1, :].to_broadcast([128, D]), writes=["lng"])
            P.dma("sp", lnb[:], ln1_b[l:l + 1, :].to_broadcast([128, D]), writes=["lnb"])
            P.op("pool", lambda e: e.memset(V[:, :, :, 64:65], 1.0), writes=["Vones"])

            def seq_of(t):
                return (0, NB) if t < NB else (NB, NT)

            def inproj(t, kv_only=False):
                s = t % R
                xs = t % 3
                hs = t % 2
                cs = t % 2
                col = 0 if t < NB else 1
                lo, hi = seq_of(t)
                P.dma("sp", xres[xs][:], tile_src(l, t), writes=[("xres", xs)])
                P.dma("sp", cosb[cs][:], cos_t[t], writes=[("cosb", cs)])
                P.dma("sp", sinb[cs][:], sin_t[t], writes=[("sinb", cs)])
                for hb in range(2):
                    def tp(e, hb=hb):
                        ins = None
                        for i in range(4):
                            j = hb * 4 + i
                            ins = e.transpose(B[2 + hb][:, i * 128:(i + 1) * 128], xres[xs][:, j * 128:(j + 1) * 128], ident_f[:])
                        return ins
                    P.op("pe", tp, reads=[("xres", xs), "ident_f"], writes=[("B", 2 + hb)])

                    def ev(e, hb=hb):
                        ins = None
                        for i in range(4):
                            j = hb * 4 + i
                            ins = e.activation(out=hT[hs][:, j, :], in_=B[2 + hb][:, i * 128:(i + 1) * 128], func=AF.Identity,
                                               scale=modT[:, 8 + j, col:col + 1], bias=modT[:, j, col:col + 1])
                        return ins
                    P.op("act", ev, reads=[("B", 2 + hb), "modT"], writes=[("hT", hs, hb)])
                hres = [("hT", hs, 0), ("hT", hs, 1)]

                def fm_group(bank, off, chunks):
                    def f(e):
                        ins = None
                        for i, c in enumerate(chunks):
                            for k in range(KC):
                                ins = e.matmul(B[bank][:, off + i * 128:off + (i + 1) * 128],
                                               lhsT=win[:, k, c * 128:(c + 1) * 128], rhs=hT[hs][:, k, :],
                                               start=(k == 0), stop=(k == KC - 1))
                        return ins
                    return f

                if not kv_only:
                    P.op("pe", fm_group(2, 0, [0, 1, 2, 3]), reads=hres + ["win"], writes=[("B", 2)])
                    P.op("pe", fm_group(3, 0, [4, 5, 6, 7]), reads=hres + ["win"], writes=[("B", 3)])
                    P.op("dve", lambda e: e.tensor_tensor(t1[:], B[2][:, :].rearrange("p (c t) -> p c t", c=4),
                                                          bc_mid(cosb[cs][:], 4), ALU.mult),
                         reads=[("B", 2), ("cosb", cs)], writes=["t1"])
                    P.op("dve", lambda e: e.tensor_tensor(t2[:], B[3][:, :].rearrange("p (c t) -> p c t", c=4),
                                                          bc_mid(sinb[cs][:], 4), ALU.mult),
                         reads=[("B", 3), ("sinb", cs)], writes=["t2"])
                    P.op("pool", lambda e: e.tensor_tensor(qrot[s][:], t1[:], t2[:], ALU.add),
                         reads=["t1", "t2"], writes=[("qrot", s)])
                P.op("pe", fm_group(2, 0, [8, 9, 10, 11]), reads=hres + ["win"], writes=[("B", 2)])
                P.op("dve", lambda e: e.tensor_tensor(t1[:, 0:2, :], B[2][:, 0:256].rearrange("p (c t) -> p c t", c=2),
                                                      bc_mid(cosb[cs][:], 2), ALU.mult),
                     reads=[("B", 2), ("cosb", cs)], writes=["t1"])
                P.op("dve", lambda e: e.tensor_tensor(t2[:, 0:2, :], B[2][:, 256:512].rearrange("p (c t) -> p c t", c=2),
                                                      bc_mid(sinb[cs][:], 2), ALU.mult),
                     reads=[("B", 2), ("sinb", cs)], writes=["t2"])
                P.op("pool", lambda e: e.tensor_tensor(kT[:, :, t * 128:(t + 1) * 128], t1[:, 0:2, :], t2[:, 0:2, :], ALU.add),
                     reads=["t1", "t2"], writes=[("kT", t)])
                if not kv_only:
                    P.op("pe", fm_group(3, 0, [12, 13, 14, 15]), reads=hres + ["win"], writes=[("B", 3)])
                    P.op("pe", fm_group(2, 0, [16, 17]), reads=hres + ["win"], writes=[("B", 2)])
                    P.op("act", lambda e: e.activation(out=cb[s][:], in_=B[3][:, 0:256].rearrange("p (c t) -> p c t", c=2),
                                                       func=AF.Copy), reads=[("B", 3)], writes=[("cb", s)])
                    P.op("act", lambda e: e.activation(out=cxs[:], in_=B[2][:, 0:256].rearrange("p (c t) -> p c t", c=2),
                                                       func=AF.Copy), reads=[("B", 2)], writes=["cxs"])
                    P.op("dve", lambda e: e.tensor_tensor(u[s][:, :, 1:129], B[3][:, 256:512].rearrange("p (c t) -> p c t", c=2),
                                                          cxs[:], ALU.mult),
                         reads=[("B", 3), "cxs"], writes=[("u", s)])
                    if t > lo:
                        sp_ = (t - 1) % R
                        P.op("pool", lambda e: e.tensor_copy(u[sp_][:, :, 129:130], u[s][:, :, 1:2]),
                             reads=[("u", s)], writes=[("u", sp_)])
                    else:
                        P.op("pool", lambda e: e.memset(u[s][:, :, 0:1], 0.0), reads=[("u", s)], writes=[("u", s)])
                    if t < hi - 1:
                        sn_ = (t + 1) % R
                        P.op("pool", lambda e: e.tensor_copy(u[sn_][:, :, 0:1], u[s][:, :, 128:129]),
                             reads=[("u", s)], writes=[("u", sn_)])
                    else:
                        P.op("pool", lambda e: e.memset(u[s][:, :, 129:130], 0.0), reads=[("u", s)], writes=[("u", s)])

                def tm(e):
                    ins = None
                    for k in range(KC):
                        ins = e.matmul(B[3][:, 0:384], lhsT=hT[hs][:, k, :], rhs=win[:, k, 2304:2688],
                                       start=(k == 0), stop=(k == KC - 1))
                    return ins
                P.op("pe", tm, reads=hres + ["win"], writes=[("B", 3)])
                P.op("act", lambda e: e.activation(out=V[:, t, :, 0:64], in_=B[3][:, 0:128].rearrange("p (h d) -> p h d", h=2),
                                                   func=AF.Copy), reads=[("B", 3)], writes=[("V", t)])
                if not kv_only:
                    def tm2(e):
                        ins = None
                        for k in range(KC):
                            ins = e.matmul(B[2][:, 256:512], lhsT=hT[hs][:, k, :], rhs=win[:, k, 2688:2944],
                                           start=(k == 0), stop=(k == KC - 1))
                        return ins
                    P.op("pe", tm2, reads=hres + ["win"], writes=[("B", 2)])
                    P.op("act", lambda e: e.activation(out=gvt[:], in_=B[3][:, 128:384], func=AF.Gelu_apprx_tanh),
                         reads=[("B", 3)], writes=["gvt"])
                    P.op("act", lambda e: e.activation(out=gut[s][:], in_=B[2][:, 256:512], func=AF.Gelu_apprx_tanh),
                         reads=[("B", 2)], writes=[("gut", s)])
                    P.op("dve", lambda e: e.bn_stats(st6[:], gvt[:]), reads=["gvt"], writes=["st6"])
                    P.op("dve", lambda e: e.bn_aggr(mv[:], st6[:]), reads=["st6"], writes=["mv"])
                    P.op("act", lambda e: e.activation(out=rstd[:], in_=mv[:, 1:2], func=AF.Sqrt, bias=LN_EPS),
                         reads=["mv"], writes=["rstd"])
                    P.op("dve", lambda e: e.reciprocal(rstd[:], rstd[:]), reads=["rstd"], writes=["rstd"])
                    P.op("dve", lambda e: e.scalar_tensor_tensor(nb_[:], mv[:, 0:1], -1.0, rstd[:], ALU.mult, ALU.mult),
                         reads=["mv", "rstd"], writes=["nb_"])
                    P.op("act", lambda e: e.activation(out=vln[s][:], in_=gvt[:], func=AF.Identity,
                                                       scale=rstd[:, 0:1], bias=nb_[:, 0:1]),
                         reads=["gvt", "rstd", "nb_"], writes=[("vln", s)])

            def mixers(t):
                s = t % R
                xs = t % 3
                ms = t % 2
                col = 0 if t < NB else 1
                lo, hi = seq_of(t)
                if t < NB:
                    kcs = [(NB, None), (NB + 1, None)]
                    if t > 0:
                        kcs.append((t - 1, maskl))
                    kcs.append((t, None))
                    if t < NB - 1:
                        kcs.append((t + 1, maskr))
                else:
                    kcs = [(NB, None), (NB + 1, None)]
                nk = len(kcs)
                for h in range(8):
                    qc, po, kv, pslot = h // 2, (h % 2) * 64, h // 4, h % 4

                    if h % 2 == 0:
                        sbank, s5, wr5 = B[6], B[4][:, 384:512], ("B", 4)
                        wr = [("B", 6)] + ([wr5] if nk > 4 else [])
                    else:
                        sbank, s5, wr5 = B[5], B[1][:, 384:512], ("B", 1)
                        wr = [("B", 5)] + ([wr5] if nk > 4 else [])

                    def sc(e, qc=qc, po=po, kv=kv, sbank=sbank, s5=s5):
                        ins = None
                        for i, (c, msk) in enumerate(kcs):
                            dst = sbank[:, i * 128:(i + 1) * 128] if i < 4 else s5
                            ins = e.matmul(dst, lhsT=kT[po:po + 64, kv, c * 128:(c + 1) * 128],
                                           rhs=qrot[s][po:po + 64, qc, :], start=True, stop=(msk is None))
                            if msk is not None:
                                ins = e.matmul(dst, lhsT=ident_b[:], rhs=msk[:], start=False, stop=True)
                        return ins
                    P.op("pe", sc, reads=[("kT", c) for c, _ in kcs] + [("qrot", s), "ident_b", "maskl", "maskr"], writes=wr)

                    def ex(e, pslot=pslot, sbank=sbank, s5=s5):
                        n1 = min(nk, 4)
                        ins = e.activation(out=PT[pslot][:, 0:n1, :], in_=sbank[:, 0:n1 * 128].rearrange("p (c t) -> p c t", c=n1),
                                           func=AF.Exp, scale=0.125)
                        if nk > 4:
                            ins = e.activation(out=PT[pslot][:, 4, :], in_=s5, func=AF.Exp, scale=0.125)
                        return ins
                    P.op("act", ex, reads=wr, writes=[("PT", pslot)])

                    def pv(e, kv=kv, pslot=pslot):
                        ins = None
                        for i, (c, msk) in enumerate(kcs):
                            ins = e.matmul(Bo[:, pslot * 65:(pslot + 1) * 65],
                                           lhsT=PT[pslot][:, i, :], rhs=V[:, c, kv, :],
                                           start=(i == 0), stop=(i == nk - 1))
                        return ins
                    P.op("pe", pv, reads=[("PT", pslot), "Vones"] + [("V", c) for c, _ in kcs], writes=[("B", 4)])
                    if pslot == 3:
                        hg = h // 4
                        bov = Bo[:, 0:260].rearrange("p (h d) -> p h d", d=65)
                        P.op("dve", lambda e, hg=hg, bov=bov: e.tensor_tensor(den[:], bov[:, :, 64], esink[:, hg * 4:(hg + 1) * 4], ALU.add),
                             reads=[("B", 4), "esink"], writes=["den"])
                        P.op("dve", lambda e: e.reciprocal(den[:], den[:]), reads=["den"], writes=["den"])
                        P.op("dve", lambda e, hg=hg, bov=bov: e.tensor_tensor(
                            mixtok[ms][:, hg * 256:(hg + 1) * 256].rearrange("p (h d) -> p h d", d=64),
                            bov[:, :, 0:64], bc_last(den[:], 64), ALU.mult),
                            reads=[("B", 4), "den"], writes=[("mixtok", ms, hg)])
                for j in range(2):
                    P.op("pool", lambda e, j=j: e.tensor_scalar(ct[:, j, :], u[s][:, j, 0:128], convw_s[:, j, 0:1], None, ALU.mult),
                         reads=[("u", s), "convw_s"], writes=[("ct", j)])
                    P.op("dve", lambda e, j=j: e.scalar_tensor_tensor(ct[:, j, :], u[s][:, j, 1:129], convw_s[:, j, 1:2], ct[:, j, :], ALU.mult, ALU.add),
                         reads=[("u", s), "convw_s", ("ct", j)], writes=[("ct", j)])
                    P.op("dve", lambda e, j=j: e.scalar_tensor_tensor(ct[:, j, :], u[s][:, j, 2:130], convw_s[:, j, 2:3], ct[:, j, :], ALU.mult, ALU.add),
                         reads=[("u", s), "convw_s", ("ct", j)], writes=[("ct", j)])
                    P.op("pool", lambda e, j=j: e.tensor_tensor(mixT[ms][:, 4 + j, :], cb[s][:, j, :], ct[:, j, :], ALU.mult),
                         reads=[("cb", s), ("ct", j)], writes=[("mixT", ms, 4 + j)])
                def gm(e):
                    ins = None
                    for g in range(4):
                        ins = e.matmul(B[1][:, g * 64:(g + 1) * 64], lhsT=wsT[:, g, :], rhs=vln[s][:, g * 64:(g + 1) * 64],
                                       start=True, stop=True)
                    return ins
                P.op("pe", gm, reads=[("vln", s), "wsT"], writes=[("B", 1)])
                P.op("dve", lambda e: e.tensor_tensor(gtmp[:].rearrange("p (g c) -> p g c", g=4),
                                                      B[1][:, 0:256].rearrange("p (g c) -> p g c", g=4),
                                                      bc_last(gmb[:], 64), ALU.add),
                     reads=[("B", 1), "gmb"], writes=["gtmp"])
                P.op("dve", lambda e: e.tensor_tensor(mixtok[ms][:, 512:768], gtmp[:], gut[s][:], ALU.mult),
                     reads=["gtmp", ("gut", s)], writes=[("mixtok", ms, 2)])
                def tpm(e):
                    ins = None
                    for i in range(6):
                        ins = e.transpose(Bbf[:, i * 128:(i + 1) * 128], mixtok[ms][:, i * 128:(i + 1) * 128], ident_b[:])
                    return ins
                P.op("pe", tpm, reads=[("mixtok", ms, i) for i in range(3)] + ["ident_b"], writes=["Bbf"])
                P.op("act", lambda e: e.activation(out=mixT[ms][:, 0:4, :], in_=Bbf[:, 0:512].rearrange("p (c t) -> p c t", c=4), func=AF.Copy),
                     reads=["Bbf"], writes=[("mixT", ms, i) for i in range(4)])
                P.op("act", lambda e: e.activation(out=mixT[ms][:, 6:8, :], in_=Bbf[:, 512:768].rearrange("p (c t) -> p c t", c=2), func=AF.Copy),
                     reads=["Bbf"], writes=[("mixT", ms, 6), ("mixT", ms, 7)])
                for hf in range(2):
                    def op_(e, hf=hf):
                        ins = None
                        for k in range(KC):
                            ins = e.matmul(B[hf][:, :], lhsT=mixT[ms][:, k, :], rhs=wout[:, k, hf * 512:(hf + 1) * 512],
                                           start=(k == 0), stop=(k == KC - 1))
                        return ins
                    P.op("pe", op_, reads=[("mixT", ms, i) for i in range(8)] + ["wout"], writes=[("B", hf)])
                    P.op("dve", lambda e, hf=hf: e.tensor_tensor(bufA[:, hf * 512:(hf + 1) * 512], B[hf][:, :],
                                                                g1b[col][:, hf * 512:(hf + 1) * 512], ALU.mult),
                         reads=[("B", hf), ("g1b", col)], writes=[("bufA", hf)])
                layer_norm_tail(xres[xs], ("xres", xs), lng, lnb, x1buf[t * 128:(t + 1) * 128, :], ("x1buf", t))

            def layer_norm_tail(xt, xres_name, g_t, b_t, dst, dst_name):
                P.op("dve", lambda e: e.scalar_tensor_tensor(bufA[:], xt[:], ALPHA, bufA[:], ALU.mult, ALU.add),
                     reads=[xres_name, ("bufA", 0), ("bufA", 1)], writes=[("bufA", 0), ("bufA", 1)])
                P.op("dve", lambda e: e.bn_stats(st12[:, 0:6], bufA[:, 0:512]), reads=[("bufA", 0)], writes=[("st12", 0)])
                P.op("dve", lambda e: e.bn_stats(st12[:, 6:12], bufA[:, 512:1024]), reads=[("bufA", 1)], writes=[("st12", 1)])
                P.op("dve", lambda e: e.bn_aggr(mv2[:], st12[:]), reads=[("st12", 0), ("st12", 1)], writes=["mv2"])
                P.op("act", lambda e: e.activation(out=rstd2[:], in_=mv2[:, 1:2], func=AF.Sqrt, bias=LN_EPS),
                     reads=["mv2"], writes=["rstd2"])
                P.op("dve", lambda e: e.reciprocal(rstd2[:], rstd2[:]), reads=["rstd2"], writes=["rstd2"])
                P.op("dve", lambda e: e.scalar_tensor_tensor(nb2[:], mv2[:, 0:1], -1.0, rstd2[:], ALU.mult, ALU.mult),
                     reads=["mv2", "rstd2"], writes=["nb2"])
                P.op("act", lambda e: e.activation(out=bufB[:], in_=bufA[:], func=AF.Identity, scale=rstd2[:, 0:1], bias=nb2[:, 0:1]),
                     reads=[("bufA", 0), ("bufA", 1), "rstd2", "nb2"], writes=["bufB"])
                P.op("pool", lambda e: e.tensor_tensor(bufB[:], bufB[:], g_t[:], ALU.mult), reads=["bufB", "lng"], writes=["bufB"])
                P.op("pool", lambda e: e.tensor_tensor(bufB[:], bufB[:], b_t[:], ALU.add), reads=["bufB", "lnb"], writes=["bufB"])
                P.dma("sp", dst, bufB[:], reads=["bufB"], writes=[dst_name])

            for t in range(NB, NT):
                inproj(t, kv_only=last)
            if not last:
                for t in range(NB, NT):
                    mixers(t)
            inproj(0)
            if NB > 1:
                inproj(1)
            for n in range(NB):
                la = P.record(mixers, n)
                lb = P.record(inproj, n + 2) if n + 2 < NB else []
                P.play(la, lb)
            P.flush()
        if stop == "p1":
            return nc

        tiles = list(range(NB)) if last else list(range(NT))
        nt2 = len(tiles)
        NBLK = (2 * nt2 * 128) // BS + NEXP
        AX = mybir.AxisListType.X
        w1v = w1.rearrange("l e p (a k) n -> (l e p a) (k n)", a=4)
        w3v = w3.rearrange("l e p (a k) n -> (l e p a) (k n)", a=4)
        w2v = w2.rearrange("l e p (a k) n -> (l e p a) (k n)", a=4)
        with ExitStack() as st:
            wr_s = alloc(st, "wr_s", [128, KC, 36], F32)
            wr_m = [alloc(st, "wr_m%d" % c, [128, KC, 36], F32) for c in range(2)]
            sh2rep = [alloc(st, "sh2rep%d" % c, [128, KC, 128], F32) for c in range(2)]
            br_row = alloc(st, "br_row", [1, 36], F32)
            lng = alloc(st, "lng2", [128, D], F32)
            lnb = alloc(st, "lnb2", [128, D], F32)
            scb = [alloc(st, "scb%d" % c, [128, D], F32) for c in range(2)]
            shb = [alloc(st, "shb%d" % c, [128, D], F32) for c in range(2)]
            g2b = [alloc(st, "g2b%d" % c, [128, D], F32) for c in range(2)]
            lt_b = alloc(st, "lt_b", [128, 128], BF16)
            ones_b = alloc(st, "ones_b", [128, 128], BF16)
            jvec = alloc(st, "jvec", [128, NBLK_MAX], F32)
            base1 = alloc(st, "base1", [128, 8], F32)
            base2 = alloc(st, "base2", [128, 4], F32)
            x1t = [alloc(st, "x1t%d" % i, [128, D], F32) for i in range(2)]
            x1T = alloc(st, "x1T", [128, KC, 128], F32)
            h2r = [alloc(st, "h2r%d" % i, [128, D], BF16) for i in range(2)]
            lg = alloc(st, "lg", [128, 36], F32)
            sm = alloc(st, "sm", [128, 16], F32)
            goh = alloc(st, "goh", [128, 4], F32)
            gex = alloc(st, "gex", [128, 4], F32)
            esel = alloc(st, "esel", [128, 8], F32)
            esel2 = alloc(st, "esel2", [128, 8], F32)
            eq1 = alloc(st, "eq1", [128, 8], F32)
            eq2 = alloc(st, "eq2", [128, 8], F32)
            oh0 = alloc(st, "oh0", [128, nt2, 32], F32)
            oh1 = alloc(st, "oh1", [128, nt2, 32], F32)
            cntb = alloc(st, "cntb", [128, nt2, 32], BF16)
            gsc = alloc(st, "gsc", [128, nt2, 2], F32)
            rank_s = alloc(st, "rank_s", [128, nt2, 32], F32)
            dtmp = alloc(st, "dtmp", [128, nt2, 32], F32)
            tot = alloc(st, "tot", [128, 32], F32)
            nblk = alloc(st, "nblk", [128, 32], F32)
            sc_a = alloc(st, "sc_a", [128, 32], F32)
            sc_b = alloc(st, "sc_b", [128, 32], F32)
            pstart = alloc(st, "pstart", [128, 32], F32)
            destf = alloc(st, "destf", [128, 2, nt2], F32)
            idx = alloc(st, "idx", [128, 2, nt2], mybir.dt.int32)
            bef = alloc(st, "bef", [128, NBLK_MAX], F32)
            be1 = alloc(st, "be1", [128, NBLK_MAX], F32)
            wif1 = alloc(st, "wif1", [128, NBLK_MAX, 4], F32)
            wi1 = alloc(st, "wi1", [128, NBLK_MAX, 4], mybir.dt.int32)
            w1s = [alloc(st, "w1s%d" % i, [128, KC, DEXP], BF16) for i in range(2)]
            w3s = [alloc(st, "w3s%d" % i, [128, KC, DEXP], BF16) for i in range(2)]
            w2s = [alloc(st, "w2s%d" % i, [128, 4, D], BF16) for i in range(2)]
            xsb = [alloc(st, "xsb%d" % i, [128, 4, D], BF16) for i in range(2)]
            hTb = [alloc(st, "hTb%d" % i, [128, KC, 512], BF16) for i in range(2)]
            sg = [alloc(st, "sg%d" % i, [128, 512], F32) for i in range(2)]
            actT = [alloc(st, "actT%d" % i, [128, 4, 512], BF16) for i in range(2)]
            ysb = [alloc(st, "ysb%d" % i, [128, D], F32) for i in range(2)]
            yg = ysb
            bufA = alloc(st, "bufA2", [128, D], F32)
            bufB = alloc(st, "bufB2", [128, D], F32)
            st12 = alloc(st, "st12b", [128, 12], F32)
            mv2 = alloc(st, "mv2b", [128, 2], F32)
            rstd2 = alloc(st, "rstd2b", [128, 1], F32)
            nb2 = alloc(st, "nb2b", [128, 1], F32)
            B = [None, None] + [palloc(st, "M%d" % i, [128, 512]) for i in range(2, 8)]
            TB = [palloc(st, "TB%d" % i, [128, 1024], BF16) for i in range(2)]

            P.dma("sp", wr_s[:], w_r[l].rearrange("(k p) n -> p k n", p=128), writes=["wr_s"])
            P.dma("sp", br_row[:], b_r[l:l + 1, :], writes=["br_row"])
            P.dma("sp", lng[:], ln2_g[l:l + 1, :].to_broadcast([128, D]), writes=["lng"])
            P.dma("sp", lnb[:], ln2_b[l:l + 1, :].to_broadcast([128, D]), writes=["lnb"])
            for c in range(2):
                P.dma("sp", shb[c][:], modrows[l, c, 1:2, :].to_broadcast([128, D]), writes=[("shb", c)])
                P.dma("sp", scb[c][:], modrows[l, c, 2:3, :].to_broadcast([128, D]), writes=[("scb", c)])
                P.dma("sp", g2b[c][:], modrows[l, c, 3:4, :].to_broadcast([128, D]), writes=[("g2b", c)])
            P.dma("pool", lt_b[:], lt_d[:, :], writes=["lt_b"])
            P.dma("sp", jvec[:], jvec_d[:, :], writes=["jvec"])
            P.dma("sp", base1[:], base1_d[:, :], writes=["base1"])
            P.dma("sp", base2[:], base2_d[:, :], writes=["base2"])
            P.op("pool", lambda e: e.memset(ones_b[:], 1.0), writes=["ones_b"])
            P.op("pool", lambda e: e.memset(xsb[1][:], 0.0), writes=[("xsb", 1)])
            zres = []
            for i in range(NBLK):
                P.dma("sp", xs_dram[i * BS:(i + 1) * BS, :].rearrange("(a p) d -> p a d", p=128), xsb[1][:],
                      reads=[("xsb", 1)], writes=[("xsz", i)])
                zres.append(("xsz", i))
            for c in range(2):
                P.op("dve", lambda e, c=c: e.tensor_tensor(wr_m[c][:], wr_s[:], bc_last(modT[:, 32:40, c], 36), ALU.mult),
                     reads=["wr_s", "modT"], writes=[("wr_m", c)])
                P.op("dve", lambda e, c=c: e.tensor_copy(sh2rep[c][:], bc_last(modT[:, 24:32, c], 128)),
                     reads=["modT"], writes=[("sh2rep", c)])

            st12s = [st12, alloc(st, "st12c", [128, 12], F32)]
            mv2s = [mv2, alloc(st, "mv2c", [128, 2], F32)]
            rstd2s = [rstd2, alloc(st, "rstd2c", [128, 1], F32)]
            nb2s = [nb2, alloc(st, "nb2c", [128, 1], F32)]

            def ln_tail2(xt, xname, dst, dname, bA, bB, se):
                nA, nB = ("bufA", se), ("bufB", se)
                s12, m2, r2, n2 = st12s[se], mv2s[se], rstd2s[se], nb2s[se]
                P.op("dve", lambda e: e.scalar_tensor_tensor(bA[:], xt[:], ALPHA, bA[:], ALU.mult, ALU.add),
                     reads=[xname, nA], writes=[nA])
                P.op("dve", lambda e: e.bn_stats(s12[:, 0:6], bA[:, 0:512]), reads=[nA], writes=[("st12", se, 0)])
                P.op("dve", lambda e: e.bn_stats(s12[:, 6:12], bA[:, 512:1024]), reads=[nA], writes=[("st12", se, 1)])
                P.op("dve", lambda e: e.bn_aggr(m2[:], s12[:]), reads=[("st12", se, 0), ("st12", se, 1)], writes=[("mv2", se)])
                P.op("act", lambda e: e.activation(out=r2[:], in_=m2[:, 1:2], func=AF.Sqrt, bias=LN_EPS),
                     reads=[("mv2", se)], writes=[("rstd2", se)])
                P.op("dve", lambda e: e.reciprocal(r2[:], r2[:]), reads=[("rstd2", se)], writes=[("rstd2", se)])
                P.op("dve", lambda e: e.scalar_tensor_tensor(n2[:], m2[:, 0:1], -1.0, r2[:], ALU.mult, ALU.mult),
                     reads=[("mv2", se), ("rstd2", se)], writes=[("nb2", se)])
                P.op("act", lambda e: e.activation(out=bB[:], in_=bA[:], func=AF.Identity, scale=r2[:, 0:1], bias=n2[:, 0:1]),
                     reads=[nA, ("rstd2", se), ("nb2", se)], writes=[nB])
                P.op("pool", lambda e: e.tensor_tensor(bB[:], bB[:], lng[:], ALU.mult), reads=[nB, "lng"], writes=[nB])
                P.op("pool", lambda e: e.tensor_tensor(bB[:], bB[:], lnb[:], ALU.add), reads=[nB, "lnb"], writes=[nB])
                P.dma("sp", dst, bB[:], reads=[nB], writes=[dname])

            def v1024(tn, nm):
                if nt2 * 32 >= 1024:
                    return tn[:].rearrange("p a b -> p (a b)")[:, 0:1024]
                return alloc(st, nm, [128, D], F32)[:]
            rank_v, dtmp_v, oh0_v = v1024(rank_s, "rank_v"), v1024(dtmp, "dtmp_v"), v1024(oh0, "oh0_v")
            x1Ts = [x1T, rank_v.rearrange("p (k t) -> p k t", k=KC)]
            bufAs = [bufA, dtmp_v]
            lgs = [lg, alloc(st, "lg_b", [128, 36], F32)]
            sms = [sm, alloc(st, "sm_b", [128, 16], F32)]
            gohs = [goh, alloc(st, "goh_b", [128, 4], F32)]
            gexs = [gex, alloc(st, "gex_b", [128, 4], F32)]
            esels = [esel, alloc(st, "esel_b", [128, 8], F32)]
            esel2s = [esel2, alloc(st, "esel2_b", [128, 8], F32)]
            eq1s = [eq1, alloc(st, "eq1_b", [128, 8], F32)]
            eq2s = [eq2, alloc(st, "eq2_b", [128, 8], F32)]

            def stageA(li):
                t = tiles[li]
                col = 0 if t < NB else 1
                xs = li % 2
                se = li % 2
                tpb = 6 if se == 0 else 4
                rtb = 2 if se == 0 else 3
                P.dma("sp", x1t[xs][:], x1buf[t * 128:(t + 1) * 128, :], writes=[("x1t", xs)])
                P.op("dve", lambda e, xs=xs, col=col: e.tensor_tensor(bufAs[se][:], x1t[xs][:], scb[col][:], ALU.mult),
                     reads=[("x1t", xs), ("scb", col)], writes=[("bufA", se)])
                P.op("pool", lambda e, xs=xs, col=col: e.tensor_tensor(h2r[xs][:], bufAs[se][:], shb[col][:], ALU.add),
                     reads=[("bufA", se), ("shb", col)], writes=[("h2r", xs)])
                P.dma("sp", h2buf[li * 128:(li + 1) * 128, :], h2r[xs][:], reads=[("h2r", xs)], writes=[("h2buf", li)])
                for hb in range(2):
                    def tp(e, hb=hb, xs=xs):
                        ins = None
                        for i in range(4):
                            j = hb * 4 + i
                            ins = e.transpose(B[tpb + hb][:, i * 128:(i + 1) * 128], x1t[xs][:, j * 128:(j + 1) * 128], ident_f[:])
                        return ins
                    P.op("pe", tp, reads=[("x1t", xs), "ident_f"], writes=[("B", tpb + hb)])

                    def cpx(e, hb=hb):
                        ins = None
                        for i in range(4):
                            ins = e.activation(out=x1Ts[se][:, hb * 4 + i, :], in_=B[tpb + hb][:, i * 128:(i + 1) * 128], func=AF.Copy)
                        return ins
                    P.op("act", cpx, reads=[("B", tpb + hb)], writes=[("x1T", se, hb)])

                def rt(e, col=col):
                    ins = None
                    for k in range(KC):
                        ins = e.matmul(B[rtb][:, 0:36], lhsT=x1Ts[se][:, k, :], rhs=wr_m[col][:, k, :], start=(k == 0), stop=False)
                    for k in range(KC):
                        ins = e.matmul(B[rtb][:, 0:36], lhsT=sh2rep[col][:, k, :], rhs=wr_s[:, k, :], start=False, stop=False)
                    ins = e.matmul(B[rtb][:, 0:36], lhsT=ones_f[0:1, :], rhs=br_row[0:1, :], start=False, stop=True)
                    return ins
                P.op("pe", rt, reads=[("x1T", se, 0), ("x1T", se, 1), ("wr_m", col), ("sh2rep", col), "wr_s", "br_row", "ones_f"], writes=[("B", rtb)])
                P.op("act", lambda e: e.activation(out=lgs[se][:], in_=B[rtb][:, 0:36], func=AF.Copy), reads=[("B", rtb)], writes=[("lg", se)])
                P.op("dve", lambda e: e.reduce_max(sms[se][:, 0:1], lgs[se][:, 0:4], AX), reads=[("lg", se)], writes=[("sm", se)])
                P.op("dve", lambda e: e.tensor_scalar(sms[se][:, 1:2], sms[se][:, 0:1], -1.0, None, ALU.mult), reads=[("sm", se)], writes=[("sm", se)])
                P.op("act", lambda e: e.activation(out=gexs[se][:], in_=lgs[se][:, 0:4], func=AF.Exp, bias=sms[se][:, 1:2], accum_out=sms[se][:, 2:3]),
                     reads=[("lg", se), ("sm", se)], writes=[("gex", se), ("sm", se)])
                P.op("dve", lambda e: e.reciprocal(sms[se][:, 3:4], sms[se][:, 2:3]), reads=[("sm", se)], writes=[("sm", se)])
                P.op("dve", lambda e: e.tensor_scalar(gohs[se][:], lgs[se][:, 0:4], sms[se][:, 0:1], None, ALU.is_equal), reads=[("lg", se), ("sm", se)], writes=[("goh", se)])
                P.op("dve", lambda e: e.tensor_scalar(esels[se][:], lgs[se][:, 4:12], gohs[se][:, 0:1], None, ALU.mult), reads=[("lg", se), ("goh", se)], writes=[("esel", se)])
                for g in range(1, 4):
                    P.op("dve", lambda e, g=g: e.scalar_tensor_tensor(esels[se][:], lgs[se][:, 4 + g * 8:12 + g * 8], gohs[se][:, g:g + 1], esels[se][:], ALU.mult, ALU.add),
                         reads=[("lg", se), ("goh", se), ("esel", se)], writes=[("esel", se)])
                P.op("dve", lambda e: e.reduce_max(sms[se][:, 4:5], esels[se][:], AX), reads=[("esel", se)], writes=[("sm", se)])
                P.op("dve", lambda e: e.tensor_scalar(eq1s[se][:], esels[se][:], sms[se][:, 4:5], None, ALU.is_equal), reads=[("esel", se), ("sm", se)], writes=[("eq1", se)])
                P.op("dve", lambda e: e.scalar_tensor_tensor(esel2s[se][:], eq1s[se][:], -1e30, esels[se][:], ALU.mult, ALU.add), reads=[("eq1", se), ("esel", se)], writes=[("esel2", se)])
                P.op("dve", lambda e: e.reduce_max(sms[se][:, 5:6], esel2s[se][:], AX), reads=[("esel2", se)], writes=[("sm", se)])
                P.op("dve", lambda e: e.tensor_scalar(eq2s[se][:], esel2s[se][:], sms[se][:, 5:6], None, ALU.is_equal), reads=[("esel2", se), ("sm", se)], writes=[("eq2", se)])
                P.op("dve", lambda e: e.tensor_tensor(sms[se][:, 6:7], sms[se][:, 4:5], sms[se][:, 5:6], ALU.subtract), reads=[("sm", se)], writes=[("sm", se)])
                P.op("act", lambda e: e.activation(out=sms[se][:, 7:8], in_=sms[se][:, 6:7], func=AF.Sigmoid), reads=[("sm", se)], writes=[("sm", se)])
                P.op("act", lambda e: e.activation(out=sms[se][:, 8:9], in_=sms[se][:, 6:7], func=AF.Sigmoid, scale=-1.0), reads=[("sm", se)], writes=[("sm", se)])
                P.op("dve", lambda e, li=li: e.tensor_scalar(gsc[:, li, :], sms[se][:, 7:9], sms[se][:, 3:4], None, ALU.mult), reads=[("sm", se)], writes=[("gsc", li)])
                P.op("dve", lambda e, li=li: e.tensor_tensor(oh0[:, li, :].rearrange("p (g x) -> p g x", g=4), bc_last(gohs[se][:], 8), bc_mid(eq1s[se][:], 4), ALU.mult),
                     reads=[("goh", se), ("eq1", se)], writes=[("oh0", li)])
                P.op("dve", lambda e, li=li: e.tensor_tensor(oh1[:, li, :].rearrange("p (g x) -> p g x", g=4), bc_last(gohs[se][:], 8), bc_mid(eq2s[se][:], 4), ALU.mult),
                     reads=[("goh", se), ("eq2", se)], writes=[("oh1", li)])
                P.op("pool", lambda e, li=li: e.tensor_tensor(cntb[:, li, :], oh0[:, li, :], oh1[:, li, :], ALU.add),
                     reads=[("oh0", li), ("oh1", li)], writes=[("cntb", li)])


            for li0 in range(0, nt2, 2):
                P.play(P.record(stageA, li0), P.record(stageA, li0 + 1) if li0 + 1 < nt2 else [])
            if stop == "p2a":
                P.flush()
                return nc
            call = [("cntb", i) for i in range(nt2)]
            nbank = (nt2 + 15) // 16
            for li in range(nt2):
                bk, off = 3 + li // 16, (li % 16) * 32

                def rk(e, li=li, bk=bk, off=off):
                    ins = None
                    for tp_ in range(li):
                        ins = e.matmul(B[bk][:, off:off + 32], lhsT=ones_b[:], rhs=cntb[:, tp_, :], start=(tp_ == 0), stop=False)
                    ins = e.matmul(B[bk][:, off:off + 32], lhsT=lt_b[:], rhs=cntb[:, li, :], start=(li == 0), stop=True)
                    return ins
                P.op("pe", rk, reads=call + ["ones_b", "lt_b"], writes=[("RK", li)])
            for bk in range(nbank):
                n_here = min(16, nt2 - bk * 16)
                P.op("act", lambda e, bk=bk, n_here=n_here: e.activation(
                    out=rank_s[:, bk * 16:bk * 16 + n_here, :], in_=B[3 + bk][:, 0:n_here * 32].rearrange("p (t x) -> p t x", x=32), func=AF.Copy),
                    reads=[("RK", i) for i in range(bk * 16, bk * 16 + n_here)], writes=[("rank_s", bk)])

            def tt(e):
                ins = None
                for tp_ in range(nt2):
                    ins = e.matmul(B[2][:, 64:96], lhsT=ones_b[:], rhs=cntb[:, tp_, :], start=(tp_ == 0), stop=(tp_ == nt2 - 1))
                return ins
            P.op("pe", tt, reads=call + ["ones_b"], writes=[("B", 2)])
            P.op("act", lambda e: e.activation(out=tot[:], in_=B[2][:, 64:96], func=AF.Copy), reads=[("B", 2)], writes=["tot"])
            P.op("dve", lambda e: e.tensor_scalar(nblk[:], tot[:], 0.0, None, ALU.is_gt), reads=["tot"], writes=["nblk"])
            for m in range(1, (2 * nt2 * 128) // BS + 1):
                P.op("dve", lambda e, m=m: e.scalar_tensor_tensor(nblk[:], tot[:], float(m * BS), nblk[:], ALU.is_gt, ALU.add),
                     reads=["tot", "nblk"], writes=["nblk"])
            P.op("dve", lambda e: e.tensor_copy(sc_a[:], nblk[:]), reads=["nblk"], writes=["sc_a"])
            cur, oth, cn, on = sc_a, sc_b, "sc_a", "sc_b"
            for dd in (1, 2, 4, 8, 16):
                P.op("dve", lambda e, cur=cur, oth=oth, dd=dd: e.tensor_copy(oth[:, 0:dd], cur[:, 0:dd]), reads=[cn], writes=[on])
                P.op("dve", lambda e, cur=cur, oth=oth, dd=dd: e.tensor_tensor(oth[:, dd:32], cur[:, dd:32], cur[:, 0:32 - dd], ALU.add),
                     reads=[cn, on], writes=[on])
                cur, oth, cn, on = oth, cur, on, cn
            pend, pendn = cur, cn
            P.op("dve", lambda e: e.tensor_tensor(pstart[:], pend[:], nblk[:], ALU.subtract), reads=[pendn, "nblk"], writes=["pstart"])
            P.op("dve", lambda e: e.tensor_scalar(pstart[:], pstart[:], float(BS), None, ALU.mult), reads=["pstart"], writes=["pstart"])
            rk_all = [("rank_s", i) for i in range(nbank)]
            P.op("dve", lambda e: e.tensor_tensor(rank_s[:], rank_s[:], bc_mid(pstart[:], nt2), ALU.add),
                 reads=rk_all + ["pstart"], writes=rk_all)
            for k, oh in enumerate((oh0, oh1)):
                P.op("dve", lambda e, oh=oh: e.tensor_tensor(dtmp[:], oh[:], rank_s[:], ALU.mult),
                     reads=rk_all + [("oh%d" % k, i) for i in range(nt2)], writes=["dtmp"])
                P.op("dve", lambda e, k=k: e.reduce_sum(destf[:, k, :], dtmp[:], AX), reads=["dtmp"], writes=[("destf", k)])
            P.op("dve", lambda e: e.tensor_copy(idx[:], destf[:]), reads=[("destf", 0), ("destf", 1)], writes=["idx"])
            P.op("dve", lambda e: e.tensor_scalar(bef[:], jvec[:], pend[:, 0:1], None, ALU.is_ge), reads=["jvec", pendn], writes=["bef"])
            for ee in range(1, 32):
                P.op("dve", lambda e, ee=ee: e.scalar_tensor_tensor(bef[:], jvec[:], pend[:, ee:ee + 1], bef[:], ALU.is_ge, ALU.add),
                     reads=["jvec", pendn, "bef"], writes=["bef"])
            P.op("dve", lambda e: e.tensor_scalar(bef[:], bef[:], 31.0, None, ALU.min), reads=["bef"], writes=["bef"])
            P.op("dve", lambda e: e.tensor_scalar(be1[:], bef[:], 128.0, float(l * NEXP * 128), ALU.mult, ALU.add), reads=["bef"], writes=["be1"])
            P.op("dve", lambda e: e.tensor_scalar(be1[:], be1[:], base1[:, 0:1], None, ALU.add), reads=["be1", "base1"], writes=["be1"])
            P.op("dve", lambda e: e.tensor_scalar(be1[:], be1[:], 4.0, None, ALU.mult), reads=["be1"], writes=["be1"])
            P.op("dve", lambda e: e.tensor_tensor(wif1[:], bc_last(be1[:], 4), bc_mid(base2[:], NBLK_MAX), ALU.add),
                 reads=["be1", "base2"], writes=["wif1"])
            P.op("dve", lambda e: e.tensor_copy(wi1[:], wif1[:]), reads=["wif1"], writes=["wi1"])
            if stop == "p2c":
                P.flush()
                return nc
            sres = []
            for li in range(nt2):
                hs_ = li % 2
                P.dma("sp", h2r[hs_][:], h2buf[li * 128:(li + 1) * 128, :], reads=[("h2buf", li)], writes=[("h2r", hs_)])
                for k in range(2):
                    P.idma(xs_dram[0:NBLK * BS, :], bass.IndirectOffsetOnAxis(ap=idx[:, k, li:li + 1], axis=0), h2r[hs_][:], None,
                           reads=[("h2r", hs_), "idx"] + zres, writes=[("xss", li, k)])
                    sres.append(("xss", li, k))

            if stop == "p2d":
                P.flush()
                return nc
            yres = []

            def blockA(j):
                ws = j % 2
                if not DBG.get("no_wgather"):
                    for a in range(4):
                        io = bass.IndirectOffsetOnAxis(ap=wi1[:, j, a:a + 1], axis=0)
                        P.idma(w1s[ws][:, 2 * a:2 * a + 2, :].rearrange("p k n -> p (k n)"), None, w1v, io, reads=["wi1"], writes=[("w1s", ws, a)])
                        P.idma(w3s[ws][:, 2 * a:2 * a + 2, :].rearrange("p k n -> p (k n)"), None, w3v, io, reads=["wi1"], writes=[("w3s", ws, a)])
                        P.idma(w2s[ws][:, a, :], None, w2v, io, reads=["wi1"], writes=[("w2s", ws, a)])
                P.dma("sp", xsb[ws][:], xs_dram[j * BS:(j + 1) * BS, :].rearrange("(a p) d -> p a d", p=128),
                      reads=sres + zres, writes=[("xsb", ws)])
                for a in range(4):
                    tb = a % 2

                    def tps_(e, a=a, tb=tb, ws=ws):
                        ins = None
                        for k in range(KC):
                            ins = e.transpose(TB[tb][:, k * 128:(k + 1) * 128], xsb[ws][:, a, k * 128:(k + 1) * 128], ident_b[:])
                        return ins
                    P.op("pe", tps_, reads=[("xsb", ws), "ident_b"], writes=[("TB", tb)])
                    P.op("act", lambda e, a=a, tb=tb, ws=ws: e.activation(out=hTb[ws][:, :, a * 128:(a + 1) * 128],
                                                                         in_=TB[tb][:, :].rearrange("p (k t) -> p k t", k=KC), func=AF.Copy),
                         reads=[("TB", tb)], writes=[("hTb", ws, a)])

            def blockB(j):
                ws = j % 2
                hres = [("hTb", ws, a) for a in range(4)]
                w13 = [("w1s", ws, a) for a in range(4)] + [("w3s", ws, a) for a in range(4)]
                for c in range(4):
                    pb = 2 + 2 * (c % 2)

                    def h13(e, c=c, pb=pb, ws=ws):
                        ins = None
                        for k in range(KC):
                            ins = e.matmul(B[pb][:, :], lhsT=w1s[ws][:, k, c * 128:(c + 1) * 128], rhs=hTb[ws][:, k, :],
                                           start=(k == 0), stop=(k == KC - 1))
                        for k in range(KC):
                            ins = e.matmul(B[pb + 1][:, :], lhsT=w3s[ws][:, k, c * 128:(c + 1) * 128], rhs=hTb[ws][:, k, :],
                                           start=(k == 0), stop=(k == KC - 1))
                        return ins
                    P.op("pe", h13, reads=w13 + hres, writes=[("B", pb), ("B", pb + 1)])
                    P.op("act", lambda e, c=c, pb=pb: e.activation(out=sg[c % 2][:], in_=B[pb][:, :], func=AF.Silu),
                         reads=[("B", pb)], writes=[("sg", c % 2)])
                    P.op("dve", lambda e, c=c, pb=pb, ws=ws: e.tensor_tensor(actT[ws][:, c, :], sg[c % 2][:], B[pb + 1][:, :], ALU.mult),
                         reads=[("sg", c % 2), ("B", pb + 1)], writes=[("actT", ws, c)])
                for a in range(4):
                    ys_ = a % 2
                    for hf in range(2):
                        yb = 6 + hf

                        def ymm(e, a=a, hf=hf, yb=yb, ws=ws):
                            ins = None
                            for c in range(4):
                                ins = e.matmul(B[yb][:, :], lhsT=actT[ws][:, c, a * 128:(a + 1) * 128],
                                               rhs=w2s[ws][:, c, hf * 512:(hf + 1) * 512], start=(c == 0), stop=(c == 3))
                            return ins
                        P.op("pe", ymm, reads=[("actT", ws, c) for c in range(4)] + [("w2s", ws, c) for c in range(4)], writes=[("B", yb)])
                        P.op("dve", lambda e, hf=hf, yb=yb, ys_=ys_: e.tensor_copy(ysb[ys_][:, hf * 512:(hf + 1) * 512], B[yb][:, :]),
                             reads=[("B", yb)], writes=[("ysb", ys_, hf)])
                    r0 = j * BS + a * 128
                    P.dma("sp", ys_dram[r0:r0 + 128, :], ysb[ys_][:], reads=[("ysb", ys_, 0), ("ysb", ys_, 1)], writes=[("ys", j, a)])
                    yres.append(("ys", j, a))


            P.play(P.record(blockA, 0))
            for j in range(NBLK):
                P.play(P.record(blockB, j), P.record(blockA, j + 1) if j + 1 < NBLK else [])
            if stop == "p2e":
                P.flush()
                return nc
            fA = [bufA, x1T[:].rearrange("p a b -> p (a b)")]
            fB = [bufB, dtmp_v]
            fY = [[ysb[0], ysb[1]], [rank_v, oh0_v]]

            def stageF(li):
                t = tiles[li]
                col = 0 if t < NB else 1
                xs = li % 2
                se = li % 2
                bA, bB, ygs = fA[se], fB[se], fY[se]
                nA = ("bufA", se)
                P.dma("sp", x1t[xs][:], x1buf[t * 128:(t + 1) * 128, :], writes=[("x1t", xs)])
                for k in range(2):
                    P.idma(ygs[k][:], None, ys_dram[0:NBLK * BS, :], bass.IndirectOffsetOnAxis(ap=idx[:, k, li:li + 1], axis=0),
                           reads=["idx"] + yres, writes=[("yg", se, k)])
                P.op("dve", lambda e: e.tensor_scalar(bA[:], ygs[0][:], gsc[:, li, 0:1], None, ALU.mult),
                     reads=[("yg", se, 0), ("gsc", li)], writes=[nA])
                P.op("dve", lambda e: e.scalar_tensor_tensor(bA[:], ygs[1][:], gsc[:, li, 1:2], bA[:], ALU.mult, ALU.add),
                     reads=[("yg", se, 1), ("gsc", li), nA], writes=[nA])
                P.op("pool", lambda e: e.tensor_tensor(bA[:], bA[:], g2b[col][:], ALU.mult),
                     reads=[nA, ("g2b", col)], writes=[nA])
                if last:
                    dst, dname = out_d[t * 128:(t + 1) * 128, :], ("out", t)
                else:
                    dst, dname = x2buf[t * 128:(t + 1) * 128, :], ("x2buf", t)
                ln_tail2(x1t[xs], ("x1t", xs), dst, dname, bA, bB, se)

            for li0 in range(0, nt2, 2):
                P.play(P.record(stageF, li0), P.record(stageF, li0 + 1) if li0 + 1 < nt2 else [])
            P.flush()
    glob.close()
    return nc


def _rope_tables(NB):
    NT = NB + NCB
    S = NB * 128
    m = 16
    freqs = (10000.0 ** (-np.arange(m, dtype=np.float32) / m)).astype(np.float32)
    tpos = np.arange(S)
    row = (tpos // GRID_W).astype(np.float32)
    colp = (tpos % GRID_W).astype(np.float32)
    cos = np.ones((128, NT * 128), np.float32)
    sin = np.zeros((128, NT * 128), np.float32)
    for p in range(128):
        d = p % 64
        pos = row if d < 32 else colp
        dd = d % 32
        f = freqs[dd % 16]
        ang = (pos * f).astype(np.float32)
        cos[p, :S] = np.cos(ang)
        sgn = -1.0 if dd < 16 else 1.0
        sin[p, :S] = sgn * np.sin(ang)
    cos_t = np.ascontiguousarray(cos.reshape(128, NT, 128).transpose(1, 0, 2))
    sin_t = np.ascontiguousarray(sin.reshape(128, NT, 128).transpose(1, 0, 2))
    return cos_t, sin_t


def make_in_maps(inputs, NB, ncores):
    f = lambda a: np.ascontiguousarray(np.asarray(a, dtype=np.float32))
    x = f(inputs["x"]); c = f(inputs["c"]); ctx = f(inputs["ctx"]); c_ctx = f(inputs["c_ctx"])
    depth = inputs["w_ada"].shape[0]
    cos_t, sin_t = _rope_tables(NB)
    j = np.arange(128)
    maskl = np.where(j[:, None] >= j[None, :], 0.0, NEG).astype(np.float32)
    maskr = np.where(j[:, None] <= j[None, :], 0.0, NEG).astype(np.float32)
    shared = {
        "w_ada": f(inputs["w_ada"]),
        "bada_p": f(np.asarray(inputs["b_ada"]).reshape(depth, 48, 128).transpose(0, 2, 1)),
        "b_ada": f(inputs["b_ada"]),
        "w_in": f(inputs["w_in"]),
        "convw": f(np.asarray(inputs["conv_w"]).reshape(depth, 3, 2, 128).transpose(0, 3, 2, 1)),
        "sink": f(inputs["attn_sink"]),
        "gm_wsT": f(np.asarray(inputs["gm_ws"]).transpose(0, 1, 3, 2)),
        "gm_bsp": f(np.asarray(inputs["gm_bs"]).transpose(0, 2, 1)),
        "w_out": f(inputs["w_out"]),
        "ln1_g": f(inputs["ln1_g"]), "ln1_b": f(inputs["ln1_b"]),
        "ln2_g": f(inputs["ln2_g"]), "ln2_b": f(inputs["ln2_b"]),
        "w_r": f(np.concatenate([np.asarray(inputs["w_rg"]), np.asarray(inputs["w_re"])], axis=-1)),
        "b_r": f(np.concatenate([np.asarray(inputs["b_rg"]), np.asarray(inputs["b_re"])], axis=-1)),
        "w1": f(np.asarray(inputs["w1"]).reshape(depth, NEXP, KC, 128, DEXP).transpose(0, 1, 3, 2, 4)),
        "w3": f(np.asarray(inputs["w3"]).reshape(depth, NEXP, KC, 128, DEXP).transpose(0, 1, 3, 2, 4)),
        "w2": f(np.asarray(inputs["w2"]).reshape(depth, NEXP, 4, 128, D).transpose(0, 1, 3, 2, 4)),
        "cos_t": cos_t, "sin_t": sin_t,
        "ident": np.eye(128, dtype=np.float32), "maskl": maskl, "maskr": maskr,
        "lt_d": (j[:, None] < j[None, :]).astype(np.float32),
        "jvec_d": np.tile(np.arange((2 * (NB + NCB) * 128) // 512 + NEXP, dtype=np.float32)[None, :], (128, 1)),
        "base1_d": (np.arange(8, dtype=np.float32)[None, :] * 128 + j[:, None]).astype(np.float32),
        "base2_d": np.tile(np.arange(4, dtype=np.float32)[None, :], (128, 1)),
    }
    maps = []
    for b in range(ncores):
        cv = np.stack([c[b].reshape(KC, 128).T, c_ctx.reshape(KC, 128).T], axis=-1)
        m = dict(shared)
        m["xin"] = np.ascontiguousarray(x[b])
        m["ctxin"] = np.ascontiguousarray(ctx[b])
        m["cvec"] = np.ascontiguousarray(cv.astype(np.float32))
        maps.append(m)
    return maps


_NC_CACHE = {}


def kernel(**inputs):
    x = np.asarray(inputs["x"])
    Bn, S, _ = x.shape
    NB = S // 128
    key = (NB,)
    if key not in _NC_CACHE:
        _NC_CACHE[key] = build_program(NB)
    nc = _NC_CACHE[key]
    maps = make_in_maps(inputs, NB, Bn)
    res = run_bass_kernel_spmd(nc, maps, core_ids=list(range(Bn)))
    return np.stack([np.asarray(r["out"], dtype=np.float32) for r in res.results], axis=0)
```

```python
from contextlib import ExitStack
import numpy as np
import ml_dtypes
import concourse.bass as bass
import concourse.mybir as mybir
from concourse.bass_utils import run_bass_kernel_spmd

F32 = mybir.dt.float32
BF16 = mybir.dt.bfloat16
AF = mybir.ActivationFunctionType
ALU = mybir.AluOpType

D = 1024
KC = 8
CTX = 256
NCB = CTX // 128
DEPTH = 2
NEXP = 32
DEXP = 512
ALPHA = (2 * DEPTH) ** 0.25
LN_EPS = 1e-6
GRID_W = 64
NEG = -30000.0
DBG = {}


class Prog:
    NDMA = 8

    def __init__(self, nc):
        self.nc = nc
        self.names = dict(pe="tensor", act="scalar", dve="vector", pool="gpsimd", sp="sync")
        self.sems = []
        self.semidx = {}
        for k in self.names:
            self.semidx[k] = len(self.sems)
            self.sems.append(nc.alloc_semaphore("s_" + k))
        self.cnt = {k: 0 for k in self.names}
        self.dq = {}
        for q in ("sp", "pool"):
            idx = []
            for i in range(self.NDMA):
                idx.append(len(self.sems))
                self.sems.append(nc.alloc_semaphore("d_%s%d" % (q, i)))
            self.dq[q] = dict(idx=idx, cnt=[0] * self.NDMA, nxt=0)
        self.seen = {k: {} for k in self.names}
        self.stream = {k: [] for k in self.names}
        self.res = {}
        self._cap = None

    def record(self, fn, *a, **kw):
        self._cap = []
        fn(*a, **kw)
        cap, self._cap = self._cap, None
        return cap

    def play(self, *lists):
        lists = [l for l in lists if l]
        pos = [0] * len(lists)
        while True:
            best, bf = None, None
            for i, l in enumerate(lists):
                if pos[i] < len(l):
                    f = pos[i] / len(l)
                    if bf is None or f < bf:
                        best, bf = i, f
            if best is None:
                break
            kind, a, kw = lists[best][pos[best]]
            pos[best] += 1
            getattr(self, kind)(*a, **kw)

    def _deps(self, reads, writes):
        deps = []
        for r in reads:
            e = self.res.get(r)
            if e and e["w"] is not None:
                deps.append(e["w"])
        for w in writes:
            e = self.res.get(w)
            if e:
                if e["w"] is not None:
                    deps.append(e["w"])
                deps.extend((si, v, src) for (si, src), v in e["r"].items())
        return deps

    def _waits(self, ek, deps):
        waits = {}
        for (si, val, src) in deps:
            if src == "pe" and ek == "pe":
                continue
            if self.seen[ek].get(si, 0) >= val:
                continue
            waits[si] = max(waits.get(si, 0), val)
        for si, v in waits.items():
            self.seen[ek][si] = v
        return list(waits.items())

    def _record(self, token, reads, writes):
        si, val, src = token
        for r in reads:
            e = self.res.setdefault(r, {"w": None, "r": {}})
            e["r"][(si, src)] = max(e["r"].get((si, src), 0), val)
        for w in writes:
            self.res[w] = {"w": token, "r": {}}

    def op(self, ek, fn, reads=(), writes=()):
        if self._cap is not None:
            self._cap.append(("op", (ek, fn), dict(reads=reads, writes=writes)))
            return None
        waits = self._waits(ek, self._deps(reads, writes))
        self.cnt[ek] += 1
        si = self.semidx[ek]
        token = (si, self.cnt[ek], ek)
        sems = self.sems

        def emit(eng):
            for (wsi, v) in waits:
                eng.wait_ge(sems[wsi], v)
            ins = fn(eng)
            ins.then_inc(sems[si], 1)

        self.stream[ek].append(emit)
        self._record(token, reads, writes)
        return token

    def dma(self, q, out, in_, reads=(), writes=()):
        if self._cap is not None:
            self._cap.append(("dma", (q, out, in_), dict(reads=reads, writes=writes)))
            return None
        d = self.dq[q]
        slot = d["nxt"]
        d["nxt"] = (slot + 1) % self.NDMA
        si = d["idx"][slot]
        deps = self._deps(reads, writes)
        if d["cnt"][slot] > 0:
            deps.append((si, 16 * d["cnt"][slot], "dma"))
        waits = self._waits(q, deps)
        d["cnt"][slot] += 1
        token = (si, 16 * d["cnt"][slot], "dma")
        sems = self.sems

        def emit(eng):
            for (wsi, v) in waits:
                eng.wait_ge(sems[wsi], v)
            eng.dma_start(out=out, in_=in_).then_inc(sems[si], 16)

        self.stream[q].append(emit)
        self._record(token, reads, writes)
        return token

    def idma(self, out, out_off, in_, in_off, reads=(), writes=()):
        if self._cap is not None:
            self._cap.append(("idma", (out, out_off, in_, in_off), dict(reads=reads, writes=writes)))
            return None
        q = "pool"
        d = self.dq[q]
        slot = d["nxt"]
        d["nxt"] = (slot + 1) % self.NDMA
        si = d["idx"][slot]
        deps = self._deps(reads, writes)
        if d["cnt"][slot] > 0:
            deps.append((si, 16 * d["cnt"][slot], "dma"))
        waits = self._waits(q, deps)
        d["cnt"][slot] += 1
        token = (si, 16 * d["cnt"][slot], "dma")
        sems = self.sems

        def emit(eng):
            for (wsi, v) in waits:
                eng.wait_ge(sems[wsi], v)
            eng.indirect_dma_start(out=out, out_offset=out_off, in_=in_, in_offset=in_off).then_inc(sems[si], 16)

        self.stream[q].append(emit)
        self._record(token, reads, writes)
        return token

    def flush(self):
        finals = []
        for k in self.names:
            if self.cnt[k] > 0:
                finals.append((self.semidx[k], self.cnt[k]))
        for q, d in self.dq.items():
            for i, si in enumerate(d["idx"]):
                if d["cnt"][i] > 0:
                    finals.append((si, 16 * d["cnt"][i]))
        with self.nc.Block() as block:
            for ek, nm in self.names.items():
                stream = self.stream[ek]
                seen = self.seen[ek]
                sems = self.sems

                def body(eng, stream=stream, seen=seen):
                    for emit in stream:
                        emit(eng)
                    for si, v in finals:
                        if seen.get(si, 0) < v:
                            eng.wait_ge(sems[si], v)
                            seen[si] = v

                getattr(block, nm)(body)
        self.stream = {k: [] for k in self.names}
        self.res = {}


def bc_mid(ap, n):
    p, f = ap.shape
    return ap.unsqueeze(1).to_broadcast([p, n, f])


def bc_last(ap, n):
    p, g = ap.shape
    return ap.unsqueeze(2).to_broadcast([p, g, n])


def build_program(NB, depth=DEPTH, tps=9, stop=None, nexp=NEXP, do_router=True, do_epi=True):
    S = NB * 128
    NT = NB + NCB
    T = NT * 128
    nc = bass.Bass("TRN2", target_bir_lowering=False)

    def din(name, shape, dt=F32):
        return nc.dram_tensor(name, list(shape), dt, kind="ExternalInput").ap()

    xin = din("xin", [S, D])
    ctxin = din("ctxin", [CTX, D])
    cvec = din("cvec", [128, KC, 2])
    w_ada = din("w_ada", [depth, D, 6 * D])
    bada_p = din("bada_p", [depth, 128, 48])
    b_ada = din("b_ada", [depth, 6 * D])
    w_in = din("w_in", [depth, D, 2048])
    convw = din("convw", [depth, 128, 2, 3])
    sink = din("sink", [depth, 8])
    gm_wsT = din("gm_wsT", [depth, 4, 128, 128])
    gm_bsp = din("gm_bsp", [depth, 128, 4])
    w_out = din("w_out", [depth, D, D])
    ln1_g = din("ln1_g", [depth, D])
    ln1_b = din("ln1_b", [depth, D])
    ln2_g = din("ln2_g", [depth, D])
    ln2_b = din("ln2_b", [depth, D])
    w_r = din("w_r", [depth, D, 36])
    b_r = din("b_r", [depth, 36])
    w1 = din("w1", [depth, NEXP, 128, KC, DEXP])
    w3 = din("w3", [depth, NEXP, 128, KC, DEXP])
    w2 = din("w2", [depth, NEXP, 128, 4, D])
    cos_t = din("cos_t", [NT, 128, 128])
    sin_t = din("sin_t", [NT, 128, 128])
    ident_d = din("ident", [128, 128])
    maskl_d = din("maskl", [128, 128])
    maskr_d = din("maskr", [128, 128])
    out_d = nc.dram_tensor("out", [S, D], F32, kind="ExternalOutput").ap()
    x1buf = nc.dram_tensor("x1buf", [T, D], F32).ap()
    x2buf = nc.dram_tensor("x2buf", [T, D], F32).ap()
    BS = 512
    NBLK_MAX = (2 * T) // BS + NEXP
    h2buf = nc.dram_tensor("h2buf", [T, D], BF16).ap()
    xs_dram = nc.dram_tensor("xs_dram", [NBLK_MAX * BS, D], BF16).ap()
    ys_dram = nc.dram_tensor("ys_dram", [NBLK_MAX * BS, D], F32).ap()
    modrows = nc.dram_tensor("modrows", [depth, 2, 4, D], F32).ap()
    lt_d = din("lt_d", [128, 128])
    jvec_d = din("jvec_d", [128, NBLK_MAX])
    base1_d = din("base1_d", [128, 8])
    base2_d = din("base2_d", [128, 4])

    P = Prog(nc)

    def tile_src(l, t):
        if l == 0:
            if t < NB:
                return xin[t * 128:(t + 1) * 128, :]
            return ctxin[(t - NB) * 128:(t - NB + 1) * 128, :]
        return x2buf[t * 128:(t + 1) * 128, :]

    glob = ExitStack()

    uid = [0]

    def alloc(st, name, shape, dt):
        uid[0] += 1
        return st.enter_context(nc.sbuf_tensor("sb%d_%s" % (uid[0], name), list(shape), dt))

    def palloc(st, name, shape, dt=F32):
        uid[0] += 1
        return st.enter_context(nc.psum_tensor("ps%d_%s" % (uid[0], name), list(shape), dt))

    ident_f = alloc(glob, "ident_f", [128, 128], F32)
    ident_b = alloc(glob, "ident_b", [128, 128], BF16)
    maskl = alloc(glob, "maskl", [128, 128], BF16)
    maskr = alloc(glob, "maskr", [128, 128], BF16)
    cact = alloc(glob, "cact", [128, KC, 2], F32)
    cact_rep = alloc(glob, "cact_rep", [128, KC, 2, 128], F32)
    ones_f = alloc(glob, "ones_f", [1, 128], F32)
    epsb = alloc(glob, "epsb", [128, 1], F32)
    modT = alloc(glob, "modT", [128, 48, 2], F32)

    P.dma("sp", ident_f[:], ident_d[:, :], writes=["ident_f"])
    P.dma("pool", ident_b[:], ident_d[:, :], writes=["ident_b"])
    P.dma("pool", maskl[:], maskl_d[:, :], writes=["maskl"])
    P.dma("pool", maskr[:], maskr_d[:, :], writes=["maskr"])
    P.dma("sp", cact[:], cvec[:, :, :], writes=["cact"])
    P.op("act", lambda e: e.activation(out=cact[:], in_=cact[:], func=AF.Silu), reads=["cact"], writes=["cact"])
    P.op("dve", lambda e: e.tensor_copy(cact_rep[:].rearrange("p k c m -> p (k c) m"),
                                        bc_last(cact[:].rearrange("p k c -> p (k c)"), 128)),
         reads=["cact"], writes=["cact_rep"])
    P.op("dve", lambda e: e.memset(ones_f[:], 1.0), writes=["ones_f"])
    P.op("dve", lambda e: e.memset(epsb[:], LN_EPS), writes=["epsb"])
    P.flush()
    if stop == "const":
        return nc

    for l in range(depth):
        last = (l == depth - 1)
        with ExitStack() as st:
            wa = [alloc(st, "wa%d" % i, [128, KC, D], F32) for i in range(2)]
            bada = alloc(st, "bada", [128, 48], F32)
            brow = [alloc(st, "brow%d" % i, [128, D], F32) for i in range(2)]
            grow = [alloc(st, "grow%d" % i, [128, D], F32) for i in range(2)]
            ps_mod = palloc(st, "ps_mod", [128, 96])
            ps_g = [palloc(st, "ps_g%d" % i, [128, 512]) for i in range(4)]
            P.dma("sp", bada[:], bada_p[l], writes=["bada"])
            for i in range(6):
                s = i % 2
                P.dma("sp", wa[s][:], w_ada[l][:, i * D:(i + 1) * D].rearrange("(k p) n -> p k n", p=128),
                      writes=[("wa", s)])

                def mm_mod(e, i=i, s=s):
                    ins = None
                    for j in range(KC):
                        for k in range(KC):
                            ins = e.matmul(ps_mod[:, (i * 8 + j) * 2:(i * 8 + j) * 2 + 2],
                                           lhsT=wa[s][:, k, j * 128:(j + 1) * 128], rhs=cact[:, k, :],
                                           start=(k == 0), stop=(k == KC - 1))
                    return ins
                P.op("pe", mm_mod, reads=[("wa", s), "cact"], writes=[("ps_mod", i)])
                if i >= 2:
                    gi = i - 2
                    P.dma("sp", brow[gi % 2][:], b_ada[l:l + 1, i * D:(i + 1) * D].to_broadcast([128, D]),
                          writes=[("brow", gi % 2)])
                    for c in range(2):
                        for h in range(2):
                            def mm_g(e, s=s, c=c, h=h):
                                ins = None
                                for k in range(KC):
                                    ins = e.matmul(ps_g[c * 2 + h][:, :], lhsT=cact_rep[:, k, c, :],
                                                   rhs=wa[s][:, k, h * 512:(h + 1) * 512],
                                                   start=(k == 0), stop=(k == KC - 1))
                                return ins
                            P.op("pe", mm_g, reads=[("wa", s), "cact_rep"], writes=[("ps_g", c * 2 + h)])
                            P.op("dve", lambda e, gi=gi, c=c, h=h: e.tensor_tensor(
                                grow[c][:, h * 512:(h + 1) * 512], ps_g[c * 2 + h][:, :],
                                brow[gi % 2][:, h * 512:(h + 1) * 512], ALU.add),
                                reads=[("ps_g", c * 2 + h), ("brow", gi % 2)], writes=[("grow", c, h)])
                        if i == 4:
                            P.op("dve", lambda e, c=c: e.tensor_scalar(grow[c][0:1, :], grow[c][0:1, :], 1.0, None, ALU.add),
                                 reads=[("grow", c, 0), ("grow", c, 1)], writes=[("grow", c, 0), ("grow", c, 1)])
                        P.dma("sp", modrows[l, c, gi:gi + 1, :], grow[c][0:1, :],
                              reads=[("grow", c, 0), ("grow", c, 1)], writes=[("modrows", c, gi)])
            P.op("dve", lambda e: e.tensor_tensor(modT[:], ps_mod[:].rearrange("p (j c) -> p j c", c=2),
                                                  bc_last(bada[:], 2), ALU.add),
                 reads=[("ps_mod", i) for i in range(6)] + ["bada"], writes=["modT"])
            P.op("dve", lambda e: e.tensor_scalar(modT[:, 8:16, :], modT[:, 8:16, :], 1.0, None, ALU.add),
                 reads=["modT"], writes=["modT"])
            P.op("dve", lambda e: e.tensor_scalar(modT[:, 32:40, :], modT[:, 32:40, :], 1.0, None, ALU.add),
                 reads=["modT"], writes=["modT"])
            P.flush()
        if stop == "p0":
            return nc

        with ExitStack() as st:
            NCOL = 18 * 128 + 640
            win = alloc(st, "win", [128, KC, NCOL], BF16)
            wout = alloc(st, "wout", [128, KC, D], BF16)
            kT = alloc(st, "kT", [128, 2, T], BF16)
            V = alloc(st, "V", [128, NT, 2, 65], BF16)
            convw_s = alloc(st, "convw_s", [128, 2, 3], F32)
            wsT = alloc(st, "wsT", [128, 4, 128], BF16)
            gmb = alloc(st, "gmb", [128, 4], F32)
            esink = alloc(st, "esink", [128, 8], F32)
            lng = alloc(st, "lng", [128, D], F32)
            lnb = alloc(st, "lnb", [128, D], F32)
            g1b = [alloc(st, "g1b%d" % c, [128, D], F32) for c in range(2)]
            for c in range(2):
                P.dma("sp", g1b[c][:], modrows[l, c, 0:1, :].to_broadcast([128, D]), writes=[("g1b", c)])
            R = 4
            xres = [alloc(st, "xres%d" % i, [128, D], F32) for i in range(3)]
            hT = [alloc(st, "hT%d" % i, [128, KC, 128], BF16) for i in range(2)]
            qrot = [alloc(st, "qrot%d" % i, [128, 4, 128], BF16) for i in range(R)]
            u = [alloc(st, "u%d" % i, [128, 2, 130], F32) for i in range(R)]
            cb = [alloc(st, "cb%d" % i, [128, 2, 128], F32) for i in range(R)]
            gut = [alloc(st, "gut%d" % i, [128, 256], F32) for i in range(R)]
            vln = [alloc(st, "vln%d" % i, [128, 256], BF16) for i in range(R)]
            cosb = [alloc(st, "cosb%d" % i, [128, 128], F32) for i in range(2)]
            sinb = [alloc(st, "sinb%d" % i, [128, 128], F32) for i in range(2)]
            t1 = alloc(st, "t1", [128, 4, 128], F32)
            t2 = alloc(st, "t2", [128, 4, 128], F32)
            cxs = alloc(st, "cxs", [128, 2, 128], F32)
            gvt = alloc(st, "gvt", [128, 256], F32)
            st6 = alloc(st, "st6", [128, 6], F32)
            mv = alloc(st, "mv", [128, 2], F32)
            rstd = alloc(st, "rstd", [128, 1], F32)
            nb_ = alloc(st, "nb_", [128, 1], F32)
            PT = [alloc(st, "PT%d" % i, [128, 5, 128], BF16) for i in range(4)]
            den = alloc(st, "den", [128, 4], F32)
            mixtok = [alloc(st, "mixtok%d" % i, [128, 768], BF16) for i in range(2)]
            mixT = [alloc(st, "mixT%d" % i, [128, KC, 128], BF16) for i in range(2)]
            ct = alloc(st, "ct", [128, 2, 128], F32)
            gtmp = alloc(st, "gtmp", [128, 256], F32)
            bufA = alloc(st, "bufA", [128, D], F32)
            bufB = alloc(st, "bufB", [128, D], F32)
            st12 = alloc(st, "st12", [128, 12], F32)
            mv2 = alloc(st, "mv2", [128, 2], F32)
            rstd2 = alloc(st, "rstd2", [128, 1], F32)
            nb2 = alloc(st, "nb2", [128, 1], F32)
            B = [palloc(st, "B%d" % i, [128, 512]) for i in range(7)]
            Bbf = palloc(st, "Bbf", [128, 1024], BF16)
            Bo = B[4]

            wv = w_in[l].rearrange("(k p) n -> p k n", p=128)
            P.dma("pool", win[:, :, 0:512], wv[:, :, 0:512], writes=["win"])
            for kv in range(2):
                for dup in range(2):
                    c0 = 1024 + kv * 128 + dup * 64
                    P.dma("pool", win[:, :, c0:c0 + 64], wv[:, :, 512 + kv * 64:512 + (kv + 1) * 64], writes=["win"])
            P.dma("pool", win[:, :, 1536:2304], wv[:, :, 768:1536], writes=["win"])
            P.dma("pool", win[:, :, 2304:2432], wv[:, :, 640:768], writes=["win"])
            P.dma("pool", win[:, :, 2432:2688], wv[:, :, 1792:2048], writes=["win"])
            P.dma("pool", win[:, :, 2688:2944], wv[:, :, 1536:1792], writes=["win"])
            P.dma("pool", wout[:], w_out[l].rearrange("(k p) n -> p k n", p=128), writes=["wout"])
            for (src0, dst0, nblk) in ((0, 512, 16), (1024, 1280, 8)):
                for a in range(2):
                    def swp(e, src0=src0, dst0=dst0, nblk=nblk, a=a):
                        srcv = win[:, :, src0:src0 + nblk * 32].rearrange("p k (b a f) -> p k b a f", a=2, f=16)
                        dstv = win[:, :, dst0:dst0 + nblk * 32].rearrange("p k (b a f) -> p k b a f", a=2, f=16)
                        return e.tensor_copy(dstv[:, :, :, a, :], srcv[:, :, :, 1 - a, :])
                    P.op("dve", swp, reads=["win"], writes=["win"])
            P.dma("sp", convw_s[:], convw[l], writes=["convw_s"])
            P.dma("pool", wsT[:], gm_wsT[l].rearrange("g q p -> q g p"), writes=["wsT"])
            P.dma("sp", gmb[:], gm_bsp[l], writes=["gmb"])
            P.dma("sp", esink[:], sink[l:l + 1, :].to_broadcast([128, 8]), writes=["esink"])
            P.op("act", lambda e: e.activation(out=esink[:], in_=esink[:], func=AF.Exp), reads=["esink"], writes=["esink"])
            P.dma("sp", lng[:], ln1_g[l:l + 1, :].to_broadcast([128, D]), writes=["lng"])
            P.dma("sp", lnb[:], ln1_b[l:l + 1, :].to_broadcast([128, D]), writes=["lnb"])
            P.op("pool", lambda e: e.memset(V[:, :, :, 64:65], 1.0), writes=["Vones"])

            def seq_of(t):
                return (0, NB) if t < NB else (NB, NT)

            def inproj(t, kv_only=False):
                s = t % R
                xs = t % 3
                hs = t % 2
                cs = t % 2
                col = 0 if t < NB else 1
                lo, hi = seq_of(t)
                P.dma("sp", xres[xs][:], tile_src(l, t), writes=[("xres", xs)])
                P.dma("sp", cosb[cs][:], cos_t[t], writes=[("cosb", cs)])
                P.dma("sp", sinb[cs][:], sin_t[t], writes=[("sinb", cs)])
                for hb in range(2):
                    def tp(e, hb=hb):
                        ins = None
                        for i in range(4):
                            j = hb * 4 + i
                            ins = e.transpose(B[2 + hb][:, i * 128:(i + 1) * 128], xres[xs][:, j * 128:(j + 1) * 128], ident_f[:])
                        return ins
                    P.op("pe", tp, reads=[("xres", xs), "ident_f"], writes=[("B", 2 + hb)])

                    def ev(e, hb=hb):
                        ins = None
                        for i in range(4):
                            j = hb * 4 + i
                            ins = e.activation(out=hT[hs][:, j, :], in_=B[2 + hb][:, i * 128:(i + 1) * 128], func=AF.Identity,
                                               scale=modT[:, 8 + j, col:col + 1], bias=modT[:, j, col:col + 1])
                        return ins
                    P.op("act", ev, reads=[("B", 2 + hb), "modT"], writes=[("hT", hs, hb)])
                hres = [("hT", hs, 0), ("hT", hs, 1)]

                def fm_group(bank, off, chunks):
                    def f(e):
                        ins = None
                        for i, c in enumerate(chunks):
                            for k in range(KC):
                                ins = e.matmul(B[bank][:, off + i * 128:off + (i + 1) * 128],
                                               lhsT=win[:, k, c * 128:(c + 1) * 128], rhs=hT[hs][:, k, :],
                                               start=(k == 0), stop=(k == KC - 1))
                        return ins
                    return f

                if not kv_only:
                    P.op("pe", fm_group(2, 0, [0, 1, 2, 3]), reads=hres + ["win"], writes=[("B", 2)])
                    P.op("pe", fm_group(3, 0, [4, 5, 6, 7]), reads=hres + ["win"], writes=[("B", 3)])
                    P.op("dve", lambda e: e.tensor_tensor(t1[:], B[2][:, :].rearrange("p (c t) -> p c t", c=4),
                                                          bc_mid(cosb[cs][:], 4), ALU.mult),
                         reads=[("B", 2), ("cosb", cs)], writes=["t1"])
                    P.op("dve", lambda e: e.tensor_tensor(t2[:], B[3][:, :].rearrange("p (c t) -> p c t", c=4),
                                                          bc_mid(sinb[cs][:], 4), ALU.mult),
                         reads=[("B", 3), ("sinb", cs)], writes=["t2"])
                    P.op("pool", lambda e: e.tensor_tensor(qrot[s][:], t1[:], t2[:], ALU.add),
                         reads=["t1", "t2"], writes=[("qrot", s)])
                P.op("pe", fm_group(2, 0, [8, 9, 10, 11]), reads=hres + ["win"], writes=[("B", 2)])
                P.op("dve", lambda e: e.tensor_tensor(t1[:, 0:2, :], B[2][:, 0:256].rearrange("p (c t) -> p c t", c=2),
                                                      bc_mid(cosb[cs][:], 2), ALU.mult),
                     reads=[("B", 2), ("cosb", cs)], writes=["t1"])
                P.op("dve", lambda e: e.tensor_tensor(t2[:, 0:2, :], B[2][:, 256:512].rearrange("p (c t) -> p c t", c=2),
                                                      bc_mid(sinb[cs][:], 2), ALU.mult),
                     reads=[("B", 2), ("sinb", cs)], writes=["t2"])
                P.op("pool", lambda e: e.tensor_tensor(kT[:, :, t * 128:(t + 1) * 128], t1[:, 0:2, :], t2[:, 0:2, :], ALU.add),
                     reads=["t1", "t2"], writes=[("kT", t)])
                if not kv_only:
                    P.op("pe", fm_group(3, 0, [12, 13, 14, 15]), reads=hres + ["win"], writes=[("B", 3)])
                    P.op("pe", fm_group(2, 0, [16, 17]), reads=hres + ["win"], writes=[("B", 2)])
                    P.op("act", lambda e: e.activation(out=cb[s][:], in_=B[3][:, 0:256].rearrange("p (c t) -> p c t", c=2),
                                                       func=AF.Copy), reads=[("B", 3)], writes=[("cb", s)])
                    P.op("act", lambda e: e.activation(out=cxs[:], in_=B[2][:, 0:256].rearrange("p (c t) -> p c t", c=2),
                                                       func=AF.Copy), reads=[("B", 2)], writes=["cxs"])
                    P.op("dve", lambda e: e.tensor_tensor(u[s][:, :, 1:129], B[3][:, 256:512].rearrange("p (c t) -> p c t", c=2),
                                                          cxs[:], ALU.mult),
                         reads=[("B", 3), "cxs"], writes=[("u", s)])
                    if t > lo:
                        sp_ = (t - 1) % R
                        P.op("pool", lambda e: e.tensor_copy(u[sp_][:, :, 129:130], u[s][:, :, 1:2]),
                             reads=[("u", s)], writes=[("u", sp_)])
                    else:
                        P.op("pool", lambda e: e.memset(u[s][:, :, 0:1], 0.0), reads=[("u", s)], writes=[("u", s)])
                    if t < hi - 1:
                        sn_ = (t + 1) % R
                        P.op("pool", lambda e: e.tensor_copy(u[sn_][:, :, 0:1], u[s][:, :, 128:129]),
                             reads=[("u", s)], writes=[("u", sn_)])
                    else:
                        P.op("pool", lambda e: e.memset(u[s][:, :, 129:130], 0.0), reads=[("u", s)], writes=[("u", s)])

                def tm(e):
                    ins = None
                    for k in range(KC):
                        ins = e.matmul(B[3][:, 0:384], lhsT=hT[hs][:, k, :], rhs=win[:, k, 2304:2688],
                                       start=(k == 0), stop=(k == KC - 1))
                    return ins
                P.op("pe", tm, reads=hres + ["win"], writes=[("B", 3)])
                P.op("act", lambda e: e.activation(out=V[:, t, :, 0:64], in_=B[3][:, 0:128].rearrange("p (h d) -> p h d", h=2),
                                                   func=AF.Copy), reads=[("B", 3)], writes=[("V", t)])
                if not kv_only:
                    def tm2(e):
                        ins = None
                        for k in range(KC):
                            ins = e.matmul(B[2][:, 256:512], lhsT=hT[hs][:, k, :], rhs=win[:, k, 2688:2944],
                                           start=(k == 0), stop=(k == KC - 1))
                        return ins
                    P.op("pe", tm2, reads=hres + ["win"], writes=[("B", 2)])
                    P.op("act", lambda e: e.activation(out=gvt[:], in_=B[3][:, 128:384], func=AF.Gelu_apprx_tanh),
                         reads=[("B", 3)], writes=["gvt"])
                    P.op("act", lambda e: e.activation(out=gut[s][:], in_=B[2][:, 256:512], func=AF.Gelu_apprx_tanh),
                         reads=[("B", 2)], writes=[("gut", s)])
                    P.op("dve", lambda e: e.bn_stats(st6[:], gvt[:]), reads=["gvt"], writes=["st6"])
                    P.op("dve", lambda e: e.bn_aggr(mv[:], st6[:]), reads=["st6"], writes=["mv"])
                    P.op("act", lambda e: e.activation(out=rstd[:], in_=mv[:, 1:2], func=AF.Ln, bias=epsb[:, 0:1]),
                         reads=["mv", "epsb"], writes=["rstd"])
                    P.op("act", lambda e: e.activation(out=rstd[:], in_=rstd[:], func=AF.Exp, scale=-0.5), reads=["rstd"], writes=["rstd"])
                    P.op("dve", lambda e: e.scalar_tensor_tensor(nb_[:], mv[:, 0:1], -1.0, rstd[:], ALU.mult, ALU.mult),
                         reads=["mv", "rstd"], writes=["nb_"])
                    P.op("act", lambda e: e.activation(out=vln[s][:], in_=gvt[:], func=AF.Identity,
                                                       scale=rstd[:, 0:1], bias=nb_[:, 0:1]),
                         reads=["gvt", "rstd", "nb_"], writes=[("vln", s)])

            def mixers(t):
                s = t % R
                xs = t % 3
                ms = t % 2
                col = 0 if t < NB else 1
                lo, hi = seq_of(t)
                if t < NB:
                    kcs = [(NB, None), (NB + 1, None)]
                    if t > 0:
                        kcs.append((t - 1, maskl))
                    kcs.append((t, None))
                    if t < NB - 1:
                        kcs.append((t + 1, maskr))
                else:
                    kcs = [(NB, None), (NB + 1, None)]
                nk = len(kcs)
                for h in range(8):
                    qc, po, kv, pslot = h // 2, (h % 2) * 64, h // 4, h % 4

                    if h % 2 == 0:
                        sbank, s5, wr5 = B[6], B[4][:, 384:512], ("B", 4)
                        wr = [("B", 6)] + ([wr5] if nk > 4 else [])
                    else:
                        sbank, s5, wr5 = B[5], B[1][:, 384:512], ("B", 1)
                        wr = [("B", 5)] + ([wr5] if nk > 4 else [])

                    def sc(e, qc=qc, po=po, kv=kv, sbank=sbank, s5=s5):
                        ins = None
                        for i, (c, msk) in enumerate(kcs):
                            dst = sbank[:, i * 128:(i + 1) * 128] if i < 4 else s5
                            ins = e.matmul(dst, lhsT=kT[po:po + 64, kv, c * 128:(c + 1) * 128],
                                           rhs=qrot[s][po:po + 64, qc, :], start=True, stop=(msk is None))
                            if msk is not None:
                                ins = e.matmul(dst, lhsT=ident_b[:], rhs=msk[:], start=False, stop=True)
                        return ins
                    P.op("pe", sc, reads=[("kT", c) for c, _ in kcs] + [("qrot", s), "ident_b", "maskl", "maskr"], writes=wr)

                    def ex(e, pslot=pslot, sbank=sbank, s5=s5):
                        n1 = min(nk, 4)
                        ins = e.activation(out=PT[pslot][:, 0:n1, :], in_=sbank[:, 0:n1 * 128].rearrange("p (c t) -> p c t", c=n1),
                                           func=AF.Exp, scale=0.125)
                        if nk > 4:
                            ins = e.activation(out=PT[pslot][:, 4, :], in_=s5, func=AF.Exp, scale=0.125)
                        return ins
                    P.op("act", ex, reads=wr, writes=[("PT", pslot)])

                    def pv(e, kv=kv, pslot=pslot):
                        ins = None
                        for i, (c, msk) in enumerate(kcs):
                            ins = e.matmul(Bo[:, pslot * 65:(pslot + 1) * 65],
                                           lhsT=PT[pslot][:, i, :], rhs=V[:, c, kv, :],
                                           start=(i == 0), stop=(i == nk - 1))
                        return ins
                    P.op("pe", pv, reads=[("PT", pslot), "Vones"] + [("V", c) for c, _ in kcs], writes=[("B", 4)])
                    if pslot == 3:
                        hg = h // 4
                        bov = Bo[:, 0:260].rearrange("p (h d) -> p h d", d=65)
                        P.op("dve", lambda e, hg=hg, bov=bov: e.tensor_tensor(den[:], bov[:, :, 64], esink[:, hg * 4:(hg + 1) * 4], ALU.add),
                             reads=[("B", 4), "esink"], writes=["den"])
                        P.op("dve", lambda e: e.reciprocal(den[:], den[:]), reads=["den"], writes=["den"])
                        P.op("dve", lambda e, hg=hg, bov=bov: e.tensor_tensor(
                            mixtok[ms][:, hg * 256:(hg + 1) * 256].rearrange("p (h d) -> p h d", d=64),
                            bov[:, :, 0:64], bc_last(den[:], 64), ALU.mult),
                            reads=[("B", 4), "den"], writes=[("mixtok", ms, hg)])
                for j in range(2):
                    P.op("pool", lambda e, j=j: e.tensor_scalar(ct[:, j, :], u[s][:, j, 0:128], convw_s[:, j, 0:1], None, ALU.mult),
                         reads=[("u", s), "convw_s"], writes=[("ct", j)])
                    P.op("dve", lambda e, j=j: e.scalar_tensor_tensor(ct[:, j, :], u[s][:, j, 1:129], convw_s[:, j, 1:2], ct[:, j, :], ALU.mult, ALU.add),
                         reads=[("u", s), "convw_s", ("ct", j)], writes=[("ct", j)])
                    P.op("dve", lambda e, j=j: e.scalar_tensor_tensor(ct[:, j, :], u[s][:, j, 2:130], convw_s[:, j, 2:3], ct[:, j, :], ALU.mult, ALU.add),
                         reads=[("u", s), "convw_s", ("ct", j)], writes=[("ct", j)])
                    P.op("pool", lambda e, j=j: e.tensor_tensor(mixT[ms][:, 4 + j, :], cb[s][:, j, :], ct[:, j, :], ALU.mult),
                         reads=[("cb", s), ("ct", j)], writes=[("mixT", ms, 4 + j)])
                def gm(e):
                    ins = None
                    for g in range(4):
                        ins = e.matmul(B[1][:, g * 64:(g + 1) * 64], lhsT=wsT[:, g, :], rhs=vln[s][:, g * 64:(g + 1) * 64],
                                       start=True, stop=True)
                    return ins
                P.op("pe", gm, reads=[("vln", s), "wsT"], writes=[("B", 1)])
                P.op("dve", lambda e: e.tensor_tensor(gtmp[:].rearrange("p (g c) -> p g c", g=4),
                                                      B[1][:, 0:256].rearrange("p (g c) -> p g c", g=4),
                                                      bc_last(gmb[:], 64), ALU.add),
                     reads=[("B", 1), "gmb"], writes=["gtmp"])
                P.op("dve", lambda e: e.tensor_tensor(mixtok[ms][:, 512:768], gtmp[:], gut[s][:], ALU.mult),
                     reads=["gtmp", ("gut", s)], writes=[("mixtok", ms, 2)])
                def tpm(e):
                    ins = None
                    for i in range(6):
                        ins = e.transpose(Bbf[:, i * 128:(i + 1) * 128], mixtok[ms][:, i * 128:(i + 1) * 128], ident_b[:])
                    return ins
                P.op("pe", tpm, reads=[("mixtok", ms, i) for i in range(3)] + ["ident_b"], writes=["Bbf"])
                P.op("act", lambda e: e.activation(out=mixT[ms][:, 0:4, :], in_=Bbf[:, 0:512].rearrange("p (c t) -> p c t", c=4), func=AF.Copy),
                     reads=["Bbf"], writes=[("mixT", ms, i) for i in range(4)])
                P.op("act", lambda e: e.activation(out=mixT[ms][:, 6:8, :], in_=Bbf[:, 512:768].rearrange("p (c t) -> p c t", c=2), func=AF.Copy),
                     reads=["Bbf"], writes=[("mixT", ms, 6), ("mixT", ms, 7)])
                for hf in range(2):
                    def op_(e, hf=hf):
                        ins = None
                        for k in range(KC):
                            ins = e.matmul(B[hf][:, :], lhsT=mixT[ms][:, k, :], rhs=wout[:, k, hf * 512:(hf + 1) * 512],
                                           start=(k == 0), stop=(k == KC - 1))
                        return ins
                    P.op("pe", op_, reads=[("mixT", ms, i) for i in range(8)] + ["wout"], writes=[("B", hf)])
                    P.op("dve", lambda e, hf=hf: e.tensor_tensor(bufA[:, hf * 512:(hf + 1) * 512], B[hf][:, :],
                                                                g1b[col][:, hf * 512:(hf + 1) * 512], ALU.mult),
                         reads=[("B", hf), ("g1b", col)], writes=[("bufA", hf)])
                layer_norm_tail(xres[xs], ("xres", xs), lng, lnb, x1buf[t * 128:(t + 1) * 128, :], ("x1buf", t))

            def layer_norm_tail(xt, xres_name, g_t, b_t, dst, dst_name):
                P.op("dve", lambda e: e.scalar_tensor_tensor(bufA[:], xt[:], ALPHA, bufA[:], ALU.mult, ALU.add),
                     reads=[xres_name, ("bufA", 0), ("bufA", 1)], writes=[("bufA", 0), ("bufA", 1)])
                P.op("dve", lambda e: e.bn_stats(st12[:, 0:6], bufA[:, 0:512]), reads=[("bufA", 0)], writes=[("st12", 0)])
                P.op("dve", lambda e: e.bn_stats(st12[:, 6:12], bufA[:, 512:1024]), reads=[("bufA", 1)], writes=[("st12", 1)])
                P.op("dve", lambda e: e.bn_aggr(mv2[:], st12[:]), reads=[("st12", 0), ("st12", 1)], writes=["mv2"])
                P.op("act", lambda e: e.activation(out=rstd2[:], in_=mv2[:, 1:2], func=AF.Ln, bias=epsb[:, 0:1]),
                     reads=["mv2", "epsb"], writes=["rstd2"])
                P.op("act", lambda e: e.activation(out=rstd2[:], in_=rstd2[:], func=AF.Exp, scale=-0.5), reads=["rstd2"], writes=["rstd2"])
                P.op("dve", lambda e: e.scalar_tensor_tensor(nb2[:], mv2[:, 0:1], -1.0, rstd2[:], ALU.mult, ALU.mult),
                     reads=["mv2", "rstd2"], writes=["nb2"])
                P.op("act", lambda e: e.activation(out=bufB[:], in_=bufA[:], func=AF.Identity, scale=rstd2[:, 0:1], bias=nb2[:, 0:1]),
                     reads=[("bufA", 0), ("bufA", 1), "rstd2", "nb2"], writes=["bufB"])
                P.op("pool", lambda e: e.tensor_tensor(bufB[:], bufB[:], g_t[:], ALU.mult), reads=["bufB", "lng"], writes=["bufB"])
                P.op("pool", lambda e: e.tensor_tensor(bufB[:], bufB[:], b_t[:], ALU.add), reads=["bufB", "lnb"], writes=["bufB"])
                P.dma("sp", dst, bufB[:], reads=["bufB"], writes=[dst_name])

            for t in range(NB, NT):
                inproj(t, kv_only=last)
            if not last:
                for t in range(NB, NT):
                    mixers(t)
            inproj(0)
            if NB > 1:
                inproj(1)
            for n in range(NB):
                la = P.record(mixers, n)
                lb = P.record(inproj, n + 2) if n + 2 < NB else []
                P.play(la, lb)
            P.flush()
        if stop == "p1":
            return nc

        tiles = list(range(NB)) if last else list(range(NT))
        nt2 = len(tiles)
        NBLK = (2 * nt2 * 128) // BS + NEXP
        AX = mybir.AxisListType.X
        w1v = w1.rearrange("l e p (a k) n -> (l e p a) (k n)", a=4)
        w3v = w3.rearrange("l e p (a k) n -> (l e p a) (k n)", a=4)
        w2v = w2.rearrange("l e p (a k) n -> (l e p a) (k n)", a=4)
        with ExitStack() as st:
            wr_s = alloc(st, "wr_s", [128, KC, 36], F32)
            wr_m = [alloc(st, "wr_m%d" % c, [128, KC, 36], F32) for c in range(2)]
            sh2rep = [alloc(st, "sh2rep%d" % c, [128, KC, 128], F32) for c in range(2)]
            br_row = alloc(st, "br_row", [1, 36], F32)
            lng = alloc(st, "lng2", [128, D], F32)
            lnb = alloc(st, "lnb2", [128, D], F32)
            scb = [alloc(st, "scb%d" % c, [128, D], F32) for c in range(2)]
            shb = [alloc(st, "shb%d" % c, [128, D], F32) for c in range(2)]
            g2b = [alloc(st, "g2b%d" % c, [128, D], F32) for c in range(2)]
            lt_b = alloc(st, "lt_b", [128, 128], BF16)
            ones_b = alloc(st, "ones_b", [128, 128], BF16)
            jvec = alloc(st, "jvec", [128, NBLK_MAX], F32)
            base1 = alloc(st, "base1", [128, 8], F32)
            base2 = alloc(st, "base2", [128, 4], F32)
            x1t = [alloc(st, "x1t%d" % i, [128, D], F32) for i in range(2)]
            x1T = alloc(st, "x1T", [128, KC, 128], F32)
            h2r = [alloc(st, "h2r%d" % i, [128, D], BF16) for i in range(2)]
            lg = alloc(st, "lg", [128, 36], F32)
            sm = alloc(st, "sm", [128, 16], F32)
            goh = alloc(st, "goh", [128, 4], F32)
            gex = alloc(st, "gex", [128, 4], F32)
            esel = alloc(st, "esel", [128, 8], F32)
            esel2 = alloc(st, "esel2", [128, 8], F32)
            eq1 = alloc(st, "eq1", [128, 8], F32)
            eq2 = alloc(st, "eq2", [128, 8], F32)
            oh0 = alloc(st, "oh0", [128, nt2, 32], F32)
            oh1 = alloc(st, "oh1", [128, nt2, 32], F32)
            cntb = alloc(st, "cntb", [128, nt2, 32], BF16)
            gsc = alloc(st, "gsc", [128, nt2, 2], F32)
            rank_s = alloc(st, "rank_s", [128, nt2, 32], F32)
            dtmp = alloc(st, "dtmp", [128, nt2, 32], F32)
            tot = alloc(st, "tot", [128, 32], F32)
            nblk = alloc(st, "nblk", [128, 32], F32)
            sc_a = alloc(st, "sc_a", [128, 32], F32)
            sc_b = alloc(st, "sc_b", [128, 32], F32)
            pstart = alloc(st, "pstart", [128, 32], F32)
            destf = alloc(st, "destf", [128, 2, nt2], F32)
            idx = alloc(st, "idx", [128, 2, nt2], mybir.dt.int32)
            bef = alloc(st, "bef", [128, NBLK_MAX], F32)
            be1 = alloc(st, "be1", [128, NBLK_MAX], F32)
            wif1 = alloc(st, "wif1", [128, NBLK_MAX, 4], F32)
            wi1 = alloc(st, "wi1", [128, NBLK_MAX, 4], mybir.dt.int32)
            w1s = [alloc(st, "w1s%d" % i, [128, KC, DEXP], BF16) for i in range(2)]
            w3s = [alloc(st, "w3s%d" % i, [128, KC, DEXP], BF16) for i in range(2)]
            w2s = [alloc(st, "w2s%d" % i, [128, 4, D], BF16) for i in range(2)]
            xsb = [alloc(st, "xsb%d" % i, [128, 4, D], BF16) for i in range(2)]
            hTb = [alloc(st, "hTb%d" % i, [128, KC, 512], BF16) for i in range(2)]
            sg = [alloc(st, "sg%d" % i, [128, 512], F32) for i in range(2)]
            actT = [alloc(st, "actT%d" % i, [128, 4, 512], BF16) for i in range(2)]
            ysb = [alloc(st, "ysb%d" % i, [128, D], F32) for i in range(2)]
            yg = ysb
            bufA = alloc(st, "bufA2", [128, D], F32)
            bufB = alloc(st, "bufB2", [128, D], F32)
            st12 = alloc(st, "st12b", [128, 12], F32)
            mv2 = alloc(st, "mv2b", [128, 2], F32)
            rstd2 = alloc(st, "rstd2b", [128, 1], F32)
            nb2 = alloc(st, "nb2b", [128, 1], F32)
            B = [None, None] + [palloc(st, "M%d" % i, [128, 512]) for i in range(2, 8)]
            TB = [palloc(st, "TB%d" % i, [128, 1024], BF16) for i in range(2)]

            P.dma("sp", wr_s[:], w_r[l].rearrange("(k p) n -> p k n", p=128), writes=["wr_s"])
            P.dma("sp", br_row[:], b_r[l:l + 1, :], writes=["br_row"])
            P.dma("sp", lng[:], ln2_g[l:l + 1, :].to_broadcast([128, D]), writes=["lng"])
            P.dma("sp", lnb[:], ln2_b[l:l + 1, :].to_broadcast([128, D]), writes=["lnb"])
            for c in range(2):
                P.dma("sp", shb[c][:], modrows[l, c, 1:2, :].to_broadcast([128, D]), writes=[("shb", c)])
                P.dma("sp", scb[c][:], modrows[l, c, 2:3, :].to_broadcast([128, D]), writes=[("scb", c)])
                P.dma("sp", g2b[c][:], modrows[l, c, 3:4, :].to_broadcast([128, D]), writes=[("g2b", c)])
            P.dma("pool", lt_b[:], lt_d[:, :], writes=["lt_b"])
            P.dma("sp", jvec[:], jvec_d[:, :], writes=["jvec"])
            P.dma("sp", base1[:], base1_d[:, :], writes=["base1"])
            P.dma("sp", base2[:], base2_d[:, :], writes=["base2"])
            P.op("pool", lambda e: e.memset(ones_b[:], 1.0), writes=["ones_b"])
            P.op("pool", lambda e: e.memset(xsb[1][:], 0.0), writes=[("xsb", 1)])
            zres = []
            for i in range(NBLK):
                P.dma("sp", xs_dram[i * BS:(i + 1) * BS, :].rearrange("(a p) d -> p a d", p=128), xsb[1][:],
                      reads=[("xsb", 1)], writes=[("xsz", i)])
                zres.append(("xsz", i))
            for c in range(2):
                P.op("dve", lambda e, c=c: e.tensor_tensor(wr_m[c][:], wr_s[:], bc_last(modT[:, 32:40, c], 36), ALU.mult),
                     reads=["wr_s", "modT"], writes=[("wr_m", c)])
                P.op("dve", lambda e, c=c: e.tensor_copy(sh2rep[c][:], bc_last(modT[:, 24:32, c], 128)),
                     reads=["modT"], writes=[("sh2rep", c)])

            st12s = [st12, alloc(st, "st12c", [128, 12], F32)]
            mv2s = [mv2, alloc(st, "mv2c", [128, 2], F32)]
            rstd2s = [rstd2, alloc(st, "rstd2c", [128, 1], F32)]
            nb2s = [nb2, alloc(st, "nb2c", [128, 1], F32)]

            def ln_tail2(xt, xname, dst, dname, bA, bB, se):
                nA, nB = ("bufA", se), ("bufB", se)
                s12, m2, r2, n2 = st12s[se], mv2s[se], rstd2s[se], nb2s[se]
                P.op("dve", lambda e: e.scalar_tensor_tensor(bA[:], xt[:], ALPHA, bA[:], ALU.mult, ALU.add),
                     reads=[xname, nA], writes=[nA])
                P.op("dve", lambda e: e.bn_stats(s12[:, 0:6], bA[:, 0:512]), reads=[nA], writes=[("st12", se, 0)])
                P.op("dve", lambda e: e.bn_stats(s12[:, 6:12], bA[:, 512:1024]), reads=[nA], writes=[("st12", se, 1)])
                P.op("dve", lambda e: e.bn_aggr(m2[:], s12[:]), reads=[("st12", se, 0), ("st12", se, 1)], writes=[("mv2", se)])
                P.op("act", lambda e: e.activation(out=r2[:], in_=m2[:, 1:2], func=AF.Sqrt, bias=LN_EPS),
                     reads=[("mv2", se)], writes=[("rstd2", se)])
                P.op("dve", lambda e: e.reciprocal(r2[:], r2[:]), reads=[("rstd2", se)], writes=[("rstd2", se)])
                P.op("dve", lambda e: e.scalar_tensor_tensor(n2[:], m2[:, 0:1], -1.0, r2[:], ALU.mult, ALU.mult),
                     reads=[("mv2", se), ("rstd2", se)], writes=[("nb2", se)])
                P.op("act", lambda e: e.activation(out=bB[:], in_=bA[:], func=AF.Identity, scale=r2[:, 0:1], bias=n2[:, 0:1]),
                     reads=[nA, ("rstd2", se), ("nb2", se)], writes=[nB])
                P.op("pool", lambda e: e.tensor_tensor(bB[:], bB[:], lng[:], ALU.mult), reads=[nB, "lng"], writes=[nB])
                P.op("pool", lambda e: e.tensor_tensor(bB[:], bB[:], lnb[:], ALU.add), reads=[nB, "lnb"], writes=[nB])
                P.dma("sp", dst, bB[:], reads=[nB], writes=[dname])

            def v1024(tn, nm):
                if nt2 * 32 >= 1024:
                    return tn[:].rearrange("p a b -> p (a b)")[:, 0:1024]
                return alloc(st, nm, [128, D], F32)[:]
            rank_v, dtmp_v, oh0_v = v1024(rank_s, "rank_v"), v1024(dtmp, "dtmp_v"), v1024(oh0, "oh0_v")
            x1Ts = [x1T, rank_v.rearrange("p (k t) -> p k t", k=KC)]
            bufAs = [bufA, dtmp_v]
            lgs = [lg, alloc(st, "lg_b", [128, 36], F32)]
            sms = [sm, alloc(st, "sm_b", [128, 16], F32)]
            gohs = [goh, alloc(st, "goh_b", [128, 4], F32)]
            gexs = [gex, alloc(st, "gex_b", [128, 4], F32)]
            esels = [esel, alloc(st, "esel_b", [128, 8], F32)]
            esel2s = [esel2, alloc(st, "esel2_b", [128, 8], F32)]
            eq1s = [eq1, alloc(st, "eq1_b", [128, 8], F32)]
            eq2s = [eq2, alloc(st, "eq2_b", [128, 8], F32)]

            def stageA(li):
                t = tiles[li]
                col = 0 if t < NB else 1
                xs = li % 2
                se = li % 2
                tpb = 6 if se == 0 else 4
                rtb = 2 if se == 0 else 3
                P.dma("sp", x1t[xs][:], x1buf[t * 128:(t + 1) * 128, :], writes=[("x1t", xs)])
                P.op("dve", lambda e, xs=xs, col=col: e.tensor_tensor(bufAs[se][:], x1t[xs][:], scb[col][:], ALU.mult),
                     reads=[("x1t", xs), ("scb", col)], writes=[("bufA", se)])
                P.op("pool", lambda e, xs=xs, col=col: e.tensor_tensor(h2r[xs][:], bufAs[se][:], shb[col][:], ALU.add),
                     reads=[("bufA", se), ("shb", col)], writes=[("h2r", xs)])
                P.dma("sp", h2buf[li * 128:(li + 1) * 128, :], h2r[xs][:], reads=[("h2r", xs)], writes=[("h2buf", li)])
                for hb in range(2):
                    def tp(e, hb=hb, xs=xs):
                        ins = None
                        for i in range(4):
                            j = hb * 4 + i
                            ins = e.transpose(B[tpb + hb][:, i * 128:(i + 1) * 128], x1t[xs][:, j * 128:(j + 1) * 128], ident_f[:])
                        return ins
                    P.op("pe", tp, reads=[("x1t", xs), "ident_f"], writes=[("B", tpb + hb)])

                    def cpx(e, hb=hb):
                        ins = None
                        for i in range(4):
                            ins = e.activation(out=x1Ts[se][:, hb * 4 + i, :], in_=B[tpb + hb][:, i * 128:(i + 1) * 128], func=AF.Copy)
                        return ins
                    P.op("act", cpx, reads=[("B", tpb + hb)], writes=[("x1T", se, hb)])

                def rt(e, col=col):
                    ins = None
                    for k in range(KC):
                        ins = e.matmul(B[rtb][:, 0:36], lhsT=x1Ts[se][:, k, :], rhs=wr_m[col][:, k, :], start=(k == 0), stop=False)
                    for k in range(KC):
                        ins = e.matmul(B[rtb][:, 0:36], lhsT=sh2rep[col][:, k, :], rhs=wr_s[:, k, :], start=False, stop=False)
                    ins = e.matmul(B[rtb][:, 0:36], lhsT=ones_f[0:1, :], rhs=br_row[0:1, :], start=False, stop=True)
                    return ins
                P.op("pe", rt, reads=[("x1T", se, 0), ("x1T", se, 1), ("wr_m", col), ("sh2rep", col), "wr_s", "br_row", "ones_f"], writes=[("B", rtb)])
                P.op("act", lambda e: e.activation(out=lgs[se][:], in_=B[rtb][:, 0:36], func=AF.Copy), reads=[("B", rtb)], writes=[("lg", se)])
                P.op("dve", lambda e: e.reduce_max(sms[se][:, 0:1], lgs[se][:, 0:4], AX), reads=[("lg", se)], writes=[("sm", se)])
                P.op("dve", lambda e: e.tensor_scalar(sms[se][:, 1:2], sms[se][:, 0:1], -1.0, None, ALU.mult), reads=[("sm", se)], writes=[("sm", se)])
                P.op("act", lambda e: e.activation(out=gexs[se][:], in_=lgs[se][:, 0:4], func=AF.Exp, bias=sms[se][:, 1:2], accum_out=sms[se][:, 2:3]),
                     reads=[("lg", se), ("sm", se)], writes=[("gex", se), ("sm", se)])
                P.op("dve", lambda e: e.reciprocal(sms[se][:, 3:4], sms[se][:, 2:3]), reads=[("sm", se)], writes=[("sm", se)])
                P.op("dve", lambda e: e.tensor_scalar(gohs[se][:], lgs[se][:, 0:4], sms[se][:, 0:1], None, ALU.is_equal), reads=[("lg", se), ("sm", se)], writes=[("goh", se)])
                P.op("dve", lambda e: e.tensor_scalar(esels[se][:], lgs[se][:, 4:12], gohs[se][:, 0:1], None, ALU.mult), reads=[("lg", se), ("goh", se)], writes=[("esel", se)])
                for g in range(1, 4):
                    P.op("dve", lambda e, g=g: e.scalar_tensor_tensor(esels[se][:], lgs[se][:, 4 + g * 8:12 + g * 8], gohs[se][:, g:g + 1], esels[se][:], ALU.mult, ALU.add),
                         reads=[("lg", se), ("goh", se), ("esel", se)], writes=[("esel", se)])
                P.op("dve", lambda e: e.reduce_max(sms[se][:, 4:5], esels[se][:], AX), reads=[("esel", se)], writes=[("sm", se)])
                P.op("dve", lambda e: e.tensor_scalar(eq1s[se][:], esels[se][:], sms[se][:, 4:5], None, ALU.is_equal), reads=[("esel", se), ("sm", se)], writes=[("eq1", se)])
                P.op("dve", lambda e: e.scalar_tensor_tensor(esel2s[se][:], eq1s[se][:], -1e30, esels[se][:], ALU.mult, ALU.add), reads=[("eq1", se), ("esel", se)], writes=[("esel2", se)])
                P.op("dve", lambda e: e.reduce_max(sms[se][:, 5:6], esel2s[se][:], AX), reads=[("esel2", se)], writes=[("sm", se)])
                P.op("dve", lambda e: e.tensor_scalar(eq2s[se][:], esel2s[se][:], sms[se][:, 5:6], None, ALU.is_equal), reads=[("esel2", se), ("sm", se)], writes=[("eq2", se)])
                P.op("dve", lambda e: e.tensor_tensor(sms[se][:, 6:7], sms[se][:, 4:5], sms[se][:, 5:6], ALU.subtract), reads=[("sm", se)], writes=[("sm", se)])
                P.op("act", lambda e: e.activation(out=sms[se][:, 7:8], in_=sms[se][:, 6:7], func=AF.Sigmoid), reads=[("sm", se)], writes=[("sm", se)])
                P.op("act", lambda e: e.activation(out=sms[se][:, 8:9], in_=sms[se][:, 6:7], func=AF.Sigmoid, scale=-1.0), reads=[("sm", se)], writes=[("sm", se)])
                P.op("dve", lambda e, li=li: e.tensor_scalar(gsc[:, li, :], sms[se][:, 7:9], sms[se][:, 3:4], None, ALU.mult), reads=[("sm", se)], writes=[("gsc", li)])
                P.op("dve", lambda e, li=li: e.tensor_tensor(oh0[:, li, :].rearrange("p (g x) -> p g x", g=4), bc_last(gohs[se][:], 8), bc_mid(eq1s[se][:], 4), ALU.mult),
                     reads=[("goh", se), ("eq1", se)], writes=[("oh0", li)])
                P.op("dve", lambda e, li=li: e.tensor_tensor(oh1[:, li, :].rearrange("p (g x) -> p g x", g=4), bc_last(gohs[se][:], 8), bc_mid(eq2s[se][:], 4), ALU.mult),
                     reads=[("goh", se), ("eq2", se)], writes=[("oh1", li)])
                P.op("pool", lambda e, li=li: e.tensor_tensor(cntb[:, li, :], oh0[:, li, :], oh1[:, li, :], ALU.add),
                     reads=[("oh0", li), ("oh1", li)], writes=[("cntb", li)])


            for li0 in range(0, nt2, 2):
                P.play(P.record(stageA, li0), P.record(stageA, li0 + 1) if li0 + 1 < nt2 else [])
            if stop == "p2a":
                P.flush()
                return nc
            call = [("cntb", i) for i in range(nt2)]
            nbank = (nt2 + 15) // 16
            for li in range(nt2):
                bk, off = 3 + li // 16, (li % 16) * 32

                def rk(e, li=li, bk=bk, off=off):
                    ins = None
                    for tp_ in range(li):
                        ins = e.matmul(B[bk][:, off:off + 32], lhsT=ones_b[:], rhs=cntb[:, tp_, :], start=(tp_ == 0), stop=False)
                    ins = e.matmul(B[bk][:, off:off + 32], lhsT=lt_b[:], rhs=cntb[:, li, :], start=(li == 0), stop=True)
                    return ins
                P.op("pe", rk, reads=call + ["ones_b", "lt_b"], writes=[("RK", li)])
            for bk in range(nbank):
                n_here = min(16, nt2 - bk * 16)
                P.op("act", lambda e, bk=bk, n_here=n_here: e.activation(
                    out=rank_s[:, bk * 16:bk * 16 + n_here, :], in_=B[3 + bk][:, 0:n_here * 32].rearrange("p (t x) -> p t x", x=32), func=AF.Copy),
                    reads=[("RK", i) for i in range(bk * 16, bk * 16 + n_here)], writes=[("rank_s", bk)])

            def tt(e):
                ins = None
                for tp_ in range(nt2):
                    ins = e.matmul(B[2][:, 64:96], lhsT=ones_b[:], rhs=cntb[:, tp_, :], start=(tp_ == 0), stop=(tp_ == nt2 - 1))
                return ins
            P.op("pe", tt, reads=call + ["ones_b"], writes=[("B", 2)])
            P.op("act", lambda e: e.activation(out=tot[:], in_=B[2][:, 64:96], func=AF.Copy), reads=[("B", 2)], writes=["tot"])
            P.op("dve", lambda e: e.tensor_scalar(nblk[:], tot[:], 0.0, None, ALU.is_gt), reads=["tot"], writes=["nblk"])
            for m in range(1, (2 * nt2 * 128) // BS + 1):
                P.op("dve", lambda e, m=m: e.scalar_tensor_tensor(nblk[:], tot[:], float(m * BS), nblk[:], ALU.is_gt, ALU.add),
                     reads=["tot", "nblk"], writes=["nblk"])
            P.op("dve", lambda e: e.tensor_copy(sc_a[:], nblk[:]), reads=["nblk"], writes=["sc_a"])
            cur, oth, cn, on = sc_a, sc_b, "sc_a", "sc_b"
            for dd in (1, 2, 4, 8, 16):
                P.op("dve", lambda e, cur=cur, oth=oth, dd=dd: e.tensor_copy(oth[:, 0:dd], cur[:, 0:dd]), reads=[cn], writes=[on])
                P.op("dve", lambda e, cur=cur, oth=oth, dd=dd: e.tensor_tensor(oth[:, dd:32], cur[:, dd:32], cur[:, 0:32 - dd], ALU.add),
                     reads=[cn, on], writes=[on])
                cur, oth, cn, on = oth, cur, on, cn
            pend, pendn = cur, cn
            P.op("dve", lambda e: e.tensor_tensor(pstart[:], pend[:], nblk[:], ALU.subtract), reads=[pendn, "nblk"], writes=["pstart"])
            P.op("dve", lambda e: e.tensor_scalar(pstart[:], pstart[:], float(BS), None, ALU.mult), reads=["pstart"], writes=["pstart"])
            rk_all = [("rank_s", i) for i in range(nbank)]
            P.op("dve", lambda e: e.tensor_tensor(rank_s[:], rank_s[:], bc_mid(pstart[:], nt2), ALU.add),
                 reads=rk_all + ["pstart"], writes=rk_all)
            for k, oh in enumerate((oh0, oh1)):
                P.op("dve", lambda e, oh=oh: e.tensor_tensor(dtmp[:], oh[:], rank_s[:], ALU.mult),
                     reads=rk_all + [("oh%d" % k, i) for i in range(nt2)], writes=["dtmp"])
                P.op("dve", lambda e, k=k: e.reduce_sum(destf[:, k, :], dtmp[:], AX), reads=["dtmp"], writes=[("destf", k)])
            P.op("dve", lambda e: e.tensor_copy(idx[:], destf[:]), reads=[("destf", 0), ("destf", 1)], writes=["idx"])
            P.op("dve", lambda e: e.tensor_scalar(bef[:], jvec[:], pend[:, 0:1], None, ALU.is_ge), reads=["jvec", pendn], writes=["bef"])
            for ee in range(1, 32):
                P.op("dve", lambda e, ee=ee: e.scalar_tensor_tensor(bef[:], jvec[:], pend[:, ee:ee + 1], bef[:], ALU.is_ge, ALU.add),
                     reads=["jvec", pendn, "bef"], writes=["bef"])
            P.op("dve", lambda e: e.tensor_scalar(bef[:], bef[:], 31.0, None, ALU.min), reads=["bef"], writes=["bef"])
            P.op("dve", lambda e: e.tensor_scalar(be1[:], bef[:], 128.0, float(l * NEXP * 128), ALU.mult, ALU.add), reads=["bef"], writes=["be1"])
            P.op("dve", lambda e: e.tensor_scalar(be1[:], be1[:], base1[:, 0:1], None, ALU.add), reads=["be1", "base1"], writes=["be1"])
            P.op("dve", lambda e: e.tensor_scalar(be1[:], be1[:], 4.0, None, ALU.mult), reads=["be1"], writes=["be1"])
            P.op("dve", lambda e: e.tensor_tensor(wif1[:], bc_last(be1[:], 4), bc_mid(base2[:], NBLK_MAX), ALU.add),
                 reads=["be1", "base2"], writes=["wif1"])
            P.op("dve", lambda e: e.tensor_copy(wi1[:], wif1[:]), reads=["wif1"], writes=["wi1"])
            if stop == "p2c":
                P.flush()
                return nc
            sres = []
            for li in range(nt2):
                hs_ = li % 2
                P.dma("sp", h2r[hs_][:], h2buf[li * 128:(li + 1) * 128, :], reads=[("h2buf", li)], writes=[("h2r", hs_)])
                for k in range(2):
                    P.idma(xs_dram[0:NBLK * BS, :], bass.IndirectOffsetOnAxis(ap=idx[:, k, li:li + 1], axis=0), h2r[hs_][:], None,
                           reads=[("h2r", hs_), "idx"] + zres, writes=[("xss", li, k)])
                    sres.append(("xss", li, k))

            if stop == "p2d":
                P.flush()
                return nc
            yres = []

            def blockA(j):
                ws = j % 2
                if not DBG.get("no_wgather"):
                    for a in range(4):
                        io = bass.IndirectOffsetOnAxis(ap=wi1[:, j, a:a + 1], axis=0)
                        P.idma(w1s[ws][:, 2 * a:2 * a + 2, :].rearrange("p k n -> p (k n)"), None, w1v, io, reads=["wi1"], writes=[("w1s", ws, a)])
                        P.idma(w3s[ws][:, 2 * a:2 * a + 2, :].rearrange("p k n -> p (k n)"), None, w3v, io, reads=["wi1"], writes=[("w3s", ws, a)])
                        P.idma(w2s[ws][:, a, :], None, w2v, io, reads=["wi1"], writes=[("w2s", ws, a)])
                P.dma("sp", xsb[ws][:], xs_dram[j * BS:(j + 1) * BS, :].rearrange("(a p) d -> p a d", p=128),
                      reads=sres + zres, writes=[("xsb", ws)])
                for a in range(4):
                    tb = a % 2

                    def tps_(e, a=a, tb=tb, ws=ws):
                        ins = None
                        for k in range(KC):
                            ins = e.transpose(TB[tb][:, k * 128:(k + 1) * 128], xsb[ws][:, a, k * 128:(k + 1) * 128], ident_b[:])
                        return ins
                    P.op("pe", tps_, reads=[("xsb", ws), "ident_b"], writes=[("TB", tb)])
                    P.op("act", lambda e, a=a, tb=tb, ws=ws: e.activation(out=hTb[ws][:, :, a * 128:(a + 1) * 128],
                                                                         in_=TB[tb][:, :].rearrange("p (k t) -> p k t", k=KC), func=AF.Copy),
                         reads=[("TB", tb)], writes=[("hTb", ws, a)])

            def blockB(j):
                ws = j % 2
                hres = [("hTb", ws, a) for a in range(4)]
                w13 = [("w1s", ws, a) for a in range(4)] + [("w3s", ws, a) for a in range(4)]
                for c in range(4):
                    pb = 2 + 2 * (c % 2)

                    def h13(e, c=c, pb=pb, ws=ws):
                        ins = None
                        for k in range(KC):
                            ins = e.matmul(B[pb][:, :], lhsT=w1s[ws][:, k, c * 128:(c + 1) * 128], rhs=hTb[ws][:, k, :],
                                           start=(k == 0), stop=(k == KC - 1))
                        for k in range(KC):
                            ins = e.matmul(B[pb + 1][:, :], lhsT=w3s[ws][:, k, c * 128:(c + 1) * 128], rhs=hTb[ws][:, k, :],
                                           start=(k == 0), stop=(k == KC - 1))
                        return ins
                    P.op("pe", h13, reads=w13 + hres, writes=[("B", pb), ("B", pb + 1)])
                    P.op("act", lambda e, c=c, pb=pb: e.activation(out=sg[c % 2][:], in_=B[pb][:, :], func=AF.Silu),
                         reads=[("B", pb)], writes=[("sg", c % 2)])
                    P.op("dve", lambda e, c=c, pb=pb, ws=ws: e.tensor_tensor(actT[ws][:, c, :], sg[c % 2][:], B[pb + 1][:, :], ALU.mult),
                         reads=[("sg", c % 2), ("B", pb + 1)], writes=[("actT", ws, c)])
                for a in range(4):
                    ys_ = a % 2
                    for hf in range(2):
                        yb = 6 + hf

                        def ymm(e, a=a, hf=hf, yb=yb, ws=ws):
                            ins = None
                            for c in range(4):
                                ins = e.matmul(B[yb][:, :], lhsT=actT[ws][:, c, a * 128:(a + 1) * 128],
                                               rhs=w2s[ws][:, c, hf * 512:(hf + 1) * 512], start=(c == 0), stop=(c == 3))
                            return ins
                        P.op("pe", ymm, reads=[("actT", ws, c) for c in range(4)] + [("w2s", ws, c) for c in range(4)], writes=[("B", yb)])
                        P.op("dve", lambda e, hf=hf, yb=yb, ys_=ys_: e.tensor_copy(ysb[ys_][:, hf * 512:(hf + 1) * 512], B[yb][:, :]),
                             reads=[("B", yb)], writes=[("ysb", ys_, hf)])
                    r0 = j * BS + a * 128
                    P.dma("sp", ys_dram[r0:r0 + 128, :], ysb[ys_][:], reads=[("ysb", ys_, 0), ("ysb", ys_, 1)], writes=[("ys", j, a)])
                    yres.append(("ys", j, a))


            P.play(P.record(blockA, 0))
            for j in range(NBLK):
                P.play(P.record(blockB, j), P.record(blockA, j + 1) if j + 1 < NBLK else [])
            if stop == "p2e":
                P.flush()
                return nc
            fA = [bufA, x1T[:].rearrange("p a b -> p (a b)")]
            fB = [bufB, dtmp_v]
            fY = [[ysb[0], ysb[1]], [rank_v, oh0_v]]

            def stageF(li):
                t = tiles[li]
                col = 0 if t < NB else 1
                xs = li % 2
                se = li % 2
                bA, bB, ygs = fA[se], fB[se], fY[se]
                nA = ("bufA", se)
                P.dma("sp", x1t[xs][:], x1buf[t * 128:(t + 1) * 128, :], writes=[("x1t", xs)])
                for k in range(2):
                    P.idma(ygs[k][:], None, ys_dram[0:NBLK * BS, :], bass.IndirectOffsetOnAxis(ap=idx[:, k, li:li + 1], axis=0),
                           reads=["idx"] + yres, writes=[("yg", se, k)])
                P.op("dve", lambda e: e.tensor_scalar(bA[:], ygs[0][:], gsc[:, li, 0:1], None, ALU.mult),
                     reads=[("yg", se, 0), ("gsc", li)], writes=[nA])
                P.op("dve", lambda e: e.scalar_tensor_tensor(bA[:], ygs[1][:], gsc[:, li, 1:2], bA[:], ALU.mult, ALU.add),
                     reads=[("yg", se, 1), ("gsc", li), nA], writes=[nA])
                P.op("pool", lambda e: e.tensor_tensor(bA[:], bA[:], g2b[col][:], ALU.mult),
                     reads=[nA, ("g2b", col)], writes=[nA])
                if last:
                    dst, dname = out_d[t * 128:(t + 1) * 128, :], ("out", t)
                else:
                    dst, dname = x2buf[t * 128:(t + 1) * 128, :], ("x2buf", t)
                ln_tail2(x1t[xs], ("x1t", xs), dst, dname, bA, bB, se)

            for li0 in range(0, nt2, 2):
                P.play(P.record(stageF, li0), P.record(stageF, li0 + 1) if li0 + 1 < nt2 else [])
            P.flush()
    glob.close()
    return nc


def _rope_tables(NB):
    NT = NB + NCB
    S = NB * 128
    m = 16
    freqs = (10000.0 ** (-np.arange(m, dtype=np.float32) / m)).astype(np.float32)
    tpos = np.arange(S)
    row = (tpos // GRID_W).astype(np.float32)
    colp = (tpos % GRID_W).astype(np.float32)
    cos = np.ones((128, NT * 128), np.float32)
    sin = np.zeros((128, NT * 128), np.float32)
    for p in range(128):
        d = p % 64
        pos = row if d < 32 else colp
        dd = d % 32
        f = freqs[dd % 16]
        ang = (pos * f).astype(np.float32)
        cos[p, :S] = np.cos(ang)
        sgn = -1.0 if dd < 16 else 1.0
        sin[p, :S] = sgn * np.sin(ang)
    cos_t = np.ascontiguousarray(cos.reshape(128, NT, 128).transpose(1, 0, 2))
    sin_t = np.ascontiguousarray(sin.reshape(128, NT, 128).transpose(1, 0, 2))
    return cos_t, sin_t


def make_in_maps(inputs, NB, ncores):
    f = lambda a: np.ascontiguousarray(np.asarray(a, dtype=np.float32))
    x = f(inputs["x"]); c = f(inputs["c"]); ctx = f(inputs["ctx"]); c_ctx = f(inputs["c_ctx"])
    depth = inputs["w_ada"].shape[0]
    cos_t, sin_t = _rope_tables(NB)
    j = np.arange(128)
    maskl = np.where(j[:, None] >= j[None, :], 0.0, NEG).astype(np.float32)
    maskr = np.where(j[:, None] <= j[None, :], 0.0, NEG).astype(np.float32)
    shared = {
        "w_ada": f(inputs["w_ada"]),
        "bada_p": f(np.asarray(inputs["b_ada"]).reshape(depth, 48, 128).transpose(0, 2, 1)),
        "b_ada": f(inputs["b_ada"]),
        "w_in": f(inputs["w_in"]),
        "convw": f(np.asarray(inputs["conv_w"]).reshape(depth, 3, 2, 128).transpose(0, 3, 2, 1)),
        "sink": f(inputs["attn_sink"]),
        "gm_wsT": f(np.asarray(inputs["gm_ws"]).transpose(0, 1, 3, 2)),
        "gm_bsp": f(np.asarray(inputs["gm_bs"]).transpose(0, 2, 1)),
        "w_out": f(inputs["w_out"]),
        "ln1_g": f(inputs["ln1_g"]), "ln1_b": f(inputs["ln1_b"]),
        "ln2_g": f(inputs["ln2_g"]), "ln2_b": f(inputs["ln2_b"]),
        "w_r": f(np.concatenate([np.asarray(inputs["w_rg"]), np.asarray(inputs["w_re"])], axis=-1)),
        "b_r": f(np.concatenate([np.asarray(inputs["b_rg"]), np.asarray(inputs["b_re"])], axis=-1)),
        "w1": f(np.asarray(inputs["w1"]).reshape(depth, NEXP, KC, 128, DEXP).transpose(0, 1, 3, 2, 4)),
        "w3": f(np.asarray(inputs["w3"]).reshape(depth, NEXP, KC, 128, DEXP).transpose(0, 1, 3, 2, 4)),
        "w2": f(np.asarray(inputs["w2"]).reshape(depth, NEXP, 4, 128, D).transpose(0, 1, 3, 2, 4)),
        "cos_t": cos_t, "sin_t": sin_t,
        "ident": np.eye(128, dtype=np.float32), "maskl": maskl, "maskr": maskr,
        "lt_d": (j[:, None] < j[None, :]).astype(np.float32),
        "jvec_d": np.tile(np.arange((2 * (NB + NCB) * 128) // 512 + NEXP, dtype=np.float32)[None, :], (128, 1)),
        "base1_d": (np.arange(8, dtype=np.float32)[None, :] * 128 + j[:, None]).astype(np.float32),
        "base2_d": np.tile(np.arange(4, dtype=np.float32)[None, :], (128, 1)),
    }
    maps = []
    for b in range(ncores):
        cv = np.stack([c[b].reshape(KC, 128).T, c_ctx.reshape(KC, 128).T], axis=-1)
        m = dict(shared)
        m["xin"] = np.ascontiguousarray(x[b])
        m["ctxin"] = np.ascontiguousarray(ctx[b])
        m["cvec"] = np.ascontiguousarray(cv.astype(np.float32))
        maps.append(m)
    return maps


_NC_CACHE = {}


def kernel(**inputs):
    x = np.asarray(inputs["x"])
    Bn, S, _ = x.shape
    NB = S // 128
    key = (NB,)
    if key not in _NC_CACHE:
        _NC_CACHE[key] = build_program(NB)
    nc = _NC_CACHE[key]
    maps = make_in_maps(inputs, NB, Bn)
    res = run_bass_kernel_spmd(nc, maps, core_ids=list(range(Bn)))
    return np.stack([np.asarray(r["out"], dtype=np.float32) for r in res.results], axis=0)
```

```python
from contextlib import ExitStack
import numpy as np
import ml_dtypes
import concourse.bass as bass
import concourse.mybir as mybir
from concourse.bass_utils import run_bass_kernel_spmd

F32 = mybir.dt.float32
BF16 = mybir.dt.bfloat16
AF = mybir.ActivationFunctionType
ALU = mybir.AluOpType

D = 1024
KC = 8
CTX = 256
NCB = CTX // 128
DEPTH = 2
NEXP = 32
DEXP = 512
ALPHA = (2 * DEPTH) ** 0.25
LN_EPS = 1e-6
GRID_W = 64
NEG = -30000.0
DBG = {}


class Prog:
    NDMA = 8

    def __init__(self, nc):
        self.nc = nc
        self.names = dict(pe="tensor", act="scalar", dve="vector", pool="gpsimd", sp="sync")
        self.sems = []
        self.semidx = {}
        for k in self.names:
            self.semidx[k] = len(self.sems)
            self.sems.append(nc.alloc_semaphore("s_" + k))
        self.cnt = {k: 0 for k in self.names}
        self.dq = {}
        for q in ("sp", "pool"):
            idx = []
            for i in range(self.NDMA):
                idx.append(len(self.sems))
                self.sems.append(nc.alloc_semaphore("d_%s%d" % (q, i)))
            self.dq[q] = dict(idx=idx, cnt=[0] * self.NDMA, nxt=0)
        self.seen = {k: {} for k in self.names}
        self.stream = {k: [] for k in self.names}
        self.res = {}
        self._cap = None

    def record(self, fn, *a, **kw):
        self._cap = []
        fn(*a, **kw)
        cap, self._cap = self._cap, None
        return cap

    def play(self, *lists):
        lists = [l for l in lists if l]
        pos = [0] * len(lists)
        while True:
            best, bf = None, None
            for i, l in enumerate(lists):
                if pos[i] < len(l):
                    f = pos[i] / len(l)
                    if bf is None or f < bf:
                        best, bf = i, f
            if best is None:
                break
            kind, a, kw = lists[best][pos[best]]
            pos[best] += 1
            getattr(self, kind)(*a, **kw)

    def _deps(self, reads, writes):
        deps = []
        for r in reads:
            e = self.res.get(r)
            if e and e["w"] is not None:
                deps.append(e["w"])
        for w in writes:
            e = self.res.get(w)
            if e:
                if e["w"] is not None:
                    deps.append(e["w"])
                deps.extend((si, v, src) for (si, src), v in e["r"].items())
        return deps

    def _waits(self, ek, deps):
        waits = {}
        for (si, val, src) in deps:
            if src == "pe" and ek == "pe":
                continue
            if self.seen[ek].get(si, 0) >= val:
                continue
            waits[si] = max(waits.get(si, 0), val)
        for si, v in waits.items():
            self.seen[ek][si] = v
        return list(waits.items())

    def _record(self, token, reads, writes):
        si, val, src = token
        for r in reads:
            e = self.res.setdefault(r, {"w": None, "r": {}})
            e["r"][(si, src)] = max(e["r"].get((si, src), 0), val)
        for w in writes:
            self.res[w] = {"w": token, "r": {}}

    def op(self, ek, fn, reads=(), writes=()):
        if self._cap is not None:
            self._cap.append(("op", (ek, fn), dict(reads=reads, writes=writes)))
            return None
        waits = self._waits(ek, self._deps(reads, writes))
        self.cnt[ek] += 1
        si = self.semidx[ek]
        token = (si, self.cnt[ek], ek)
        sems = self.sems

        def emit(eng):
            for (wsi, v) in waits:
                eng.wait_ge(sems[wsi], v)
            ins = fn(eng)
            ins.then_inc(sems[si], 1)

        self.stream[ek].append(emit)
        self._record(token, reads, writes)
        return token

    def dma(self, q, out, in_, reads=(), writes=()):
        if self._cap is not None:
            self._cap.append(("dma", (q, out, in_), dict(reads=reads, writes=writes)))
            return None
        d = self.dq[q]
        slot = d["nxt"]
        d["nxt"] = (slot + 1) % self.NDMA
        si = d["idx"][slot]
        deps = self._deps(reads, writes)
        if d["cnt"][slot] > 0:
            deps.append((si, 16 * d["cnt"][slot], "dma"))
        waits = self._waits(q, deps)
        d["cnt"][slot] += 1
        token = (si, 16 * d["cnt"][slot], "dma")
        sems = self.sems

        def emit(eng):
            for (wsi, v) in waits:
                eng.wait_ge(sems[wsi], v)
            eng.dma_start(out=out, in_=in_).then_inc(sems[si], 16)

        self.stream[q].append(emit)
        self._record(token, reads, writes)
        return token

    def idma(self, out, out_off, in_, in_off, reads=(), writes=()):
        if self._cap is not None:
            self._cap.append(("idma", (out, out_off, in_, in_off), dict(reads=reads, writes=writes)))
            return None
        q = "pool"
        d = self.dq[q]
        slot = d["nxt"]
        d["nxt"] = (slot + 1) % self.NDMA
        si = d["idx"][slot]
        deps = self._deps(reads, writes)
        if d["cnt"][slot] > 0:
            deps.append((si, 16 * d["cnt"][slot], "dma"))
        waits = self._waits(q, deps)
        d["cnt"][slot] += 1
        token = (si, 16 * d["cnt"][slot], "dma")
        sems = self.sems

        def emit(eng):
            for (wsi, v) in waits:
                eng.wait_ge(sems[wsi], v)
            eng.indirect_dma_start(out=out, out_offset=out_off, in_=in_, in_offset=in_off).then_inc(sems[si], 16)

        self.stream[q].append(emit)
        self._record(token, reads, writes)
        return token

    def flush(self):
        finals = []
        for k in self.names:
            if self.cnt[k] > 0:
                finals.append((self.semidx[k], self.cnt[k]))
        for q, d in self.dq.items():
            for i, si in enumerate(d["idx"]):
                if d["cnt"][i] > 0:
                    finals.append((si, 16 * d["cnt"][i]))
        with self.nc.Block() as block:
            for ek, nm in self.names.items():
                stream = self.stream[ek]
                seen = self.seen[ek]
                sems = self.sems

                def body(eng, stream=stream, seen=seen):
                    for emit in stream:
                        emit(eng)
                    for si, v in finals:
                        if seen.get(si, 0) < v:
                            eng.wait_ge(sems[si], v)
                            seen[si] = v

                getattr(block, nm)(body)
        self.stream = {k: [] for k in self.names}
        self.res = {}


def bc_mid(ap, n):
    p, f = ap.shape
    return ap.unsqueeze(1).to_broadcast([p, n, f])


def bc_last(ap, n):
    p, g = ap.shape
    return ap.unsqueeze(2).to_broadcast([p, g, n])


def build_program(NB, depth=DEPTH, tps=9, stop=None, nexp=NEXP, do_router=True, do_epi=True):
    S = NB * 128
    NT = NB + NCB
    T = NT * 128
    nc = bass.Bass("TRN2", target_bir_lowering=False)

    def din(name, shape, dt=F32):
        return nc.dram_tensor(name, list(shape), dt, kind="ExternalInput").ap()

    xin = din("xin", [S, D])
    ctxin = din("ctxin", [CTX, D])
    cvec = din("cvec", [128, KC, 2])
    w_ada = din("w_ada", [depth, D, 6 * D])
    bada_p = din("bada_p", [depth, 128, 48])
    b_ada = din("b_ada", [depth, 6 * D])
    w_in = din("w_in", [depth, D, 2048])
    convw = din("convw", [depth, 128, 2, 3])
    sink = din("sink", [depth, 8])
    gm_wsT = din("gm_wsT", [depth, 4, 128, 128])
    gm_bsp = din("gm_bsp", [depth, 128, 4])
    w_out = din("w_out", [depth, D, D])
    ln1_g = din("ln1_g", [depth, D])
    ln1_b = din("ln1_b", [depth, D])
    ln2_g = din("ln2_g", [depth, D])
    ln2_b = din("ln2_b", [depth, D])
    w_r = din("w_r", [depth, D, 36])
    b_r = din("b_r", [depth, 36])
    w1 = din("w1", [depth, NEXP, 128, KC, DEXP])
    w3 = din("w3", [depth, NEXP, 128, KC, DEXP])
    w2 = din("w2", [depth, NEXP, 128, 4, D])
    cos_t = din("cos_t", [NT, 128, 128])
    sin_t = din("sin_t", [NT, 128, 128])
    ident_d = din("ident", [128, 128])
    maskl_d = din("maskl", [128, 128])
    maskr_d = din("maskr", [128, 128])
    out_d = nc.dram_tensor("out", [S, D], F32, kind="ExternalOutput").ap()
    x1buf = nc.dram_tensor("x1buf", [T, D], F32).ap()
    x2buf = nc.dram_tensor("x2buf", [T, D], F32).ap()
    BS = 512
    NBLK_MAX = (2 * T) // BS + NEXP
    h2buf = nc.dram_tensor("h2buf", [T, D], BF16).ap()
    xs_dram = nc.dram_tensor("xs_dram", [NBLK_MAX * BS, D], BF16).ap()
    ys_dram = nc.dram_tensor("ys_dram", [NBLK_MAX * BS, D], F32).ap()
    modrows = nc.dram_tensor("modrows", [depth, 2, 4, D], F32).ap()
    lt_d = din("lt_d", [128, 128])
    jvec_d = din("jvec_d", [128, NBLK_MAX])
    base1_d = din("base1_d", [128, 8])
    base2_d = din("base2_d", [128, 4])

    P = Prog(nc)

    def tile_src(l, t):
        if l == 0:
            if t < NB:
                return xin[t * 128:(t + 1) * 128, :]
            return ctxin[(t - NB) * 128:(t - NB + 1) * 128, :]
        return x2buf[t * 128:(t + 1) * 128, :]

    glob = ExitStack()

    uid = [0]

    def alloc(st, name, shape, dt):
        uid[0] += 1
        return st.enter_context(nc.sbuf_tensor("sb%d_%s" % (uid[0], name), list(shape), dt))

    def palloc(st, name, shape, dt=F32):
        uid[0] += 1
        return st.enter_context(nc.psum_tensor("ps%d_%s" % (uid[0], name), list(shape), dt))

    ident_f = alloc(glob, "ident_f", [128, 128], F32)
    ident_b = alloc(glob, "ident_b", [128, 128], BF16)
    maskl = alloc(glob, "maskl", [128, 128], BF16)
    maskr = alloc(glob, "maskr", [128, 128], BF16)
    cact = alloc(glob, "cact", [128, KC, 2], F32)
    cact_rep = alloc(glob, "cact_rep", [128, KC, 2, 128], F32)
    ones_f = alloc(glob, "ones_f", [1, 128], F32)
    epsb = alloc(glob, "epsb", [128, 1], F32)
    modT = alloc(glob, "modT", [128, 48, 2], F32)

    P.dma("sp", ident_f[:], ident_d[:, :], writes=["ident_f"])
    P.dma("pool", ident_b[:], ident_d[:, :], writes=["ident_b"])
    P.dma("pool", maskl[:], maskl_d[:, :], writes=["maskl"])
    P.dma("pool", maskr[:], maskr_d[:, :], writes=["maskr"])
    P.dma("sp", cact[:], cvec[:, :, :], writes=["cact"])
    P.op("act", lambda e: e.activation(out=cact[:], in_=cact[:], func=AF.Silu), reads=["cact"], writes=["cact"])
    P.op("dve", lambda e: e.tensor_copy(cact_rep[:].rearrange("p k c m -> p (k c) m"),
                                        bc_last(cact[:].rearrange("p k c -> p (k c)"), 128)),
         reads=["cact"], writes=["cact_rep"])
    P.op("dve", lambda e: e.memset(ones_f[:], 1.0), writes=["ones_f"])
    P.op("dve", lambda e: e.memset(epsb[:], LN_EPS), writes=["epsb"])
    P.flush()
    if stop == "const":
        return nc

    for l in range(depth):
        last = (l == depth - 1)
        with ExitStack() as st:
            wa = [alloc(st, "wa%d" % i, [128, KC, D], F32) for i in range(2)]
            bada = alloc(st, "bada", [128, 48], F32)
            brow = [alloc(st, "brow%d" % i, [128, D], F32) for i in range(2)]
            grow = [alloc(st, "grow%d" % i, [128, D], F32) for i in range(2)]
            ps_mod = palloc(st, "ps_mod", [128, 96])
            ps_g = [palloc(st, "ps_g%d" % i, [128, 512]) for i in range(4)]
            P.dma("sp", bada[:], bada_p[l], writes=["bada"])
            for i in range(6):
                s = i % 2
                P.dma("sp", wa[s][:], w_ada[l][:, i * D:(i + 1) * D].rearrange("(k p) n -> p k n", p=128),
                      writes=[("wa", s)])

                def mm_mod(e, i=i, s=s):
                    ins = None
                    for j in range(KC):
                        for k in range(KC):
                            ins = e.matmul(ps_mod[:, (i * 8 + j) * 2:(i * 8 + j) * 2 + 2],
                                           lhsT=wa[s][:, k, j * 128:(j + 1) * 128], rhs=cact[:, k, :],
                                           start=(k == 0), stop=(k == KC - 1))
                    return ins
                P.op("pe", mm_mod, reads=[("wa", s), "cact"], writes=[("ps_mod", i)])
                if i >= 2:
                    gi = i - 2
                    P.dma("sp", brow[gi % 2][:], b_ada[l:l + 1, i * D:(i + 1) * D].to_broadcast([128, D]),
                          writes=[("brow", gi % 2)])
                    for c in range(2):
                        for h in range(2):
                            def mm_g(e, s=s, c=c, h=h):
                                ins = None
                                for k in range(KC):
                                    ins = e.matmul(ps_g[c * 2 + h][:, :], lhsT=cact_rep[:, k, c, :],
                                                   rhs=wa[s][:, k, h * 512:(h + 1) * 512],
                                                   start=(k == 0), stop=(k == KC - 1))
                                return ins
                            P.op("pe", mm_g, reads=[("wa", s), "cact_rep"], writes=[("ps_g", c * 2 + h)])
                            P.op("dve", lambda e, gi=gi, c=c, h=h: e.tensor_tensor(
                                grow[c][:, h * 512:(h + 1) * 512], ps_g[c * 2 + h][:, :],
                                brow[gi % 2][:, h * 512:(h + 1) * 512], ALU.add),
                                reads=[("ps_g", c * 2 + h), ("brow", gi % 2)], writes=[("grow", c, h)])
                        if i == 4:
                            P.op("dve", lambda e, c=c: e.tensor_scalar(grow[c][0:1, :], grow[c][0:1, :], 1.0, None, ALU.add),
                                 reads=[("grow", c, 0), ("grow", c, 1)], writes=[("grow", c, 0), ("grow", c, 1)])
                        P.dma("sp", modrows[l, c, gi:gi + 1, :], grow[c][0:1, :],
                              reads=[("grow", c, 0), ("grow", c, 1)], writes=[("modrows", c, gi)])
            P.op("dve", lambda e: e.tensor_tensor(modT[:], ps_mod[:].rearrange("p (j c) -> p j c", c=2),
                                                  bc_last(bada[:], 2), ALU.add),
                 reads=[("ps_mod", i) for i in range(6)] + ["bada"], writes=["modT"])
            P.op("dve", lambda e: e.tensor_scalar(modT[:, 8:16, :], modT[:, 8:16, :], 1.0, None, ALU.add),
                 reads=["modT"], writes=["modT"])
            P.op("dve", lambda e: e.tensor_scalar(modT[:, 32:40, :], modT[:, 32:40, :], 1.0, None, ALU.add),
                 reads=["modT"], writes=["modT"])
            P.flush()
        if stop == "p0":
            return nc

        with ExitStack() as st:
            NCOL = 18 * 128 + 640
            win = alloc(st, "win", [128, KC, NCOL], BF16)
            wout = alloc(st, "wout", [128, KC, D], BF16)
            kT = alloc(st, "kT", [128, 2, T], BF16)
            V = alloc(st, "V", [128, NT, 2, 65], BF16)
            convw_s = alloc(st, "convw_s", [128, 2, 3], F32)
            wsT = alloc(st, "wsT", [128, 4, 128], BF16)
            gmb = alloc(st, "gmb", [128, 4], F32)
            esink = alloc(st, "esink", [128, 8], F32)
            lng = alloc(st, "lng", [128, D], F32)
            lnb = alloc(st, "lnb", [128, D], F32)
            g1b = [alloc(st, "g1b%d" % c, [128, D], F32) for c in range(2)]
            for c in range(2):
                P.dma("sp", g1b[c][:], modrows[l, c, 0:1, :].to_broadcast([128, D]), writes=[("g1b", c)])
            R = 4
            xres = [alloc(st, "xres%d" % i, [128, D], F32) for i in range(4)]
            hT = [alloc(st, "hT%d" % i, [128, KC, 128], BF16) for i in range(2)]
            qrot = [alloc(st, "qrot%d" % i, [128, 4, 128], BF16) for i in range(R)]
            u = [alloc(st, "u%d" % i, [128, 2, 130], F32) for i in range(R)]
            cb = [alloc(st, "cb%d" % i, [128, 2, 128], F32) for i in range(R)]
            gut = [alloc(st, "gut%d" % i, [128, 256], F32) for i in range(R)]
            vln = [alloc(st, "vln%d" % i, [128, 256], BF16) for i in range(R)]
            cosb = [alloc(st, "cosb%d" % i, [128, 128], F32) for i in range(2)]
            sinb = [alloc(st, "sinb%d" % i, [128, 128], F32) for i in range(2)]
            t1 = alloc(st, "t1", [128, 4, 128], F32)
            t2 = alloc(st, "t2", [128, 4, 128], F32)
            cxs = alloc(st, "cxs", [128, 2, 128], F32)
            gvt = alloc(st, "gvt", [128, 256], F32)
            st6 = alloc(st, "st6", [128, 6], F32)
            mv = alloc(st, "mv", [128, 2], F32)
            rstd = alloc(st, "rstd", [128, 1], F32)
            nb_ = alloc(st, "nb_", [128, 1], F32)
            PT = [alloc(st, "PT%d" % i, [128, 5, 128], BF16) for i in range(4)]
            den = alloc(st, "den", [128, 4], F32)
            mixtok = [alloc(st, "mixtok%d" % i, [128, 768], BF16) for i in range(2)]
            mixT = [alloc(st, "mixT%d" % i, [128, KC, 128], BF16) for i in range(2)]
            ct = alloc(st, "ct", [128, 2, 128], F32)
            gtmp = alloc(st, "gtmp", [128, 256], F32)
            bufAs = [alloc(st, "bufA%d" % i, [128, D], F32) for i in range(2)]
            bufB = alloc(st, "bufB", [128, D], F32)
            st12 = alloc(st, "st12", [128, 12], F32)
            mv2 = alloc(st, "mv2", [128, 2], F32)
            rstd2 = alloc(st, "rstd2", [128, 1], F32)
            nb2 = alloc(st, "nb2", [128, 1], F32)
            B = [palloc(st, "B%d" % i, [128, 512]) for i in range(7)]
            Bbf = palloc(st, "Bbf", [128, 1024], BF16)
            Bo = B[4]

            wv = w_in[l].rearrange("(k p) n -> p k n", p=128)
            P.dma("pool", win[:, :, 0:512], wv[:, :, 0:512], writes=["win"])
            for kv in range(2):
                for dup in range(2):
                    c0 = 1024 + kv * 128 + dup * 64
                    P.dma("pool", win[:, :, c0:c0 + 64], wv[:, :, 512 + kv * 64:512 + (kv + 1) * 64], writes=["win"])
            P.dma("pool", win[:, :, 1536:2304], wv[:, :, 768:1536], writes=["win"])
            P.dma("pool", win[:, :, 2304:2432], wv[:, :, 640:768], writes=["win"])
            P.dma("pool", win[:, :, 2432:2688], wv[:, :, 1792:2048], writes=["win"])
            P.dma("pool", win[:, :, 2688:2944], wv[:, :, 1536:1792], writes=["win"])
            P.dma("pool", wout[:], w_out[l].rearrange("(k p) n -> p k n", p=128), writes=["wout"])
            for (src0, dst0, nblk) in ((0, 512, 16), (1024, 1280, 8)):
                for a in range(2):
                    def swp(e, src0=src0, dst0=dst0, nblk=nblk, a=a):
                        srcv = win[:, :, src0:src0 + nblk * 32].rearrange("p k (b a f) -> p k b a f", a=2, f=16)
                        dstv = win[:, :, dst0:dst0 + nblk * 32].rearrange("p k (b a f) -> p k b a f", a=2, f=16)
                        return e.tensor_copy(dstv[:, :, :, a, :], srcv[:, :, :, 1 - a, :])
                    P.op("dve", swp, reads=["win"], writes=["win"])
            P.dma("sp", convw_s[:], convw[l], writes=["convw_s"])
            P.dma("pool", wsT[:], gm_wsT[l].rearrange("g q p -> q g p"), writes=["wsT"])
            P.dma("sp", gmb[:], gm_bsp[l], writes=["gmb"])
            P.dma("sp", esink[:], sink[l:l + 1, :].to_broadcast([128, 8]), writes=["esink"])
            P.op("act", lambda e: e.activation(out=esink[:], in_=esink[:], func=AF.Exp), reads=["esink"], writes=["esink"])
            P.dma("sp", lng[:], ln1_g[l:l + 1, :].to_broadcast([128, D]), writes=["lng"])
            P.dma("sp", lnb[:], ln1_b[l:l + 1, :].to_broadcast([128, D]), writes=["lnb"])
            P.op("pool", lambda e: e.memset(V[:, :, :, 64:65], 1.0), writes=["Vones"])

            def seq_of(t):
                return (0, NB) if t < NB else (NB, NT)

            def inproj(t, kv_only=False):
                s = t % R
                xs = t % 4
                hs = t % 2
                cs = t % 2
                col = 0 if t < NB else 1
                lo, hi = seq_of(t)
                P.dma("sp", xres[xs][:], tile_src(l, t), writes=[("xres", xs)])
                P.dma("sp", cosb[cs][:], cos_t[t], writes=[("cosb", cs)])
                P.dma("sp", sinb[cs][:], sin_t[t], writes=[("sinb", cs)])
                for hb in range(2):
                    def tp(e, hb=hb):
                        ins = None
                        for i in range(4):
                            j = hb * 4 + i
                            ins = e.transpose(B[2 + hb][:, i * 128:(i + 1) * 128], xres[xs][:, j * 128:(j + 1) * 128], ident_f[:])
                        return ins
                    P.op("pe", tp, reads=[("xres", xs), "ident_f"], writes=[("B", 2 + hb)])

                    def ev(e, hb=hb):
                        ins = None
                        for i in range(4):
                            j = hb * 4 + i
                            ins = e.activation(out=hT[hs][:, j, :], in_=B[2 + hb][:, i * 128:(i + 1) * 128], func=AF.Identity,
                                               scale=modT[:, 8 + j, col:col + 1], bias=modT[:, j, col:col + 1])
                        return ins
                    P.op("act", ev, reads=[("B", 2 + hb), "modT"], writes=[("hT", hs, hb)])
                hres = [("hT", hs, 0), ("hT", hs, 1)]

                def fm_group(bank, off, chunks):
                    def f(e):
                        ins = None
                        for i, c in enumerate(chunks):
                            for k in range(KC):
                                ins = e.matmul(B[bank][:, off + i * 128:off + (i + 1) * 128],
                                               lhsT=win[:, k, c * 128:(c + 1) * 128], rhs=hT[hs][:, k, :],
                                               start=(k == 0), stop=(k == KC - 1))
                        return ins
                    return f

                if not kv_only:
                    P.op("pe", fm_group(2, 0, [0, 1, 2, 3]), reads=hres + ["win"], writes=[("B", 2)])
                    P.op("pe", fm_group(3, 0, [4, 5, 6, 7]), reads=hres + ["win"], writes=[("B", 3)])
                    P.op("dve", lambda e: e.tensor_tensor(t1[:], B[2][:, :].rearrange("p (c t) -> p c t", c=4),
                                                          bc_mid(cosb[cs][:], 4), ALU.mult),
                         reads=[("B", 2), ("cosb", cs)], writes=["t1"])
                    P.op("dve", lambda e: e.tensor_tensor(t2[:], B[3][:, :].rearrange("p (c t) -> p c t", c=4),
                                                          bc_mid(sinb[cs][:], 4), ALU.mult),
                         reads=[("B", 3), ("sinb", cs)], writes=["t2"])
                    P.op("pool", lambda e: e.tensor_tensor(qrot[s][:], t1[:], t2[:], ALU.add),
                         reads=["t1", "t2"], writes=[("qrot", s)])
                P.op("pe", fm_group(2, 0, [8, 9, 10, 11]), reads=hres + ["win"], writes=[("B", 2)])
                P.op("dve", lambda e: e.tensor_tensor(t1[:, 0:2, :], B[2][:, 0:256].rearrange("p (c t) -> p c t", c=2),
                                                      bc_mid(cosb[cs][:], 2), ALU.mult),
                     reads=[("B", 2), ("cosb", cs)], writes=["t1"])
                P.op("dve", lambda e: e.tensor_tensor(t2[:, 0:2, :], B[2][:, 256:512].rearrange("p (c t) -> p c t", c=2),
                                                      bc_mid(sinb[cs][:], 2), ALU.mult),
                     reads=[("B", 2), ("sinb", cs)], writes=["t2"])
                P.op("pool", lambda e: e.tensor_tensor(kT[:, :, t * 128:(t + 1) * 128], t1[:, 0:2, :], t2[:, 0:2, :], ALU.add),
                     reads=["t1", "t2"], writes=[("kT", t)])
                if not kv_only:
                    P.op("pe", fm_group(3, 0, [12, 13, 14, 15]), reads=hres + ["win"], writes=[("B", 3)])
                    P.op("pe", fm_group(2, 0, [16, 17]), reads=hres + ["win"], writes=[("B", 2)])
                    P.op("act", lambda e: e.activation(out=cb[s][:], in_=B[3][:, 0:256].rearrange("p (c t) -> p c t", c=2),
                                                       func=AF.Copy), reads=[("B", 3)], writes=[("cb", s)])
                    P.op("act", lambda e: e.activation(out=cxs[:], in_=B[2][:, 0:256].rearrange("p (c t) -> p c t", c=2),
                                                       func=AF.Copy), reads=[("B", 2)], writes=["cxs"])
                    P.op("dve", lambda e: e.tensor_tensor(u[s][:, :, 1:129], B[3][:, 256:512].rearrange("p (c t) -> p c t", c=2),
                                                          cxs[:], ALU.mult),
                         reads=[("B", 3), "cxs"], writes=[("u", s)])
                    if t > lo:
                        sp_ = (t - 1) % R
                        P.op("pool", lambda e: e.tensor_copy(u[sp_][:, :, 129:130], u[s][:, :, 1:2]),
                             reads=[("u", s)], writes=[("u", sp_)])
                    else:
                        P.op("pool", lambda e: e.memset(u[s][:, :, 0:1], 0.0), reads=[("u", s)], writes=[("u", s)])
                    if t < hi - 1:
                        sn_ = (t + 1) % R
                        P.op("pool", lambda e: e.tensor_copy(u[sn_][:, :, 0:1], u[s][:, :, 128:129]),
                             reads=[("u", s)], writes=[("u", sn_)])
                    else:
                        P.op("pool", lambda e: e.memset(u[s][:, :, 129:130], 0.0), reads=[("u", s)], writes=[("u", s)])

                def tm(e):
                    ins = None
                    for k in range(KC):
                        ins = e.matmul(B[3][:, 0:384], lhsT=hT[hs][:, k, :], rhs=win[:, k, 2304:2688],
                                       start=(k == 0), stop=(k == KC - 1))
                    return ins
                P.op("pe", tm, reads=hres + ["win"], writes=[("B", 3)])
                P.op("act", lambda e: e.activation(out=V[:, t, :, 0:64], in_=B[3][:, 0:128].rearrange("p (h d) -> p h d", h=2),
                                                   func=AF.Copy), reads=[("B", 3)], writes=[("V", t)])
                if not kv_only:
                    def tm2(e):
                        ins = None
                        for k in range(KC):
                            ins = e.matmul(B[2][:, 256:512], lhsT=hT[hs][:, k, :], rhs=win[:, k, 2688:2944],
                                           start=(k == 0), stop=(k == KC - 1))
                        return ins
                    P.op("pe", tm2, reads=hres + ["win"], writes=[("B", 2)])
                    P.op("act", lambda e: e.activation(out=gvt[:], in_=B[3][:, 128:384], func=AF.Gelu_apprx_tanh),
                         reads=[("B", 3)], writes=["gvt"])
                    P.op("act", lambda e: e.activation(out=gut[s][:], in_=B[2][:, 256:512], func=AF.Gelu_apprx_tanh),
                         reads=[("B", 2)], writes=[("gut", s)])
                    P.op("dve", lambda e: e.bn_stats(st6[:], gvt[:]), reads=["gvt"], writes=["st6"])
                    P.op("dve", lambda e: e.bn_aggr(mv[:], st6[:]), reads=["st6"], writes=["mv"])
                    P.op("act", lambda e: e.activation(out=rstd[:], in_=mv[:, 1:2], func=AF.Ln, bias=epsb[:, 0:1]),
                         reads=["mv", "epsb"], writes=["rstd"])
                    P.op("act", lambda e: e.activation(out=rstd[:], in_=rstd[:], func=AF.Exp, scale=-0.5), reads=["rstd"], writes=["rstd"])
                    P.op("dve", lambda e: e.scalar_tensor_tensor(nb_[:], mv[:, 0:1], -1.0, rstd[:], ALU.mult, ALU.mult),
                         reads=["mv", "rstd"], writes=["nb_"])
                    P.op("act", lambda e: e.activation(out=vln[s][:], in_=gvt[:], func=AF.Identity,
                                                       scale=rstd[:, 0:1], bias=nb_[:, 0:1]),
                         reads=["gvt", "rstd", "nb_"], writes=[("vln", s)])

            def mixers(t):
                s = t % R
                xs = t % 4
                ms = t % 2
                col = 0 if t < NB else 1
                lo, hi = seq_of(t)
                if t < NB:
                    kcs = [(NB, None), (NB + 1, None)]
                    if t > 0:
                        kcs.append((t - 1, maskl))
                    kcs.append((t, None))
                    if t < NB - 1:
                        kcs.append((t + 1, maskr))
                else:
                    kcs = [(NB, None), (NB + 1, None)]
                nk = len(kcs)
                for h in range(8):
                    qc, po, kv, pslot = h // 2, (h % 2) * 64, h // 4, h % 4

                    if h % 2 == 0:
                        sbank, s5, wr5 = B[6], B[4][:, 384:512], ("B", 4)
                        wr = [("B", 6)] + ([wr5] if nk > 4 else [])
                    else:
                        sbank, s5, wr5 = B[5], B[1][:, 384:512], ("B", 1)
                        wr = [("B", 5)] + ([wr5] if nk > 4 else [])

                    def sc(e, qc=qc, po=po, kv=kv, sbank=sbank, s5=s5):
                        ins = None
                        for i, (c, msk) in enumerate(kcs):
                            dst = sbank[:, i * 128:(i + 1) * 128] if i < 4 else s5
                            ins = e.matmul(dst, lhsT=kT[po:po + 64, kv, c * 128:(c + 1) * 128],
                                           rhs=qrot[s][po:po + 64, qc, :], start=True, stop=(msk is None))
                            if msk is not None:
                                ins = e.matmul(dst, lhsT=ident_b[:], rhs=msk[:], start=False, stop=True)
                        return ins
                    P.op("pe", sc, reads=[("kT", c) for c, _ in kcs] + [("qrot", s), "ident_b", "maskl", "maskr"], writes=wr)

                    def ex(e, pslot=pslot, sbank=sbank, s5=s5):
                        n1 = min(nk, 4)
                        ins = e.activation(out=PT[pslot][:, 0:n1, :], in_=sbank[:, 0:n1 * 128].rearrange("p (c t) -> p c t", c=n1),
                                           func=AF.Exp, scale=0.125)
                        if nk > 4:
                            ins = e.activation(out=PT[pslot][:, 4, :], in_=s5, func=AF.Exp, scale=0.125)
                        return ins
                    P.op("act", ex, reads=wr, writes=[("PT", pslot)])

                    def pv(e, kv=kv, pslot=pslot):
                        ins = None
                        for i, (c, msk) in enumerate(kcs):
                            ins = e.matmul(Bo[:, pslot * 65:(pslot + 1) * 65],
                                           lhsT=PT[pslot][:, i, :], rhs=V[:, c, kv, :],
                                           start=(i == 0), stop=(i == nk - 1))
                        return ins
                    P.op("pe", pv, reads=[("PT", pslot), "Vones"] + [("V", c) for c, _ in kcs], writes=[("B", 4)])
                    if pslot == 3:
                        hg = h // 4
                        bov = Bo[:, 0:260].rearrange("p (h d) -> p h d", d=65)
                        P.op("dve", lambda e, hg=hg, bov=bov: e.tensor_tensor(den[:], bov[:, :, 64], esink[:, hg * 4:(hg + 1) * 4], ALU.add),
                             reads=[("B", 4), "esink"], writes=["den"])
                        P.op("dve", lambda e: e.reciprocal(den[:], den[:]), reads=["den"], writes=["den"])
                        P.op("dve", lambda e, hg=hg, bov=bov: e.tensor_tensor(
                            mixtok[ms][:, hg * 256:(hg + 1) * 256].rearrange("p (h d) -> p h d", d=64),
                            bov[:, :, 0:64], bc_last(den[:], 64), ALU.mult),
                            reads=[("B", 4), "den"], writes=[("mixtok", ms, hg)])
                for j in range(2):
                    P.op("pool", lambda e, j=j: e.tensor_scalar(ct[:, j, :], u[s][:, j, 0:128], convw_s[:, j, 0:1], None, ALU.mult),
                         reads=[("u", s), "convw_s"], writes=[("ct", j)])
                    P.op("dve", lambda e, j=j: e.scalar_tensor_tensor(ct[:, j, :], u[s][:, j, 1:129], convw_s[:, j, 1:2], ct[:, j, :], ALU.mult, ALU.add),
                         reads=[("u", s), "convw_s", ("ct", j)], writes=[("ct", j)])
                    P.op("dve", lambda e, j=j: e.scalar_tensor_tensor(ct[:, j, :], u[s][:, j, 2:130], convw_s[:, j, 2:3], ct[:, j, :], ALU.mult, ALU.add),
                         reads=[("u", s), "convw_s", ("ct", j)], writes=[("ct", j)])
                    P.op("pool", lambda e, j=j: e.tensor_tensor(mixT[ms][:, 4 + j, :], cb[s][:, j, :], ct[:, j, :], ALU.mult),
                         reads=[("cb", s), ("ct", j)], writes=[("mixT", ms, 4 + j)])
                def gm(e):
                    ins = None
                    for g in range(4):
                        ins = e.matmul(B[1][:, g * 64:(g + 1) * 64], lhsT=wsT[:, g, :], rhs=vln[s][:, g * 64:(g + 1) * 64],
                                       start=True, stop=True)
                    return ins
                P.op("pe", gm, reads=[("vln", s), "wsT"], writes=[("B", 1)])
                P.op("dve", lambda e: e.tensor_tensor(gtmp[:].rearrange("p (g c) -> p g c", g=4),
                                                      B[1][:, 0:256].rearrange("p (g c) -> p g c", g=4),
                                                      bc_last(gmb[:], 64), ALU.add),
                     reads=[("B", 1), "gmb"], writes=["gtmp"])
                P.op("dve", lambda e: e.tensor_tensor(mixtok[ms][:, 512:768], gtmp[:], gut[s][:], ALU.mult),
                     reads=["gtmp", ("gut", s)], writes=[("mixtok", ms, 2)])
                def tpm(e):
                    ins = None
                    for i in range(6):
                        ins = e.transpose(Bbf[:, i * 128:(i + 1) * 128], mixtok[ms][:, i * 128:(i + 1) * 128], ident_b[:])
                    return ins
                P.op("pe", tpm, reads=[("mixtok", ms, i) for i in range(3)] + ["ident_b"], writes=["Bbf"])
                P.op("act", lambda e: e.activation(out=mixT[ms][:, 0:4, :], in_=Bbf[:, 0:512].rearrange("p (c t) -> p c t", c=4), func=AF.Copy),
                     reads=["Bbf"], writes=[("mixT", ms, i) for i in range(4)])
                P.op("act", lambda e: e.activation(out=mixT[ms][:, 6:8, :], in_=Bbf[:, 512:768].rearrange("p (c t) -> p c t", c=2), func=AF.Copy),
                     reads=["Bbf"], writes=[("mixT", ms, 6), ("mixT", ms, 7)])
                for hf in range(2):
                    def op_(e, hf=hf):
                        ins = None
                        for k in range(KC):
                            ins = e.matmul(B[hf][:, :], lhsT=mixT[ms][:, k, :], rhs=wout[:, k, hf * 512:(hf + 1) * 512],
                                           start=(k == 0), stop=(k == KC - 1))
                        return ins
                    P.op("pe", op_, reads=[("mixT", ms, i) for i in range(8)] + ["wout"], writes=[("B", hf)])
                    P.op("dve", lambda e, hf=hf: e.tensor_tensor(bufAs[t % 2][:, hf * 512:(hf + 1) * 512], B[hf][:, :],
                                                                g1b[col][:, hf * 512:(hf + 1) * 512], ALU.mult),
                         reads=[("B", hf), ("g1b", col)], writes=[("bufA", t % 2, hf)])

            def mixB(t):
                xs = t % 4
                layer_norm_tail(xres[xs], ("xres", xs), lng, lnb, x1buf[t * 128:(t + 1) * 128, :], ("x1buf", t), t % 2)

            def layer_norm_tail(xt, xres_name, g_t, b_t, dst, dst_name, ab):
                bufA = bufAs[ab]
                P.op("dve", lambda e: e.scalar_tensor_tensor(bufA[:], xt[:], ALPHA, bufA[:], ALU.mult, ALU.add),
                     reads=[xres_name, ("bufA", ab, 0), ("bufA", ab, 1)], writes=[("bufA", ab, 0), ("bufA", ab, 1)])
                P.op("dve", lambda e: e.bn_stats(st12[:, 0:6], bufA[:, 0:512]), reads=[("bufA", ab, 0)], writes=[("st12", 0)])
                P.op("dve", lambda e: e.bn_stats(st12[:, 6:12], bufA[:, 512:1024]), reads=[("bufA", ab, 1)], writes=[("st12", 1)])
                P.op("dve", lambda e: e.bn_aggr(mv2[:], st12[:]), reads=[("st12", 0), ("st12", 1)], writes=["mv2"])
                P.op("act", lambda e: e.activation(out=rstd2[:], in_=mv2[:, 1:2], func=AF.Ln, bias=epsb[:, 0:1]),
                     reads=["mv2", "epsb"], writes=["rstd2"])
                P.op("act", lambda e: e.activation(out=rstd2[:], in_=rstd2[:], func=AF.Exp, scale=-0.5), reads=["rstd2"], writes=["rstd2"])
                P.op("dve", lambda e: e.scalar_tensor_tensor(nb2[:], mv2[:, 0:1], -1.0, rstd2[:], ALU.mult, ALU.mult),
                     reads=["mv2", "rstd2"], writes=["nb2"])
                P.op("act", lambda e: e.activation(out=bufB[:], in_=bufA[:], func=AF.Identity, scale=rstd2[:, 0:1], bias=nb2[:, 0:1]),
                     reads=[("bufA", ab, 0), ("bufA", ab, 1), "rstd2", "nb2"], writes=["bufB"])
                P.op("pool", lambda e: e.tensor_tensor(bufB[:], bufB[:], g_t[:], ALU.mult), reads=["bufB", "lng"], writes=["bufB"])
                P.op("pool", lambda e: e.tensor_tensor(bufB[:], bufB[:], b_t[:], ALU.add), reads=["bufB", "lnb"], writes=["bufB"])
                P.dma("sp", dst, bufB[:], reads=["bufB"], writes=[dst_name])

            for t in range(NB, NT):
                inproj(t, kv_only=last)
            if not last:
                for t in range(NB, NT):
                    mixers(t)
                    mixB(t)
            inproj(0)
            if NB > 1:
                inproj(1)
            for n in range(NB):
                la = P.record(mixers, n)
                lb = P.record(inproj, n + 2) if n + 2 < NB else []
                lc = P.record(mixB, n - 1) if n >= 1 else []
                P.play(la, lb, lc)
            mixB(NB - 1)
            P.flush()
        if stop == "p1":
            return nc

        tiles = list(range(NB)) if last else list(range(NT))
        nt2 = len(tiles)
        NBLK = (2 * nt2 * 128) // BS + NEXP
        AX = mybir.AxisListType.X
        w1v = w1.rearrange("l e p (a k) n -> (l e p a) (k n)", a=4)
        w3v = w3.rearrange("l e p (a k) n -> (l e p a) (k n)", a=4)
        w2v = w2.rearrange("l e p (a k) n -> (l e p a) (k n)", a=4)
        with ExitStack() as st:
            wr_s = alloc(st, "wr_s", [128, KC, 36], F32)
            wr_m = [alloc(st, "wr_m%d" % c, [128, KC, 36], F32) for c in range(2)]
            sh2rep = [alloc(st, "sh2rep%d" % c, [128, KC, 128], F32) for c in range(2)]
            br_row = alloc(st, "br_row", [1, 36], F32)
            lng = alloc(st, "lng2", [128, D], F32)
            lnb = alloc(st, "lnb2", [128, D], F32)
            scb = [alloc(st, "scb%d" % c, [128, D], F32) for c in range(2)]
            shb = [alloc(st, "shb%d" % c, [128, D], F32) for c in range(2)]
            g2b = [alloc(st, "g2b%d" % c, [128, D], F32) for c in range(2)]
            lt_b = alloc(st, "lt_b", [128, 128], BF16)
            ones_b = alloc(st, "ones_b", [128, 128], BF16)
            jvec = alloc(st, "jvec", [128, NBLK_MAX], F32)
            base1 = alloc(st, "base1", [128, 8], F32)
            base2 = alloc(st, "base2", [128, 4], F32)
            x1t = [alloc(st, "x1t%d" % i, [128, D], F32) for i in range(2)]
            x1T = alloc(st, "x1T", [128, KC, 128], F32)
            h2r = [alloc(st, "h2r%d" % i, [128, D], BF16) for i in range(2)]
            lg = alloc(st, "lg", [128, 36], F32)
            sm = alloc(st, "sm", [128, 16], F32)
            goh = alloc(st, "goh", [128, 4], F32)
            gex = alloc(st, "gex", [128, 4], F32)
            esel = alloc(st, "esel", [128, 8], F32)
            esel2 = alloc(st, "esel2", [128, 8], F32)
            eq1 = alloc(st, "eq1", [128, 8], F32)
            eq2 = alloc(st, "eq2", [128, 8], F32)
            oh0 = alloc(st, "oh0", [128, nt2, 32], F32)
            oh1 = alloc(st, "oh1", [128, nt2, 32], F32)
            cntb = alloc(st, "cntb", [128, nt2, 32], BF16)
            gsc = alloc(st, "gsc", [128, nt2, 2], F32)
            rank_s = alloc(st, "rank_s", [128, nt2, 32], F32)
            dtmp = alloc(st, "dtmp", [128, nt2, 32], F32)
            tot = alloc(st, "tot", [128, 32], F32)
            nblk = alloc(st, "nblk", [128, 32], F32)
            sc_a = alloc(st, "sc_a", [128, 32], F32)
            sc_b = alloc(st, "sc_b", [128, 32], F32)
            pstart = alloc(st, "pstart", [128, 32], F32)
            destf = alloc(st, "destf", [128, 2, nt2], F32)
            idx = alloc(st, "idx", [128, 2, nt2], mybir.dt.int32)
            bef = alloc(st, "bef", [128, NBLK_MAX], F32)
            be1 = alloc(st, "be1", [128, NBLK_MAX], F32)
            wif1 = alloc(st, "wif1", [128, NBLK_MAX, 4], F32)
            wi1 = alloc(st, "wi1", [128, NBLK_MAX, 4], mybir.dt.int32)
            w1s = [alloc(st, "w1s%d" % i, [128, KC, DEXP], BF16) for i in range(2)]
            w3s = [alloc(st, "w3s%d" % i, [128, KC, DEXP], BF16) for i in range(2)]
            w2s = [alloc(st, "w2s%d" % i, [128, 4, D], BF16) for i in range(2)]
            xsb = [alloc(st, "xsb%d" % i, [128, 4, D], BF16) for i in range(2)]
            hTb = [alloc(st, "hTb%d" % i, [128, KC, 512], BF16) for i in range(2)]
            sg = [alloc(st, "sg%d" % i, [128, 512], F32) for i in range(2)]
            actT = [alloc(st, "actT%d" % i, [128, 4, 512], BF16) for i in range(2)]
            ysb = [alloc(st, "ysb%d" % i, [128, D], F32) for i in range(2)]
            yg = ysb
            bufA = alloc(st, "bufA2", [128, D], F32)
            bufB = alloc(st, "bufB2", [128, D], F32)
            st12 = alloc(st, "st12b", [128, 12], F32)
            mv2 = alloc(st, "mv2b", [128, 2], F32)
            rstd2 = alloc(st, "rstd2b", [128, 1], F32)
            nb2 = alloc(st, "nb2b", [128, 1], F32)
            B = [None, None] + [palloc(st, "M%d" % i, [128, 512]) for i in range(2, 8)]
            TB = [palloc(st, "TB%d" % i, [128, 1024], BF16) for i in range(2)]

            P.dma("sp", wr_s[:], w_r[l].rearrange("(k p) n -> p k n", p=128), writes=["wr_s"])
            P.dma("sp", br_row[:], b_r[l:l + 1, :], writes=["br_row"])
            P.dma("sp", lng[:], ln2_g[l:l + 1, :].to_broadcast([128, D]), writes=["lng"])
            P.dma("sp", lnb[:], ln2_b[l:l + 1, :].to_broadcast([128, D]), writes=["lnb"])
            for c in range(2):
                P.dma("sp", shb[c][:], modrows[l, c, 1:2, :].to_broadcast([128, D]), writes=[("shb", c)])
                P.dma("sp", scb[c][:], modrows[l, c, 2:3, :].to_broadcast([128, D]), writes=[("scb", c)])
                P.dma("sp", g2b[c][:], modrows[l, c, 3:4, :].to_broadcast([128, D]), writes=[("g2b", c)])
            P.dma("pool", lt_b[:], lt_d[:, :], writes=["lt_b"])
            P.dma("sp", jvec[:], jvec_d[:, :], writes=["jvec"])
            P.dma("sp", base1[:], base1_d[:, :], writes=["base1"])
            P.dma("sp", base2[:], base2_d[:, :], writes=["base2"])
            P.op("pool", lambda e: e.memset(ones_b[:], 1.0), writes=["ones_b"])
            P.op("pool", lambda e: e.memset(xsb[1][:], 0.0), writes=[("xsb", 1)])
            zres = []
            for i in range(NBLK):
                P.dma("sp", xs_dram[i * BS:(i + 1) * BS, :].rearrange("(a p) d -> p a d", p=128), xsb[1][:],
                      reads=[("xsb", 1)], writes=[("xsz", i)])
                zres.append(("xsz", i))
            for c in range(2):
                P.op("dve", lambda e, c=c: e.tensor_tensor(wr_m[c][:], wr_s[:], bc_last(modT[:, 32:40, c], 36), ALU.mult),
                     reads=["wr_s", "modT"], writes=[("wr_m", c)])
                P.op("dve", lambda e, c=c: e.tensor_copy(sh2rep[c][:], bc_last(modT[:, 24:32, c], 128)),
                     reads=["modT"], writes=[("sh2rep", c)])

            st12s = [st12, alloc(st, "st12c", [128, 12], F32)]
            mv2s = [mv2, alloc(st, "mv2c", [128, 2], F32)]
            rstd2s = [rstd2, alloc(st, "rstd2c", [128, 1], F32)]
            nb2s = [nb2, alloc(st, "nb2c", [128, 1], F32)]

            def ln_tail2(xt, xname, dst, dname, bA, bB, se):
                nA, nB = ("bufA", se), ("bufB", se)
                s12, m2, r2, n2 = st12s[se], mv2s[se], rstd2s[se], nb2s[se]
                P.op("dve", lambda e: e.scalar_tensor_tensor(bA[:], xt[:], ALPHA, bA[:], ALU.mult, ALU.add),
                     reads=[xname, nA], writes=[nA])
                P.op("dve", lambda e: e.bn_stats(s12[:, 0:6], bA[:, 0:512]), reads=[nA], writes=[("st12", se, 0)])
                P.op("dve", lambda e: e.bn_stats(s12[:, 6:12], bA[:, 512:1024]), reads=[nA], writes=[("st12", se, 1)])
                P.op("dve", lambda e: e.bn_aggr(m2[:], s12[:]), reads=[("st12", se, 0), ("st12", se, 1)], writes=[("mv2", se)])
                P.op("act", lambda e: e.activation(out=r2[:], in_=m2[:, 1:2], func=AF.Sqrt, bias=LN_EPS),
                     reads=[("mv2", se)], writes=[("rstd2", se)])
                P.op("dve", lambda e: e.reciprocal(r2[:], r2[:]), reads=[("rstd2", se)], writes=[("rstd2", se)])
                P.op("dve", lambda e: e.scalar_tensor_tensor(n2[:], m2[:, 0:1], -1.0, r2[:], ALU.mult, ALU.mult),
                     reads=[("mv2", se), ("rstd2", se)], writes=[("nb2", se)])
                P.op("act", lambda e: e.activation(out=bB[:], in_=bA[:], func=AF.Identity, scale=r2[:, 0:1], bias=n2[:, 0:1]),
                     reads=[nA, ("rstd2", se), ("nb2", se)], writes=[nB])
                P.op("pool", lambda e: e.tensor_tensor(bB[:], bB[:], lng[:], ALU.mult), reads=[nB, "lng"], writes=[nB])
                P.op("pool", lambda e: e.tensor_tensor(bB[:], bB[:], lnb[:], ALU.add), reads=[nB, "lnb"], writes=[nB])
                P.dma("sp", dst, bB[:], reads=[nB], writes=[dname])

            def v1024(tn, nm):
                if nt2 * 32 >= 1024:
                    return tn[:].rearrange("p a b -> p (a b)")[:, 0:1024]
                return alloc(st, nm, [128, D], F32)[:]
            rank_v, dtmp_v, oh0_v = v1024(rank_s, "rank_v"), v1024(dtmp, "dtmp_v"), v1024(oh0, "oh0_v")
            x1Ts = [x1T, rank_v.rearrange("p (k t) -> p k t", k=KC)]
            bufAs = [bufA, dtmp_v]
            lgs = [lg, alloc(st, "lg_b", [128, 36], F32)]
            sms = [sm, alloc(st, "sm_b", [128, 16], F32)]
            gohs = [goh, alloc(st, "goh_b", [128, 4], F32)]
            gexs = [gex, alloc(st, "gex_b", [128, 4], F32)]
            esels = [esel, alloc(st, "esel_b", [128, 8], F32)]
            esel2s = [esel2, alloc(st, "esel2_b", [128, 8], F32)]
            eq1s = [eq1, alloc(st, "eq1_b", [128, 8], F32)]
            eq2s = [eq2, alloc(st, "eq2_b", [128, 8], F32)]

            def stageA(li):
                t = tiles[li]
                col = 0 if t < NB else 1
                xs = li % 2
                se = li % 2
                tpb = 6 if se == 0 else 4
                rtb = 2 if se == 0 else 3
                P.dma("sp", x1t[xs][:], x1buf[t * 128:(t + 1) * 128, :], writes=[("x1t", xs)])
                P.op("dve", lambda e, xs=xs, col=col: e.tensor_tensor(bufAs[se][:], x1t[xs][:], scb[col][:], ALU.mult),
                     reads=[("x1t", xs), ("scb", col)], writes=[("bufA", se)])
                P.op("pool", lambda e, xs=xs, col=col: e.tensor_tensor(h2r[xs][:], bufAs[se][:], shb[col][:], ALU.add),
                     reads=[("bufA", se), ("shb", col)], writes=[("h2r", xs)])
                P.dma("sp", h2buf[li * 128:(li + 1) * 128, :], h2r[xs][:], reads=[("h2r", xs)], writes=[("h2buf", li)])
                for hb in range(2):
                    def tp(e, hb=hb, xs=xs):
                        ins = None
                        for i in range(4):
                            j = hb * 4 + i
                            ins = e.transpose(B[tpb + hb][:, i * 128:(i + 1) * 128], x1t[xs][:, j * 128:(j + 1) * 128], ident_f[:])
                        return ins
                    P.op("pe", tp, reads=[("x1t", xs), "ident_f"], writes=[("B", tpb + hb)])

                    def cpx(e, hb=hb):
                        ins = None
                        for i in range(4):
                            ins = e.activation(out=x1Ts[se][:, hb * 4 + i, :], in_=B[tpb + hb][:, i * 128:(i + 1) * 128], func=AF.Copy)
                        return ins
                    P.op("act", cpx, reads=[("B", tpb + hb)], writes=[("x1T", se, hb)])

                def rt(e, col=col):
                    ins = None
                    for k in range(KC):
                        ins = e.matmul(B[rtb][:, 0:36], lhsT=x1Ts[se][:, k, :], rhs=wr_m[col][:, k, :], start=(k == 0), stop=False)
                    for k in range(KC):
                        ins = e.matmul(B[rtb][:, 0:36], lhsT=sh2rep[col][:, k, :], rhs=wr_s[:, k, :], start=False, stop=False)
                    ins = e.matmul(B[rtb][:, 0:36], lhsT=ones_f[0:1, :], rhs=br_row[0:1, :], start=False, stop=True)
                    return ins
                P.op("pe", rt, reads=[("x1T", se, 0), ("x1T", se, 1), ("wr_m", col), ("sh2rep", col), "wr_s", "br_row", "ones_f"], writes=[("B", rtb)])
                P.op("act", lambda e: e.activation(out=lgs[se][:], in_=B[rtb][:, 0:36], func=AF.Copy), reads=[("B", rtb)], writes=[("lg", se)])
                P.op("dve", lambda e: e.reduce_max(sms[se][:, 0:1], lgs[se][:, 0:4], AX), reads=[("lg", se)], writes=[("sm", se)])
                P.op("dve", lambda e: e.tensor_scalar(sms[se][:, 1:2], sms[se][:, 0:1], -1.0, None, ALU.mult), reads=[("sm", se)], writes=[("sm", se)])
                P.op("act", lambda e: e.activation(out=gexs[se][:], in_=lgs[se][:, 0:4], func=AF.Exp, bias=sms[se][:, 1:2], accum_out=sms[se][:, 2:3]),
                     reads=[("lg", se), ("sm", se)], writes=[("gex", se), ("sm", se)])
                P.op("dve", lambda e: e.reciprocal(sms[se][:, 3:4], sms[se][:, 2:3]), reads=[("sm", se)], writes=[("sm", se)])
                P.op("dve", lambda e: e.tensor_scalar(gohs[se][:], lgs[se][:, 0:4], sms[se][:, 0:1], None, ALU.is_equal), reads=[("lg", se), ("sm", se)], writes=[("goh", se)])
                P.op("dve", lambda e: e.tensor_scalar(esels[se][:], lgs[se][:, 4:12], gohs[se][:, 0:1], None, ALU.mult), reads=[("lg", se), ("goh", se)], writes=[("esel", se)])
                for g in range(1, 4):
                    P.op("dve", lambda e, g=g: e.scalar_tensor_tensor(esels[se][:], lgs[se][:, 4 + g * 8:12 + g * 8], gohs[se][:, g:g + 1], esels[se][:], ALU.mult, ALU.add),
                         reads=[("lg", se), ("goh", se), ("esel", se)], writes=[("esel", se)])
                P.op("dve", lambda e: e.reduce_max(sms[se][:, 4:5], esels[se][:], AX), reads=[("esel", se)], writes=[("sm", se)])
                P.op("dve", lambda e: e.tensor_scalar(eq1s[se][:], esels[se][:], sms[se][:, 4:5], None, ALU.is_equal), reads=[("esel", se), ("sm", se)], writes=[("eq1", se)])
                P.op("dve", lambda e: e.scalar_tensor_tensor(esel2s[se][:], eq1s[se][:], -1e30, esels[se][:], ALU.mult, ALU.add), reads=[("eq1", se), ("esel", se)], writes=[("esel2", se)])
                P.op("dve", lambda e: e.reduce_max(sms[se][:, 5:6], esel2s[se][:], AX), reads=[("esel2", se)], writes=[("sm", se)])
                P.op("dve", lambda e: e.tensor_scalar(eq2s[se][:], esel2s[se][:], sms[se][:, 5:6], None, ALU.is_equal), reads=[("esel2", se), ("sm", se)], writes=[("eq2", se)])
                P.op("dve", lambda e: e.tensor_tensor(sms[se][:, 6:7], sms[se][:, 4:5], sms[se][:, 5:6], ALU.subtract), reads=[("sm", se)], writes=[("sm", se)])
                P.op("act", lambda e: e.activation(out=sms[se][:, 7:8], in_=sms[se][:, 6:7], func=AF.Sigmoid), reads=[("sm", se)], writes=[("sm", se)])
                P.op("act", lambda e: e.activation(out=sms[se][:, 8:9], in_=sms[se][:, 6:7], func=AF.Sigmoid, scale=-1.0), reads=[("sm", se)], writes=[("sm", se)])
                P.op("dve", lambda e, li=li: e.tensor_scalar(gsc[:, li, :], sms[se][:, 7:9], sms[se][:, 3:4], None, ALU.mult), reads=[("sm", se)], writes=[("gsc", li)])
                P.op("dve", lambda e, li=li: e.tensor_tensor(oh0[:, li, :].rearrange("p (g x) -> p g x", g=4), bc_last(gohs[se][:], 8), bc_mid(eq1s[se][:], 4), ALU.mult),
                     reads=[("goh", se), ("eq1", se)], writes=[("oh0", li)])
                P.op("dve", lambda e, li=li: e.tensor_tensor(oh1[:, li, :].rearrange("p (g x) -> p g x", g=4), bc_last(gohs[se][:], 8), bc_mid(eq2s[se][:], 4), ALU.mult),
                     reads=[("goh", se), ("eq2", se)], writes=[("oh1", li)])
                P.op("pool", lambda e, li=li: e.tensor_tensor(cntb[:, li, :], oh0[:, li, :], oh1[:, li, :], ALU.add),
                     reads=[("oh0", li), ("oh1", li)], writes=[("cntb", li)])


            for li0 in range(0, nt2, 2):
                P.play(P.record(stageA, li0), P.record(stageA, li0 + 1) if li0 + 1 < nt2 else [])
            if stop == "p2a":
                P.flush()
                return nc
            call = [("cntb", i) for i in range(nt2)]
            nbank = (nt2 + 15) // 16
            for li in range(nt2):
                bk, off = 3 + li // 16, (li % 16) * 32

                def rk(e, li=li, bk=bk, off=off):
                    ins = None
                    for tp_ in range(li):
                        ins = e.matmul(B[bk][:, off:off + 32], lhsT=ones_b[:], rhs=cntb[:, tp_, :], start=(tp_ == 0), stop=False)
                    ins = e.matmul(B[bk][:, off:off + 32], lhsT=lt_b[:], rhs=cntb[:, li, :], start=(li == 0), stop=True)
                    return ins
                P.op("pe", rk, reads=call + ["ones_b", "lt_b"], writes=[("RK", li)])
            for bk in range(nbank):
                n_here = min(16, nt2 - bk * 16)
                P.op("act", lambda e, bk=bk, n_here=n_here: e.activation(
                    out=rank_s[:, bk * 16:bk * 16 + n_here, :], in_=B[3 + bk][:, 0:n_here * 32].rearrange("p (t x) -> p t x", x=32), func=AF.Copy),
                    reads=[("RK", i) for i in range(bk * 16, bk * 16 + n_here)], writes=[("rank_s", bk)])

            def tt(e):
                ins = None
                for tp_ in range(nt2):
                    ins = e.matmul(B[2][:, 64:96], lhsT=ones_b[:], rhs=cntb[:, tp_, :], start=(tp_ == 0), stop=(tp_ == nt2 - 1))
                return ins
            P.op("pe", tt, reads=call + ["ones_b"], writes=[("B", 2)])
            P.op("act", lambda e: e.activation(out=tot[:], in_=B[2][:, 64:96], func=AF.Copy), reads=[("B", 2)], writes=["tot"])
            P.op("dve", lambda e: e.tensor_scalar(nblk[:], tot[:], 0.0, None, ALU.is_gt), reads=["tot"], writes=["nblk"])
            for m in range(1, (2 * nt2 * 128) // BS + 1):
                P.op("dve", lambda e, m=m: e.scalar_tensor_tensor(nblk[:], tot[:], float(m * BS), nblk[:], ALU.is_gt, ALU.add),
                     reads=["tot", "nblk"], writes=["nblk"])
            P.op("dve", lambda e: e.tensor_copy(sc_a[:], nblk[:]), reads=["nblk"], writes=["sc_a"])
            cur, oth, cn, on = sc_a, sc_b, "sc_a", "sc_b"
            for dd in (1, 2, 4, 8, 16):
                P.op("dve", lambda e, cur=cur, oth=oth, dd=dd: e.tensor_copy(oth[:, 0:dd], cur[:, 0:dd]), reads=[cn], writes=[on])
                P.op("dve", lambda e, cur=cur, oth=oth, dd=dd: e.tensor_tensor(oth[:, dd:32], cur[:, dd:32], cur[:, 0:32 - dd], ALU.add),
                     reads=[cn, on], writes=[on])
                cur, oth, cn, on = oth, cur, on, cn
            pend, pendn = cur, cn
            P.op("dve", lambda e: e.tensor_tensor(pstart[:], pend[:], nblk[:], ALU.subtract), reads=[pendn, "nblk"], writes=["pstart"])
            P.op("dve", lambda e: e.tensor_scalar(pstart[:], pstart[:], float(BS), None, ALU.mult), reads=["pstart"], writes=["pstart"])
            rk_all = [("rank_s", i) for i in range(nbank)]
            P.op("dve", lambda e: e.tensor_tensor(rank_s[:], rank_s[:], bc_mid(pstart[:], nt2), ALU.add),
                 reads=rk_all + ["pstart"], writes=rk_all)
            for k, oh in enumerate((oh0, oh1)):
                P.op("dve", lambda e, oh=oh: e.tensor_tensor(dtmp[:], oh[:], rank_s[:], ALU.mult),
                     reads=rk_all + [("oh%d" % k, i) for i in range(nt2)], writes=["dtmp"])
                P.op("dve", lambda e, k=k: e.reduce_sum(destf[:, k, :], dtmp[:], AX), reads=["dtmp"], writes=[("destf", k)])
            P.op("dve", lambda e: e.tensor_copy(idx[:], destf[:]), reads=[("destf", 0), ("destf", 1)], writes=["idx"])
            P.op("dve", lambda e: e.tensor_scalar(bef[:], jvec[:], pend[:, 0:1], None, ALU.is_ge), reads=["jvec", pendn], writes=["bef"])
            for ee in range(1, 32):
                P.op("dve", lambda e, ee=ee: e.scalar_tensor_tensor(bef[:], jvec[:], pend[:, ee:ee + 1], bef[:], ALU.is_ge, ALU.add),
                     reads=["jvec", pendn, "bef"], writes=["bef"])
            P.op("dve", lambda e: e.tensor_scalar(bef[:], bef[:], 31.0, None, ALU.min), reads=["bef"], writes=["bef"])
            P.op("dve", lambda e: e.tensor_scalar(be1[:], bef[:], 128.0, float(l * NEXP * 128), ALU.mult, ALU.add), reads=["bef"], writes=["be1"])
            P.op("dve", lambda e: e.tensor_scalar(be1[:], be1[:], base1[:, 0:1], None, ALU.add), reads=["be1", "base1"], writes=["be1"])
            P.op("dve", lambda e: e.tensor_scalar(be1[:], be1[:], 4.0, None, ALU.mult), reads=["be1"], writes=["be1"])
            P.op("dve", lambda e: e.tensor_tensor(wif1[:], bc_last(be1[:], 4), bc_mid(base2[:], NBLK_MAX), ALU.add),
                 reads=["be1", "base2"], writes=["wif1"])
            P.op("dve", lambda e: e.tensor_copy(wi1[:], wif1[:]), reads=["wif1"], writes=["wi1"])
            if stop == "p2c":
                P.flush()
                return nc
            sres = []
            for li in range(nt2):
                hs_ = li % 2
                P.dma("sp", h2r[hs_][:], h2buf[li * 128:(li + 1) * 128, :], reads=[("h2buf", li)], writes=[("h2r", hs_)])
                for k in range(2):
                    P.idma(xs_dram[0:NBLK * BS, :], bass.IndirectOffsetOnAxis(ap=idx[:, k, li:li + 1], axis=0), h2r[hs_][:], None,
                           reads=[("h2r", hs_), "idx"] + zres, writes=[("xss", li, k)])
                    sres.append(("xss", li, k))

            if stop == "p2d":
                P.flush()
                return nc
            yres = []

            def blockA(j):
                ws = j % 2
                if not DBG.get("no_wgather"):
                    for a in range(4):
                        io = bass.IndirectOffsetOnAxis(ap=wi1[:, j, a:a + 1], axis=0)
                        P.idma(w1s[ws][:, 2 * a:2 * a + 2, :].rearrange("p k n -> p (k n)"), None, w1v, io, reads=["wi1"], writes=[("w1s", ws, a)])
                        P.idma(w3s[ws][:, 2 * a:2 * a + 2, :].rearrange("p k n -> p (k n)"), None, w3v, io, reads=["wi1"], writes=[("w3s", ws, a)])
                        P.idma(w2s[ws][:, a, :], None, w2v, io, reads=["wi1"], writes=[("w2s", ws, a)])
                P.dma("sp", xsb[ws][:], xs_dram[j * BS:(j + 1) * BS, :].rearrange("(a p) d -> p a d", p=128),
                      reads=sres + zres, writes=[("xsb", ws)])
                for a in range(4):
                    tb = a % 2

                    def tps_(e, a=a, tb=tb, ws=ws):
                        ins = None
                        for k in range(KC):
                            ins = e.transpose(TB[tb][:, k * 128:(k + 1) * 128], xsb[ws][:, a, k * 128:(k + 1) * 128], ident_b[:])
                        return ins
                    P.op("pe", tps_, reads=[("xsb", ws), "ident_b"], writes=[("TB", tb)])
                    P.op("act", lambda e, a=a, tb=tb, ws=ws: e.activation(out=hTb[ws][:, :, a * 128:(a + 1) * 128],
                                                                         in_=TB[tb][:, :].rearrange("p (k t) -> p k t", k=KC), func=AF.Copy),
                         reads=[("TB", tb)], writes=[("hTb", ws, a)])

            def blockB(j):
                ws = j % 2
                hres = [("hTb", ws, a) for a in range(4)]
                w13 = [("w1s", ws, a) for a in range(4)] + [("w3s", ws, a) for a in range(4)]
                for c in range(4):
                    pb = 2 + 2 * (c % 2)

                    def h13(e, c=c, pb=pb, ws=ws):
                        ins = None
                        for k in range(KC):
                            ins = e.matmul(B[pb][:, :], lhsT=w1s[ws][:, k, c * 128:(c + 1) * 128], rhs=hTb[ws][:, k, :],
                                           start=(k == 0), stop=(k == KC - 1))
                        for k in range(KC):
                            ins = e.matmul(B[pb + 1][:, :], lhsT=w3s[ws][:, k, c * 128:(c + 1) * 128], rhs=hTb[ws][:, k, :],
                                           start=(k == 0), stop=(k == KC - 1))
                        return ins
                    P.op("pe", h13, reads=w13 + hres, writes=[("B", pb), ("B", pb + 1)])
                    P.op("act", lambda e, c=c, pb=pb: e.activation(out=sg[c % 2][:], in_=B[pb][:, :], func=AF.Silu),
                         reads=[("B", pb)], writes=[("sg", c % 2)])
                    P.op("dve", lambda e, c=c, pb=pb, ws=ws: e.tensor_tensor(actT[ws][:, c, :], sg[c % 2][:], B[pb + 1][:, :], ALU.mult),
                         reads=[("sg", c % 2), ("B", pb + 1)], writes=[("actT", ws, c)])
                for a in range(4):
                    ys_ = a % 2
                    for hf in range(2):
                        yb = 6 + hf

                        def ymm(e, a=a, hf=hf, yb=yb, ws=ws):
                            ins = None
                            for c in range(4):
                                ins = e.matmul(B[yb][:, :], lhsT=actT[ws][:, c, a * 128:(a + 1) * 128],
                                               rhs=w2s[ws][:, c, hf * 512:(hf + 1) * 512], start=(c == 0), stop=(c == 3))
                            return ins
                        P.op("pe", ymm, reads=[("actT", ws, c) for c in range(4)] + [("w2s", ws, c) for c in range(4)], writes=[("B", yb)])
                        P.op("dve", lambda e, hf=hf, yb=yb, ys_=ys_: e.tensor_copy(ysb[ys_][:, hf * 512:(hf + 1) * 512], B[yb][:, :]),
                             reads=[("B", yb)], writes=[("ysb", ys_, hf)])
                    r0 = j * BS + a * 128
                    P.dma("sp", ys_dram[r0:r0 + 128, :], ysb[ys_][:], reads=[("ysb", ys_, 0), ("ysb", ys_, 1)], writes=[("ys", j, a)])
                    yres.append(("ys", j, a))


            P.play(P.record(blockA, 0))
            for j in range(NBLK):
                P.play(P.record(blockB, j), P.record(blockA, j + 1) if j + 1 < NBLK else [])
            if stop == "p2e":
                P.flush()
                return nc
            fA = [bufA, x1T[:].rearrange("p a b -> p (a b)")]
            fB = [bufB, dtmp_v]
            fY = [[ysb[0], ysb[1]], [rank_v, oh0_v]]

            def stageF(li):
                t = tiles[li]
                col = 0 if t < NB else 1
                xs = li % 2
                se = li % 2
                bA, bB, ygs = fA[se], fB[se], fY[se]
                nA = ("bufA", se)
                P.dma("sp", x1t[xs][:], x1buf[t * 128:(t + 1) * 128, :], writes=[("x1t", xs)])
                for k in range(2):
                    P.idma(ygs[k][:], None, ys_dram[0:NBLK * BS, :], bass.IndirectOffsetOnAxis(ap=idx[:, k, li:li + 1], axis=0),
                           reads=["idx"] + yres, writes=[("yg", se, k)])
                P.op("dve", lambda e: e.tensor_scalar(bA[:], ygs[0][:], gsc[:, li, 0:1], None, ALU.mult),
                     reads=[("yg", se, 0), ("gsc", li)], writes=[nA])
                P.op("dve", lambda e: e.scalar_tensor_tensor(bA[:], ygs[1][:], gsc[:, li, 1:2], bA[:], ALU.mult, ALU.add),
                     reads=[("yg", se, 1), ("gsc", li), nA], writes=[nA])
                P.op("pool", lambda e: e.tensor_tensor(bA[:], bA[:], g2b[col][:], ALU.mult),
                     reads=[nA, ("g2b", col)], writes=[nA])
                if last:
                    dst, dname = out_d[t * 128:(t + 1) * 128, :], ("out", t)
                else:
                    dst, dname = x2buf[t * 128:(t + 1) * 128, :], ("x2buf", t)
                ln_tail2(x1t[xs], ("x1t", xs), dst, dname, bA, bB, se)

            for li0 in range(0, nt2, 2):
                P.play(P.record(stageF, li0), P.record(stageF, li0 + 1) if li0 + 1 < nt2 else [])
            P.flush()
    glob.close()
    return nc


def _rope_tables(NB):
    NT = NB + NCB
    S = NB * 128
    m = 16
    freqs = (10000.0 ** (-np.arange(m, dtype=np.float32) / m)).astype(np.float32)
    tpos = np.arange(S)
    row = (tpos // GRID_W).astype(np.float32)
    colp = (tpos % GRID_W).astype(np.float32)
    cos = np.ones((128, NT * 128), np.float32)
    sin = np.zeros((128, NT * 128), np.float32)
    for p in range(128):
        d = p % 64
        pos = row if d < 32 else colp
        dd = d % 32
        f = freqs[dd % 16]
        ang = (pos * f).astype(np.float32)
        cos[p, :S] = np.cos(ang)
        sgn = -1.0 if dd < 16 else 1.0
        sin[p, :S] = sgn * np.sin(ang)
    cos_t = np.ascontiguousarray(cos.reshape(128, NT, 128).transpose(1, 0, 2))
    sin_t = np.ascontiguousarray(sin.reshape(128, NT, 128).transpose(1, 0, 2))
    return cos_t, sin_t


def make_in_maps(inputs, NB, ncores):
    f = lambda a: np.ascontiguousarray(np.asarray(a, dtype=np.float32))
    x = f(inputs["x"]); c = f(inputs["c"]); ctx = f(inputs["ctx"]); c_ctx = f(inputs["c_ctx"])
    depth = inputs["w_ada"].shape[0]
    cos_t, sin_t = _rope_tables(NB)
    j = np.arange(128)
    maskl = np.where(j[:, None] >= j[None, :], 0.0, NEG).astype(np.float32)
    maskr = np.where(j[:, None] <= j[None, :], 0.0, NEG).astype(np.float32)
    shared = {
        "w_ada": f(inputs["w_ada"]),
        "bada_p": f(np.asarray(inputs["b_ada"]).reshape(depth, 48, 128).transpose(0, 2, 1)),
        "b_ada": f(inputs["b_ada"]),
        "w_in": f(inputs["w_in"]),
        "convw": f(np.asarray(inputs["conv_w"]).reshape(depth, 3, 2, 128).transpose(0, 3, 2, 1)),
        "sink": f(inputs["attn_sink"]),
        "gm_wsT": f(np.asarray(inputs["gm_ws"]).transpose(0, 1, 3, 2)),
        "gm_bsp": f(np.asarray(inputs["gm_bs"]).transpose(0, 2, 1)),
        "w_out": f(inputs["w_out"]),
        "ln1_g": f(inputs["ln1_g"]), "ln1_b": f(inputs["ln1_b"]),
        "ln2_g": f(inputs["ln2_g"]), "ln2_b": f(inputs["ln2_b"]),
        "w_r": f(np.concatenate([np.asarray(inputs["w_rg"]), np.asarray(inputs["w_re"])], axis=-1)),
        "b_r": f(np.concatenate([np.asarray(inputs["b_rg"]), np.asarray(inputs["b_re"])], axis=-1)),
        "w1": f(np.asarray(inputs["w1"]).reshape(depth, NEXP, KC, 128, DEXP).transpose(0, 1, 3, 2, 4)),
        "w3": f(np.asarray(inputs["w3"]).reshape(depth, NEXP, KC, 128, DEXP).transpose(0, 1, 3, 2, 4)),
        "w2": f(np.asarray(inputs["w2"]).reshape(depth, NEXP, 4, 128, D).transpose(0, 1, 3, 2, 4)),
        "cos_t": cos_t, "sin_t": sin_t,
        "ident": np.eye(128, dtype=np.float32), "maskl": maskl, "maskr": maskr,
        "lt_d": (j[:, None] < j[None, :]).astype(np.float32),
        "jvec_d": np.tile(np.arange((2 * (NB + NCB) * 128) // 512 + NEXP, dtype=np.float32)[None, :], (128, 1)),
        "base1_d": (np.arange(8, dtype=np.float32)[None, :] * 128 + j[:, None]).astype(np.float32),
        "base2_d": np.tile(np.arange(4, dtype=np.float32)[None, :], (128, 1)),
    }
    maps = []
    for b in range(ncores):
        cv = np.stack([c[b].reshape(KC, 128).T, c_ctx.reshape(KC, 128).T], axis=-1)
        m = dict(shared)
        m["xin"] = np.ascontiguousarray(x[b])
        m["ctxin"] = np.ascontiguousarray(ctx[b])
        m["cvec"] = np.ascontiguousarray(cv.astype(np.float32))
        maps.append(m)
    return maps


_NC_CACHE = {}


def kernel(**inputs):
    x = np.asarray(inputs["x"])
    Bn, S, _ = x.shape
    NB = S // 128
    key = (NB,)
    if key not in _NC_CACHE:
        _NC_CACHE[key] = build_program(NB)
    nc = _NC_CACHE[key]
    maps = make_in_maps(inputs, NB, Bn)
    res = run_bass_kernel_spmd(nc, maps, core_ids=list(range(Bn)))
    return np.stack([np.asarray(r["out"], dtype=np.float32) for r in res.results], axis=0)
```

```python
from contextlib import ExitStack
import numpy as np
import ml_dtypes
import concourse.bass as bass
import concourse.mybir as mybir
from concourse.bass_utils import run_bass_kernel_spmd

F32 = mybir.dt.float32
BF16 = mybir.dt.bfloat16
AF = mybir.ActivationFunctionType
ALU = mybir.AluOpType

D = 1024
KC = 8
CTX = 256
NCB = CTX // 128
DEPTH = 2
NEXP = 32
DEXP = 512
ALPHA = (2 * DEPTH) ** 0.25
LN_EPS = 1e-6
GRID_W = 64
NEG = -30000.0
DBG = {}


class Prog:
    NDMA = 8

    def __init__(self, nc):
        self.nc = nc
        self.names = dict(pe="tensor", act="scalar", dve="vector", pool="gpsimd", sp="sync")
        self.sems = []
        self.semidx = {}
        for k in self.names:
            self.semidx[k] = len(self.sems)
            self.sems.append(nc.alloc_semaphore("s_" + k))
        self.cnt = {k: 0 for k in self.names}
        self.dq = {}
        for q in ("sp", "pool"):
            idx = []
            for i in range(self.NDMA):
                idx.append(len(self.sems))
                self.sems.append(nc.alloc_semaphore("d_%s%d" % (q, i)))
            self.dq[q] = dict(idx=idx, cnt=[0] * self.NDMA, nxt=0)
        self.seen = {k: {} for k in self.names}
        self.stream = {k: [] for k in self.names}
        self.res = {}
        self._cap = None

    def record(self, fn, *a, **kw):
        self._cap = []
        fn(*a, **kw)
        cap, self._cap = self._cap, None
        return cap

    def play(self, *lists):
        lists = [l for l in lists if l]
        pos = [0] * len(lists)
        while True:
            best, bf = None, None
            for i, l in enumerate(lists):
                if pos[i] < len(l):
                    f = pos[i] / len(l)
                    if bf is None or f < bf:
                        best, bf = i, f
            if best is None:
                break
            kind, a, kw = lists[best][pos[best]]
            pos[best] += 1
            getattr(self, kind)(*a, **kw)

    def _deps(self, reads, writes):
        deps = []
        for r in reads:
            e = self.res.get(r)
            if e and e["w"] is not None:
                deps.append(e["w"])
        for w in writes:
            e = self.res.get(w)
            if e:
                if e["w"] is not None:
                    deps.append(e["w"])
                deps.extend((si, v, src) for (si, src), v in e["r"].items())
        return deps

    def _waits(self, ek, deps):
        waits = {}
        for (si, val, src) in deps:
            if src == "pe" and ek == "pe":
                continue
            if self.seen[ek].get(si, 0) >= val:
                continue
            waits[si] = max(waits.get(si, 0), val)
        for si, v in waits.items():
            self.seen[ek][si] = v
        return list(waits.items())

    def _record(self, token, reads, writes):
        si, val, src = token
        for r in reads:
            e = self.res.setdefault(r, {"w": None, "r": {}})
            e["r"][(si, src)] = max(e["r"].get((si, src), 0), val)
        for w in writes:
            self.res[w] = {"w": token, "r": {}}

    def op(self, ek, fn, reads=(), writes=()):
        if self._cap is not None:
            self._cap.append(("op", (ek, fn), dict(reads=reads, writes=writes)))
            return None
        waits = self._waits(ek, self._deps(reads, writes))
        self.cnt[ek] += 1
        si = self.semidx[ek]
        token = (si, self.cnt[ek], ek)
        sems = self.sems

        def emit(eng):
            for (wsi, v) in waits:
                eng.wait_ge(sems[wsi], v)
            ins = fn(eng)
            ins.then_inc(sems[si], 1)

        self.stream[ek].append(emit)
        self._record(token, reads, writes)
        return token

    def dma(self, q, out, in_, reads=(), writes=()):
        if self._cap is not None:
            self._cap.append(("dma", (q, out, in_), dict(reads=reads, writes=writes)))
            return None
        d = self.dq[q]
        slot = d["nxt"]
        d["nxt"] = (slot + 1) % self.NDMA
        si = d["idx"][slot]
        deps = self._deps(reads, writes)
        if d["cnt"][slot] > 0:
            deps.append((si, 16 * d["cnt"][slot], "dma"))
        waits = self._waits(q, deps)
        d["cnt"][slot] += 1
        token = (si, 16 * d["cnt"][slot], "dma")
        sems = self.sems

        def emit(eng):
            for (wsi, v) in waits:
                eng.wait_ge(sems[wsi], v)
            eng.dma_start(out=out, in_=in_).then_inc(sems[si], 16)

        self.stream[q].append(emit)
        self._record(token, reads, writes)
        return token

    def idma(self, out, out_off, in_, in_off, reads=(), writes=()):
        if self._cap is not None:
            self._cap.append(("idma", (out, out_off, in_, in_off), dict(reads=reads, writes=writes)))
            return None
        q = "pool"
        d = self.dq[q]
        slot = d["nxt"]
        d["nxt"] = (slot + 1) % self.NDMA
        si = d["idx"][slot]
        deps = self._deps(reads, writes)
        if d["cnt"][slot] > 0:
            deps.append((si, 16 * d["cnt"][slot], "dma"))
        waits = self._waits(q, deps)
        d["cnt"][slot] += 1
        token = (si, 16 * d["cnt"][slot], "dma")
        sems = self.sems

        def emit(eng):
            for (wsi, v) in waits:
                eng.wait_ge(sems[wsi], v)
            eng.indirect_dma_start(out=out, out_offset=out_off, in_=in_, in_offset=in_off).then_inc(sems[si], 16)

        self.stream[q].append(emit)
        self._record(token, reads, writes)
        return token

    def flush(self):
        finals = []
        for k in self.names:
            if self.cnt[k] > 0:
                finals.append((self.semidx[k], self.cnt[k]))
        for q, d in self.dq.items():
            for i, si in enumerate(d["idx"]):
                if d["cnt"][i] > 0:
                    finals.append((si, 16 * d["cnt"][i]))
        with self.nc.Block() as block:
            for ek, nm in self.names.items():
                stream = self.stream[ek]
                seen = self.seen[ek]
                sems = self.sems

                def body(eng, stream=stream, seen=seen):
                    for emit in stream:
                        emit(eng)
                    for si, v in finals:
                        if seen.get(si, 0) < v:
                            eng.wait_ge(sems[si], v)
                            seen[si] = v

                getattr(block, nm)(body)
        self.stream = {k: [] for k in self.names}
        self.res = {}


def bc_mid(ap, n):
    p, f = ap.shape
    return ap.unsqueeze(1).to_broadcast([p, n, f])


def bc_last(ap, n):
    p, g = ap.shape
    return ap.unsqueeze(2).to_broadcast([p, g, n])


def build_program(NB, depth=DEPTH, tps=9, stop=None, nexp=NEXP, do_router=True, do_epi=True):
    S = NB * 128
    NT = NB + NCB
    T = NT * 128
    nc = bass.Bass("TRN2", target_bir_lowering=False)

    def din(name, shape, dt=F32):
        return nc.dram_tensor(name, list(shape), dt, kind="ExternalInput").ap()

    xin = din("xin", [S, D])
    ctxin = din("ctxin", [CTX, D])
    cvec = din("cvec", [128, KC, 2])
    w_ada = din("w_ada", [depth, D, 6 * D])
    bada_p = din("bada_p", [depth, 128, 48])
    b_ada = din("b_ada", [depth, 6 * D])
    w_in = din("w_in", [depth, D, 2048])
    convw = din("convw", [depth, 128, 2, 3])
    sink = din("sink", [depth, 8])
    gm_wsT = din("gm_wsT", [depth, 4, 128, 128])
    gm_bsp = din("gm_bsp", [depth, 128, 4])
    w_out = din("w_out", [depth, D, D])
    ln1_g = din("ln1_g", [depth, D])
    ln1_b = din("ln1_b", [depth, D])
    ln2_g = din("ln2_g", [depth, D])
    ln2_b = din("ln2_b", [depth, D])
    w_r = din("w_r", [depth, D, 36])
    b_r = din("b_r", [depth, 36])
    w1 = din("w1", [depth, NEXP, 128, KC, DEXP])
    w3 = din("w3", [depth, NEXP, 128, KC, DEXP])
    w2 = din("w2", [depth, NEXP, 128, 4, D])
    cos_t = din("cos_t", [NT, 128, 128])
    sin_t = din("sin_t", [NT, 128, 128])
    ident_d = din("ident", [128, 128])
    maskl_d = din("maskl", [128, 128])
    maskr_d = din("maskr", [128, 128])
    out_d = nc.dram_tensor("out", [S, D], F32, kind="ExternalOutput").ap()
    x1buf = nc.dram_tensor("x1buf", [T, D], F32).ap()
    x2buf = nc.dram_tensor("x2buf", [T, D], F32).ap()
    BS = 512
    NBLK_MAX = (2 * T) // BS + NEXP
    h2buf = nc.dram_tensor("h2buf", [T, D], BF16).ap()
    xs_dram = nc.dram_tensor("xs_dram", [NBLK_MAX * BS, D], BF16).ap()
    ys_dram = nc.dram_tensor("ys_dram", [NBLK_MAX * BS, D], F32).ap()
    modrows = nc.dram_tensor("modrows", [depth, 2, 4, D], F32).ap()
    lt_d = din("lt_d", [128, 128])
    jvec_d = din("jvec_d", [128, NBLK_MAX])
    base1_d = din("base1_d", [128, 8])
    base2_d = din("base2_d", [128, 4])

    P = Prog(nc)

    def tile_src(l, t):
        if l == 0:
            if t < NB:
                return xin[t * 128:(t + 1) * 128, :]
            return ctxin[(t - NB) * 128:(t - NB + 1) * 128, :]
        return x2buf[t * 128:(t + 1) * 128, :]

    glob = ExitStack()

    uid = [0]

    def alloc(st, name, shape, dt):
        uid[0] += 1
        return st.enter_context(nc.sbuf_tensor("sb%d_%s" % (uid[0], name), list(shape), dt))

    def palloc(st, name, shape, dt=F32):
        uid[0] += 1
        return st.enter_context(nc.psum_tensor("ps%d_%s" % (uid[0], name), list(shape), dt))

    ident_f = alloc(glob, "ident_f", [128, 128], F32)
    ident_b = alloc(glob, "ident_b", [128, 128], BF16)
    maskl = alloc(glob, "maskl", [128, 128], BF16)
    maskr = alloc(glob, "maskr", [128, 128], BF16)
    cact = alloc(glob, "cact", [128, KC, 2], F32)
    cact_rep = alloc(glob, "cact_rep", [128, KC, 2, 128], F32)
    ones_f = alloc(glob, "ones_f", [1, 128], F32)
    epsb = alloc(glob, "epsb", [128, 1], F32)
    modT = alloc(glob, "modT", [128, 48, 2], F32)

    P.dma("sp", ident_f[:], ident_d[:, :], writes=["ident_f"])
    P.dma("pool", ident_b[:], ident_d[:, :], writes=["ident_b"])
    P.dma("pool", maskl[:], maskl_d[:, :], writes=["maskl"])
    P.dma("pool", maskr[:], maskr_d[:, :], writes=["maskr"])
    P.dma("sp", cact[:], cvec[:, :, :], writes=["cact"])
    P.op("act", lambda e: e.activation(out=cact[:], in_=cact[:], func=AF.Silu), reads=["cact"], writes=["cact"])
    P.op("dve", lambda e: e.tensor_copy(cact_rep[:].rearrange("p k c m -> p (k c) m"),
                                        bc_last(cact[:].rearrange("p k c -> p (k c)"), 128)),
         reads=["cact"], writes=["cact_rep"])
    P.op("dve", lambda e: e.memset(ones_f[:], 1.0), writes=["ones_f"])
    P.op("dve", lambda e: e.memset(epsb[:], LN_EPS), writes=["epsb"])
    P.flush()
    if stop == "const":
        return nc

    for l in range(depth):
        last = (l == depth - 1)
        with ExitStack() as st:
            wa = [alloc(st, "wa%d" % i, [128, KC, D], F32) for i in range(2)]
            bada = alloc(st, "bada", [128, 48], F32)
            brow = [alloc(st, "brow%d" % i, [128, D], F32) for i in range(2)]
            grow = [alloc(st, "grow%d" % i, [128, D], F32) for i in range(2)]
            ps_mod = palloc(st, "ps_mod", [128, 96])
            ps_g = [palloc(st, "ps_g%d" % i, [128, 512]) for i in range(4)]
            P.dma("sp", bada[:], bada_p[l], writes=["bada"])
            for i in range(6):
                s = i % 2
                P.dma("sp", wa[s][:], w_ada[l][:, i * D:(i + 1) * D].rearrange("(k p) n -> p k n", p=128),
                      writes=[("wa", s)])

                def mm_mod(e, i=i, s=s):
                    ins = None
                    for j in range(KC):
                        for k in range(KC):
                            ins = e.matmul(ps_mod[:, (i * 8 + j) * 2:(i * 8 + j) * 2 + 2],
                                           lhsT=wa[s][:, k, j * 128:(j + 1) * 128], rhs=cact[:, k, :],
                                           start=(k == 0), stop=(k == KC - 1))
                    return ins
                P.op("pe", mm_mod, reads=[("wa", s), "cact"], writes=[("ps_mod", i)])
                if i >= 2:
                    gi = i - 2
                    P.dma("sp", brow[gi % 2][:], b_ada[l:l + 1, i * D:(i + 1) * D].to_broadcast([128, D]),
                          writes=[("brow", gi % 2)])
                    for c in range(2):
                        for h in range(2):
                            def mm_g(e, s=s, c=c, h=h):
                                ins = None
                                for k in range(KC):
                                    ins = e.matmul(ps_g[c * 2 + h][:, :], lhsT=cact_rep[:, k, c, :],
                                                   rhs=wa[s][:, k, h * 512:(h + 1) * 512],
                                                   start=(k == 0), stop=(k == KC - 1))
                                return ins
                            P.op("pe", mm_g, reads=[("wa", s), "cact_rep"], writes=[("ps_g", c * 2 + h)])
                            P.op("dve", lambda e, gi=gi, c=c, h=h: e.tensor_tensor(
                                grow[c][:, h * 512:(h + 1) * 512], ps_g[c * 2 + h][:, :],
                                brow[gi % 2][:, h * 512:(h + 1) * 512], ALU.add),
                                reads=[("ps_g", c * 2 + h), ("brow", gi % 2)], writes=[("grow", c, h)])
                        if i == 4:
                            P.op("dve", lambda e, c=c: e.tensor_scalar(grow[c][0:1, :], grow[c][0:1, :], 1.0, None, ALU.add),
                                 reads=[("grow", c, 0), ("grow", c, 1)], writes=[("grow", c, 0), ("grow", c, 1)])
                        P.dma("sp", modrows[l, c, gi:gi + 1, :], grow[c][0:1, :],
                              reads=[("grow", c, 0), ("grow", c, 1)], writes=[("modrows", c, gi)])
            P.op("dve", lambda e: e.tensor_tensor(modT[:], ps_mod[:].rearrange("p (j c) -> p j c", c=2),
                                                  bc_last(bada[:], 2), ALU.add),
                 reads=[("ps_mod", i) for i in range(6)] + ["bada"], writes=["modT"])
            P.op("dve", lambda e: e.tensor_scalar(modT[:, 8:16, :], modT[:, 8:16, :], 1.0, None, ALU.add),
                 reads=["modT"], writes=["modT"])
            P.op("dve", lambda e: e.tensor_scalar(modT[:, 32:40, :], modT[:, 32:40, :], 1.0, None, ALU.add),
                 reads=["modT"], writes=["modT"])
            P.flush()
        if stop == "p0":
            return nc

        with ExitStack() as st:
            NCOL = 18 * 128 + 640
            win = alloc(st, "win", [128, KC, NCOL], BF16)
            wout = alloc(st, "wout", [128, KC, D], BF16)
            kT = alloc(st, "kT", [128, 2, T], BF16)
            V = alloc(st, "V", [128, NT, 2, 65], BF16)
            convw_s = alloc(st, "convw_s", [128, 2, 3], F32)
            wsT = alloc(st, "wsT", [128, 4, 128], BF16)
            gmb = alloc(st, "gmb", [128, 4], F32)
            esink = alloc(st, "esink", [128, 8], F32)
            lng = alloc(st, "lng", [128, D], F32)
            lnb = alloc(st, "lnb", [128, D], F32)
            g1b = [alloc(st, "g1b%d" % c, [128, D], F32) for c in range(2)]
            for c in range(2):
                P.dma("sp", g1b[c][:], modrows[l, c, 0:1, :].to_broadcast([128, D]), writes=[("g1b", c)])
            R = 5
            xres = [alloc(st, "xres%d" % i, [128, D], F32) for i in range(5)]
            hT = [alloc(st, "hT%d" % i, [128, KC, 128], BF16) for i in range(2)]
            qrot = [alloc(st, "qrot%d" % i, [128, 4, 128], BF16) for i in range(R)]
            u = [alloc(st, "u%d" % i, [128, 2, 130], F32) for i in range(R)]
            cb = [alloc(st, "cb%d" % i, [128, 2, 128], F32) for i in range(R)]
            gut = [alloc(st, "gut%d" % i, [128, 256], F32) for i in range(R)]
            vln = [alloc(st, "vln%d" % i, [128, 256], BF16) for i in range(R)]
            cosb = [alloc(st, "cosb%d" % i, [128, 128], F32) for i in range(2)]
            sinb = [alloc(st, "sinb%d" % i, [128, 128], F32) for i in range(2)]
            t1 = alloc(st, "t1", [128, 4, 128], F32)
            t2 = alloc(st, "t2", [128, 4, 128], F32)
            cxs = alloc(st, "cxs", [128, 2, 128], F32)
            gvt = alloc(st, "gvt", [128, 256], F32)
            st6 = alloc(st, "st6", [128, 6], F32)
            mv = alloc(st, "mv", [128, 2], F32)
            rstd = alloc(st, "rstd", [128, 1], F32)
            nb_ = alloc(st, "nb_", [128, 1], F32)
            PT = [alloc(st, "PT%d" % i, [128, 5, 128], BF16) for i in range(4)]
            den = alloc(st, "den", [128, 4], F32)
            mixtok = [alloc(st, "mixtok%d" % i, [128, 768], BF16) for i in range(2)]
            mixT = [alloc(st, "mixT%d" % i, [128, KC, 128], BF16) for i in range(2)]
            ct = alloc(st, "ct", [128, 2, 128], F32)
            gtmp = alloc(st, "gtmp", [128, 256], F32)
            bufAs = [alloc(st, "bufA%d" % i, [128, D], F32) for i in range(2)]
            bufB = alloc(st, "bufB", [128, D], F32)
            st12 = alloc(st, "st12", [128, 12], F32)
            mv2 = alloc(st, "mv2", [128, 2], F32)
            rstd2 = alloc(st, "rstd2", [128, 1], F32)
            nb2 = alloc(st, "nb2", [128, 1], F32)
            B = [palloc(st, "B%d" % i, [128, 512]) for i in range(7)]
            Bbf = palloc(st, "Bbf", [128, 1024], BF16)
            Bo = B[4]

            wv = w_in[l].rearrange("(k p) n -> p k n", p=128)
            P.dma("pool", win[:, :, 0:512], wv[:, :, 0:512], writes=["win"])
            for kv in range(2):
                for dup in range(2):
                    c0 = 1024 + kv * 128 + dup * 64
                    P.dma("pool", win[:, :, c0:c0 + 64], wv[:, :, 512 + kv * 64:512 + (kv + 1) * 64], writes=["win"])
            P.dma("pool", win[:, :, 1536:2304], wv[:, :, 768:1536], writes=["win"])
            P.dma("pool", win[:, :, 2304:2432], wv[:, :, 640:768], writes=["win"])
            P.dma("pool", win[:, :, 2432:2688], wv[:, :, 1792:2048], writes=["win"])
            P.dma("pool", win[:, :, 2688:2944], wv[:, :, 1536:1792], writes=["win"])
            P.dma("pool", wout[:], w_out[l].rearrange("(k p) n -> p k n", p=128), writes=["wout"])
            for (src0, dst0, nblk) in ((0, 512, 16), (1024, 1280, 8)):
                for a in range(2):
                    def swp(e, src0=src0, dst0=dst0, nblk=nblk, a=a):
                        srcv = win[:, :, src0:src0 + nblk * 32].rearrange("p k (b a f) -> p k b a f", a=2, f=16)
                        dstv = win[:, :, dst0:dst0 + nblk * 32].rearrange("p k (b a f) -> p k b a f", a=2, f=16)
                        return e.tensor_copy(dstv[:, :, :, a, :], srcv[:, :, :, 1 - a, :])
                    P.op("dve", swp, reads=["win"], writes=["win"])
            P.dma("sp", convw_s[:], convw[l], writes=["convw_s"])
            P.dma("pool", wsT[:], gm_wsT[l].rearrange("g q p -> q g p"), writes=["wsT"])
            P.dma("sp", gmb[:], gm_bsp[l], writes=["gmb"])
            P.dma("sp", esink[:], sink[l:l + 1, :].to_broadcast([128, 8]), writes=["esink"])
            P.op("act", lambda e: e.activation(out=esink[:], in_=esink[:], func=AF.Exp), reads=["esink"], writes=["esink"])
            P.dma("sp", lng[:], ln1_g[l:l + 1, :].to_broadcast([128, D]), writes=["lng"])
            P.dma("sp", lnb[:], ln1_b[l:l + 1, :].to_broadcast([128, D]), writes=["lnb"])
            P.op("pool", lambda e: e.memset(V[:, :, :, 64:65], 1.0), writes=["Vones"])

            def seq_of(t):
                return (0, NB) if t < NB else (NB, NT)

            def inproj(t, kv_only=False):
                s = t % R
                xs = t % 5
                hs = t % 2
                cs = t % 2
                col = 0 if t < NB else 1
                lo, hi = seq_of(t)
                P.dma("sp", xres[xs][:], tile_src(l, t), writes=[("xres", xs)])
                P.dma("sp", cosb[cs][:], cos_t[t], writes=[("cosb", cs)])
                P.dma("sp", sinb[cs][:], sin_t[t], writes=[("sinb", cs)])
                for hb in range(2):
                    def tp(e, hb=hb):
                        ins = None
                        for i in range(4):
                            j = hb * 4 + i
                            ins = e.transpose(B[2 + hb][:, i * 128:(i + 1) * 128], xres[xs][:, j * 128:(j + 1) * 128], ident_f[:])
                        return ins
                    P.op("pe", tp, reads=[("xres", xs), "ident_f"], writes=[("B", 2 + hb)])

                    def ev(e, hb=hb):
                        ins = None
                        for i in range(4):
                            j = hb * 4 + i
                            ins = e.activation(out=hT[hs][:, j, :], in_=B[2 + hb][:, i * 128:(i + 1) * 128], func=AF.Identity,
                                               scale=modT[:, 8 + j, col:col + 1], bias=modT[:, j, col:col + 1])
                        return ins
                    P.op("act", ev, reads=[("B", 2 + hb), "modT"], writes=[("hT", hs, hb)])
                hres = [("hT", hs, 0), ("hT", hs, 1)]

                def fm_group(bank, off, chunks):
                    def f(e):
                        ins = None
                        for i, c in enumerate(chunks):
                            for k in range(KC):
                                ins = e.matmul(B[bank][:, off + i * 128:off + (i + 1) * 128],
                                               lhsT=win[:, k, c * 128:(c + 1) * 128], rhs=hT[hs][:, k, :],
                                               start=(k == 0), stop=(k == KC - 1))
                        return ins
                    return f

                if not kv_only:
                    P.op("pe", fm_group(2, 0, [0, 1, 2, 3]), reads=hres + ["win"], writes=[("B", 2)])
                    P.op("pe", fm_group(3, 0, [4, 5, 6, 7]), reads=hres + ["win"], writes=[("B", 3)])
                    P.op("dve", lambda e: e.tensor_tensor(t1[:], B[2][:, :].rearrange("p (c t) -> p c t", c=4),
                                                          bc_mid(cosb[cs][:], 4), ALU.mult),
                         reads=[("B", 2), ("cosb", cs)], writes=["t1"])
                    P.op("dve", lambda e: e.tensor_tensor(t2[:], B[3][:, :].rearrange("p (c t) -> p c t", c=4),
                                                          bc_mid(sinb[cs][:], 4), ALU.mult),
                         reads=[("B", 3), ("sinb", cs)], writes=["t2"])
                    P.op("pool", lambda e: e.tensor_tensor(qrot[s][:], t1[:], t2[:], ALU.add),
                         reads=["t1", "t2"], writes=[("qrot", s)])
                P.op("pe", fm_group(2, 0, [8, 9, 10, 11]), reads=hres + ["win"], writes=[("B", 2)])
                P.op("dve", lambda e: e.tensor_tensor(t1[:, 0:2, :], B[2][:, 0:256].rearrange("p (c t) -> p c t", c=2),
                                                      bc_mid(cosb[cs][:], 2), ALU.mult),
                     reads=[("B", 2), ("cosb", cs)], writes=["t1"])
                P.op("dve", lambda e: e.tensor_tensor(t2[:, 0:2, :], B[2][:, 256:512].rearrange("p (c t) -> p c t", c=2),
                                                      bc_mid(sinb[cs][:], 2), ALU.mult),
                     reads=[("B", 2), ("sinb", cs)], writes=["t2"])
                P.op("pool", lambda e: e.tensor_tensor(kT[:, :, t * 128:(t + 1) * 128], t1[:, 0:2, :], t2[:, 0:2, :], ALU.add),
                     reads=["t1", "t2"], writes=[("kT", t)])
                if not kv_only:
                    P.op("pe", fm_group(3, 0, [12, 13, 14, 15]), reads=hres + ["win"], writes=[("B", 3)])
                    P.op("pe", fm_group(2, 0, [16, 17]), reads=hres + ["win"], writes=[("B", 2)])
                    P.op("act", lambda e: e.activation(out=cb[s][:], in_=B[3][:, 0:256].rearrange("p (c t) -> p c t", c=2),
                                                       func=AF.Copy), reads=[("B", 3)], writes=[("cb", s)])
                    P.op("act", lambda e: e.activation(out=cxs[:], in_=B[2][:, 0:256].rearrange("p (c t) -> p c t", c=2),
                                                       func=AF.Copy), reads=[("B", 2)], writes=["cxs"])
                    P.op("dve", lambda e: e.tensor_tensor(u[s][:, :, 1:129], B[3][:, 256:512].rearrange("p (c t) -> p c t", c=2),
                                                          cxs[:], ALU.mult),
                         reads=[("B", 3), "cxs"], writes=[("u", s)])
                    if t > lo:
                        sp_ = (t - 1) % R
                        P.op("pool", lambda e: e.tensor_copy(u[sp_][:, :, 129:130], u[s][:, :, 1:2]),
                             reads=[("u", s)], writes=[("u", sp_)])
                    else:
                        P.op("pool", lambda e: e.memset(u[s][:, :, 0:1], 0.0), reads=[("u", s)], writes=[("u", s)])
                    if t < hi - 1:
                        sn_ = (t + 1) % R
                        P.op("pool", lambda e: e.tensor_copy(u[sn_][:, :, 0:1], u[s][:, :, 128:129]),
                             reads=[("u", s)], writes=[("u", sn_)])
                    else:
                        P.op("pool", lambda e: e.memset(u[s][:, :, 129:130], 0.0), reads=[("u", s)], writes=[("u", s)])

                def tm(e):
                    ins = None
                    for k in range(KC):
                        ins = e.matmul(B[3][:, 0:384], lhsT=hT[hs][:, k, :], rhs=win[:, k, 2304:2688],
                                       start=(k == 0), stop=(k == KC - 1))
                    return ins
                P.op("pe", tm, reads=hres + ["win"], writes=[("B", 3)])
                P.op("act", lambda e: e.activation(out=V[:, t, :, 0:64], in_=B[3][:, 0:128].rearrange("p (h d) -> p h d", h=2),
                                                   func=AF.Copy), reads=[("B", 3)], writes=[("V", t)])
                if not kv_only:
                    def tm2(e):
                        ins = None
                        for k in range(KC):
                            ins = e.matmul(B[2][:, 256:512], lhsT=hT[hs][:, k, :], rhs=win[:, k, 2688:2944],
                                           start=(k == 0), stop=(k == KC - 1))
                        return ins
                    P.op("pe", tm2, reads=hres + ["win"], writes=[("B", 2)])
                    P.op("act", lambda e: e.activation(out=gvt[:], in_=B[3][:, 128:384], func=AF.Gelu_apprx_tanh),
                         reads=[("B", 3)], writes=["gvt"])
                    P.op("act", lambda e: e.activation(out=gut[s][:], in_=B[2][:, 256:512], func=AF.Gelu_apprx_tanh),
                         reads=[("B", 2)], writes=[("gut", s)])
                    P.op("dve", lambda e: e.bn_stats(st6[:], gvt[:]), reads=["gvt"], writes=["st6"])
                    P.op("dve", lambda e: e.bn_aggr(mv[:], st6[:]), reads=["st6"], writes=["mv"])
                    P.op("act", lambda e: e.activation(out=rstd[:], in_=mv[:, 1:2], func=AF.Ln, bias=epsb[:, 0:1]),
                         reads=["mv", "epsb"], writes=["rstd"])
                    P.op("act", lambda e: e.activation(out=rstd[:], in_=rstd[:], func=AF.Exp, scale=-0.5), reads=["rstd"], writes=["rstd"])
                    P.op("dve", lambda e: e.scalar_tensor_tensor(nb_[:], mv[:, 0:1], -1.0, rstd[:], ALU.mult, ALU.mult),
                         reads=["mv", "rstd"], writes=["nb_"])
                    P.op("act", lambda e: e.activation(out=vln[s][:], in_=gvt[:], func=AF.Identity,
                                                       scale=rstd[:, 0:1], bias=nb_[:, 0:1]),
                         reads=["gvt", "rstd", "nb_"], writes=[("vln", s)])

            def attn(t):
                s = t % R
                xs = t % 5
                ms = t % 2
                col = 0 if t < NB else 1
                lo, hi = seq_of(t)
                if t < NB:
                    kcs = [(NB, None), (NB + 1, None)]
                    if t > 0:
                        kcs.append((t - 1, maskl))
                    kcs.append((t, None))
                    if t < NB - 1:
                        kcs.append((t + 1, maskr))
                else:
                    kcs = [(NB, None), (NB + 1, None)]
                nk = len(kcs)
                for h in range(8):
                    qc, po, kv, pslot = h // 2, (h % 2) * 64, h // 4, h % 4

                    if h % 2 == 0:
                        sbank, s5, wr5 = B[6], B[4][:, 384:512], ("B", 4)
                        wr = [("B", 6)] + ([wr5] if nk > 4 else [])
                    else:
                        sbank, s5, wr5 = B[5], B[1][:, 384:512], ("B", 1)
                        wr = [("B", 5)] + ([wr5] if nk > 4 else [])

                    def sc(e, qc=qc, po=po, kv=kv, sbank=sbank, s5=s5):
                        ins = None
                        for i, (c, msk) in enumerate(kcs):
                            dst = sbank[:, i * 128:(i + 1) * 128] if i < 4 else s5
                            ins = e.matmul(dst, lhsT=kT[po:po + 64, kv, c * 128:(c + 1) * 128],
                                           rhs=qrot[s][po:po + 64, qc, :], start=True, stop=(msk is None))
                            if msk is not None:
                                ins = e.matmul(dst, lhsT=ident_b[:], rhs=msk[:], start=False, stop=True)
                        return ins
                    P.op("pe", sc, reads=[("kT", c) for c, _ in kcs] + [("qrot", s), "ident_b", "maskl", "maskr"], writes=wr)

                    def ex(e, pslot=pslot, sbank=sbank, s5=s5):
                        n1 = min(nk, 4)
                        ins = e.activation(out=PT[pslot][:, 0:n1, :], in_=sbank[:, 0:n1 * 128].rearrange("p (c t) -> p c t", c=n1),
                                           func=AF.Exp, scale=0.125)
                        if nk > 4:
                            ins = e.activation(out=PT[pslot][:, 4, :], in_=s5, func=AF.Exp, scale=0.125)
                        return ins
                    P.op("act", ex, reads=wr, writes=[("PT", pslot)])

                    def pv(e, kv=kv, pslot=pslot):
                        ins = None
                        for i, (c, msk) in enumerate(kcs):
                            ins = e.matmul(Bo[:, pslot * 65:(pslot + 1) * 65],
                                           lhsT=PT[pslot][:, i, :], rhs=V[:, c, kv, :],
                                           start=(i == 0), stop=(i == nk - 1))
                        return ins
                    P.op("pe", pv, reads=[("PT", pslot), "Vones"] + [("V", c) for c, _ in kcs], writes=[("B", 4)])
                    if pslot == 3:
                        hg = h // 4
                        bov = Bo[:, 0:260].rearrange("p (h d) -> p h d", d=65)
                        P.op("dve", lambda e, hg=hg, bov=bov: e.tensor_tensor(den[:], bov[:, :, 64], esink[:, hg * 4:(hg + 1) * 4], ALU.add),
                             reads=[("B", 4), "esink"], writes=["den"])
                        P.op("dve", lambda e: e.reciprocal(den[:], den[:]), reads=["den"], writes=["den"])
                        P.op("dve", lambda e, hg=hg, bov=bov: e.tensor_tensor(
                            mixtok[ms][:, hg * 256:(hg + 1) * 256].rearrange("p (h d) -> p h d", d=64),
                            bov[:, :, 0:64], bc_last(den[:], 64), ALU.mult),
                            reads=[("B", 4), "den"], writes=[("mixtok", ms, hg)])

            def post(t):
                s = t % R
                ms = t % 2
                col = 0 if t < NB else 1
                for j in range(2):
                    P.op("pool", lambda e, j=j: e.tensor_scalar(ct[:, j, :], u[s][:, j, 0:128], convw_s[:, j, 0:1], None, ALU.mult),
                         reads=[("u", s), "convw_s"], writes=[("ct", j)])
                    P.op("dve", lambda e, j=j: e.scalar_tensor_tensor(ct[:, j, :], u[s][:, j, 1:129], convw_s[:, j, 1:2], ct[:, j, :], ALU.mult, ALU.add),
                         reads=[("u", s), "convw_s", ("ct", j)], writes=[("ct", j)])
                    P.op("dve", lambda e, j=j: e.scalar_tensor_tensor(ct[:, j, :], u[s][:, j, 2:130], convw_s[:, j, 2:3], ct[:, j, :], ALU.mult, ALU.add),
                         reads=[("u", s), "convw_s", ("ct", j)], writes=[("ct", j)])
                    P.op("pool", lambda e, j=j: e.tensor_tensor(mixT[ms][:, 4 + j, :], cb[s][:, j, :], ct[:, j, :], ALU.mult),
                         reads=[("cb", s), ("ct", j)], writes=[("mixT", ms, 4 + j)])
                def gm(e):
                    ins = None
                    for g in range(4):
                        ins = e.matmul(B[1][:, g * 64:(g + 1) * 64], lhsT=wsT[:, g, :], rhs=vln[s][:, g * 64:(g + 1) * 64],
                                       start=True, stop=True)
                    return ins
                P.op("pe", gm, reads=[("vln", s), "wsT"], writes=[("B", 1)])
                P.op("dve", lambda e: e.tensor_tensor(gtmp[:].rearrange("p (g c) -> p g c", g=4),
                                                      B[1][:, 0:256].rearrange("p (g c) -> p g c", g=4),
                                                      bc_last(gmb[:], 64), ALU.add),
                     reads=[("B", 1), "gmb"], writes=["gtmp"])
                P.op("dve", lambda e: e.tensor_tensor(mixtok[ms][:, 512:768], gtmp[:], gut[s][:], ALU.mult),
                     reads=["gtmp", ("gut", s)], writes=[("mixtok", ms, 2)])
                def tpm(e):
                    ins = None
                    for i in range(6):
                        ins = e.transpose(Bbf[:, i * 128:(i + 1) * 128], mixtok[ms][:, i * 128:(i + 1) * 128], ident_b[:])
                    return ins
                P.op("pe", tpm, reads=[("mixtok", ms, i) for i in range(3)] + ["ident_b"], writes=["Bbf"])
                P.op("act", lambda e: e.activation(out=mixT[ms][:, 0:4, :], in_=Bbf[:, 0:512].rearrange("p (c t) -> p c t", c=4), func=AF.Copy),
                     reads=["Bbf"], writes=[("mixT", ms, i) for i in range(4)])
                P.op("act", lambda e: e.activation(out=mixT[ms][:, 6:8, :], in_=Bbf[:, 512:768].rearrange("p (c t) -> p c t", c=2), func=AF.Copy),
                     reads=["Bbf"], writes=[("mixT", ms, 6), ("mixT", ms, 7)])
                for hf in range(2):
                    def op_(e, hf=hf):
                        ins = None
                        for k in range(KC):
                            ins = e.matmul(B[0][:, :], lhsT=mixT[ms][:, k, :], rhs=wout[:, k, hf * 512:(hf + 1) * 512],
                                           start=(k == 0), stop=(k == KC - 1))
                        return ins
                    P.op("pe", op_, reads=[("mixT", ms, i) for i in range(8)] + ["wout"], writes=[("B", 0)])
                    P.op("dve", lambda e, hf=hf: e.tensor_tensor(bufAs[t % 2][:, hf * 512:(hf + 1) * 512], B[0][:, :],
                                                                g1b[col][:, hf * 512:(hf + 1) * 512], ALU.mult),
                         reads=[("B", 0), ("g1b", col)], writes=[("bufA", t % 2, hf)])

            def mixB(t):
                xs = t % 5
                layer_norm_tail(xres[xs], ("xres", xs), lng, lnb, x1buf[t * 128:(t + 1) * 128, :], ("x1buf", t), t % 2)

            def layer_norm_tail(xt, xres_name, g_t, b_t, dst, dst_name, ab):
                bufA = bufAs[ab]
                P.op("dve", lambda e: e.scalar_tensor_tensor(bufA[:], xt[:], ALPHA, bufA[:], ALU.mult, ALU.add),
                     reads=[xres_name, ("bufA", ab, 0), ("bufA", ab, 1)], writes=[("bufA", ab, 0), ("bufA", ab, 1)])
                P.op("dve", lambda e: e.bn_stats(st12[:, 0:6], bufA[:, 0:512]), reads=[("bufA", ab, 0)], writes=[("st12", 0)])
                P.op("dve", lambda e: e.bn_stats(st12[:, 6:12], bufA[:, 512:1024]), reads=[("bufA", ab, 1)], writes=[("st12", 1)])
                P.op("dve", lambda e: e.bn_aggr(mv2[:], st12[:]), reads=[("st12", 0), ("st12", 1)], writes=["mv2"])
                P.op("act", lambda e: e.activation(out=rstd2[:], in_=mv2[:, 1:2], func=AF.Ln, bias=epsb[:, 0:1]),
                     reads=["mv2", "epsb"], writes=["rstd2"])
                P.op("act", lambda e: e.activation(out=rstd2[:], in_=rstd2[:], func=AF.Exp, scale=-0.5), reads=["rstd2"], writes=["rstd2"])
                P.op("dve", lambda e: e.scalar_tensor_tensor(nb2[:], mv2[:, 0:1], -1.0, rstd2[:], ALU.mult, ALU.mult),
                     reads=["mv2", "rstd2"], writes=["nb2"])
                P.op("act", lambda e: e.activation(out=bufB[:], in_=bufA[:], func=AF.Identity, scale=rstd2[:, 0:1], bias=nb2[:, 0:1]),
                     reads=[("bufA", ab, 0), ("bufA", ab, 1), "rstd2", "nb2"], writes=["bufB"])
                P.op("pool", lambda e: e.tensor_tensor(bufB[:], bufB[:], g_t[:], ALU.mult), reads=["bufB", "lng"], writes=["bufB"])
                P.op("pool", lambda e: e.tensor_tensor(bufB[:], bufB[:], b_t[:], ALU.add), reads=["bufB", "lnb"], writes=["bufB"])
                P.dma("sp", dst, bufB[:], reads=["bufB"], writes=[dst_name])

            for t in range(NB, NT):
                inproj(t, kv_only=last)
            if not last:
                for t in range(NB, NT):
                    attn(t)
                    post(t)
                    mixB(t)
            inproj(0)
            if NB > 1:
                inproj(1)
            for n in range(NB + 2):
                ls = []
                if n < NB:
                    ls.append(P.record(attn, n))
                if 0 <= n - 1 < NB:
                    ls.append(P.record(post, n - 1))
                if 0 <= n - 2 < NB:
                    ls.append(P.record(mixB, n - 2))
                if n + 2 < NB:
                    ls.append(P.record(inproj, n + 2))
                P.play(*ls)
            P.flush()
        if stop == "p1":
            return nc

        tiles = list(range(NB)) if last else list(range(NT))
        nt2 = len(tiles)
        NBLK = (2 * nt2 * 128) // BS + NEXP
        AX = mybir.AxisListType.X
        w1v = w1.rearrange("l e p (a k) n -> (l e p a) (k n)", a=4)
        w3v = w3.rearrange("l e p (a k) n -> (l e p a) (k n)", a=4)
        w2v = w2.rearrange("l e p (a k) n -> (l e p a) (k n)", a=4)
        with ExitStack() as st:
            wr_s = alloc(st, "wr_s", [128, KC, 36], F32)
            wr_m = [alloc(st, "wr_m%d" % c, [128, KC, 36], F32) for c in range(2)]
            sh2rep = [alloc(st, "sh2rep%d" % c, [128, KC, 128], F32) for c in range(2)]
            br_row = alloc(st, "br_row", [1, 36], F32)
            lng = alloc(st, "lng2", [128, D], F32)
            lnb = alloc(st, "lnb2", [128, D], F32)
            scb = [alloc(st, "scb%d" % c, [128, D], F32) for c in range(2)]
            shb = [alloc(st, "shb%d" % c, [128, D], F32) for c in range(2)]
            g2b = [alloc(st, "g2b%d" % c, [128, D], F32) for c in range(2)]
            lt_b = alloc(st, "lt_b", [128, 128], BF16)
            ones_b = alloc(st, "ones_b", [128, 128], BF16)
            jvec = alloc(st, "jvec", [128, NBLK_MAX], F32)
            base1 = alloc(st, "base1", [128, 8], F32)
            base2 = alloc(st, "base2", [128, 4], F32)
            x1t = [alloc(st, "x1t%d" % i, [128, D], F32) for i in range(2)]
            x1T = alloc(st, "x1T", [128, KC, 128], F32)
            h2r = [alloc(st, "h2r%d" % i, [128, D], BF16) for i in range(2)]
            lg = alloc(st, "lg", [128, 36], F32)
            sm = alloc(st, "sm", [128, 16], F32)
            goh = alloc(st, "goh", [128, 4], F32)
            gex = alloc(st, "gex", [128, 4], F32)
            esel = alloc(st, "esel", [128, 8], F32)
            esel2 = alloc(st, "esel2", [128, 8], F32)
            eq1 = alloc(st, "eq1", [128, 8], F32)
            eq2 = alloc(st, "eq2", [128, 8], F32)
            oh0 = alloc(st, "oh0", [128, nt2, 32], F32)
            oh1 = alloc(st, "oh1", [128, nt2, 32], F32)
            cntb = alloc(st, "cntb", [128, nt2, 32], BF16)
            gsc = alloc(st, "gsc", [128, nt2, 2], F32)
            rank_s = alloc(st, "rank_s", [128, nt2, 32], F32)
            dtmp = alloc(st, "dtmp", [128, nt2, 32], F32)
            tot = alloc(st, "tot", [128, 32], F32)
            nblk = alloc(st, "nblk", [128, 32], F32)
            sc_a = alloc(st, "sc_a", [128, 32], F32)
            sc_b = alloc(st, "sc_b", [128, 32], F32)
            pstart = alloc(st, "pstart", [128, 32], F32)
            destf = alloc(st, "destf", [128, 2, nt2], F32)
            idx = alloc(st, "idx", [128, 2, nt2], mybir.dt.int32)
            bef = alloc(st, "bef", [128, NBLK_MAX], F32)
            be1 = alloc(st, "be1", [128, NBLK_MAX], F32)
            wif1 = alloc(st, "wif1", [128, NBLK_MAX, 4], F32)
            wi1 = alloc(st, "wi1", [128, NBLK_MAX, 4], mybir.dt.int32)
            w1s = [alloc(st, "w1s%d" % i, [128, KC, DEXP], BF16) for i in range(2)]
            w3s = [alloc(st, "w3s%d" % i, [128, KC, DEXP], BF16) for i in range(2)]
            w2s = [alloc(st, "w2s%d" % i, [128, 4, D], BF16) for i in range(2)]
            xsb = [alloc(st, "xsb%d" % i, [128, 4, D], BF16) for i in range(2)]
            hTb = [alloc(st, "hTb%d" % i, [128, KC, 512], BF16) for i in range(2)]
            sg = [alloc(st, "sg%d" % i, [128, 512], F32) for i in range(2)]
            actT = [alloc(st, "actT%d" % i, [128, 4, 512], BF16) for i in range(2)]
            ysb = [alloc(st, "ysb%d" % i, [128, D], F32) for i in range(2)]
            yg = ysb
            bufA = alloc(st, "bufA2", [128, D], F32)
            bufB = alloc(st, "bufB2", [128, D], F32)
            st12 = alloc(st, "st12b", [128, 12], F32)
            mv2 = alloc(st, "mv2b", [128, 2], F32)
            rstd2 = alloc(st, "rstd2b", [128, 1], F32)
            nb2 = alloc(st, "nb2b", [128, 1], F32)
            B = [None, None] + [palloc(st, "M%d" % i, [128, 512]) for i in range(2, 8)]
            TB = [palloc(st, "TB%d" % i, [128, 1024], BF16) for i in range(2)]

            P.dma("sp", wr_s[:], w_r[l].rearrange("(k p) n -> p k n", p=128), writes=["wr_s"])
            P.dma("sp", br_row[:], b_r[l:l + 1, :], writes=["br_row"])
            P.dma("sp", lng[:], ln2_g[l:l + 1, :].to_broadcast([128, D]), writes=["lng"])
            P.dma("sp", lnb[:], ln2_b[l:l + 1, :].to_broadcast([128, D]), writes=["lnb"])
            for c in range(2):
                P.dma("sp", shb[c][:], modrows[l, c, 1:2, :].to_broadcast([128, D]), writes=[("shb", c)])
                P.dma("sp", scb[c][:], modrows[l, c, 2:3, :].to_broadcast([128, D]), writes=[("scb", c)])
                P.dma("sp", g2b[c][:], modrows[l, c, 3:4, :].to_broadcast([128, D]), writes=[("g2b", c)])
            P.dma("pool", lt_b[:], lt_d[:, :], writes=["lt_b"])
            P.dma("sp", jvec[:], jvec_d[:, :], writes=["jvec"])
            P.dma("sp", base1[:], base1_d[:, :], writes=["base1"])
            P.dma("sp", base2[:], base2_d[:, :], writes=["base2"])
            P.op("pool", lambda e: e.memset(ones_b[:], 1.0), writes=["ones_b"])
            P.op("pool", lambda e: e.memset(xsb[1][:], 0.0), writes=[("xsb", 1)])
            zres = []
            for i in range(NBLK):
                P.dma("sp", xs_dram[i * BS:(i + 1) * BS, :].rearrange("(a p) d -> p a d", p=128), xsb[1][:],
                      reads=[("xsb", 1)], writes=[("xsz", i)])
                zres.append(("xsz", i))
            for c in range(2):
                P.op("dve", lambda e, c=c: e.tensor_tensor(wr_m[c][:], wr_s[:], bc_last(modT[:, 32:40, c], 36), ALU.mult),
                     reads=["wr_s", "modT"], writes=[("wr_m", c)])
                P.op("dve", lambda e, c=c: e.tensor_copy(sh2rep[c][:], bc_last(modT[:, 24:32, c], 128)),
                     reads=["modT"], writes=[("sh2rep", c)])

            st12s = [st12, alloc(st, "st12c", [128, 12], F32)]
            mv2s = [mv2, alloc(st, "mv2c", [128, 2], F32)]
            rstd2s = [rstd2, alloc(st, "rstd2c", [128, 1], F32)]
            nb2s = [nb2, alloc(st, "nb2c", [128, 1], F32)]

            def ln_tail2(xt, xname, dst, dname, bA, bB, se):
                nA, nB = ("bufA", se), ("bufB", se)
                s12, m2, r2, n2 = st12s[se], mv2s[se], rstd2s[se], nb2s[se]
                P.op("dve", lambda e: e.scalar_tensor_tensor(bA[:], xt[:], ALPHA, bA[:], ALU.mult, ALU.add),
                     reads=[xname, nA], writes=[nA])
                P.op("dve", lambda e: e.bn_stats(s12[:, 0:6], bA[:, 0:512]), reads=[nA], writes=[("st12", se, 0)])
                P.op("dve", lambda e: e.bn_stats(s12[:, 6:12], bA[:, 512:1024]), reads=[nA], writes=[("st12", se, 1)])
                P.op("dve", lambda e: e.bn_aggr(m2[:], s12[:]), reads=[("st12", se, 0), ("st12", se, 1)], writes=[("mv2", se)])
                P.op("act", lambda e: e.activation(out=r2[:], in_=m2[:, 1:2], func=AF.Sqrt, bias=LN_EPS),
                     reads=[("mv2", se)], writes=[("rstd2", se)])
                P.op("dve", lambda e: e.reciprocal(r2[:], r2[:]), reads=[("rstd2", se)], writes=[("rstd2", se)])
                P.op("dve", lambda e: e.scalar_tensor_tensor(n2[:], m2[:, 0:1], -1.0, r2[:], ALU.mult, ALU.mult),
                     reads=[("mv2", se), ("rstd2", se)], writes=[("nb2", se)])
                P.op("act", lambda e: e.activation(out=bB[:], in_=bA[:], func=AF.Identity, scale=r2[:, 0:1], bias=n2[:, 0:1]),
                     reads=[nA, ("rstd2", se), ("nb2", se)], writes=[nB])
                P.op("pool", lambda e: e.tensor_tensor(bB[:], bB[:], lng[:], ALU.mult), reads=[nB, "lng"], writes=[nB])
                P.op("pool", lambda e: e.tensor_tensor(bB[:], bB[:], lnb[:], ALU.add), reads=[nB, "lnb"], writes=[nB])
                P.dma("sp", dst, bB[:], reads=[nB], writes=[dname])

            def v1024(tn, nm):
                if nt2 * 32 >= 1024:
                    return tn[:].rearrange("p a b -> p (a b)")[:, 0:1024]
                return alloc(st, nm, [128, D], F32)[:]
            rank_v, dtmp_v, oh0_v = v1024(rank_s, "rank_v"), v1024(dtmp, "dtmp_v"), v1024(oh0, "oh0_v")
            x1Ts = [x1T, rank_v.rearrange("p (k t) -> p k t", k=KC)]
            bufAs = [bufA, dtmp_v]
            lgs = [lg, alloc(st, "lg_b", [128, 36], F32)]
            sms = [sm, alloc(st, "sm_b", [128, 16], F32)]
            gohs = [goh, alloc(st, "goh_b", [128, 4], F32)]
            gexs = [gex, alloc(st, "gex_b", [128, 4], F32)]
            esels = [esel, alloc(st, "esel_b", [128, 8], F32)]
            esel2s = [esel2, alloc(st, "esel2_b", [128, 8], F32)]
            eq1s = [eq1, alloc(st, "eq1_b", [128, 8], F32)]
            eq2s = [eq2, alloc(st, "eq2_b", [128, 8], F32)]

            def stageA(li):
                t = tiles[li]
                col = 0 if t < NB else 1
                xs = li % 2
                se = li % 2
                tpb = 6 if se == 0 else 4
                rtb = 2 if se == 0 else 3
                P.dma("sp", x1t[xs][:], x1buf[t * 128:(t + 1) * 128, :], writes=[("x1t", xs)])
                P.op("dve", lambda e, xs=xs, col=col: e.tensor_tensor(bufAs[se][:], x1t[xs][:], scb[col][:], ALU.mult),
                     reads=[("x1t", xs), ("scb", col)], writes=[("bufA", se)])
                P.op("pool", lambda e, xs=xs, col=col: e.tensor_tensor(h2r[xs][:], bufAs[se][:], shb[col][:], ALU.add),
                     reads=[("bufA", se), ("shb", col)], writes=[("h2r", xs)])
                P.dma("sp", h2buf[li * 128:(li + 1) * 128, :], h2r[xs][:], reads=[("h2r", xs)], writes=[("h2buf", li)])
                for hb in range(2):
                    def tp(e, hb=hb, xs=xs):
                        ins = None
                        for i in range(4):
                            j = hb * 4 + i
                            ins = e.transpose(B[tpb + hb][:, i * 128:(i + 1) * 128], x1t[xs][:, j * 128:(j + 1) * 128], ident_f[:])
                        return ins
                    P.op("pe", tp, reads=[("x1t", xs), "ident_f"], writes=[("B", tpb + hb)])

                    def cpx(e, hb=hb):
                        ins = None
                        for i in range(4):
                            ins = e.activation(out=x1Ts[se][:, hb * 4 + i, :], in_=B[tpb + hb][:, i * 128:(i + 1) * 128], func=AF.Copy)
                        return ins
                    P.op("act", cpx, reads=[("B", tpb + hb)], writes=[("x1T", se, hb)])

                def rt(e, col=col):
                    ins = None
                    for k in range(KC):
                        ins = e.matmul(B[rtb][:, 0:36], lhsT=x1Ts[se][:, k, :], rhs=wr_m[col][:, k, :], start=(k == 0), stop=False)
                    for k in range(KC):
                        ins = e.matmul(B[rtb][:, 0:36], lhsT=sh2rep[col][:, k, :], rhs=wr_s[:, k, :], start=False, stop=False)
                    ins = e.matmul(B[rtb][:, 0:36], lhsT=ones_f[0:1, :], rhs=br_row[0:1, :], start=False, stop=True)
                    return ins
                P.op("pe", rt, reads=[("x1T", se, 0), ("x1T", se, 1), ("wr_m", col), ("sh2rep", col), "wr_s", "br_row", "ones_f"], writes=[("B", rtb)])
                P.op("act", lambda e: e.activation(out=lgs[se][:], in_=B[rtb][:, 0:36], func=AF.Copy), reads=[("B", rtb)], writes=[("lg", se)])
                P.op("dve", lambda e: e.reduce_max(sms[se][:, 0:1], lgs[se][:, 0:4], AX), reads=[("lg", se)], writes=[("sm", se)])
                P.op("dve", lambda e: e.tensor_scalar(sms[se][:, 1:2], sms[se][:, 0:1], -1.0, None, ALU.mult), reads=[("sm", se)], writes=[("sm", se)])
                P.op("act", lambda e: e.activation(out=gexs[se][:], in_=lgs[se][:, 0:4], func=AF.Exp, bias=sms[se][:, 1:2], accum_out=sms[se][:, 2:3]),
                     reads=[("lg", se), ("sm", se)], writes=[("gex", se), ("sm", se)])
                P.op("dve", lambda e: e.reciprocal(sms[se][:, 3:4], sms[se][:, 2:3]), reads=[("sm", se)], writes=[("sm", se)])
                P.op("dve", lambda e: e.tensor_scalar(gohs[se][:], lgs[se][:, 0:4], sms[se][:, 0:1], None, ALU.is_equal), reads=[("lg", se), ("sm", se)], writes=[("goh", se)])
                P.op("dve", lambda e: e.tensor_scalar(esels[se][:], lgs[se][:, 4:12], gohs[se][:, 0:1], None, ALU.mult), reads=[("lg", se), ("goh", se)], writes=[("esel", se)])
                for g in range(1, 4):
                    P.op("dve", lambda e, g=g: e.scalar_tensor_tensor(esels[se][:], lgs[se][:, 4 + g * 8:12 + g * 8], gohs[se][:, g:g + 1], esels[se][:], ALU.mult, ALU.add),
                         reads=[("lg", se), ("goh", se), ("esel", se)], writes=[("esel", se)])
                P.op("dve", lambda e: e.reduce_max(sms[se][:, 4:5], esels[se][:], AX), reads=[("esel", se)], writes=[("sm", se)])
                P.op("dve", lambda e: e.tensor_scalar(eq1s[se][:], esels[se][:], sms[se][:, 4:5], None, ALU.is_equal), reads=[("esel", se), ("sm", se)], writes=[("eq1", se)])
                P.op("dve", lambda e: e.scalar_tensor_tensor(esel2s[se][:], eq1s[se][:], -1e30, esels[se][:], ALU.mult, ALU.add), reads=[("eq1", se), ("esel", se)], writes=[("esel2", se)])
                P.op("dve", lambda e: e.reduce_max(sms[se][:, 5:6], esel2s[se][:], AX), reads=[("esel2", se)], writes=[("sm", se)])
                P.op("dve", lambda e: e.tensor_scalar(eq2s[se][:], esel2s[se][:], sms[se][:, 5:6], None, ALU.is_equal), reads=[("esel2", se), ("sm", se)], writes=[("eq2", se)])
                P.op("dve", lambda e: e.tensor_tensor(sms[se][:, 6:7], sms[se][:, 4:5], sms[se][:, 5:6], ALU.subtract), reads=[("sm", se)], writes=[("sm", se)])
                P.op("act", lambda e: e.activation(out=sms[se][:, 7:8], in_=sms[se][:, 6:7], func=AF.Sigmoid), reads=[("sm", se)], writes=[("sm", se)])
                P.op("act", lambda e: e.activation(out=sms[se][:, 8:9], in_=sms[se][:, 6:7], func=AF.Sigmoid, scale=-1.0), reads=[("sm", se)], writes=[("sm", se)])
                P.op("dve", lambda e, li=li: e.tensor_scalar(gsc[:, li, :], sms[se][:, 7:9], sms[se][:, 3:4], None, ALU.mult), reads=[("sm", se)], writes=[("gsc", li)])
                P.op("dve", lambda e, li=li: e.tensor_tensor(oh0[:, li, :].rearrange("p (g x) -> p g x", g=4), bc_last(gohs[se][:], 8), bc_mid(eq1s[se][:], 4), ALU.mult),
                     reads=[("goh", se), ("eq1", se)], writes=[("oh0", li)])
                P.op("dve", lambda e, li=li: e.tensor_tensor(oh1[:, li, :].rearrange("p (g x) -> p g x", g=4), bc_last(gohs[se][:], 8), bc_mid(eq2s[se][:], 4), ALU.mult),
                     reads=[("goh", se), ("eq2", se)], writes=[("oh1", li)])
                P.op("pool", lambda e, li=li: e.tensor_tensor(cntb[:, li, :], oh0[:, li, :], oh1[:, li, :], ALU.add),
                     reads=[("oh0", li), ("oh1", li)], writes=[("cntb", li)])


            for li0 in range(0, nt2, 2):
                P.play(P.record(stageA, li0), P.record(stageA, li0 + 1) if li0 + 1 < nt2 else [])
            if stop == "p2a":
                P.flush()
                return nc
            call = [("cntb", i) for i in range(nt2)]
            nbank = (nt2 + 15) // 16
            for li in range(nt2):
                bk, off = 3 + li // 16, (li % 16) * 32

                def rk(e, li=li, bk=bk, off=off):
                    ins = None
                    for tp_ in range(li):
                        ins = e.matmul(B[bk][:, off:off + 32], lhsT=ones_b[:], rhs=cntb[:, tp_, :], start=(tp_ == 0), stop=False)
                    ins = e.matmul(B[bk][:, off:off + 32], lhsT=lt_b[:], rhs=cntb[:, li, :], start=(li == 0), stop=True)
                    return ins
                P.op("pe", rk, reads=call + ["ones_b", "lt_b"], writes=[("RK", li)])
            for bk in range(nbank):
                n_here = min(16, nt2 - bk * 16)
                P.op("act", lambda e, bk=bk, n_here=n_here: e.activation(
                    out=rank_s[:, bk * 16:bk * 16 + n_here, :], in_=B[3 + bk][:, 0:n_here * 32].rearrange("p (t x) -> p t x", x=32), func=AF.Copy),
                    reads=[("RK", i) for i in range(bk * 16, bk * 16 + n_here)], writes=[("rank_s", bk)])

            def tt(e):
                ins = None
                for tp_ in range(nt2):
                    ins = e.matmul(B[2][:, 64:96], lhsT=ones_b[:], rhs=cntb[:, tp_, :], start=(tp_ == 0), stop=(tp_ == nt2 - 1))
                return ins
            P.op("pe", tt, reads=call + ["ones_b"], writes=[("B", 2)])
            P.op("act", lambda e: e.activation(out=tot[:], in_=B[2][:, 64:96], func=AF.Copy), reads=[("B", 2)], writes=["tot"])
            P.op("dve", lambda e: e.tensor_scalar(nblk[:], tot[:], 0.0, None, ALU.is_gt), reads=["tot"], writes=["nblk"])
            for m in range(1, (2 * nt2 * 128) // BS + 1):
                P.op("dve", lambda e, m=m: e.scalar_tensor_tensor(nblk[:], tot[:], float(m * BS), nblk[:], ALU.is_gt, ALU.add),
                     reads=["tot", "nblk"], writes=["nblk"])
            P.op("dve", lambda e: e.tensor_copy(sc_a[:], nblk[:]), reads=["nblk"], writes=["sc_a"])
            cur, oth, cn, on = sc_a, sc_b, "sc_a", "sc_b"
            for dd in (1, 2, 4, 8, 16):
                P.op("dve", lambda e, cur=cur, oth=oth, dd=dd: e.tensor_copy(oth[:, 0:dd], cur[:, 0:dd]), reads=[cn], writes=[on])
                P.op("dve", lambda e, cur=cur, oth=oth, dd=dd: e.tensor_tensor(oth[:, dd:32], cur[:, dd:32], cur[:, 0:32 - dd], ALU.add),
                     reads=[cn, on], writes=[on])
                cur, oth, cn, on = oth, cur, on, cn
            pend, pendn = cur, cn
            P.op("dve", lambda e: e.tensor_tensor(pstart[:], pend[:], nblk[:], ALU.subtract), reads=[pendn, "nblk"], writes=["pstart"])
            P.op("dve", lambda e: e.tensor_scalar(pstart[:], pstart[:], float(BS), None, ALU.mult), reads=["pstart"], writes=["pstart"])
            rk_all = [("rank_s", i) for i in range(nbank)]
            P.op("dve", lambda e: e.tensor_tensor(rank_s[:], rank_s[:], bc_mid(pstart[:], nt2), ALU.add),
                 reads=rk_all + ["pstart"], writes=rk_all)
            for k, oh in enumerate((oh0, oh1)):
                P.op("dve", lambda e, oh=oh: e.tensor_tensor(dtmp[:], oh[:], rank_s[:], ALU.mult),
                     reads=rk_all + [("oh%d" % k, i) for i in range(nt2)], writes=["dtmp"])
                P.op("dve", lambda e, k=k: e.reduce_sum(destf[:, k, :], dtmp[:], AX), reads=["dtmp"], writes=[("destf", k)])
            P.op("dve", lambda e: e.tensor_copy(idx[:], destf[:]), reads=[("destf", 0), ("destf", 1)], writes=["idx"])
            P.op("dve", lambda e: e.tensor_scalar(bef[:], jvec[:], pend[:, 0:1], None, ALU.is_ge), reads=["jvec", pendn], writes=["bef"])
            for ee in range(1, 32):
                P.op("dve", lambda e, ee=ee: e.scalar_tensor_tensor(bef[:], jvec[:], pend[:, ee:ee + 1], bef[:], ALU.is_ge, ALU.add),
                     reads=["jvec", pendn, "bef"], writes=["bef"])
            P.op("dve", lambda e: e.tensor_scalar(bef[:], bef[:], 31.0, None, ALU.min), reads=["bef"], writes=["bef"])
            P.op("dve", lambda e: e.tensor_scalar(be1[:], bef[:], 128.0, float(l * NEXP * 128), ALU.mult, ALU.add), reads=["bef"], writes=["be1"])
            P.op("dve", lambda e: e.tensor_scalar(be1[:], be1[:], base1[:, 0:1], None, ALU.add), reads=["be1", "base1"], writes=["be1"])
            P.op("dve", lambda e: e.tensor_scalar(be1[:], be1[:], 4.0, None, ALU.mult), reads=["be1"], writes=["be1"])
            P.op("dve", lambda e: e.tensor_tensor(wif1[:], bc_last(be1[:], 4), bc_mid(base2[:], NBLK_MAX), ALU.add),
                 reads=["be1", "base2"], writes=["wif1"])
            P.op("dve", lambda e: e.tensor_copy(wi1[:], wif1[:]), reads=["wif1"], writes=["wi1"])
            if stop == "p2c":
                P.flush()
                return nc
            sres = []
            for li in range(nt2):
                hs_ = li % 2
                P.dma("sp", h2r[hs_][:], h2buf[li * 128:(li + 1) * 128, :], reads=[("h2buf", li)], writes=[("h2r", hs_)])
                for k in range(2):
                    P.idma(xs_dram[0:NBLK * BS, :], bass.IndirectOffsetOnAxis(ap=idx[:, k, li:li + 1], axis=0), h2r[hs_][:], None,
                           reads=[("h2r", hs_), "idx"] + zres, writes=[("xss", li, k)])
                    sres.append(("xss", li, k))

            if stop == "p2d":
                P.flush()
                return nc
            yres = []

            def blockA(j):
                ws = j % 2
                if not DBG.get("no_wgather"):
                    for a in range(4):
                        io = bass.IndirectOffsetOnAxis(ap=wi1[:, j, a:a + 1], axis=0)
                        P.idma(w1s[ws][:, 2 * a:2 * a + 2, :].rearrange("p k n -> p (k n)"), None, w1v, io, reads=["wi1"], writes=[("w1s", ws, a)])
                        P.idma(w3s[ws][:, 2 * a:2 * a + 2, :].rearrange("p k n -> p (k n)"), None, w3v, io, reads=["wi1"], writes=[("w3s", ws, a)])
                        P.idma(w2s[ws][:, a, :], None, w2v, io, reads=["wi1"], writes=[("w2s", ws, a)])
                P.dma("sp", xsb[ws][:], xs_dram[j * BS:(j + 1) * BS, :].rearrange("(a p) d -> p a d", p=128),
                      reads=sres + zres, writes=[("xsb", ws)])
                for a in range(4):
                    tb = a % 2

                    def tps_(e, a=a, tb=tb, ws=ws):
                        ins = None
                        for k in range(KC):
                            ins = e.transpose(TB[tb][:, k * 128:(k + 1) * 128], xsb[ws][:, a, k * 128:(k + 1) * 128], ident_b[:])
                        return ins
                    P.op("pe", tps_, reads=[("xsb", ws), "ident_b"], writes=[("TB", tb)])
                    P.op("act", lambda e, a=a, tb=tb, ws=ws: e.activation(out=hTb[ws][:, :, a * 128:(a + 1) * 128],
                                                                         in_=TB[tb][:, :].rearrange("p (k t) -> p k t", k=KC), func=AF.Copy),
                         reads=[("TB", tb)], writes=[("hTb", ws, a)])

            def blockB(j):
                ws = j % 2
                hres = [("hTb", ws, a) for a in range(4)]
                w13 = [("w1s", ws, a) for a in range(4)] + [("w3s", ws, a) for a in range(4)]
                for c in range(4):
                    pb = 2 + 2 * (c % 2)

                    def h13(e, c=c, pb=pb, ws=ws):
                        ins = None
                        for k in range(KC):
                            ins = e.matmul(B[pb][:, :], lhsT=w1s[ws][:, k, c * 128:(c + 1) * 128], rhs=hTb[ws][:, k, :],
                                           start=(k == 0), stop=(k == KC - 1))
                        for k in range(KC):
                            ins = e.matmul(B[pb + 1][:, :], lhsT=w3s[ws][:, k, c * 128:(c + 1) * 128], rhs=hTb[ws][:, k, :],
                                           start=(k == 0), stop=(k == KC - 1))
                        return ins
                    P.op("pe", h13, reads=w13 + hres, writes=[("B", pb), ("B", pb + 1)])
                    P.op("act", lambda e, c=c, pb=pb: e.activation(out=sg[c % 2][:], in_=B[pb][:, :], func=AF.Silu),
                         reads=[("B", pb)], writes=[("sg", c % 2)])
                    P.op("dve", lambda e, c=c, pb=pb, ws=ws: e.tensor_tensor(actT[ws][:, c, :], sg[c % 2][:], B[pb + 1][:, :], ALU.mult),
                         reads=[("sg", c % 2), ("B", pb + 1)], writes=[("actT", ws, c)])
                for a in range(4):
                    ys_ = a % 2
                    for hf in range(2):
                        yb = 6 + hf

                        def ymm(e, a=a, hf=hf, yb=yb, ws=ws):
                            ins = None
                            for c in range(4):
                                ins = e.matmul(B[yb][:, :], lhsT=actT[ws][:, c, a * 128:(a + 1) * 128],
                                               rhs=w2s[ws][:, c, hf * 512:(hf + 1) * 512], start=(c == 0), stop=(c == 3))
                            return ins
                        P.op("pe", ymm, reads=[("actT", ws, c) for c in range(4)] + [("w2s", ws, c) for c in range(4)], writes=[("B", yb)])
                        P.op("dve", lambda e, hf=hf, yb=yb, ys_=ys_: e.tensor_copy(ysb[ys_][:, hf * 512:(hf + 1) * 512], B[yb][:, :]),
                             reads=[("B", yb)], writes=[("ysb", ys_, hf)])
                    r0 = j * BS + a * 128
                    P.dma("sp", ys_dram[r0:r0 + 128, :], ysb[ys_][:], reads=[("ysb", ys_, 0), ("ysb", ys_, 1)], writes=[("ys", j, a)])
                    yres.append(("ys", j, a))


            P.play(P.record(blockA, 0))
            for j in range(NBLK):
                P.play(P.record(blockB, j), P.record(blockA, j + 1) if j + 1 < NBLK else [])
            if stop == "p2e":
                P.flush()
                return nc
            fA = [bufA, x1T[:].rearrange("p a b -> p (a b)")]
            fB = [bufB, dtmp_v]
            fY = [[ysb[0], ysb[1]], [rank_v, oh0_v]]

            def stageF(li):
                t = tiles[li]
                col = 0 if t < NB else 1
                xs = li % 2
                se = li % 2
                bA, bB, ygs = fA[se], fB[se], fY[se]
                nA = ("bufA", se)
                P.dma("sp", x1t[xs][:], x1buf[t * 128:(t + 1) * 128, :], writes=[("x1t", xs)])
                for k in range(2):
                    P.idma(ygs[k][:], None, ys_dram[0:NBLK * BS, :], bass.IndirectOffsetOnAxis(ap=idx[:, k, li:li + 1], axis=0),
                           reads=["idx"] + yres, writes=[("yg", se, k)])
                P.op("dve", lambda e: e.tensor_scalar(bA[:], ygs[0][:], gsc[:, li, 0:1], None, ALU.mult),
                     reads=[("yg", se, 0), ("gsc", li)], writes=[nA])
                P.op("dve", lambda e: e.scalar_tensor_tensor(bA[:], ygs[1][:], gsc[:, li, 1:2], bA[:], ALU.mult, ALU.add),
                     reads=[("yg", se, 1), ("gsc", li), nA], writes=[nA])
                P.op("pool", lambda e: e.tensor_tensor(bA[:], bA[:], g2b[col][:], ALU.mult),
                     reads=[nA, ("g2b", col)], writes=[nA])
                if last:
                    dst, dname = out_d[t * 128:(t + 1) * 128, :], ("out", t)
                else:
                    dst, dname = x2buf[t * 128:(t + 1) * 128, :], ("x2buf", t)
                ln_tail2(x1t[xs], ("x1t", xs), dst, dname, bA, bB, se)

            for li0 in range(0, nt2, 2):
                P.play(P.record(stageF, li0), P.record(stageF, li0 + 1) if li0 + 1 < nt2 else [])
            P.flush()
    glob.close()
    return nc


def _rope_tables(NB):
    NT = NB + NCB
    S = NB * 128
    m = 16
    freqs = (10000.0 ** (-np.arange(m, dtype=np.float32) / m)).astype(np.float32)
    tpos = np.arange(S)
    row = (tpos // GRID_W).astype(np.float32)
    colp = (tpos % GRID_W).astype(np.float32)
    cos = np.ones((128, NT * 128), np.float32)
    sin = np.zeros((128, NT * 128), np.float32)
    for p in range(128):
        d = p % 64
        pos = row if d < 32 else colp
        dd = d % 32
        f = freqs[dd % 16]
        ang = (pos * f).astype(np.float32)
        cos[p, :S] = np.cos(ang)
        sgn = -1.0 if dd < 16 else 1.0
        sin[p, :S] = sgn * np.sin(ang)
    cos_t = np.ascontiguousarray(cos.reshape(128, NT, 128).transpose(1, 0, 2))
    sin_t = np.ascontiguousarray(sin.reshape(128, NT, 128).transpose(1, 0, 2))
    return cos_t, sin_t


def make_in_maps(inputs, NB, ncores):
    f = lambda a: np.ascontiguousarray(np.asarray(a, dtype=np.float32))
    x = f(inputs["x"]); c = f(inputs["c"]); ctx = f(inputs["ctx"]); c_ctx = f(inputs["c_ctx"])
    depth = inputs["w_ada"].shape[0]
    cos_t, sin_t = _rope_tables(NB)
    j = np.arange(128)
    maskl = np.where(j[:, None] >= j[None, :], 0.0, NEG).astype(np.float32)
    maskr = np.where(j[:, None] <= j[None, :], 0.0, NEG).astype(np.float32)
    shared = {
        "w_ada": f(inputs["w_ada"]),
        "bada_p": f(np.asarray(inputs["b_ada"]).reshape(depth, 48, 128).transpose(0, 2, 1)),
        "b_ada": f(inputs["b_ada"]),
        "w_in": f(inputs["w_in"]),
        "convw": f(np.asarray(inputs["conv_w"]).reshape(depth, 3, 2, 128).transpose(0, 3, 2, 1)),
        "sink": f(inputs["attn_sink"]),
        "gm_wsT": f(np.asarray(inputs["gm_ws"]).transpose(0, 1, 3, 2)),
        "gm_bsp": f(np.asarray(inputs["gm_bs"]).transpose(0, 2, 1)),
        "w_out": f(inputs["w_out"]),
        "ln1_g": f(inputs["ln1_g"]), "ln1_b": f(inputs["ln1_b"]),
        "ln2_g": f(inputs["ln2_g"]), "ln2_b": f(inputs["ln2_b"]),
        "w_r": f(np.concatenate([np.asarray(inputs["w_rg"]), np.asarray(inputs["w_re"])], axis=-1)),
        "b_r": f(np.concatenate([np.asarray(inputs["b_rg"]), np.asarray(inputs["b_re"])], axis=-1)),
        "w1": f(np.asarray(inputs["w1"]).reshape(depth, NEXP, KC, 128, DEXP).transpose(0, 1, 3, 2, 4)),
        "w3": f(np.asarray(inputs["w3"]).reshape(depth, NEXP, KC, 128, DEXP).transpose(0, 1, 3, 2, 4)),
        "w2": f(np.asarray(inputs["w2"]).reshape(depth, NEXP, 4, 128, D).transpose(0, 1, 3, 2, 4)),
        "cos_t": cos_t, "sin_t": sin_t,
        "ident": np.eye(128, dtype=np.float32), "maskl": maskl, "maskr": maskr,
        "lt_d": (j[:, None] < j[None, :]).astype(np.float32),
        "jvec_d": np.tile(np.arange((2 * (NB + NCB) * 128) // 512 + NEXP, dtype=np.float32)[None, :], (128, 1)),
        "base1_d": (np.arange(8, dtype=np.float32)[None, :] * 128 + j[:, None]).astype(np.float32),
        "base2_d": np.tile(np.arange(4, dtype=np.float32)[None, :], (128, 1)),
    }
    maps = []
    for b in range(ncores):
        cv = np.stack([c[b].reshape(KC, 128).T, c_ctx.reshape(KC, 128).T], axis=-1)
        m = dict(shared)
        m["xin"] = np.ascontiguousarray(x[b])
        m["ctxin"] = np.ascontiguousarray(ctx[b])
        m["cvec"] = np.ascontiguousarray(cv.astype(np.float32))
        maps.append(m)
    return maps


_NC_CACHE = {}


def kernel(**inputs):
    x = np.asarray(inputs["x"])
    Bn, S, _ = x.shape
    NB = S // 128
    key = (NB,)
    if key not in _NC_CACHE:
        _NC_CACHE[key] = build_program(NB)
    nc = _NC_CACHE[key]
    maps = make_in_maps(inputs, NB, Bn)
    res = run_bass_kernel_spmd(nc, maps, core_ids=list(range(Bn)))
    return np.stack([np.asarray(r["out"], dtype=np.float32) for r in res.results], axis=0)
```

```python
from contextlib import ExitStack
import numpy as np
import ml_dtypes
import concourse.bass as bass
import concourse.mybir as mybir
from concourse.bass_utils import run_bass_kernel_spmd

F32 = mybir.dt.float32
BF16 = mybir.dt.bfloat16
AF = mybir.ActivationFunctionType
ALU = mybir.AluOpType

D = 1024
KC = 8
CTX = 256
NCB = CTX // 128
DEPTH = 2
NEXP = 32
DEXP = 512
ALPHA = (2 * DEPTH) ** 0.25
LN_EPS = 1e-6
GRID_W = 64
NEG = -30000.0
DBG = {}


class Prog:
    NDMA = 8

    def __init__(self, nc):
        self.nc = nc
        self.names = dict(pe="tensor", act="scalar", dve="vector", pool="gpsimd", sp="sync")
        self.sems = []
        self.semidx = {}
        for k in self.names:
            self.semidx[k] = len(self.sems)
            self.sems.append(nc.alloc_semaphore("s_" + k))
        self.cnt = {k: 0 for k in self.names}
        self.dq = {}
        for q in ("sp", "pool"):
            idx = []
            for i in range(self.NDMA):
                idx.append(len(self.sems))
                self.sems.append(nc.alloc_semaphore("d_%s%d" % (q, i)))
            self.dq[q] = dict(idx=idx, cnt=[0] * self.NDMA, nxt=0)
        self.seen = {k: {} for k in self.names}
        self.stream = {k: [] for k in self.names}
        self.res = {}
        self._cap = None

    def record(self, fn, *a, **kw):
        self._cap = []
        fn(*a, **kw)
        cap, self._cap = self._cap, None
        return cap

    def play(self, *lists):
        lists = [l for l in lists if l]
        pos = [0] * len(lists)
        while True:
            best, bf = None, None
            for i, l in enumerate(lists):
                if pos[i] < len(l):
                    f = pos[i] / len(l)
                    if bf is None or f < bf:
                        best, bf = i, f
            if best is None:
                break
            kind, a, kw = lists[best][pos[best]]
            pos[best] += 1
            getattr(self, kind)(*a, **kw)

    def _deps(self, reads, writes):
        deps = []
        for r in reads:
            e = self.res.get(r)
            if e and e["w"] is not None:
                deps.append(e["w"])
        for w in writes:
            e = self.res.get(w)
            if e:
                if e["w"] is not None:
                    deps.append(e["w"])
                deps.extend((si, v, src) for (si, src), v in e["r"].items())
        return deps

    def _waits(self, ek, deps):
        waits = {}
        for (si, val, src) in deps:
            if src == "pe" and ek == "pe":
                continue
            if self.seen[ek].get(si, 0) >= val:
                continue
            waits[si] = max(waits.get(si, 0), val)
        for si, v in waits.items():
            self.seen[ek][si] = v
        return list(waits.items())

    def _record(self, token, reads, writes):
        si, val, src = token
        for r in reads:
            e = self.res.setdefault(r, {"w": None, "r": {}})
            e["r"][(si, src)] = max(e["r"].get((si, src), 0), val)
        for w in writes:
            self.res[w] = {"w": token, "r": {}}

    def op(self, ek, fn, reads=(), writes=()):
        if self._cap is not None:
            self._cap.append(("op", (ek, fn), dict(reads=reads, writes=writes)))
            return None
        waits = self._waits(ek, self._deps(reads, writes))
        self.cnt[ek] += 1
        si = self.semidx[ek]
        token = (si, self.cnt[ek], ek)
        sems = self.sems

        def emit(eng):
            for (wsi, v) in waits:
                eng.wait_ge(sems[wsi], v)
            ins = fn(eng)
            ins.then_inc(sems[si], 1)

        self.stream[ek].append(emit)
        self._record(token, reads, writes)
        return token

    def dma(self, q, out, in_, reads=(), writes=()):
        if self._cap is not None:
            self._cap.append(("dma", (q, out, in_), dict(reads=reads, writes=writes)))
            return None
        d = self.dq[q]
        slot = d["nxt"]
        d["nxt"] = (slot + 1) % self.NDMA
        si = d["idx"][slot]
        deps = self._deps(reads, writes)
        if d["cnt"][slot] > 0:
            deps.append((si, 16 * d["cnt"][slot], "dma"))
        waits = self._waits(q, deps)
        d["cnt"][slot] += 1
        token = (si, 16 * d["cnt"][slot], "dma")
        sems = self.sems

        def emit(eng):
            for (wsi, v) in waits:
                eng.wait_ge(sems[wsi], v)
            eng.dma_start(out=out, in_=in_).then_inc(sems[si], 16)

        self.stream[q].append(emit)
        self._record(token, reads, writes)
        return token

    def idma(self, out, out_off, in_, in_off, reads=(), writes=()):
        if self._cap is not None:
            self._cap.append(("idma", (out, out_off, in_, in_off), dict(reads=reads, writes=writes)))
            return None
        q = "pool"
        d = self.dq[q]
        slot = d["nxt"]
        d["nxt"] = (slot + 1) % self.NDMA
        si = d["idx"][slot]
        deps = self._deps(reads, writes)
        if d["cnt"][slot] > 0:
            deps.append((si, 16 * d["cnt"][slot], "dma"))
        waits = self._waits(q, deps)
        d["cnt"][slot] += 1
        token = (si, 16 * d["cnt"][slot], "dma")
        sems = self.sems

        def emit(eng):
            for (wsi, v) in waits:
                eng.wait_ge(sems[wsi], v)
            eng.indirect_dma_start(out=out, out_offset=out_off, in_=in_, in_offset=in_off).then_inc(sems[si], 16)

        self.stream[q].append(emit)
        self._record(token, reads, writes)
        return token

    def flush(self):
        finals = []
        for k in self.names:
            if self.cnt[k] > 0:
                finals.append((self.semidx[k], self.cnt[k]))
        for q, d in self.dq.items():
            for i, si in enumerate(d["idx"]):
                if d["cnt"][i] > 0:
                    finals.append((si, 16 * d["cnt"][i]))
        with self.nc.Block() as block:
            for ek, nm in self.names.items():
                stream = self.stream[ek]
                seen = self.seen[ek]
                sems = self.sems

                def body(eng, stream=stream, seen=seen):
                    for emit in stream:
                        emit(eng)
                    for si, v in finals:
                        if seen.get(si, 0) < v:
                            eng.wait_ge(sems[si], v)
                            seen[si] = v

                getattr(block, nm)(body)
        self.stream = {k: [] for k in self.names}
        self.res = {}


def bc_mid(ap, n):
    p, f = ap.shape
    return ap.unsqueeze(1).to_broadcast([p, n, f])


def bc_last(ap, n):
    p, g = ap.shape
    return ap.unsqueeze(2).to_broadcast([p, g, n])


def build_program(NB, depth=DEPTH, tps=9, stop=None, nexp=NEXP, do_router=True, do_epi=True):
    S = NB * 128
    NT = NB + NCB
    T = NT * 128
    nc = bass.Bass("TRN2", target_bir_lowering=False)

    def din(name, shape, dt=F32):
        return nc.dram_tensor(name, list(shape), dt, kind="ExternalInput").ap()

    xin = din("xin", [S, D])
    ctxin = din("ctxin", [CTX, D])
    cvec = din("cvec", [128, KC, 2])
    w_ada = din("w_ada", [depth, D, 6 * D])
    bada_p = din("bada_p", [depth, 128, 48])
    b_ada = din("b_ada", [depth, 6 * D])
    w_in = din("w_in", [depth, D, 2048])
    convw = din("convw", [depth, 128, 2, 3])
    sink = din("sink", [depth, 8])
    gm_wsT = din("gm_wsT", [depth, 4, 128, 128])
    gm_bsp = din("gm_bsp", [depth, 128, 4])
    w_out = din("w_out", [depth, D, D])
    ln1_g = din("ln1_g", [depth, D])
    ln1_b = din("ln1_b", [depth, D])
    ln2_g = din("ln2_g", [depth, D])
    ln2_b = din("ln2_b", [depth, D])
    w_r = din("w_r", [depth, D, 36])
    b_r = din("b_r", [depth, 36])
    w1 = din("w1", [depth, NEXP, 128, KC, DEXP])
    w3 = din("w3", [depth, NEXP, 128, KC, DEXP])
    w2 = din("w2", [depth, NEXP, 128, 4, D])
    cos_t = din("cos_t", [NT, 128, 128])
    sin_t = din("sin_t", [NT, 128, 128])
    ident_d = din("ident", [128, 128])
    maskl_d = din("maskl", [128, 128])
    maskr_d = din("maskr", [128, 128])
    out_d = nc.dram_tensor("out", [S, D], F32, kind="ExternalOutput").ap()
    x1buf = nc.dram_tensor("x1buf", [T, D], F32).ap()
    x2buf = nc.dram_tensor("x2buf", [T, D], F32).ap()
    BS = 512
    NBLK_MAX = (2 * T) // BS + NEXP
    h2buf = nc.dram_tensor("h2buf", [T, D], BF16).ap()
    xs_dram = nc.dram_tensor("xs_dram", [NBLK_MAX * BS, D], BF16).ap()
    ys_dram = nc.dram_tensor("ys_dram", [NBLK_MAX * BS, D], F32).ap()
    modrows = nc.dram_tensor("modrows", [depth, 2, 4, D], F32).ap()
    lt_d = din("lt_d", [128, 128])
    jvec_d = din("jvec_d", [128, NBLK_MAX])
    base1_d = din("base1_d", [128, 8])
    base2_d = din("base2_d", [128, 4])

    P = Prog(nc)

    def tile_src(l, t):
        if l == 0:
            if t < NB:
                return xin[t * 128:(t + 1) * 128, :]
            return ctxin[(t - NB) * 128:(t - NB + 1) * 128, :]
        return x2buf[t * 128:(t + 1) * 128, :]

    glob = ExitStack()

    uid = [0]

    def alloc(st, name, shape, dt):
        uid[0] += 1
        return st.enter_context(nc.sbuf_tensor("sb%d_%s" % (uid[0], name), list(shape), dt))

    def palloc(st, name, shape, dt=F32):
        uid[0] += 1
        return st.enter_context(nc.psum_tensor("ps%d_%s" % (uid[0], name), list(shape), dt))

    ident_f = alloc(glob, "ident_f", [128, 128], F32)
    ident_b = alloc(glob, "ident_b", [128, 128], BF16)
    maskl = alloc(glob, "maskl", [128, 128], BF16)
    maskr = alloc(glob, "maskr", [128, 128], BF16)
    cact = alloc(glob, "cact", [128, KC, 2], F32)
    cact_rep = alloc(glob, "cact_rep", [128, KC, 2, 128], F32)
    ones_f = alloc(glob, "ones_f", [1, 128], F32)
    epsb = alloc(glob, "epsb", [128, 1], F32)
    modT = alloc(glob, "modT", [128, 48, 2], F32)

    P.dma("sp", ident_f[:], ident_d[:, :], writes=["ident_f"])
    P.dma("pool", ident_b[:], ident_d[:, :], writes=["ident_b"])
    P.dma("pool", maskl[:], maskl_d[:, :], writes=["maskl"])
    P.dma("pool", maskr[:], maskr_d[:, :], writes=["maskr"])
    P.dma("sp", cact[:], cvec[:, :, :], writes=["cact"])
    P.op("act", lambda e: e.activation(out=cact[:], in_=cact[:], func=AF.Silu), reads=["cact"], writes=["cact"])
    P.op("dve", lambda e: e.tensor_copy(cact_rep[:].rearrange("p k c m -> p (k c) m"),
                                        bc_last(cact[:].rearrange("p k c -> p (k c)"), 128)),
         reads=["cact"], writes=["cact_rep"])
    P.op("dve", lambda e: e.memset(ones_f[:], 1.0), writes=["ones_f"])
    P.op("dve", lambda e: e.memset(epsb[:], LN_EPS), writes=["epsb"])
    P.flush()
    if stop == "const":
        return nc

    for l in range(depth):
        last = (l == depth - 1)
        with ExitStack() as st:
            wa = [alloc(st, "wa%d" % i, [128, KC, D], F32) for i in range(2)]
            bada = alloc(st, "bada", [128, 48], F32)
            brow = [alloc(st, "brow%d" % i, [128, D], F32) for i in range(2)]
            grow = [alloc(st, "grow%d" % i, [128, D], F32) for i in range(2)]
            ps_mod = palloc(st, "ps_mod", [128, 96])
            ps_g = [palloc(st, "ps_g%d" % i, [128, 512]) for i in range(4)]
            P.dma("sp", bada[:], bada_p[l], writes=["bada"])
            for i in range(6):
                s = i % 2
                P.dma("sp", wa[s][:], w_ada[l][:, i * D:(i + 1) * D].rearrange("(k p) n -> p k n", p=128),
                      writes=[("wa", s)])

                def mm_mod(e, i=i, s=s):
                    ins = None
                    for j in range(KC):
                        for k in range(KC):
                            ins = e.matmul(ps_mod[:, (i * 8 + j) * 2:(i * 8 + j) * 2 + 2],
                                           lhsT=wa[s][:, k, j * 128:(j + 1) * 128], rhs=cact[:, k, :],
                                           start=(k == 0), stop=(k == KC - 1))
                    return ins
                P.op("pe", mm_mod, reads=[("wa", s), "cact"], writes=[("ps_mod", i)])
                if i >= 2:
                    gi = i - 2
                    P.dma("sp", brow[gi % 2][:], b_ada[l:l + 1, i * D:(i + 1) * D].to_broadcast([128, D]),
                          writes=[("brow", gi % 2)])
                    for c in range(2):
                        for h in range(2):
                            def mm_g(e, s=s, c=c, h=h):
                                ins = None
                                for k in range(KC):
                                    ins = e.matmul(ps_g[c * 2 + h][:, :], lhsT=cact_rep[:, k, c, :],
                                                   rhs=wa[s][:, k, h * 512:(h + 1) * 512],
                                                   start=(k == 0), stop=(k == KC - 1))
                                return ins
                            P.op("pe", mm_g, reads=[("wa", s), "cact_rep"], writes=[("ps_g", c * 2 + h)])
                            P.op("dve", lambda e, gi=gi, c=c, h=h: e.tensor_tensor(
                                grow[c][:, h * 512:(h + 1) * 512], ps_g[c * 2 + h][:, :],
                                brow[gi % 2][:, h * 512:(h + 1) * 512], ALU.add),
                                reads=[("ps_g", c * 2 + h), ("brow", gi % 2)], writes=[("grow", c, h)])
                        if i == 4:
                            P.op("dve", lambda e, c=c: e.tensor_scalar(grow[c][0:1, :], grow[c][0:1, :], 1.0, None, ALU.add),
                                 reads=[("grow", c, 0), ("grow", c, 1)], writes=[("grow", c, 0), ("grow", c, 1)])
                        P.dma("sp", modrows[l, c, gi:gi + 1, :], grow[c][0:1, :],
                              reads=[("grow", c, 0), ("grow", c, 1)], writes=[("modrows", c, gi)])
            P.op("dve", lambda e: e.tensor_tensor(modT[:], ps_mod[:].rearrange("p (j c) -> p j c", c=2),
                                                  bc_last(bada[:], 2), ALU.add),
                 reads=[("ps_mod", i) for i in range(6)] + ["bada"], writes=["modT"])
            P.op("dve", lambda e: e.tensor_scalar(modT[:, 8:16, :], modT[:, 8:16, :], 1.0, None, ALU.add),
                 reads=["modT"], writes=["modT"])
            P.op("dve", lambda e: e.tensor_scalar(modT[:, 32:40, :], modT[:, 32:40, :], 1.0, None, ALU.add),
                 reads=["modT"], writes=["modT"])
            P.flush()
        if stop == "p0":
            return nc

        with ExitStack() as st:
            NCOL = 18 * 128 + 640
            win = alloc(st, "win", [128, KC, NCOL], BF16)
            wout = alloc(st, "wout", [128, KC, D], BF16)
            kT = alloc(st, "kT", [128, 2, T], BF16)
            V = alloc(st, "V", [128, NT, 2, 65], BF16)
            convw_s = alloc(st, "convw_s", [128, 2, 3], F32)
            wsT = alloc(st, "wsT", [128, 4, 128], BF16)
            gmb = alloc(st, "gmb", [128, 4], F32)
            esink = alloc(st, "esink", [128, 8], F32)
            lng = alloc(st, "lng", [128, D], F32)
            lnb = alloc(st, "lnb", [128, D], F32)
            g1b = [alloc(st, "g1b%d" % c, [128, D], F32) for c in range(2)]
            for c in range(2):
                P.dma("sp", g1b[c][:], modrows[l, c, 0:1, :].to_broadcast([128, D]), writes=[("g1b", c)])
            R = 5
            xres = [alloc(st, "xres%d" % i, [128, D], F32) for i in range(5)]
            hT = [alloc(st, "hT%d" % i, [128, KC, 128], BF16) for i in range(2)]
            qrot = [alloc(st, "qrot%d" % i, [128, 4, 128], BF16) for i in range(R)]
            u = [alloc(st, "u%d" % i, [128, 2, 130], F32) for i in range(R)]
            cb = [alloc(st, "cb%d" % i, [128, 2, 128], F32) for i in range(R)]
            gut = [alloc(st, "gut%d" % i, [128, 256], F32) for i in range(R)]
            vln = [alloc(st, "vln%d" % i, [128, 256], BF16) for i in range(R)]
            cosb = [alloc(st, "cosb%d" % i, [128, 128], F32) for i in range(2)]
            sinb = [alloc(st, "sinb%d" % i, [128, 128], F32) for i in range(2)]
            t1 = alloc(st, "t1", [128, 4, 128], F32)
            t2 = alloc(st, "t2", [128, 4, 128], F32)
            cxs = alloc(st, "cxs", [128, 2, 128], F32)
            gvt = alloc(st, "gvt", [128, 256], F32)
            st6 = alloc(st, "st6", [128, 6], F32)
            mv = alloc(st, "mv", [128, 2], F32)
            rstd = alloc(st, "rstd", [128, 1], F32)
            nb_ = alloc(st, "nb_", [128, 1], F32)
            PT = [alloc(st, "PT%d" % i, [128, 5, 128], BF16) for i in range(4)]
            den = alloc(st, "den", [128, 4], F32)
            mixtok = [alloc(st, "mixtok%d" % i, [128, 768], BF16) for i in range(2)]
            mixT = [alloc(st, "mixT%d" % i, [128, KC, 128], BF16) for i in range(2)]
            ct = alloc(st, "ct", [128, 2, 128], F32)
            gtmp = alloc(st, "gtmp", [128, 256], F32)
            bufAs = [alloc(st, "bufA%d" % i, [128, D], F32) for i in range(2)]
            bufB = alloc(st, "bufB", [128, D], F32)
            st12 = alloc(st, "st12", [128, 12], F32)
            mv2 = alloc(st, "mv2", [128, 2], F32)
            rstd2 = alloc(st, "rstd2", [128, 1], F32)
            nb2 = alloc(st, "nb2", [128, 1], F32)
            B = [palloc(st, "B%d" % i, [128, 512]) for i in range(7)]
            Bbf = palloc(st, "Bbf", [128, 1024], BF16)
            Bo = B[4]

            wv = w_in[l].rearrange("(k p) n -> p k n", p=128)
            P.dma("pool", win[:, :, 0:512], wv[:, :, 0:512], writes=["win"])
            for kv in range(2):
                for dup in range(2):
                    c0 = 1024 + kv * 128 + dup * 64
                    P.dma("pool", win[:, :, c0:c0 + 64], wv[:, :, 512 + kv * 64:512 + (kv + 1) * 64], writes=["win"])
            P.dma("pool", win[:, :, 1536:2304], wv[:, :, 768:1536], writes=["win"])
            P.dma("pool", win[:, :, 2304:2432], wv[:, :, 640:768], writes=["win"])
            P.dma("pool", win[:, :, 2432:2688], wv[:, :, 1792:2048], writes=["win"])
            P.dma("pool", win[:, :, 2688:2944], wv[:, :, 1536:1792], writes=["win"])
            P.dma("pool", wout[:], w_out[l].rearrange("(k p) n -> p k n", p=128), writes=["wout"])
            for (src0, dst0, nblk) in ((0, 512, 16), (1024, 1280, 8)):
                for a in range(2):
                    def swp(e, src0=src0, dst0=dst0, nblk=nblk, a=a):
                        srcv = win[:, :, src0:src0 + nblk * 32].rearrange("p k (b a f) -> p k b a f", a=2, f=16)
                        dstv = win[:, :, dst0:dst0 + nblk * 32].rearrange("p k (b a f) -> p k b a f", a=2, f=16)
                        return e.tensor_copy(dstv[:, :, :, a, :], srcv[:, :, :, 1 - a, :])
                    P.op("dve", swp, reads=["win"], writes=["win"])
            P.dma("sp", convw_s[:], convw[l], writes=["convw_s"])
            P.dma("pool", wsT[:], gm_wsT[l].rearrange("g q p -> q g p"), writes=["wsT"])
            P.dma("sp", gmb[:], gm_bsp[l], writes=["gmb"])
            P.dma("sp", esink[:], sink[l:l + 1, :].to_broadcast([128, 8]), writes=["esink"])
            P.op("act", lambda e: e.activation(out=esink[:], in_=esink[:], func=AF.Exp), reads=["esink"], writes=["esink"])
            P.dma("sp", lng[:], ln1_g[l:l + 1, :].to_broadcast([128, D]), writes=["lng"])
            P.dma("sp", lnb[:], ln1_b[l:l + 1, :].to_broadcast([128, D]), writes=["lnb"])
            P.op("pool", lambda e: e.memset(V[:, :, :, 64:65], 1.0), writes=["Vones"])

            def seq_of(t):
                return (0, NB) if t < NB else (NB, NT)

            def inproj(t, kv_only=False):
                s = t % R
                xs = t % 5
                hs = t % 2
                cs = t % 2
                col = 0 if t < NB else 1
                lo, hi = seq_of(t)
                P.dma("sp", xres[xs][:], tile_src(l, t), writes=[("xres", xs)])
                P.dma("sp", cosb[cs][:], cos_t[t], writes=[("cosb", cs)])
                P.dma("sp", sinb[cs][:], sin_t[t], writes=[("sinb", cs)])
                for hb in range(2):
                    def tp(e, hb=hb):
                        ins = None
                        for i in range(4):
                            j = hb * 4 + i
                            ins = e.transpose(B[2 + hb][:, i * 128:(i + 1) * 128], xres[xs][:, j * 128:(j + 1) * 128], ident_f[:])
                        return ins
                    P.op("pe", tp, reads=[("xres", xs), "ident_f"], writes=[("B", 2 + hb)])

                    def ev(e, hb=hb):
                        ins = None
                        for i in range(4):
                            j = hb * 4 + i
                            ins = e.activation(out=hT[hs][:, j, :], in_=B[2 + hb][:, i * 128:(i + 1) * 128], func=AF.Identity,
                                               scale=modT[:, 8 + j, col:col + 1], bias=modT[:, j, col:col + 1])
                        return ins
                    P.op("act", ev, reads=[("B", 2 + hb), "modT"], writes=[("hT", hs, hb)])
                hres = [("hT", hs, 0), ("hT", hs, 1)]

                def fm_group(bank, off, chunks):
                    def f(e):
                        ins = None
                        for i, c in enumerate(chunks):
                            for k in range(KC):
                                ins = e.matmul(B[bank][:, off + i * 128:off + (i + 1) * 128],
                                               lhsT=win[:, k, c * 128:(c + 1) * 128], rhs=hT[hs][:, k, :],
                                               start=(k == 0), stop=(k == KC - 1))
                        return ins
                    return f

                if not kv_only:
                    P.op("pe", fm_group(2, 0, [0, 1, 2, 3]), reads=hres + ["win"], writes=[("B", 2)])
                    P.op("pe", fm_group(3, 0, [4, 5, 6, 7]), reads=hres + ["win"], writes=[("B", 3)])
                    P.op("dve", lambda e: e.tensor_tensor(t1[:], B[2][:, :].rearrange("p (c t) -> p c t", c=4),
                                                          bc_mid(cosb[cs][:], 4), ALU.mult),
                         reads=[("B", 2), ("cosb", cs)], writes=["t1"])
                    P.op("dve", lambda e: e.tensor_tensor(t2[:], B[3][:, :].rearrange("p (c t) -> p c t", c=4),
                                                          bc_mid(sinb[cs][:], 4), ALU.mult),
                         reads=[("B", 3), ("sinb", cs)], writes=["t2"])
                    P.op("pool", lambda e: e.tensor_tensor(qrot[s][:], t1[:], t2[:], ALU.add),
                         reads=["t1", "t2"], writes=[("qrot", s)])
                P.op("pe", fm_group(2, 0, [8, 9, 10, 11]), reads=hres + ["win"], writes=[("B", 2)])
                P.op("dve", lambda e: e.tensor_tensor(t1[:, 0:2, :], B[2][:, 0:256].rearrange("p (c t) -> p c t", c=2),
                                                      bc_mid(cosb[cs][:], 2), ALU.mult),
                     reads=[("B", 2), ("cosb", cs)], writes=["t1"])
                P.op("dve", lambda e: e.tensor_tensor(t2[:, 0:2, :], B[2][:, 256:512].rearrange("p (c t) -> p c t", c=2),
                                                      bc_mid(sinb[cs][:], 2), ALU.mult),
                     reads=[("B", 2), ("sinb", cs)], writes=["t2"])
                P.op("pool", lambda e: e.tensor_tensor(kT[:, :, t * 128:(t + 1) * 128], t1[:, 0:2, :], t2[:, 0:2, :], ALU.add),
                     reads=["t1", "t2"], writes=[("kT", t)])
                if not kv_only:
                    P.op("pe", fm_group(3, 0, [12, 13, 14, 15]), reads=hres + ["win"], writes=[("B", 3)])
                    P.op("pe", fm_group(2, 0, [16, 17]), reads=hres + ["win"], writes=[("B", 2)])
                    P.op("act", lambda e: e.activation(out=cb[s][:], in_=B[3][:, 0:256].rearrange("p (c t) -> p c t", c=2),
                                                       func=AF.Copy), reads=[("B", 3)], writes=[("cb", s)])
                    P.op("act", lambda e: e.activation(out=cxs[:], in_=B[2][:, 0:256].rearrange("p (c t) -> p c t", c=2),
                                                       func=AF.Copy), reads=[("B", 2)], writes=["cxs"])
                    P.op("dve", lambda e: e.tensor_tensor(u[s][:, :, 1:129], B[3][:, 256:512].rearrange("p (c t) -> p c t", c=2),
                                                          cxs[:], ALU.mult),
                         reads=[("B", 3), "cxs"], writes=[("u", s)])
                    if t > lo:
                        sp_ = (t - 1) % R
                        P.op("pool", lambda e: e.tensor_copy(u[sp_][:, :, 129:130], u[s][:, :, 1:2]),
                             reads=[("u", s)], writes=[("u", sp_)])
                    else:
                        P.op("pool", lambda e: e.memset(u[s][:, :, 0:1], 0.0), reads=[("u", s)], writes=[("u", s)])
                    if t < hi - 1:
                        sn_ = (t + 1) % R
                        P.op("pool", lambda e: e.tensor_copy(u[sn_][:, :, 0:1], u[s][:, :, 128:129]),
                             reads=[("u", s)], writes=[("u", sn_)])
                    else:
                        P.op("pool", lambda e: e.memset(u[s][:, :, 129:130], 0.0), reads=[("u", s)], writes=[("u", s)])

                def tm(e):
                    ins = None
                    for k in range(KC):
                        ins = e.matmul(B[3][:, 0:384], lhsT=hT[hs][:, k, :], rhs=win[:, k, 2304:2688],
                                       start=(k == 0), stop=(k == KC - 1))
                    return ins
                P.op("pe", tm, reads=hres + ["win"], writes=[("B", 3)])
                P.op("act", lambda e: e.activation(out=V[:, t, :, 0:64], in_=B[3][:, 0:128].rearrange("p (h d) -> p h d", h=2),
                                                   func=AF.Copy), reads=[("B", 3)], writes=[("V", t)])
                if not kv_only:
                    def tm2(e):
                        ins = None
                        for k in range(KC):
                            ins = e.matmul(B[2][:, 256:512], lhsT=hT[hs][:, k, :], rhs=win[:, k, 2688:2944],
                                           start=(k == 0), stop=(k == KC - 1))
                        return ins
                    P.op("pe", tm2, reads=hres + ["win"], writes=[("B", 2)])
                    P.op("act", lambda e: e.activation(out=gvt[:], in_=B[3][:, 128:384], func=AF.Gelu_apprx_tanh),
                         reads=[("B", 3)], writes=["gvt"])
                    P.op("act", lambda e: e.activation(out=gut[s][:], in_=B[2][:, 256:512], func=AF.Gelu_apprx_tanh),
                         reads=[("B", 2)], writes=[("gut", s)])
                    P.op("dve", lambda e: e.bn_stats(st6[:], gvt[:]), reads=["gvt"], writes=["st6"])
                    P.op("dve", lambda e: e.bn_aggr(mv[:], st6[:]), reads=["st6"], writes=["mv"])
                    P.op("act", lambda e: e.activation(out=rstd[:], in_=mv[:, 1:2], func=AF.Ln, bias=epsb[:, 0:1]),
                         reads=["mv", "epsb"], writes=["rstd"])
                    P.op("act", lambda e: e.activation(out=rstd[:], in_=rstd[:], func=AF.Exp, scale=-0.5), reads=["rstd"], writes=["rstd"])
                    P.op("dve", lambda e: e.scalar_tensor_tensor(nb_[:], mv[:, 0:1], -1.0, rstd[:], ALU.mult, ALU.mult),
                         reads=["mv", "rstd"], writes=["nb_"])
                    P.op("act", lambda e: e.activation(out=vln[s][:], in_=gvt[:], func=AF.Identity,
                                                       scale=rstd[:, 0:1], bias=nb_[:, 0:1]),
                         reads=["gvt", "rstd", "nb_"], writes=[("vln", s)])

            def attn(t):
                s = t % R
                xs = t % 5
                ms = t % 2
                col = 0 if t < NB else 1
                lo, hi = seq_of(t)
                if t < NB:
                    kcs = [(NB, None), (NB + 1, None)]
                    if t > 0:
                        kcs.append((t - 1, maskl))
                    kcs.append((t, None))
                    if t < NB - 1:
                        kcs.append((t + 1, maskr))
                else:
                    kcs = [(NB, None), (NB + 1, None)]
                nk = len(kcs)
                for h in range(8):
                    qc, po, kv, pslot = h // 2, (h % 2) * 64, h // 4, h % 4

                    if h % 2 == 0:
                        sbank, s5, wr5 = B[6], B[4][:, 384:512], ("B", 4)
                        wr = [("B", 6)] + ([wr5] if nk > 4 else [])
                    else:
                        sbank, s5, wr5 = B[5], B[1][:, 384:512], ("B", 1)
                        wr = [("B", 5)] + ([wr5] if nk > 4 else [])

                    def sc(e, qc=qc, po=po, kv=kv, sbank=sbank, s5=s5):
                        ins = None
                        for i, (c, msk) in enumerate(kcs):
                            dst = sbank[:, i * 128:(i + 1) * 128] if i < 4 else s5
                            ins = e.matmul(dst, lhsT=kT[po:po + 64, kv, c * 128:(c + 1) * 128],
                                           rhs=qrot[s][po:po + 64, qc, :], start=True, stop=(msk is None))
                            if msk is not None:
                                ins = e.matmul(dst, lhsT=ident_b[:], rhs=msk[:], start=False, stop=True)
                        return ins
                    P.op("pe", sc, reads=[("kT", c) for c, _ in kcs] + [("qrot", s), "ident_b", "maskl", "maskr"], writes=wr)

                    def ex(e, pslot=pslot, sbank=sbank, s5=s5):
                        n1 = min(nk, 4)
                        ins = e.activation(out=PT[pslot][:, 0:n1, :], in_=sbank[:, 0:n1 * 128].rearrange("p (c t) -> p c t", c=n1),
                                           func=AF.Exp, scale=0.125)
                        if nk > 4:
                            ins = e.activation(out=PT[pslot][:, 4, :], in_=s5, func=AF.Exp, scale=0.125)
                        return ins
                    P.op("act", ex, reads=wr, writes=[("PT", pslot)])

                    def pv(e, kv=kv, pslot=pslot):
                        ins = None
                        for i, (c, msk) in enumerate(kcs):
                            ins = e.matmul(Bo[:, pslot * 65:(pslot + 1) * 65],
                                           lhsT=PT[pslot][:, i, :], rhs=V[:, c, kv, :],
                                           start=(i == 0), stop=(i == nk - 1))
                        return ins
                    P.op("pe", pv, reads=[("PT", pslot), "Vones"] + [("V", c) for c, _ in kcs], writes=[("B", 4)])
                    if pslot == 3:
                        hg = h // 4
                        bov = Bo[:, 0:260].rearrange("p (h d) -> p h d", d=65)
                        P.op("dve", lambda e, hg=hg, bov=bov: e.tensor_tensor(den[:], bov[:, :, 64], esink[:, hg * 4:(hg + 1) * 4], ALU.add),
                             reads=[("B", 4), "esink"], writes=["den"])
                        P.op("dve", lambda e: e.reciprocal(den[:], den[:]), reads=["den"], writes=["den"])
                        P.op("dve", lambda e, hg=hg, bov=bov: e.tensor_tensor(
                            mixtok[ms][:, hg * 256:(hg + 1) * 256].rearrange("p (h d) -> p h d", d=64),
                            bov[:, :, 0:64], bc_last(den[:], 64), ALU.mult),
                            reads=[("B", 4), "den"], writes=[("mixtok", ms, hg)])

            def post(t):
                s = t % R
                ms = t % 2
                col = 0 if t < NB else 1
                for j in range(2):
                    P.op("pool", lambda e, j=j: e.tensor_scalar(ct[:, j, :], u[s][:, j, 0:128], convw_s[:, j, 0:1], None, ALU.mult),
                         reads=[("u", s), "convw_s"], writes=[("ct", j)])
                    P.op("dve", lambda e, j=j: e.scalar_tensor_tensor(ct[:, j, :], u[s][:, j, 1:129], convw_s[:, j, 1:2], ct[:, j, :], ALU.mult, ALU.add),
                         reads=[("u", s), "convw_s", ("ct", j)], writes=[("ct", j)])
                    P.op("dve", lambda e, j=j: e.scalar_tensor_tensor(ct[:, j, :], u[s][:, j, 2:130], convw_s[:, j, 2:3], ct[:, j, :], ALU.mult, ALU.add),
                         reads=[("u", s), "convw_s", ("ct", j)], writes=[("ct", j)])
                    P.op("pool", lambda e, j=j: e.tensor_tensor(mixT[ms][:, 4 + j, :], cb[s][:, j, :], ct[:, j, :], ALU.mult),
                         reads=[("cb", s), ("ct", j)], writes=[("mixT", ms, 4 + j)])
                def gm(e):
                    ins = None
                    for g in range(4):
                        ins = e.matmul(B[1][:, g * 64:(g + 1) * 64], lhsT=wsT[:, g, :], rhs=vln[s][:, g * 64:(g + 1) * 64],
                                       start=True, stop=True)
                    return ins
                P.op("pe", gm, reads=[("vln", s), "wsT"], writes=[("B", 1)])
                P.op("dve", lambda e: e.tensor_tensor(gtmp[:].rearrange("p (g c) -> p g c", g=4),
                                                      B[1][:, 0:256].rearrange("p (g c) -> p g c", g=4),
                                                      bc_last(gmb[:], 64), ALU.add),
                     reads=[("B", 1), "gmb"], writes=["gtmp"])
                P.op("dve", lambda e: e.tensor_tensor(mixtok[ms][:, 512:768], gtmp[:], gut[s][:], ALU.mult),
                     reads=["gtmp", ("gut", s)], writes=[("mixtok", ms, 2)])
                def tpm(e):
                    ins = None
                    for i in range(6):
                        ins = e.transpose(Bbf[:, i * 128:(i + 1) * 128], mixtok[ms][:, i * 128:(i + 1) * 128], ident_b[:])
                    return ins
                P.op("pe", tpm, reads=[("mixtok", ms, i) for i in range(3)] + ["ident_b"], writes=["Bbf"])
                P.op("act", lambda e: e.activation(out=mixT[ms][:, 0:4, :], in_=Bbf[:, 0:512].rearrange("p (c t) -> p c t", c=4), func=AF.Copy),
                     reads=["Bbf"], writes=[("mixT", ms, i) for i in range(4)])
                P.op("act", lambda e: e.activation(out=mixT[ms][:, 6:8, :], in_=Bbf[:, 512:768].rearrange("p (c t) -> p c t", c=2), func=AF.Copy),
                     reads=["Bbf"], writes=[("mixT", ms, 6), ("mixT", ms, 7)])
                for hf in range(2):
                    def op_(e, hf=hf):
                        ins = None
                        for k in range(KC):
                            ins = e.matmul(B[0][:, :], lhsT=mixT[ms][:, k, :], rhs=wout[:, k, hf * 512:(hf + 1) * 512],
                                           start=(k == 0), stop=(k == KC - 1))
                        return ins
                    P.op("pe", op_, reads=[("mixT", ms, i) for i in range(8)] + ["wout"], writes=[("B", 0)])
                    P.op("dve", lambda e, hf=hf: e.tensor_tensor(bufAs[t % 2][:, hf * 512:(hf + 1) * 512], B[0][:, :],
                                                                g1b[col][:, hf * 512:(hf + 1) * 512], ALU.mult),
                         reads=[("B", 0), ("g1b", col)], writes=[("bufA", t % 2, hf)])

            def mixB(t):
                xs = t % 5
                layer_norm_tail(xres[xs], ("xres", xs), lng, lnb, x1buf[t * 128:(t + 1) * 128, :], ("x1buf", t), t % 2)

            def layer_norm_tail(xt, xres_name, g_t, b_t, dst, dst_name, ab):
                bufA = bufAs[ab]
                P.op("dve", lambda e: e.scalar_tensor_tensor(bufA[:], xt[:], ALPHA, bufA[:], ALU.mult, ALU.add),
                     reads=[xres_name, ("bufA", ab, 0), ("bufA", ab, 1)], writes=[("bufA", ab, 0), ("bufA", ab, 1)])
                P.op("dve", lambda e: e.bn_stats(st12[:, 0:6], bufA[:, 0:512]), reads=[("bufA", ab, 0)], writes=[("st12", 0)])
                P.op("dve", lambda e: e.bn_stats(st12[:, 6:12], bufA[:, 512:1024]), reads=[("bufA", ab, 1)], writes=[("st12", 1)])
                P.op("dve", lambda e: e.bn_aggr(mv2[:], st12[:]), reads=[("st12", 0), ("st12", 1)], writes=["mv2"])
                P.op("act", lambda e: e.activation(out=rstd2[:], in_=mv2[:, 1:2], func=AF.Ln, bias=epsb[:, 0:1]),
                     reads=["mv2", "epsb"], writes=["rstd2"])
                P.op("act", lambda e: e.activation(out=rstd2[:], in_=rstd2[:], func=AF.Exp, scale=-0.5), reads=["rstd2"], writes=["rstd2"])
                P.op("dve", lambda e: e.scalar_tensor_tensor(nb2[:], mv2[:, 0:1], -1.0, rstd2[:], ALU.mult, ALU.mult),
                     reads=["mv2", "rstd2"], writes=["nb2"])
                P.op("act", lambda e: e.activation(out=bufB[:], in_=bufA[:], func=AF.Identity, scale=rstd2[:, 0:1], bias=nb2[:, 0:1]),
                     reads=[("bufA", ab, 0), ("bufA", ab, 1), "rstd2", "nb2"], writes=["bufB"])
                P.op("pool", lambda e: e.tensor_tensor(bufB[:], bufB[:], g_t[:], ALU.mult), reads=["bufB", "lng"], writes=["bufB"])
                P.op("pool", lambda e: e.tensor_tensor(bufB[:], bufB[:], b_t[:], ALU.add), reads=["bufB", "lnb"], writes=["bufB"])
                P.dma("sp", dst, bufB[:], reads=["bufB"], writes=[dst_name])

            for t in range(NB, NT):
                inproj(t, kv_only=last)
            if not last:
                for t in range(NB, NT):
                    attn(t)
                    post(t)
                    mixB(t)
            inproj(0)
            if NB > 1:
                inproj(1)
            for n in range(NB + 2):
                ls = []
                if n < NB:
                    ls.append(P.record(attn, n))
                if 0 <= n - 1 < NB:
                    ls.append(P.record(post, n - 1))
                if 0 <= n - 2 < NB:
                    ls.append(P.record(mixB, n - 2))
                if n + 2 < NB:
                    ls.append(P.record(inproj, n + 2))
                P.play(*ls)
            P.flush()
        if stop == "p1":
            return nc

        tiles = list(range(NB)) if last else list(range(NT))
        nt2 = len(tiles)
        NBLK = (2 * nt2 * 128) // BS + NEXP
        AX = mybir.AxisListType.X
        w1v = w1.rearrange("l e p (a k) n -> (l e p a) (k n)", a=4)
        w3v = w3.rearrange("l e p (a k) n -> (l e p a) (k n)", a=4)
        w2v = w2.rearrange("l e p (a k) n -> (l e p a) (k n)", a=4)
        with ExitStack() as st:
            wr_s = alloc(st, "wr_s", [128, KC, 36], F32)
            wr_m = [alloc(st, "wr_m%d" % c, [128, KC, 36], F32) for c in range(2)]
            sh2rep = [alloc(st, "sh2rep%d" % c, [128, KC, 128], F32) for c in range(2)]
            br_row = alloc(st, "br_row", [1, 36], F32)
            lng = alloc(st, "lng2", [128, D], F32)
            lnb = alloc(st, "lnb2", [128, D], F32)
            scb = [alloc(st, "scb%d" % c, [128, D], F32) for c in range(2)]
            shb = [alloc(st, "shb%d" % c, [128, D], F32) for c in range(2)]
            g2b = [alloc(st, "g2b%d" % c, [128, D], F32) for c in range(2)]
            lt_b = alloc(st, "lt_b", [128, 128], BF16)
            ones_b = alloc(st, "ones_b", [128, 128], BF16)
            jvec = alloc(st, "jvec", [128, NBLK_MAX], F32)
            base1 = alloc(st, "base1", [128, 8], F32)
            base2 = alloc(st, "base2", [128, 4], F32)
            x1t = [alloc(st, "x1t%d" % i, [128, D], F32) for i in range(2)]
            x1T = alloc(st, "x1T", [128, KC, 128], F32)
            h2r = [alloc(st, "h2r%d" % i, [128, D], BF16) for i in range(2)]
            lg = alloc(st, "lg", [128, 36], F32)
            sm = alloc(st, "sm", [128, 16], F32)
            goh = alloc(st, "goh", [128, 4], F32)
            gex = alloc(st, "gex", [128, 4], F32)
            esel = alloc(st, "esel", [128, 8], F32)
            esel2 = alloc(st, "esel2", [128, 8], F32)
            eq1 = alloc(st, "eq1", [128, 8], F32)
            eq2 = alloc(st, "eq2", [128, 8], F32)
            oh0 = alloc(st, "oh0", [128, nt2, 32], F32)
            oh1 = alloc(st, "oh1", [128, nt2, 32], F32)
            cntb = alloc(st, "cntb", [128, nt2, 32], BF16)
            gsc = alloc(st, "gsc", [128, nt2, 2], F32)
            rank_s = alloc(st, "rank_s", [128, nt2, 32], F32)
            dtmp = alloc(st, "dtmp", [128, nt2, 32], F32)
            tot = alloc(st, "tot", [128, 32], F32)
            nblk = alloc(st, "nblk", [128, 32], F32)
            sc_a = alloc(st, "sc_a", [128, 32], F32)
            sc_b = alloc(st, "sc_b", [128, 32], F32)
            pstart = alloc(st, "pstart", [128, 32], F32)
            destf = alloc(st, "destf", [128, 2, nt2], F32)
            idx = alloc(st, "idx", [128, 2, nt2], mybir.dt.int32)
            bef = alloc(st, "bef", [128, NBLK_MAX], F32)
            be1 = alloc(st, "be1", [128, NBLK_MAX], F32)
            wif1 = alloc(st, "wif1", [128, NBLK_MAX, 4], F32)
            wi1 = alloc(st, "wi1", [128, NBLK_MAX, 4], mybir.dt.int32)
            w1s = [alloc(st, "w1s%d" % i, [128, KC, DEXP], BF16) for i in range(2)]
            w3s = [alloc(st, "w3s%d" % i, [128, KC, DEXP], BF16) for i in range(2)]
            w2s = [alloc(st, "w2s%d" % i, [128, 4, D], BF16) for i in range(2)]
            xsb = [alloc(st, "xsb%d" % i, [128, 4, D], BF16) for i in range(2)]
            hTb = [alloc(st, "hTb%d" % i, [128, KC, 512], BF16) for i in range(2)]
            sg = [alloc(st, "sg%d" % i, [128, 512], F32) for i in range(2)]
            actT = [alloc(st, "actT%d" % i, [128, 4, 512], BF16) for i in range(2)]
            ysb = [alloc(st, "ysb%d" % i, [128, D], F32) for i in range(2)]
            yg = ysb
            bufA = alloc(st, "bufA2", [128, D], F32)
            bufB = alloc(st, "bufB2", [128, D], F32)
            st12 = alloc(st, "st12b", [128, 12], F32)
            mv2 = alloc(st, "mv2b", [128, 2], F32)
            rstd2 = alloc(st, "rstd2b", [128, 1], F32)
            nb2 = alloc(st, "nb2b", [128, 1], F32)
            B = [None, None] + [palloc(st, "M%d" % i, [128, 512]) for i in range(2, 8)]
            TB = [palloc(st, "TB%d" % i, [128, 1024], BF16) for i in range(2)]

            P.dma("sp", wr_s[:], w_r[l].rearrange("(k p) n -> p k n", p=128), writes=["wr_s"])
            P.dma("sp", br_row[:], b_r[l:l + 1, :], writes=["br_row"])
            P.dma("sp", lng[:], ln2_g[l:l + 1, :].to_broadcast([128, D]), writes=["lng"])
            P.dma("sp", lnb[:], ln2_b[l:l + 1, :].to_broadcast([128, D]), writes=["lnb"])
            for c in range(2):
                P.dma("sp", shb[c][:], modrows[l, c, 1:2, :].to_broadcast([128, D]), writes=[("shb", c)])
                P.dma("sp", scb[c][:], modrows[l, c, 2:3, :].to_broadcast([128, D]), writes=[("scb", c)])
                P.dma("sp", g2b[c][:], modrows[l, c, 3:4, :].to_broadcast([128, D]), writes=[("g2b", c)])
            P.dma("pool", lt_b[:], lt_d[:, :], writes=["lt_b"])
            P.dma("sp", jvec[:], jvec_d[:, :], writes=["jvec"])
            P.dma("sp", base1[:], base1_d[:, :], writes=["base1"])
            P.dma("sp", base2[:], base2_d[:, :], writes=["base2"])
            P.op("pool", lambda e: e.memset(ones_b[:], 1.0), writes=["ones_b"])
            P.op("pool", lambda e: e.memset(xsb[1][:], 0.0), writes=[("xsb", 1)])
            zres = []
            for i in range(NBLK):
                P.dma("pool", xs_dram[i * BS:(i + 1) * BS, :].rearrange("(a p) d -> p a d", p=128), xsb[1][:],
                      reads=[("xsb", 1)], writes=[("xsz", i)])
                zres.append(("xsz", i))
            for c in range(2):
                P.op("dve", lambda e, c=c: e.tensor_tensor(wr_m[c][:], wr_s[:], bc_last(modT[:, 32:40, c], 36), ALU.mult),
                     reads=["wr_s", "modT"], writes=[("wr_m", c)])
                P.op("dve", lambda e, c=c: e.tensor_copy(sh2rep[c][:], bc_last(modT[:, 24:32, c], 128)),
                     reads=["modT"], writes=[("sh2rep", c)])

            st12s = [st12, alloc(st, "st12c", [128, 12], F32)]
            mv2s = [mv2, alloc(st, "mv2c", [128, 2], F32)]
            rstd2s = [rstd2, alloc(st, "rstd2c", [128, 1], F32)]
            nb2s = [nb2, alloc(st, "nb2c", [128, 1], F32)]

            def ln_tail2(xt, xname, dst, dname, bA, bB, se):
                nA, nB = ("bufA", se), ("bufB", se)
                s12, m2, r2, n2 = st12s[se], mv2s[se], rstd2s[se], nb2s[se]
                P.op("dve", lambda e: e.scalar_tensor_tensor(bA[:], xt[:], ALPHA, bA[:], ALU.mult, ALU.add),
                     reads=[xname, nA], writes=[nA])
                P.op("dve", lambda e: e.bn_stats(s12[:, 0:6], bA[:, 0:512]), reads=[nA], writes=[("st12", se, 0)])
                P.op("dve", lambda e: e.bn_stats(s12[:, 6:12], bA[:, 512:1024]), reads=[nA], writes=[("st12", se, 1)])
                P.op("dve", lambda e: e.bn_aggr(m2[:], s12[:]), reads=[("st12", se, 0), ("st12", se, 1)], writes=[("mv2", se)])
                P.op("act", lambda e: e.activation(out=r2[:], in_=m2[:, 1:2], func=AF.Sqrt, bias=LN_EPS),
                     reads=[("mv2", se)], writes=[("rstd2", se)])
                P.op("dve", lambda e: e.reciprocal(r2[:], r2[:]), reads=[("rstd2", se)], writes=[("rstd2", se)])
                P.op("dve", lambda e: e.scalar_tensor_tensor(n2[:], m2[:, 0:1], -1.0, r2[:], ALU.mult, ALU.mult),
                     reads=[("mv2", se), ("rstd2", se)], writes=[("nb2", se)])
                P.op("act", lambda e: e.activation(out=bB[:], in_=bA[:], func=AF.Identity, scale=r2[:, 0:1], bias=n2[:, 0:1]),
                     reads=[nA, ("rstd2", se), ("nb2", se)], writes=[nB])
                P.op("pool", lambda e: e.tensor_tensor(bB[:], bB[:], lng[:], ALU.mult), reads=[nB, "lng"], writes=[nB])
                P.op("pool", lambda e: e.tensor_tensor(bB[:], bB[:], lnb[:], ALU.add), reads=[nB, "lnb"], writes=[nB])
                P.dma("sp", dst, bB[:], reads=[nB], writes=[dname])

            def v1024(tn, nm):
                if nt2 * 32 >= 1024:
                    return tn[:].rearrange("p a b -> p (a b)")[:, 0:1024]
                return alloc(st, nm, [128, D], F32)[:]
            rank_v, dtmp_v, oh0_v = v1024(rank_s, "rank_v"), v1024(dtmp, "dtmp_v"), v1024(oh0, "oh0_v")
            x1Ts = [x1T, rank_v.rearrange("p (k t) -> p k t", k=KC)]
            bufAs = [bufA, dtmp_v]
            lgs = [lg, alloc(st, "lg_b", [128, 36], F32)]
            sms = [sm, alloc(st, "sm_b", [128, 16], F32)]
            gohs = [goh, alloc(st, "goh_b", [128, 4], F32)]
            gexs = [gex, alloc(st, "gex_b", [128, 4], F32)]
            esels = [esel, alloc(st, "esel_b", [128, 8], F32)]
            esel2s = [esel2, alloc(st, "esel2_b", [128, 8], F32)]
            eq1s = [eq1, alloc(st, "eq1_b", [128, 8], F32)]
            eq2s = [eq2, alloc(st, "eq2_b", [128, 8], F32)]

            def stageA(li):
                t = tiles[li]
                col = 0 if t < NB else 1
                xs = li % 2
                se = li % 2
                tpb = 6 if se == 0 else 4
                rtb = 2 if se == 0 else 3
                P.dma("sp", x1t[xs][:], x1buf[t * 128:(t + 1) * 128, :], writes=[("x1t", xs)])
                P.op("dve", lambda e, xs=xs, col=col: e.tensor_tensor(bufAs[se][:], x1t[xs][:], scb[col][:], ALU.mult),
                     reads=[("x1t", xs), ("scb", col)], writes=[("bufA", se)])
                P.op("pool", lambda e, xs=xs, col=col: e.tensor_tensor(h2r[xs][:], bufAs[se][:], shb[col][:], ALU.add),
                     reads=[("bufA", se), ("shb", col)], writes=[("h2r", xs)])
                P.dma("sp", h2buf[li * 128:(li + 1) * 128, :], h2r[xs][:], reads=[("h2r", xs)], writes=[("h2buf", li)])
                for hb in range(2):
                    def tp(e, hb=hb, xs=xs):
                        ins = None
                        for i in range(4):
                            j = hb * 4 + i
                            ins = e.transpose(B[tpb + hb][:, i * 128:(i + 1) * 128], x1t[xs][:, j * 128:(j + 1) * 128], ident_f[:])
                        return ins
                    P.op("pe", tp, reads=[("x1t", xs), "ident_f"], writes=[("B", tpb + hb)])

                    def cpx(e, hb=hb):
                        ins = None
                        for i in range(4):
                            ins = e.activation(out=x1Ts[se][:, hb * 4 + i, :], in_=B[tpb + hb][:, i * 128:(i + 1) * 128], func=AF.Copy)
                        return ins
                    P.op("act", cpx, reads=[("B", tpb + hb)], writes=[("x1T", se, hb)])

                def rt(e, col=col):
                    ins = None
                    for k in range(KC):
                        ins = e.matmul(B[rtb][:, 0:36], lhsT=x1Ts[se][:, k, :], rhs=wr_m[col][:, k, :], start=(k == 0), stop=False)
                    for k in range(KC):
                        ins = e.matmul(B[rtb][:, 0:36], lhsT=sh2rep[col][:, k, :], rhs=wr_s[:, k, :], start=False, stop=False)
                    ins = e.matmul(B[rtb][:, 0:36], lhsT=ones_f[0:1, :], rhs=br_row[0:1, :], start=False, stop=True)
                    return ins
                P.op("pe", rt, reads=[("x1T", se, 0), ("x1T", se, 1), ("wr_m", col), ("sh2rep", col), "wr_s", "br_row", "ones_f"], writes=[("B", rtb)])
                P.op("act", lambda e: e.activation(out=lgs[se][:], in_=B[rtb][:, 0:36], func=AF.Copy), reads=[("B", rtb)], writes=[("lg", se)])
                P.op("dve", lambda e: e.reduce_max(sms[se][:, 0:1], lgs[se][:, 0:4], AX), reads=[("lg", se)], writes=[("sm", se)])
                P.op("dve", lambda e: e.tensor_scalar(sms[se][:, 1:2], sms[se][:, 0:1], -1.0, None, ALU.mult), reads=[("sm", se)], writes=[("sm", se)])
                P.op("act", lambda e: e.activation(out=gexs[se][:], in_=lgs[se][:, 0:4], func=AF.Exp, bias=sms[se][:, 1:2], accum_out=sms[se][:, 2:3]),
                     reads=[("lg", se), ("sm", se)], writes=[("gex", se), ("sm", se)])
                P.op("dve", lambda e: e.reciprocal(sms[se][:, 3:4], sms[se][:, 2:3]), reads=[("sm", se)], writes=[("sm", se)])
                P.op("dve", lambda e: e.tensor_scalar(gohs[se][:], lgs[se][:, 0:4], sms[se][:, 0:1], None, ALU.is_equal), reads=[("lg", se), ("sm", se)], writes=[("goh", se)])
                P.op("dve", lambda e: e.tensor_scalar(esels[se][:], lgs[se][:, 4:12], gohs[se][:, 0:1], None, ALU.mult), reads=[("lg", se), ("goh", se)], writes=[("esel", se)])
                for g in range(1, 4):
                    P.op("dve", lambda e, g=g: e.scalar_tensor_tensor(esels[se][:], lgs[se][:, 4 + g * 8:12 + g * 8], gohs[se][:, g:g + 1], esels[se][:], ALU.mult, ALU.add),
                         reads=[("lg", se), ("goh", se), ("esel", se)], writes=[("esel", se)])
                P.op("dve", lambda e: e.reduce_max(sms[se][:, 4:5], esels[se][:], AX), reads=[("esel", se)], writes=[("sm", se)])
                P.op("dve", lambda e: e.tensor_scalar(eq1s[se][:], esels[se][:], sms[se][:, 4:5], None, ALU.is_equal), reads=[("esel", se), ("sm", se)], writes=[("eq1", se)])
                P.op("dve", lambda e: e.scalar_tensor_tensor(esel2s[se][:], eq1s[se][:], -1e30, esels[se][:], ALU.mult, ALU.add), reads=[("eq1", se), ("esel", se)], writes=[("esel2", se)])
                P.op("dve", lambda e: e.reduce_max(sms[se][:, 5:6], esel2s[se][:], AX), reads=[("esel2", se)], writes=[("sm", se)])
                P.op("dve", lambda e: e.tensor_scalar(eq2s[se][:], esel2s[se][:], sms[se][:, 5:6], None, ALU.is_equal), reads=[("esel2", se), ("sm", se)], writes=[("eq2", se)])
                P.op("dve", lambda e: e.tensor_tensor(sms[se][:, 6:7], sms[se][:, 4:5], sms[se][:, 5:6], ALU.subtract), reads=[("sm", se)], writes=[("sm", se)])
                P.op("act", lambda e: e.activation(out=sms[se][:, 7:8], in_=sms[se][:, 6:7], func=AF.Sigmoid), reads=[("sm", se)], writes=[("sm", se)])
                P.op("act", lambda e: e.activation(out=sms[se][:, 8:9], in_=sms[se][:, 6:7], func=AF.Sigmoid, scale=-1.0), reads=[("sm", se)], writes=[("sm", se)])
                P.op("dve", lambda e, li=li: e.tensor_scalar(gsc[:, li, :], sms[se][:, 7:9], sms[se][:, 3:4], None, ALU.mult), reads=[("sm", se)], writes=[("gsc", li)])
                P.op("dve", lambda e, li=li: e.tensor_tensor(oh0[:, li, :].rearrange("p (g x) -> p g x", g=4), bc_last(gohs[se][:], 8), bc_mid(eq1s[se][:], 4), ALU.mult),
                     reads=[("goh", se), ("eq1", se)], writes=[("oh0", li)])
                P.op("dve", lambda e, li=li: e.tensor_tensor(oh1[:, li, :].rearrange("p (g x) -> p g x", g=4), bc_last(gohs[se][:], 8), bc_mid(eq2s[se][:], 4), ALU.mult),
                     reads=[("goh", se), ("eq2", se)], writes=[("oh1", li)])
                P.op("pool", lambda e, li=li: e.tensor_tensor(cntb[:, li, :], oh0[:, li, :], oh1[:, li, :], ALU.add),
                     reads=[("oh0", li), ("oh1", li)], writes=[("cntb", li)])


            for li0 in range(0, nt2, 2):
                P.play(P.record(stageA, li0), P.record(stageA, li0 + 1) if li0 + 1 < nt2 else [])
            if stop == "p2a":
                P.flush()
                return nc
            call = [("cntb", i) for i in range(nt2)]
            nbank = (nt2 + 15) // 16
            for li in range(nt2):
                bk, off = 3 + li // 16, (li % 16) * 32

                def rk(e, li=li, bk=bk, off=off):
                    ins = None
                    for tp_ in range(li):
                        ins = e.matmul(B[bk][:, off:off + 32], lhsT=ones_b[:], rhs=cntb[:, tp_, :], start=(tp_ == 0), stop=False)
                    ins = e.matmul(B[bk][:, off:off + 32], lhsT=lt_b[:], rhs=cntb[:, li, :], start=(li == 0), stop=True)
                    return ins
                P.op("pe", rk, reads=call + ["ones_b", "lt_b"], writes=[("RK", li)])
            for bk in range(nbank):
                n_here = min(16, nt2 - bk * 16)
                P.op("act", lambda e, bk=bk, n_here=n_here: e.activation(
                    out=rank_s[:, bk * 16:bk * 16 + n_here, :], in_=B[3 + bk][:, 0:n_here * 32].rearrange("p (t x) -> p t x", x=32), func=AF.Copy),
                    reads=[("RK", i) for i in range(bk * 16, bk * 16 + n_here)], writes=[("rank_s", bk)])

            def tt(e):
                ins = None
                for tp_ in range(nt2):
                    ins = e.matmul(B[2][:, 64:96], lhsT=ones_b[:], rhs=cntb[:, tp_, :], start=(tp_ == 0), stop=(tp_ == nt2 - 1))
                return ins
            P.op("pe", tt, reads=call + ["ones_b"], writes=[("B", 2)])
            P.op("act", lambda e: e.activation(out=tot[:], in_=B[2][:, 64:96], func=AF.Copy), reads=[("B", 2)], writes=["tot"])
            P.op("dve", lambda e: e.tensor_scalar(nblk[:], tot[:], 0.0, None, ALU.is_gt), reads=["tot"], writes=["nblk"])
            for m in range(1, (2 * nt2 * 128) // BS + 1):
                P.op("dve", lambda e, m=m: e.scalar_tensor_tensor(nblk[:], tot[:], float(m * BS), nblk[:], ALU.is_gt, ALU.add),
                     reads=["tot", "nblk"], writes=["nblk"])
            P.op("dve", lambda e: e.tensor_copy(sc_a[:], nblk[:]), reads=["nblk"], writes=["sc_a"])
            cur, oth, cn, on = sc_a, sc_b, "sc_a", "sc_b"
            for dd in (1, 2, 4, 8, 16):
                P.op("dve", lambda e, cur=cur, oth=oth, dd=dd: e.tensor_copy(oth[:, 0:dd], cur[:, 0:dd]), reads=[cn], writes=[on])
                P.op("dve", lambda e, cur=cur, oth=oth, dd=dd: e.tensor_tensor(oth[:, dd:32], cur[:, dd:32], cur[:, 0:32 - dd], ALU.add),
                     reads=[cn, on], writes=[on])
                cur, oth, cn, on = oth, cur, on, cn
            pend, pendn = cur, cn
            P.op("dve", lambda e: e.tensor_tensor(pstart[:], pend[:], nblk[:], ALU.subtract), reads=[pendn, "nblk"], writes=["pstart"])
            P.op("dve", lambda e: e.tensor_scalar(pstart[:], pstart[:], float(BS), None, ALU.mult), reads=["pstart"], writes=["pstart"])
            rk_all = [("rank_s", i) for i in range(nbank)]
            P.op("dve", lambda e: e.tensor_tensor(rank_s[:], rank_s[:], bc_mid(pstart[:], nt2), ALU.add),
                 reads=rk_all + ["pstart"], writes=rk_all)
            for k, oh in enumerate((oh0, oh1)):
                P.op("dve", lambda e, oh=oh: e.tensor_tensor(dtmp[:], oh[:], rank_s[:], ALU.mult),
                     reads=rk_all + [("oh%d" % k, i) for i in range(nt2)], writes=["dtmp"])
                P.op("dve", lambda e, k=k: e.reduce_sum(destf[:, k, :], dtmp[:], AX), reads=["dtmp"], writes=[("destf", k)])
            P.op("dve", lambda e: e.tensor_copy(idx[:], destf[:]), reads=[("destf", 0), ("destf", 1)], writes=["idx"])
            P.op("dve", lambda e: e.tensor_scalar(bef[:], jvec[:], pend[:, 0:1], None, ALU.is_ge), reads=["jvec", pendn], writes=["bef"])
            for ee in range(1, 32):
                P.op("dve", lambda e, ee=ee: e.scalar_tensor_tensor(bef[:], jvec[:], pend[:, ee:ee + 1], bef[:], ALU.is_ge, ALU.add),
                     reads=["jvec", pendn, "bef"], writes=["bef"])
            P.op("dve", lambda e: e.tensor_scalar(bef[:], bef[:], 31.0, None, ALU.min), reads=["bef"], writes=["bef"])
            P.op("dve", lambda e: e.tensor_scalar(be1[:], bef[:], 128.0, float(l * NEXP * 128), ALU.mult, ALU.add), reads=["bef"], writes=["be1"])
            P.op("dve", lambda e: e.tensor_scalar(be1[:], be1[:], base1[:, 0:1], None, ALU.add), reads=["be1", "base1"], writes=["be1"])
            P.op("dve", lambda e: e.tensor_scalar(be1[:], be1[:], 4.0, None, ALU.mult), reads=["be1"], writes=["be1"])
            P.op("dve", lambda e: e.tensor_tensor(wif1[:], bc_last(be1[:], 4), bc_mid(base2[:], NBLK_MAX), ALU.add),
                 reads=["be1", "base2"], writes=["wif1"])
            P.op("dve", lambda e: e.tensor_copy(wi1[:], wif1[:]), reads=["wif1"], writes=["wi1"])
            if stop == "p2c":
                P.flush()
                return nc
            sres = []
            for li in range(nt2):
                hs_ = li % 2
                P.dma("sp", h2r[hs_][:], h2buf[li * 128:(li + 1) * 128, :], reads=[("h2buf", li)], writes=[("h2r", hs_)])
                for k in range(2):
                    P.idma(xs_dram[0:NBLK * BS, :], bass.IndirectOffsetOnAxis(ap=idx[:, k, li:li + 1], axis=0), h2r[hs_][:], None,
                           reads=[("h2r", hs_), "idx"] + zres, writes=[("xss", li, k)])
                    sres.append(("xss", li, k))

            if stop == "p2d":
                P.flush()
                return nc
            yres = []

            def blockA(j):
                ws = j % 2
                if not DBG.get("no_wgather"):
                    for a in range(4):
                        io = bass.IndirectOffsetOnAxis(ap=wi1[:, j, a:a + 1], axis=0)
                        P.idma(w1s[ws][:, 2 * a:2 * a + 2, :].rearrange("p k n -> p (k n)"), None, w1v, io, reads=["wi1"], writes=[("w1s", ws, a)])
                        P.idma(w3s[ws][:, 2 * a:2 * a + 2, :].rearrange("p k n -> p (k n)"), None, w3v, io, reads=["wi1"], writes=[("w3s", ws, a)])
                        P.idma(w2s[ws][:, a, :], None, w2v, io, reads=["wi1"], writes=[("w2s", ws, a)])
                P.dma("sp", xsb[ws][:], xs_dram[j * BS:(j + 1) * BS, :].rearrange("(a p) d -> p a d", p=128),
                      reads=sres + zres, writes=[("xsb", ws)])
                for a in range(4):
                    tb = a % 2

                    def tps_(e, a=a, tb=tb, ws=ws):
                        ins = None
                        for k in range(KC):
                            ins = e.transpose(TB[tb][:, k * 128:(k + 1) * 128], xsb[ws][:, a, k * 128:(k + 1) * 128], ident_b[:])
                        return ins
                    P.op("pe", tps_, reads=[("xsb", ws), "ident_b"], writes=[("TB", tb)])
                    P.op("act", lambda e, a=a, tb=tb, ws=ws: e.activation(out=hTb[ws][:, :, a * 128:(a + 1) * 128],
                                                                         in_=TB[tb][:, :].rearrange("p (k t) -> p k t", k=KC), func=AF.Copy),
                         reads=[("TB", tb)], writes=[("hTb", ws, a)])

            def blockB(j):
                ws = j % 2
                hres = [("hTb", ws, a) for a in range(4)]
                w13 = [("w1s", ws, a) for a in range(4)] + [("w3s", ws, a) for a in range(4)]
                for c in range(4):
                    pb = 2 + 2 * (c % 2)

                    def h13(e, c=c, pb=pb, ws=ws):
                        ins = None
                        for k in range(KC):
                            ins = e.matmul(B[pb][:, :], lhsT=w1s[ws][:, k, c * 128:(c + 1) * 128], rhs=hTb[ws][:, k, :],
                                           start=(k == 0), stop=(k == KC - 1))
                        for k in range(KC):
                            ins = e.matmul(B[pb + 1][:, :], lhsT=w3s[ws][:, k, c * 128:(c + 1) * 128], rhs=hTb[ws][:, k, :],
                                           start=(k == 0), stop=(k == KC - 1))
                        return ins
                    P.op("pe", h13, reads=w13 + hres, writes=[("B", pb), ("B", pb + 1)])
                    P.op("act", lambda e, c=c, pb=pb: e.activation(out=sg[c % 2][:], in_=B[pb][:, :], func=AF.Silu),
                         reads=[("B", pb)], writes=[("sg", c % 2)])
                    P.op("dve", lambda e, c=c, pb=pb, ws=ws: e.tensor_tensor(actT[ws][:, c, :], sg[c % 2][:], B[pb + 1][:, :], ALU.mult),
                         reads=[("sg", c % 2), ("B", pb + 1)], writes=[("actT", ws, c)])
                for a in range(4):
                    ys_ = a % 2
                    for hf in range(2):
                        yb = 6 + hf

                        def ymm(e, a=a, hf=hf, yb=yb, ws=ws):
                            ins = None
                            for c in range(4):
                                ins = e.matmul(B[yb][:, :], lhsT=actT[ws][:, c, a * 128:(a + 1) * 128],
                                               rhs=w2s[ws][:, c, hf * 512:(hf + 1) * 512], start=(c == 0), stop=(c == 3))
                            return ins
                        P.op("pe", ymm, reads=[("actT", ws, c) for c in range(4)] + [("w2s", ws, c) for c in range(4)], writes=[("B", yb)])
                        P.op("dve", lambda e, hf=hf, yb=yb, ys_=ys_: e.tensor_copy(ysb[ys_][:, hf * 512:(hf + 1) * 512], B[yb][:, :]),
                             reads=[("B", yb)], writes=[("ysb", ys_, hf)])
                    r0 = j * BS + a * 128
                    P.dma("sp", ys_dram[r0:r0 + 128, :], ysb[ys_][:], reads=[("ysb", ys_, 0), ("ysb", ys_, 1)], writes=[("ys", j, a)])
                    yres.append(("ys", j, a))


            P.play(P.record(blockA, 0))
            for j in range(NBLK):
                P.play(P.record(blockB, j), P.record(blockA, j + 1) if j + 1 < NBLK else [])
            if stop == "p2e":
                P.flush()
                return nc
            fA = [bufA, x1T[:].rearrange("p a b -> p (a b)")]
            fB = [bufB, dtmp_v]
            fY = [[ysb[0], ysb[1]], [rank_v, oh0_v]]

            def stageF(li):
                t = tiles[li]
                col = 0 if t < NB else 1
                xs = li % 2
                se = li % 2
                bA, bB, ygs = fA[se], fB[se], fY[se]
                nA = ("bufA", se)
                P.dma("sp", x1t[xs][:], x1buf[t * 128:(t + 1) * 128, :], writes=[("x1t", xs)])
                for k in range(2):
                    P.idma(ygs[k][:], None, ys_dram[0:NBLK * BS, :], bass.IndirectOffsetOnAxis(ap=idx[:, k, li:li + 1], axis=0),
                           reads=["idx"] + yres, writes=[("yg", se, k)])
                P.op("dve", lambda e: e.tensor_scalar(bA[:], ygs[0][:], gsc[:, li, 0:1], None, ALU.mult),
                     reads=[("yg", se, 0), ("gsc", li)], writes=[nA])
                P.op("dve", lambda e: e.scalar_tensor_tensor(bA[:], ygs[1][:], gsc[:, li, 1:2], bA[:], ALU.mult, ALU.add),
                     reads=[("yg", se, 1), ("gsc", li), nA], writes=[nA])
                P.op("pool", lambda e: e.tensor_tensor(bA[:], bA[:], g2b[col][:], ALU.mult),
                     reads=[nA, ("g2b", col)], writes=[nA])
                if last:
                    dst, dname = out_d[t * 128:(t + 1) * 128, :], ("out", t)
                else:
                    dst, dname = x2buf[t * 128:(t + 1) * 128, :], ("x2buf", t)
                ln_tail2(x1t[xs], ("x1t", xs), dst, dname, bA, bB, se)

            for li0 in range(0, nt2, 2):
                P.play(P.record(stageF, li0), P.record(stageF, li0 + 1) if li0 + 1 < nt2 else [])
            P.flush()
    glob.close()
    return nc


def _rope_tables(NB):
    NT = NB + NCB
    S = NB * 128
    m = 16
    freqs = (10000.0 ** (-np.arange(m, dtype=np.float32) / m)).astype(np.float32)
    tpos = np.arange(S)
    row = (tpos // GRID_W).astype(np.float32)
    colp = (tpos % GRID_W).astype(np.float32)
    cos = np.ones((128, NT * 128), np.float32)
    sin = np.zeros((128, NT * 128), np.float32)
    for p in range(128):
        d = p % 64
        pos = row if d < 32 else colp
        dd = d % 32
        f = freqs[dd % 16]
        ang = (pos * f).astype(np.float32)
        cos[p, :S] = np.cos(ang)
        sgn = -1.0 if dd < 16 else 1.0
        sin[p, :S] = sgn * np.sin(ang)
    cos_t = np.ascontiguousarray(cos.reshape(128, NT, 128).transpose(1, 0, 2))
    sin_t = np.ascontiguousarray(sin.reshape(128, NT, 128).transpose(1, 0, 2))
    return cos_t, sin_t


def make_in_maps(inputs, NB, ncores):
    f = lambda a: np.ascontiguousarray(np.asarray(a, dtype=np.float32))
    x = f(inputs["x"]); c = f(inputs["c"]); ctx = f(inputs["ctx"]); c_ctx = f(inputs["c_ctx"])
    depth = inputs["w_ada"].shape[0]
    cos_t, sin_t = _rope_tables(NB)
    j = np.arange(128)
    maskl = np.where(j[:, None] >= j[None, :], 0.0, NEG).astype(np.float32)
    maskr = np.where(j[:, None] <= j[None, :], 0.0, NEG).astype(np.float32)
    shared = {
        "w_ada": f(inputs["w_ada"]),
        "bada_p": f(np.asarray(inputs["b_ada"]).reshape(depth, 48, 128).transpose(0, 2, 1)),
        "b_ada": f(inputs["b_ada"]),
        "w_in": f(inputs["w_in"]),
        "convw": f(np.asarray(inputs["conv_w"]).reshape(depth, 3, 2, 128).transpose(0, 3, 2, 1)),
        "sink": f(inputs["attn_sink"]),
        "gm_wsT": f(np.asarray(inputs["gm_ws"]).transpose(0, 1, 3, 2)),
        "gm_bsp": f(np.asarray(inputs["gm_bs"]).transpose(0, 2, 1)),
        "w_out": f(inputs["w_out"]),
        "ln1_g": f(inputs["ln1_g"]), "ln1_b": f(inputs["ln1_b"]),
        "ln2_g": f(inputs["ln2_g"]), "ln2_b": f(inputs["ln2_b"]),
        "w_r": f(np.concatenate([np.asarray(inputs["w_rg"]), np.asarray(inputs["w_re"])], axis=-1)),
        "b_r": f(np.concatenate([np.asarray(inputs["b_rg"]), np.asarray(inputs["b_re"])], axis=-1)),
        "w1": f(np.asarray(inputs["w1"]).reshape(depth, NEXP, KC, 128, DEXP).transpose(0, 1, 3, 2, 4)),
        "w3": f(np.asarray(inputs["w3"]).reshape(depth, NEXP, KC, 128, DEXP).transpose(0, 1, 3, 2, 4)),
        "w2": f(np.asarray(inputs["w2"]).reshape(depth, NEXP, 4, 128, D).transpose(0, 1, 3, 2, 4)),
        "cos_t": cos_t, "sin_t": sin_t,
        "ident": np.eye(128, dtype=np.float32), "maskl": maskl, "maskr": maskr,
        "lt_d": (j[:, None] < j[None, :]).astype(np.float32),
        "jvec_d": np.tile(np.arange((2 * (NB + NCB) * 128) // 512 + NEXP, dtype=np.float32)[None, :], (128, 1)),
        "base1_d": (np.arange(8, dtype=np.float32)[None, :] * 128 + j[:, None]).astype(np.float32),
        "base2_d": np.tile(np.arange(4, dtype=np.float32)[None, :], (128, 1)),
    }
    maps = []
    for b in range(ncores):
        cv = np.stack([c[b].reshape(KC, 128).T, c_ctx.reshape(KC, 128).T], axis=-1)
        m = dict(shared)
        m["xin"] = np.ascontiguousarray(x[b])
        m["ctxin"] = np.ascontiguousarray(ctx[b])
        m["cvec"] = np.ascontiguousarray(cv.astype(np.float32))
        maps.append(m)
    return maps


_NC_CACHE = {}


def kernel(**inputs):
    x = np.asarray(inputs["x"])
    Bn, S, _ = x.shape
    NB = S // 128
    key = (NB,)
    if key not in _NC_CACHE:
        _NC_CACHE[key] = build_program(NB)
    nc = _NC_CACHE[key]
    maps = make_in_maps(inputs, NB, Bn)
    res = run_bass_kernel_spmd(nc, maps, core_ids=list(range(Bn)))
    return np.stack([np.asarray(r["out"], dtype=np.float32) for r in res.results], axis=0)
```

```python
from contextlib import ExitStack
import numpy as np
import ml_dtypes
import concourse.bass as bass
import concourse.mybir as mybir
from concourse.bass_utils import run_bass_kernel_spmd

F32 = mybir.dt.float32
BF16 = mybir.dt.bfloat16
AF = mybir.ActivationFunctionType
ALU = mybir.AluOpType

D = 1024
KC = 8
CTX = 256
NCB = CTX // 128
DEPTH = 2
NEXP = 32
DEXP = 512
ALPHA = (2 * DEPTH) ** 0.25
LN_EPS = 1e-6
GRID_W = 64
NEG = -30000.0
DBG = {}


class Prog:
    NDMA = 8

    def __init__(self, nc):
        self.nc = nc
        self.names = dict(pe="tensor", act="scalar", dve="vector", pool="gpsimd", sp="sync")
        self.sems = []
        self.semidx = {}
        for k in self.names:
            self.semidx[k] = len(self.sems)
            self.sems.append(nc.alloc_semaphore("s_" + k))
        self.cnt = {k: 0 for k in self.names}
        self.dq = {}
        for q in ("sp", "pool"):
            idx = []
            for i in range(self.NDMA):
                idx.append(len(self.sems))
                self.sems.append(nc.alloc_semaphore("d_%s%d" % (q, i)))
            self.dq[q] = dict(idx=idx, cnt=[0] * self.NDMA, nxt=0)
        self.seen = {k: {} for k in self.names}
        self.stream = {k: [] for k in self.names}
        self.res = {}
        self._cap = None

    def record(self, fn, *a, **kw):
        self._cap = []
        fn(*a, **kw)
        cap, self._cap = self._cap, None
        return cap

    def play(self, *lists):
        lists = [l for l in lists if l]
        pos = [0] * len(lists)
        while True:
            best, bf = None, None
            for i, l in enumerate(lists):
                if pos[i] < len(l):
                    f = pos[i] / len(l)
                    if bf is None or f < bf:
                        best, bf = i, f
            if best is None:
                break
            kind, a, kw = lists[best][pos[best]]
            pos[best] += 1
            getattr(self, kind)(*a, **kw)

    def _deps(self, reads, writes):
        deps = []
        for r in reads:
            e = self.res.get(r)
            if e and e["w"] is not None:
                deps.append(e["w"])
        for w in writes:
            e = self.res.get(w)
            if e:
                if e["w"] is not None:
                    deps.append(e["w"])
                deps.extend((si, v, src) for (si, src), v in e["r"].items())
        return deps

    def _waits(self, ek, deps):
        waits = {}
        for (si, val, src) in deps:
            if src == "pe" and ek == "pe":
                continue
            if self.seen[ek].get(si, 0) >= val:
                continue
            waits[si] = max(waits.get(si, 0), val)
        for si, v in waits.items():
            self.seen[ek][si] = v
        return list(waits.items())

    def _record(self, token, reads, writes):
        si, val, src = token
        for r in reads:
            e = self.res.setdefault(r, {"w": None, "r": {}})
            e["r"][(si, src)] = max(e["r"].get((si, src), 0), val)
        for w in writes:
            self.res[w] = {"w": token, "r": {}}

    def op(self, ek, fn, reads=(), writes=()):
        if self._cap is not None:
            self._cap.append(("op", (ek, fn), dict(reads=reads, writes=writes)))
            return None
        waits = self._waits(ek, self._deps(reads, writes))
        self.cnt[ek] += 1
        si = self.semidx[ek]
        token = (si, self.cnt[ek], ek)
        sems = self.sems

        def emit(eng):
            for (wsi, v) in waits:
                eng.wait_ge(sems[wsi], v)
            ins = fn(eng)
            ins.then_inc(sems[si], 1)

        self.stream[ek].append(emit)
        self._record(token, reads, writes)
        return token

    def dma(self, q, out, in_, reads=(), writes=()):
        if self._cap is not None:
            self._cap.append(("dma", (q, out, in_), dict(reads=reads, writes=writes)))
            return None
        d = self.dq[q]
        slot = d["nxt"]
        d["nxt"] = (slot + 1) % self.NDMA
        si = d["idx"][slot]
        deps = self._deps(reads, writes)
        if d["cnt"][slot] > 0:
            deps.append((si, 16 * d["cnt"][slot], "dma"))
        waits = self._waits(q, deps)
        d["cnt"][slot] += 1
        token = (si, 16 * d["cnt"][slot], "dma")
        sems = self.sems

        def emit(eng):
            for (wsi, v) in waits:
                eng.wait_ge(sems[wsi], v)
            eng.dma_start(out=out, in_=in_).then_inc(sems[si], 16)

        self.stream[q].append(emit)
        self._record(token, reads, writes)
        return token

    def idma(self, out, out_off, in_, in_off, reads=(), writes=()):
        if self._cap is not None:
            self._cap.append(("idma", (out, out_off, in_, in_off), dict(reads=reads, writes=writes)))
            return None
        q = "pool"
        d = self.dq[q]
        slot = d["nxt"]
        d["nxt"] = (slot + 1) % self.NDMA
        si = d["idx"][slot]
        deps = self._deps(reads, writes)
        if d["cnt"][slot] > 0:
            deps.append((si, 16 * d["cnt"][slot], "dma"))
        waits = self._waits(q, deps)
        d["cnt"][slot] += 1
        token = (si, 16 * d["cnt"][slot], "dma")
        sems = self.sems

        def emit(eng):
            for (wsi, v) in waits:
                eng.wait_ge(sems[wsi], v)
            eng.indirect_dma_start(out=out, out_offset=out_off, in_=in_, in_offset=in_off).then_inc(sems[si], 16)

        self.stream[q].append(emit)
        self._record(token, reads, writes)
        return token

    def flush(self):
        finals = []
        for k in self.names:
            if self.cnt[k] > 0:
                finals.append((self.semidx[k], self.cnt[k]))
        for q, d in self.dq.items():
            for i, si in enumerate(d["idx"]):
                if d["cnt"][i] > 0:
                    finals.append((si, 16 * d["cnt"][i]))
        with self.nc.Block() as block:
            for ek, nm in self.names.items():
                stream = self.stream[ek]
                seen = self.seen[ek]
                sems = self.sems

                def body(eng, stream=stream, seen=seen):
                    for emit in stream:
                        emit(eng)
                    for si, v in finals:
                        if seen.get(si, 0) < v:
                            eng.wait_ge(sems[si], v)
                            seen[si] = v

                getattr(block, nm)(body)
        self.stream = {k: [] for k in self.names}
        self.res = {}


def bc_mid(ap, n):
    p, f = ap.shape
    return ap.unsqueeze(1).to_broadcast([p, n, f])


def bc_last(ap, n):
    p, g = ap.shape
    return ap.unsqueeze(2).to_broadcast([p, g, n])


def build_program(NB, depth=DEPTH, tps=9, stop=None, nexp=NEXP, do_router=True, do_epi=True):
    S = NB * 128
    NT = NB + NCB
    T = NT * 128
    nc = bass.Bass("TRN2", target_bir_lowering=False)

    def din(name, shape, dt=F32):
        return nc.dram_tensor(name, list(shape), dt, kind="ExternalInput").ap()

    xin = din("xin", [S, D])
    ctxin = din("ctxin", [CTX, D])
    cvec = din("cvec", [128, KC, 2])
    w_ada = din("w_ada", [depth, D, 6 * D])
    bada_p = din("bada_p", [depth, 128, 48])
    b_ada = din("b_ada", [depth, 6 * D])
    w_in = din("w_in", [depth, D, 2048])
    convw = din("convw", [depth, 128, 2, 3])
    sink = din("sink", [depth, 8])
    gm_wsT = din("gm_wsT", [depth, 4, 128, 128])
    gm_bsp = din("gm_bsp", [depth, 128, 4])
    w_out = din("w_out", [depth, D, D])
    ln1_g = din("ln1_g", [depth, D])
    ln1_b = din("ln1_b", [depth, D])
    ln2_g = din("ln2_g", [depth, D])
    ln2_b = din("ln2_b", [depth, D])
    w_r = din("w_r", [depth, D, 36])
    b_r = din("b_r", [depth, 36])
    w1 = din("w1", [depth, NEXP, 128, KC, DEXP])
    w3 = din("w3", [depth, NEXP, 128, KC, DEXP])
    w2 = din("w2", [depth, NEXP, 128, 4, D])
    cos_t = din("cos_t", [NT, 128, 128])
    sin_t = din("sin_t", [NT, 128, 128])
    ident_d = din("ident", [128, 128])
    maskl_d = din("maskl", [128, 128])
    maskr_d = din("maskr", [128, 128])
    out_d = nc.dram_tensor("out", [S, D], F32, kind="ExternalOutput").ap()
    x1buf = nc.dram_tensor("x1buf", [T, D], F32).ap()
    x2buf = nc.dram_tensor("x2buf", [T, D], F32).ap()
    BS = 512
    NBLK_MAX = (2 * T) // BS + NEXP
    h2buf = nc.dram_tensor("h2buf", [T, D], BF16).ap()
    xs_dram = nc.dram_tensor("xs_dram", [NBLK_MAX * BS, D], BF16).ap()
    ys_dram = nc.dram_tensor("ys_dram", [NBLK_MAX * BS, D], F32).ap()
    modrows = nc.dram_tensor("modrows", [depth, 2, 4, D], F32).ap()
    lt_d = din("lt_d", [128, 128])
    jvec_d = din("jvec_d", [128, NBLK_MAX])
    base1_d = din("base1_d", [128, 8])
    base2_d = din("base2_d", [128, 4])

    P = Prog(nc)

    def tile_src(l, t):
        if l == 0:
            if t < NB:
                return xin[t * 128:(t + 1) * 128, :]
            return ctxin[(t - NB) * 128:(t - NB + 1) * 128, :]
        return x2buf[t * 128:(t + 1) * 128, :]

    glob = ExitStack()

    uid = [0]

    def alloc(st, name, shape, dt):
        uid[0] += 1
        return st.enter_context(nc.sbuf_tensor("sb%d_%s" % (uid[0], name), list(shape), dt))

    def palloc(st, name, shape, dt=F32):
        uid[0] += 1
        return st.enter_context(nc.psum_tensor("ps%d_%s" % (uid[0], name), list(shape), dt))

    ident_f = alloc(glob, "ident_f", [128, 128], F32)
    ident_b = alloc(glob, "ident_b", [128, 128], BF16)
    maskl = alloc(glob, "maskl", [128, 128], BF16)
    maskr = alloc(glob, "maskr", [128, 128], BF16)
    cact = alloc(glob, "cact", [128, KC, 2], F32)
    cact_rep = alloc(glob, "cact_rep", [128, KC, 2, 128], F32)
    ones_f = alloc(glob, "ones_f", [1, 128], F32)
    epsb = alloc(glob, "epsb", [128, 1], F32)
    modT = alloc(glob, "modT", [128, 48, 2], F32)

    P.dma("sp", ident_f[:], ident_d[:, :], writes=["ident_f"])
    P.dma("pool", ident_b[:], ident_d[:, :], writes=["ident_b"])
    P.dma("pool", maskl[:], maskl_d[:, :], writes=["maskl"])
    P.dma("pool", maskr[:], maskr_d[:, :], writes=["maskr"])
    P.dma("sp", cact[:], cvec[:, :, :], writes=["cact"])
    P.op("act", lambda e: e.activation(out=cact[:], in_=cact[:], func=AF.Silu), reads=["cact"], writes=["cact"])
    P.op("dve", lambda e: e.tensor_copy(cact_rep[:].rearrange("p k c m -> p (k c) m"),
                                        bc_last(cact[:].rearrange("p k c -> p (k c)"), 128)),
         reads=["cact"], writes=["cact_rep"])
    P.op("dve", lambda e: e.memset(ones_f[:], 1.0), writes=["ones_f"])
    P.op("dve", lambda e: e.memset(epsb[:], LN_EPS), writes=["epsb"])
    P.flush()
    if stop == "const":
        return nc

    for l in range(depth):
        last = (l == depth - 1)
        with ExitStack() as st:
            wa = [alloc(st, "wa%d" % i, [128, KC, D], F32) for i in range(2)]
            bada = alloc(st, "bada", [128, 48], F32)
            brow = [alloc(st, "brow%d" % i, [128, D], F32) for i in range(2)]
            grow = [alloc(st, "grow%d" % i, [128, D], F32) for i in range(2)]
            ps_mod = palloc(st, "ps_mod", [128, 96])
            ps_g = [palloc(st, "ps_g%d" % i, [128, 512]) for i in range(4)]
            P.dma("sp", bada[:], bada_p[l], writes=["bada"])
            for i in range(6):
                s = i % 2
                P.dma("sp", wa[s][:], w_ada[l][:, i * D:(i + 1) * D].rearrange("(k p) n -> p k n", p=128),
                      writes=[("wa", s)])

                def mm_mod(e, i=i, s=s):
                    ins = None
                    for j in range(KC):
                        for k in range(KC):
                            ins = e.matmul(ps_mod[:, (i * 8 + j) * 2:(i * 8 + j) * 2 + 2],
                                           lhsT=wa[s][:, k, j * 128:(j + 1) * 128], rhs=cact[:, k, :],
                                           start=(k == 0), stop=(k == KC - 1))
                    return ins
                P.op("pe", mm_mod, reads=[("wa", s), "cact"], writes=[("ps_mod", i)])
                if i >= 2:
                    gi = i - 2
                    P.dma("sp", brow[gi % 2][:], b_ada[l:l + 1, i * D:(i + 1) * D].to_broadcast([128, D]),
                          writes=[("brow", gi % 2)])
                    for c in range(2):
                        for h in range(2):
                            def mm_g(e, s=s, c=c, h=h):
                                ins = None
                                for k in range(KC):
                                    ins = e.matmul(ps_g[c * 2 + h][:, :], lhsT=cact_rep[:, k, c, :],
                                                   rhs=wa[s][:, k, h * 512:(h + 1) * 512],
                                                   start=(k == 0), stop=(k == KC - 1))
                                return ins
                            P.op("pe", mm_g, reads=[("wa", s), "cact_rep"], writes=[("ps_g", c * 2 + h)])
                            P.op("dve", lambda e, gi=gi, c=c, h=h: e.tensor_tensor(
                                grow[c][:, h * 512:(h + 1) * 512], ps_g[c * 2 + h][:, :],
                                brow[gi % 2][:, h * 512:(h + 1) * 512], ALU.add),
                                reads=[("ps_g", c * 2 + h), ("brow", gi % 2)], writes=[("grow", c, h)])
                        if i == 4:
                            P.op("dve", lambda e, c=c: e.tensor_scalar(grow[c][0:1, :], grow[c][0:1, :], 1.0, None, ALU.add),
                                 reads=[("grow", c, 0), ("grow", c, 1)], writes=[("grow", c, 0), ("grow", c, 1)])
                        P.dma("sp", modrows[l, c, gi:gi + 1, :], grow[c][0:1, :],
                              reads=[("grow", c, 0), ("grow", c, 1)], writes=[("modrows", c, gi)])
            P.op("dve", lambda e: e.tensor_tensor(modT[:], ps_mod[:].rearrange("p (j c) -> p j c", c=2),
                                                  bc_last(bada[:], 2), ALU.add),
                 reads=[("ps_mod", i) for i in range(6)] + ["bada"], writes=["modT"])
            P.op("dve", lambda e: e.tensor_scalar(modT[:, 8:16, :], modT[:, 8:16, :], 1.0, None, ALU.add),
                 reads=["modT"], writes=["modT"])
            P.op("dve", lambda e: e.tensor_scalar(modT[:, 32:40, :], modT[:, 32:40, :], 1.0, None, ALU.add),
                 reads=["modT"], writes=["modT"])
            P.flush()
        if stop == "p0":
            return nc

        with ExitStack() as st:
            NCOL = 18 * 128 + 640
            win = alloc(st, "win", [128, KC, NCOL], BF16)
            wout = alloc(st, "wout", [128, KC, D], BF16)
            kT = alloc(st, "kT", [128, 2, T], BF16)
            V = alloc(st, "V", [128, NT, 2, 65], BF16)
            convw_s = alloc(st, "convw_s", [128, 2, 3], F32)
            wsT = alloc(st, "wsT", [128, 4, 128], BF16)
            gmb = alloc(st, "gmb", [128, 4], F32)
            esink = alloc(st, "esink", [128, 8], F32)
            lng = alloc(st, "lng", [128, D], F32)
            lnb = alloc(st, "lnb", [128, D], F32)
            g1b = [alloc(st, "g1b%d" % c, [128, D], F32) for c in range(2)]
            for c in range(2):
                P.dma("sp", g1b[c][:], modrows[l, c, 0:1, :].to_broadcast([128, D]), writes=[("g1b", c)])
            R = 5
            xres = [alloc(st, "xres%d" % i, [128, D], F32) for i in range(5)]
            hT = [alloc(st, "hT%d" % i, [128, KC, 128], BF16) for i in range(2)]
            qrot = [alloc(st, "qrot%d" % i, [128, 4, 128], BF16) for i in range(R)]
            u = [alloc(st, "u%d" % i, [128, 2, 130], F32) for i in range(R)]
            cb = [alloc(st, "cb%d" % i, [128, 2, 128], F32) for i in range(R)]
            gut = [alloc(st, "gut%d" % i, [128, 256], F32) for i in range(R)]
            vln = [alloc(st, "vln%d" % i, [128, 256], BF16) for i in range(R)]
            cosb = [alloc(st, "cosb%d" % i, [128, 128], F32) for i in range(2)]
            sinb = [alloc(st, "sinb%d" % i, [128, 128], F32) for i in range(2)]
            t1 = alloc(st, "t1", [128, 4, 128], F32)
            t2 = alloc(st, "t2", [128, 4, 128], F32)
            cxs = alloc(st, "cxs", [128, 2, 128], F32)
            gvt = alloc(st, "gvt", [128, 256], F32)
            st6 = alloc(st, "st6", [128, 6], F32)
            mv = alloc(st, "mv", [128, 2], F32)
            rstd = alloc(st, "rstd", [128, 1], F32)
            nb_ = alloc(st, "nb_", [128, 1], F32)
            PT = [alloc(st, "PT%d" % i, [128, 5, 128], BF16) for i in range(4)]
            den = alloc(st, "den", [128, 4], F32)
            mixtok = [alloc(st, "mixtok%d" % i, [128, 768], BF16) for i in range(2)]
            mixT = [alloc(st, "mixT%d" % i, [128, KC, 128], BF16) for i in range(2)]
            ct = alloc(st, "ct", [128, 2, 128], F32)
            gtmp = alloc(st, "gtmp", [128, 256], F32)
            bufAs = [alloc(st, "bufA%d" % i, [128, D], F32) for i in range(2)]
            bufB = alloc(st, "bufB", [128, D], F32)
            st12 = alloc(st, "st12", [128, 12], F32)
            mv2 = alloc(st, "mv2", [128, 2], F32)
            rstd2 = alloc(st, "rstd2", [128, 1], F32)
            nb2 = alloc(st, "nb2", [128, 1], F32)
            B = [palloc(st, "B%d" % i, [128, 512]) for i in range(7)]
            Bbf = palloc(st, "Bbf", [128, 1024], BF16)
            Bo = B[4]

            wv = w_in[l].rearrange("(k p) n -> p k n", p=128)
            P.dma("pool", win[:, :, 0:512], wv[:, :, 0:512], writes=["win"])
            for kv in range(2):
                for dup in range(2):
                    c0 = 1024 + kv * 128 + dup * 64
                    P.dma("pool", win[:, :, c0:c0 + 64], wv[:, :, 512 + kv * 64:512 + (kv + 1) * 64], writes=["win"])
            P.dma("pool", win[:, :, 1536:2304], wv[:, :, 768:1536], writes=["win"])
            P.dma("pool", win[:, :, 2304:2432], wv[:, :, 640:768], writes=["win"])
            P.dma("pool", win[:, :, 2432:2688], wv[:, :, 1792:2048], writes=["win"])
            P.dma("pool", win[:, :, 2688:2944], wv[:, :, 1536:1792], writes=["win"])
            P.dma("pool", wout[:], w_out[l].rearrange("(k p) n -> p k n", p=128), writes=["wout"])
            for (src0, dst0, nblk) in ((0, 512, 16), (1024, 1280, 8)):
                for a in range(2):
                    def swp(e, src0=src0, dst0=dst0, nblk=nblk, a=a):
                        srcv = win[:, :, src0:src0 + nblk * 32].rearrange("p k (b a f) -> p k b a f", a=2, f=16)
                        dstv = win[:, :, dst0:dst0 + nblk * 32].rearrange("p k (b a f) -> p k b a f", a=2, f=16)
                        return e.tensor_copy(dstv[:, :, :, a, :], srcv[:, :, :, 1 - a, :])
                    P.op("dve", swp, reads=["win"], writes=["win"])
            P.dma("sp", convw_s[:], convw[l], writes=["convw_s"])
            P.dma("pool", wsT[:], gm_wsT[l].rearrange("g q p -> q g p"), writes=["wsT"])
            P.dma("sp", gmb[:], gm_bsp[l], writes=["gmb"])
            P.dma("sp", esink[:], sink[l:l + 1, :].to_broadcast([128, 8]), writes=["esink"])
            P.op("act", lambda e: e.activation(out=esink[:], in_=esink[:], func=AF.Exp), reads=["esink"], writes=["esink"])
            P.dma("sp", lng[:], ln1_g[l:l + 1, :].to_broadcast([128, D]), writes=["lng"])
            P.dma("sp", lnb[:], ln1_b[l:l + 1, :].to_broadcast([128, D]), writes=["lnb"])
            P.op("pool", lambda e: e.memset(V[:, :, :, 64:65], 1.0), writes=["Vones"])

            def seq_of(t):
                return (0, NB) if t < NB else (NB, NT)

            def inproj(t, kv_only=False):
                s = t % R
                xs = t % 5
                hs = t % 2
                cs = t % 2
                col = 0 if t < NB else 1
                lo, hi = seq_of(t)
                P.dma("sp", xres[xs][:], tile_src(l, t), writes=[("xres", xs)])
                P.dma("sp", cosb[cs][:], cos_t[t], writes=[("cosb", cs)])
                P.dma("sp", sinb[cs][:], sin_t[t], writes=[("sinb", cs)])
                for hb in range(2):
                    def tp(e, hb=hb):
                        ins = None
                        for i in range(4):
                            j = hb * 4 + i
                            ins = e.transpose(B[2 + hb][:, i * 128:(i + 1) * 128], xres[xs][:, j * 128:(j + 1) * 128], ident_f[:])
                        return ins
                    P.op("pe", tp, reads=[("xres", xs), "ident_f"], writes=[("B", 2 + hb)])

                    def ev(e, hb=hb):
                        ins = None
                        for i in range(4):
                            j = hb * 4 + i
                            ins = e.activation(out=hT[hs][:, j, :], in_=B[2 + hb][:, i * 128:(i + 1) * 128], func=AF.Identity,
                                               scale=modT[:, 8 + j, col:col + 1], bias=modT[:, j, col:col + 1])
                        return ins
                    P.op("act", ev, reads=[("B", 2 + hb), "modT"], writes=[("hT", hs, hb)])
                hres = [("hT", hs, 0), ("hT", hs, 1)]

                def fm_group(bank, off, chunks):
                    def f(e):
                        ins = None
                        for i, c in enumerate(chunks):
                            for k in range(KC):
                                ins = e.matmul(B[bank][:, off + i * 128:off + (i + 1) * 128],
                                               lhsT=win[:, k, c * 128:(c + 1) * 128], rhs=hT[hs][:, k, :],
                                               start=(k == 0), stop=(k == KC - 1))
                        return ins
                    return f

                if not kv_only:
                    P.op("pe", fm_group(2, 0, [0, 1, 2, 3]), reads=hres + ["win"], writes=[("B", 2)])
                    P.op("pe", fm_group(3, 0, [4, 5, 6, 7]), reads=hres + ["win"], writes=[("B", 3)])
                    P.op("dve", lambda e: e.tensor_tensor(t1[:], B[2][:, :].rearrange("p (c t) -> p c t", c=4),
                                                          bc_mid(cosb[cs][:], 4), ALU.mult),
                         reads=[("B", 2), ("cosb", cs)], writes=["t1"])
                    P.op("dve", lambda e: e.tensor_tensor(t2[:], B[3][:, :].rearrange("p (c t) -> p c t", c=4),
                                                          bc_mid(sinb[cs][:], 4), ALU.mult),
                         reads=[("B", 3), ("sinb", cs)], writes=["t2"])
                    P.op("pool", lambda e: e.tensor_tensor(qrot[s][:], t1[:], t2[:], ALU.add),
                         reads=["t1", "t2"], writes=[("qrot", s)])
                P.op("pe", fm_group(2, 0, [8, 9, 10, 11]), reads=hres + ["win"], writes=[("B", 2)])
                P.op("dve", lambda e: e.tensor_tensor(t1[:, 0:2, :], B[2][:, 0:256].rearrange("p (c t) -> p c t", c=2),
                                                      bc_mid(cosb[cs][:], 2), ALU.mult),
                     reads=[("B", 2), ("cosb", cs)], writes=["t1"])
                P.op("dve", lambda e: e.tensor_tensor(t2[:, 0:2, :], B[2][:, 256:512].rearrange("p (c t) -> p c t", c=2),
                                                      bc_mid(sinb[cs][:], 2), ALU.mult),
                     reads=[("B", 2), ("sinb", cs)], writes=["t2"])
                P.op("pool", lambda e: e.tensor_tensor(kT[:, :, t * 128:(t + 1) * 128], t1[:, 0:2, :], t2[:, 0:2, :], ALU.add),
                     reads=["t1", "t2"], writes=[("kT", t)])
                if not kv_only:
                    P.op("pe", fm_group(3, 0, [12, 13, 14, 15]), reads=hres + ["win"], writes=[("B", 3)])
                    P.op("pe", fm_group(2, 0, [16, 17]), reads=hres + ["win"], writes=[("B", 2)])
                    P.op("act", lambda e: e.activation(out=cb[s][:], in_=B[3][:, 0:256].rearrange("p (c t) -> p c t", c=2),
                                                       func=AF.Copy), reads=[("B", 3)], writes=[("cb", s)])
                    P.op("act", lambda e: e.activation(out=cxs[:], in_=B[2][:, 0:256].rearrange("p (c t) -> p c t", c=2),
                                                       func=AF.Copy), reads=[("B", 2)], writes=["cxs"])
                    P.op("dve", lambda e: e.tensor_tensor(u[s][:, :, 1:129], B[3][:, 256:512].rearrange("p (c t) -> p c t", c=2),
                                                          cxs[:], ALU.mult),
                         reads=[("B", 3), "cxs"], writes=[("u", s)])
                    if t > lo:
                        sp_ = (t - 1) % R
                        P.op("pool", lambda e: e.tensor_copy(u[sp_][:, :, 129:130], u[s][:, :, 1:2]),
                             reads=[("u", s)], writes=[("u", sp_)])
                    else:
                        P.op("pool", lambda e: e.memset(u[s][:, :, 0:1], 0.0), reads=[("u", s)], writes=[("u", s)])
                    if t < hi - 1:
                        sn_ = (t + 1) % R
                        P.op("pool", lambda e: e.tensor_copy(u[sn_][:, :, 0:1], u[s][:, :, 128:129]),
                             reads=[("u", s)], writes=[("u", sn_)])
                    else:
                        P.op("pool", lambda e: e.memset(u[s][:, :, 129:130], 0.0), reads=[("u", s)], writes=[("u", s)])

                def tm(e):
                    ins = None
                    for k in range(KC):
                        ins = e.matmul(B[3][:, 0:384], lhsT=hT[hs][:, k, :], rhs=win[:, k, 2304:2688],
                                       start=(k == 0), stop=(k == KC - 1))
                    return ins
                P.op("pe", tm, reads=hres + ["win"], writes=[("B", 3)])
                P.op("act", lambda e: e.activation(out=V[:, t, :, 0:64], in_=B[3][:, 0:128].rearrange("p (h d) -> p h d", h=2),
                                                   func=AF.Copy), reads=[("B", 3)], writes=[("V", t)])
                if not kv_only:
                    def tm2(e):
                        ins = None
                        for k in range(KC):
                            ins = e.matmul(B[2][:, 256:512], lhsT=hT[hs][:, k, :], rhs=win[:, k, 2688:2944],
                                           start=(k == 0), stop=(k == KC - 1))
                        return ins
                    P.op("pe", tm2, reads=hres + ["win"], writes=[("B", 2)])
                    P.op("act", lambda e: e.activation(out=gvt[:], in_=B[3][:, 128:384], func=AF.Gelu_apprx_tanh),
                         reads=[("B", 3)], writes=["gvt"])
                    P.op("act", lambda e: e.activation(out=gut[s][:], in_=B[2][:, 256:512], func=AF.Gelu_apprx_tanh),
                         reads=[("B", 2)], writes=[("gut", s)])
                    P.op("dve", lambda e: e.bn_stats(st6[:], gvt[:]), reads=["gvt"], writes=["st6"])
                    P.op("dve", lambda e: e.bn_aggr(mv[:], st6[:]), reads=["st6"], writes=["mv"])
                    P.op("act", lambda e: e.activation(out=rstd[:], in_=mv[:, 1:2], func=AF.Ln, bias=epsb[:, 0:1]),
                         reads=["mv", "epsb"], writes=["rstd"])
                    P.op("act", lambda e: e.activation(out=rstd[:], in_=rstd[:], func=AF.Exp, scale=-0.5), reads=["rstd"], writes=["rstd"])
                    P.op("dve", lambda e: e.scalar_tensor_tensor(nb_[:], mv[:, 0:1], -1.0, rstd[:], ALU.mult, ALU.mult),
                         reads=["mv", "rstd"], writes=["nb_"])
                    P.op("act", lambda e: e.activation(out=vln[s][:], in_=gvt[:], func=AF.Identity,
                                                       scale=rstd[:, 0:1], bias=nb_[:, 0:1]),
                         reads=["gvt", "rstd", "nb_"], writes=[("vln", s)])

            def attn(t):
                s = t % R
                xs = t % 5
                ms = t % 2
                col = 0 if t < NB else 1
                lo, hi = seq_of(t)
                if t < NB:
                    kcs = [(NB, None), (NB + 1, None)]
                    if t > 0:
                        kcs.append((t - 1, maskl))
                    kcs.append((t, None))
                    if t < NB - 1:
                        kcs.append((t + 1, maskr))
                else:
                    kcs = [(NB, None), (NB + 1, None)]
                nk = len(kcs)
                for h in range(8):
                    qc, po, kv, pslot = h // 2, (h % 2) * 64, h // 4, h % 4

                    if h % 2 == 0:
                        sbank, s5, wr5 = B[6], B[4][:, 384:512], ("B", 4)
                        wr = [("B", 6)] + ([wr5] if nk > 4 else [])
                    else:
                        sbank, s5, wr5 = B[5], B[1][:, 384:512], ("B", 1)
                        wr = [("B", 5)] + ([wr5] if nk > 4 else [])

                    def sc(e, qc=qc, po=po, kv=kv, sbank=sbank, s5=s5):
                        ins = None
                        for i, (c, msk) in enumerate(kcs):
                            dst = sbank[:, i * 128:(i + 1) * 128] if i < 4 else s5
                            ins = e.matmul(dst, lhsT=kT[po:po + 64, kv, c * 128:(c + 1) * 128],
                                           rhs=qrot[s][po:po + 64, qc, :], start=True, stop=(msk is None))
                            if msk is not None:
                                ins = e.matmul(dst, lhsT=ident_b[:], rhs=msk[:], start=False, stop=True)
                        return ins
                    P.op("pe", sc, reads=[("kT", c) for c, _ in kcs] + [("qrot", s), "ident_b", "maskl", "maskr"], writes=wr)

                    def ex(e, pslot=pslot, sbank=sbank, s5=s5):
                        n1 = min(nk, 4)
                        ins = e.activation(out=PT[pslot][:, 0:n1, :], in_=sbank[:, 0:n1 * 128].rearrange("p (c t) -> p c t", c=n1),
                                           func=AF.Exp, scale=0.125)
                        if nk > 4:
                            ins = e.activation(out=PT[pslot][:, 4, :], in_=s5, func=AF.Exp, scale=0.125)
                        return ins
                    P.op("act", ex, reads=wr, writes=[("PT", pslot)])

                    def pv(e, kv=kv, pslot=pslot):
                        ins = None
                        for i, (c, msk) in enumerate(kcs):
                            ins = e.matmul(Bo[:, pslot * 65:(pslot + 1) * 65],
                                           lhsT=PT[pslot][:, i, :], rhs=V[:, c, kv, :],
                                           start=(i == 0), stop=(i == nk - 1))
                        return ins
                    P.op("pe", pv, reads=[("PT", pslot), "Vones"] + [("V", c) for c, _ in kcs], writes=[("B", 4)])
                    if pslot == 3:
                        hg = h // 4
                        bov = Bo[:, 0:260].rearrange("p (h d) -> p h d", d=65)
                        P.op("dve", lambda e, hg=hg, bov=bov: e.tensor_tensor(den[:], bov[:, :, 64], esink[:, hg * 4:(hg + 1) * 4], ALU.add),
                             reads=[("B", 4), "esink"], writes=["den"])
                        P.op("dve", lambda e: e.reciprocal(den[:], den[:]), reads=["den"], writes=["den"])
                        P.op("dve", lambda e, hg=hg, bov=bov: e.tensor_tensor(
                            mixtok[ms][:, hg * 256:(hg + 1) * 256].rearrange("p (h d) -> p h d", d=64),
                            bov[:, :, 0:64], bc_last(den[:], 64), ALU.mult),
                            reads=[("B", 4), "den"], writes=[("mixtok", ms, hg)])

            def post(t):
                s = t % R
                ms = t % 2
                col = 0 if t < NB else 1
                for j in range(2):
                    P.op("pool", lambda e, j=j: e.tensor_scalar(ct[:, j, :], u[s][:, j, 0:128], convw_s[:, j, 0:1], None, ALU.mult),
                         reads=[("u", s), "convw_s"], writes=[("ct", j)])
                    P.op("dve", lambda e, j=j: e.scalar_tensor_tensor(ct[:, j, :], u[s][:, j, 1:129], convw_s[:, j, 1:2], ct[:, j, :], ALU.mult, ALU.add),
                         reads=[("u", s), "convw_s", ("ct", j)], writes=[("ct", j)])
                    P.op("dve", lambda e, j=j: e.scalar_tensor_tensor(ct[:, j, :], u[s][:, j, 2:130], convw_s[:, j, 2:3], ct[:, j, :], ALU.mult, ALU.add),
                         reads=[("u", s), "convw_s", ("ct", j)], writes=[("ct", j)])
                    P.op("pool", lambda e, j=j: e.tensor_tensor(mixT[ms][:, 4 + j, :], cb[s][:, j, :], ct[:, j, :], ALU.mult),
                         reads=[("cb", s), ("ct", j)], writes=[("mixT", ms, 4 + j)])
                def gm(e):
                    ins = None
                    for g in range(4):
                        ins = e.matmul(B[1][:, g * 64:(g + 1) * 64], lhsT=wsT[:, g, :], rhs=vln[s][:, g * 64:(g + 1) * 64],
                                       start=True, stop=True)
                    return ins
                P.op("pe", gm, reads=[("vln", s), "wsT"], writes=[("B", 1)])
                P.op("dve", lambda e: e.tensor_tensor(gtmp[:].rearrange("p (g c) -> p g c", g=4),
                                                      B[1][:, 0:256].rearrange("p (g c) -> p g c", g=4),
                                                      bc_last(gmb[:], 64), ALU.add),
                     reads=[("B", 1), "gmb"], writes=["gtmp"])
                P.op("dve", lambda e: e.tensor_tensor(mixtok[ms][:, 512:768], gtmp[:], gut[s][:], ALU.mult),
                     reads=["gtmp", ("gut", s)], writes=[("mixtok", ms, 2)])
                def tpm(e):
                    ins = None
                    for i in range(6):
                        ins = e.transpose(Bbf[:, i * 128:(i + 1) * 128], mixtok[ms][:, i * 128:(i + 1) * 128], ident_b[:])
                    return ins
                P.op("pe", tpm, reads=[("mixtok", ms, i) for i in range(3)] + ["ident_b"], writes=["Bbf"])
                P.op("act", lambda e: e.activation(out=mixT[ms][:, 0:4, :], in_=Bbf[:, 0:512].rearrange("p (c t) -> p c t", c=4), func=AF.Copy),
                     reads=["Bbf"], writes=[("mixT", ms, i) for i in range(4)])
                P.op("act", lambda e: e.activation(out=mixT[ms][:, 6:8, :], in_=Bbf[:, 512:768].rearrange("p (c t) -> p c t", c=2), func=AF.Copy),
                     reads=["Bbf"], writes=[("mixT", ms, 6), ("mixT", ms, 7)])
                for hf in range(2):
                    def op_(e, hf=hf):
                        ins = None
                        for k in range(KC):
                            ins = e.matmul(B[0][:, :], lhsT=mixT[ms][:, k, :], rhs=wout[:, k, hf * 512:(hf + 1) * 512],
                                           start=(k == 0), stop=(k == KC - 1))
                        return ins
                    P.op("pe", op_, reads=[("mixT", ms, i) for i in range(8)] + ["wout"], writes=[("B", 0)])
                    P.op("dve", lambda e, hf=hf: e.tensor_tensor(bufAs[t % 2][:, hf * 512:(hf + 1) * 512], B[0][:, :],
                                                                g1b[col][:, hf * 512:(hf + 1) * 512], ALU.mult),
                         reads=[("B", 0), ("g1b", col)], writes=[("bufA", t % 2, hf)])

            def mixB(t):
                xs = t % 5
                layer_norm_tail(xres[xs], ("xres", xs), lng, lnb, x1buf[t * 128:(t + 1) * 128, :], ("x1buf", t), t % 2)

            def layer_norm_tail(xt, xres_name, g_t, b_t, dst, dst_name, ab):
                bufA = bufAs[ab]
                P.op("dve", lambda e: e.scalar_tensor_tensor(bufA[:], xt[:], ALPHA, bufA[:], ALU.mult, ALU.add),
                     reads=[xres_name, ("bufA", ab, 0), ("bufA", ab, 1)], writes=[("bufA", ab, 0), ("bufA", ab, 1)])
                P.op("dve", lambda e: e.bn_stats(st12[:, 0:6], bufA[:, 0:512]), reads=[("bufA", ab, 0)], writes=[("st12", 0)])
                P.op("dve", lambda e: e.bn_stats(st12[:, 6:12], bufA[:, 512:1024]), reads=[("bufA", ab, 1)], writes=[("st12", 1)])
                P.op("dve", lambda e: e.bn_aggr(mv2[:], st12[:]), reads=[("st12", 0), ("st12", 1)], writes=["mv2"])
                P.op("act", lambda e: e.activation(out=rstd2[:], in_=mv2[:, 1:2], func=AF.Ln, bias=epsb[:, 0:1]),
                     reads=["mv2", "epsb"], writes=["rstd2"])
                P.op("act", lambda e: e.activation(out=rstd2[:], in_=rstd2[:], func=AF.Exp, scale=-0.5), reads=["rstd2"], writes=["rstd2"])
                P.op("dve", lambda e: e.scalar_tensor_tensor(nb2[:], mv2[:, 0:1], -1.0, rstd2[:], ALU.mult, ALU.mult),
                     reads=["mv2", "rstd2"], writes=["nb2"])
                P.op("act", lambda e: e.activation(out=bufB[:], in_=bufA[:], func=AF.Identity, scale=rstd2[:, 0:1], bias=nb2[:, 0:1]),
                     reads=[("bufA", ab, 0), ("bufA", ab, 1), "rstd2", "nb2"], writes=["bufB"])
                P.op("pool", lambda e: e.tensor_tensor(bufB[:], bufB[:], g_t[:], ALU.mult), reads=["bufB", "lng"], writes=["bufB"])
                P.op("pool", lambda e: e.tensor_tensor(bufB[:], bufB[:], b_t[:], ALU.add), reads=["bufB", "lnb"], writes=["bufB"])
                P.dma("sp", dst, bufB[:], reads=["bufB"], writes=[dst_name])

            for t in range(NB, NT):
                inproj(t, kv_only=last)
            if not last:
                for t in range(NB, NT):
                    attn(t)
                    post(t)
                    mixB(t)
            inproj(0)
            if NB > 1:
                inproj(1)
            for n in range(NB + 2):
                ls = []
                if n < NB:
                    ls.append(P.record(attn, n))
                if 0 <= n - 1 < NB:
                    ls.append(P.record(post, n - 1))
                if 0 <= n - 2 < NB:
                    ls.append(P.record(mixB, n - 2))
                if n + 2 < NB:
                    ls.append(P.record(inproj, n + 2))
                P.play(*ls)
            P.flush()
        if stop == "p1":
            return nc

        tiles = list(range(NB)) if last else list(range(NT))
        nt2 = len(tiles)
        NBLK = (2 * nt2 * 128) // BS + NEXP
        AX = mybir.AxisListType.X
        w1v = w1.rearrange("l e p (a k) n -> (l e p a) (k n)", a=4)
        w3v = w3.rearrange("l e p (a k) n -> (l e p a) (k n)", a=4)
        w2v = w2.rearrange("l e p (a k) n -> (l e p a) (k n)", a=4)
        with ExitStack() as st:
            wr_s = alloc(st, "wr_s", [128, KC, 36], F32)
            wr_m = [alloc(st, "wr_m%d" % c, [128, KC, 36], F32) for c in range(2)]
            sh2rep = [alloc(st, "sh2rep%d" % c, [128, KC, 128], F32) for c in range(2)]
            br_row = alloc(st, "br_row", [1, 36], F32)
            lng = alloc(st, "lng2", [128, D], F32)
            lnb = alloc(st, "lnb2", [128, D], F32)
            scb = [alloc(st, "scb%d" % c, [128, D], F32) for c in range(2)]
            shb = [alloc(st, "shb%d" % c, [128, D], F32) for c in range(2)]
            g2b = [alloc(st, "g2b%d" % c, [128, D], F32) for c in range(2)]
            lt_b = alloc(st, "lt_b", [128, 128], BF16)
            ones_b = alloc(st, "ones_b", [128, 128], BF16)
            jvec = alloc(st, "jvec", [128, NBLK_MAX], F32)
            base1 = alloc(st, "base1", [128, 8], F32)
            base2 = alloc(st, "base2", [128, 4], F32)
            x1t = [alloc(st, "x1t%d" % i, [128, D], F32) for i in range(2)]
            x1T = alloc(st, "x1T", [128, KC, 128], F32)
            h2r = [alloc(st, "h2r%d" % i, [128, D], BF16) for i in range(2)]
            lg = alloc(st, "lg", [128, 36], F32)
            sm = alloc(st, "sm", [128, 16], F32)
            goh = alloc(st, "goh", [128, 4], F32)
            gex = alloc(st, "gex", [128, 4], F32)
            esel = alloc(st, "esel", [128, 8], F32)
            esel2 = alloc(st, "esel2", [128, 8], F32)
            eq1 = alloc(st, "eq1", [128, 8], F32)
            eq2 = alloc(st, "eq2", [128, 8], F32)
            oh0 = alloc(st, "oh0", [128, nt2, 32], F32)
            oh1 = alloc(st, "oh1", [128, nt2, 32], F32)
            cntb = alloc(st, "cntb", [128, nt2, 32], BF16)
            gsc = alloc(st, "gsc", [128, nt2, 2], F32)
            rank_s = alloc(st, "rank_s", [128, nt2, 32], F32)
            dtmp = alloc(st, "dtmp", [128, nt2, 32], F32)
            tot = alloc(st, "tot", [128, 32], F32)
            nblk = alloc(st, "nblk", [128, 32], F32)
            sc_a = alloc(st, "sc_a", [128, 32], F32)
            sc_b = alloc(st, "sc_b", [128, 32], F32)
            pstart = alloc(st, "pstart", [128, 32], F32)
            destf = alloc(st, "destf", [128, 2, nt2], F32)
            idx = alloc(st, "idx", [128, 2, nt2], mybir.dt.int32)
            bef = alloc(st, "bef", [128, NBLK_MAX], F32)
            be1 = alloc(st, "be1", [128, NBLK_MAX], F32)
            wif1 = alloc(st, "wif1", [128, NBLK_MAX, 4], F32)
            wi1 = alloc(st, "wi1", [128, NBLK_MAX, 4], mybir.dt.int32)
            w1s = [alloc(st, "w1s%d" % i, [128, KC, DEXP], BF16) for i in range(2)]
            w3s = [alloc(st, "w3s%d" % i, [128, KC, DEXP], BF16) for i in range(2)]
            w2s = [alloc(st, "w2s%d" % i, [128, 4, D], BF16) for i in range(2)]
            xsb = [alloc(st, "xsb%d" % i, [128, 4, D], BF16) for i in range(2)]
            hTb = [alloc(st, "hTb%d" % i, [128, KC, 512], BF16) for i in range(2)]
            sg = [alloc(st, "sg%d" % i, [128, 512], F32) for i in range(2)]
            actT = [alloc(st, "actT%d" % i, [128, 4, 512], BF16) for i in range(2)]
            ysb = [alloc(st, "ysb%d" % i, [128, D], F32) for i in range(2)]
            yg = ysb
            bufA = alloc(st, "bufA2", [128, D], F32)
            bufB = alloc(st, "bufB2", [128, D], F32)
            st12 = alloc(st, "st12b", [128, 12], F32)
            mv2 = alloc(st, "mv2b", [128, 2], F32)
            rstd2 = alloc(st, "rstd2b", [128, 1], F32)
            nb2 = alloc(st, "nb2b", [128, 1], F32)
            B = [None, None] + [palloc(st, "M%d" % i, [128, 512]) for i in range(2, 8)]
            TB = [palloc(st, "TB%d" % i, [128, 1024], BF16) for i in range(2)]

            P.dma("sp", wr_s[:], w_r[l].rearrange("(k p) n -> p k n", p=128), writes=["wr_s"])
            P.dma("sp", br_row[:], b_r[l:l + 1, :], writes=["br_row"])
            P.dma("sp", lng[:], ln2_g[l:l + 1, :].to_broadcast([128, D]), writes=["lng"])
            P.dma("sp", lnb[:], ln2_b[l:l + 1, :].to_broadcast([128, D]), writes=["lnb"])
            for c in range(2):
                P.dma("sp", shb[c][:], modrows[l, c, 1:2, :].to_broadcast([128, D]), writes=[("shb", c)])
                P.dma("sp", scb[c][:], modrows[l, c, 2:3, :].to_broadcast([128, D]), writes=[("scb", c)])
                P.dma("sp", g2b[c][:], modrows[l, c, 3:4, :].to_broadcast([128, D]), writes=[("g2b", c)])
            P.dma("pool", lt_b[:], lt_d[:, :], writes=["lt_b"])
            P.dma("sp", jvec[:], jvec_d[:, :], writes=["jvec"])
            P.dma("sp", base1[:], base1_d[:, :], writes=["base1"])
            P.dma("sp", base2[:], base2_d[:, :], writes=["base2"])
            P.op("pool", lambda e: e.memset(ones_b[:], 1.0), writes=["ones_b"])
            P.op("pool", lambda e: e.memset(xsb[1][:], 0.0), writes=[("xsb", 1)])
            zres = []
            for i in range(NBLK):
                P.dma("pool", xs_dram[i * BS:(i + 1) * BS, :].rearrange("(a p) d -> p a d", p=128), xsb[1][:],
                      reads=[("xsb", 1)], writes=[("xsz", i)])
                zres.append(("xsz", i))
            for c in range(2):
                P.op("dve", lambda e, c=c: e.tensor_tensor(wr_m[c][:], wr_s[:], bc_last(modT[:, 32:40, c], 36), ALU.mult),
                     reads=["wr_s", "modT"], writes=[("wr_m", c)])
                P.op("dve", lambda e, c=c: e.tensor_copy(sh2rep[c][:], bc_last(modT[:, 24:32, c], 128)),
                     reads=["modT"], writes=[("sh2rep", c)])

            st12s = [st12, alloc(st, "st12c", [128, 12], F32)]
            mv2s = [mv2, alloc(st, "mv2c", [128, 2], F32)]
            rstd2s = [rstd2, alloc(st, "rstd2c", [128, 1], F32)]
            nb2s = [nb2, alloc(st, "nb2c", [128, 1], F32)]

            def ln_tail2(xt, xname, dst, dname, bA, bB, se):
                nA, nB = ("bufA", se), ("bufB", se)
                s12, m2, r2, n2 = st12s[se], mv2s[se], rstd2s[se], nb2s[se]
                P.op("dve", lambda e: e.scalar_tensor_tensor(bA[:], xt[:], ALPHA, bA[:], ALU.mult, ALU.add),
                     reads=[xname, nA], writes=[nA])
                P.op("dve", lambda e: e.bn_stats(s12[:, 0:6], bA[:, 0:512]), reads=[nA], writes=[("st12", se, 0)])
                P.op("dve", lambda e: e.bn_stats(s12[:, 6:12], bA[:, 512:1024]), reads=[nA], writes=[("st12", se, 1)])
                P.op("dve", lambda e: e.bn_aggr(m2[:], s12[:]), reads=[("st12", se, 0), ("st12", se, 1)], writes=[("mv2", se)])
                P.op("act", lambda e: e.activation(out=r2[:], in_=m2[:, 1:2], func=AF.Sqrt, bias=LN_EPS),
                     reads=[("mv2", se)], writes=[("rstd2", se)])
                P.op("dve", lambda e: e.reciprocal(r2[:], r2[:]), reads=[("rstd2", se)], writes=[("rstd2", se)])
                P.op("dve", lambda e: e.scalar_tensor_tensor(n2[:], m2[:, 0:1], -1.0, r2[:], ALU.mult, ALU.mult),
                     reads=[("mv2", se), ("rstd2", se)], writes=[("nb2", se)])
                P.op("act", lambda e: e.activation(out=bB[:], in_=bA[:], func=AF.Identity, scale=r2[:, 0:1], bias=n2[:, 0:1]),
                     reads=[nA, ("rstd2", se), ("nb2", se)], writes=[nB])
                P.op("pool", lambda e: e.tensor_tensor(bB[:], bB[:], lng[:], ALU.mult), reads=[nB, "lng"], writes=[nB])
                P.op("pool", lambda e: e.tensor_tensor(bB[:], bB[:], lnb[:], ALU.add), reads=[nB, "lnb"], writes=[nB])
                P.dma("sp", dst, bB[:], reads=[nB], writes=[dname])

            def v1024(tn, nm):
                if nt2 * 32 >= 1024:
                    return tn[:].rearrange("p a b -> p (a b)")[:, 0:1024]
                return alloc(st, nm, [128, D], F32)[:]
            rank_v, dtmp_v, oh0_v = v1024(rank_s, "rank_v"), v1024(dtmp, "dtmp_v"), v1024(oh0, "oh0_v")
            x1Ts = [x1T, rank_v.rearrange("p (k t) -> p k t", k=KC)]
            bufAs = [bufA, dtmp_v]
            lgs = [lg, alloc(st, "lg_b", [128, 36], F32)]
            sms = [sm, alloc(st, "sm_b", [128, 16], F32)]
            gohs = [goh, alloc(st, "goh_b", [128, 4], F32)]
            gexs = [gex, alloc(st, "gex_b", [128, 4], F32)]
            esels = [esel, alloc(st, "esel_b", [128, 8], F32)]
            esel2s = [esel2, alloc(st, "esel2_b", [128, 8], F32)]
            eq1s = [eq1, alloc(st, "eq1_b", [128, 8], F32)]
            eq2s = [eq2, alloc(st, "eq2_b", [128, 8], F32)]

            def stageA(li):
                t = tiles[li]
                col = 0 if t < NB else 1
                xs = li % 2
                se = li % 2
                tpb = 6 if se == 0 else 4
                rtb = 2 if se == 0 else 3
                P.dma("sp", x1t[xs][:], x1buf[t * 128:(t + 1) * 128, :], writes=[("x1t", xs)])
                P.op("dve", lambda e, xs=xs, col=col: e.tensor_tensor(bufAs[se][:], x1t[xs][:], scb[col][:], ALU.mult),
                     reads=[("x1t", xs), ("scb", col)], writes=[("bufA", se)])
                P.op("pool", lambda e, xs=xs, col=col: e.tensor_tensor(h2r[xs][:], bufAs[se][:], shb[col][:], ALU.add),
                     reads=[("bufA", se), ("shb", col)], writes=[("h2r", xs)])
                P.dma("sp", h2buf[li * 128:(li + 1) * 128, :], h2r[xs][:], reads=[("h2r", xs)], writes=[("h2buf", li)])
                for hb in range(2):
                    def tp(e, hb=hb, xs=xs):
                        ins = None
                        for i in range(4):
                            j = hb * 4 + i
                            ins = e.transpose(B[tpb + hb][:, i * 128:(i + 1) * 128], x1t[xs][:, j * 128:(j + 1) * 128], ident_f[:])
                        return ins
                    P.op("pe", tp, reads=[("x1t", xs), "ident_f"], writes=[("B", tpb + hb)])

                    def cpx(e, hb=hb):
                        ins = None
                        for i in range(4):
                            ins = e.activation(out=x1Ts[se][:, hb * 4 + i, :], in_=B[tpb + hb][:, i * 128:(i + 1) * 128], func=AF.Copy)
                        return ins
                    P.op("act", cpx, reads=[("B", tpb + hb)], writes=[("x1T", se, hb)])

                def rt(e, col=col):
                    ins = None
                    for k in range(KC):
                        ins = e.matmul(B[rtb][:, 0:36], lhsT=x1Ts[se][:, k, :], rhs=wr_m[col][:, k, :], start=(k == 0), stop=False)
                    for k in range(KC):
                        ins = e.matmul(B[rtb][:, 0:36], lhsT=sh2rep[col][:, k, :], rhs=wr_s[:, k, :], start=False, stop=False)
                    ins = e.matmul(B[rtb][:, 0:36], lhsT=ones_f[0:1, :], rhs=br_row[0:1, :], start=False, stop=True)
                    return ins
                P.op("pe", rt, reads=[("x1T", se, 0), ("x1T", se, 1), ("wr_m", col), ("sh2rep", col), "wr_s", "br_row", "ones_f"], writes=[("B", rtb)])
                P.op("act", lambda e: e.activation(out=lgs[se][:], in_=B[rtb][:, 0:36], func=AF.Copy), reads=[("B", rtb)], writes=[("lg", se)])
                P.op("dve", lambda e: e.reduce_max(sms[se][:, 0:1], lgs[se][:, 0:4], AX), reads=[("lg", se)], writes=[("sm", se)])
                P.op("dve", lambda e: e.tensor_scalar(sms[se][:, 1:2], sms[se][:, 0:1], -1.0, None, ALU.mult), reads=[("sm", se)], writes=[("sm", se)])
                P.op("act", lambda e: e.activation(out=gexs[se][:], in_=lgs[se][:, 0:4], func=AF.Exp, bias=sms[se][:, 1:2], accum_out=sms[se][:, 2:3]),
                     reads=[("lg", se), ("sm", se)], writes=[("gex", se), ("sm", se)])
                P.op("dve", lambda e: e.reciprocal(sms[se][:, 3:4], sms[se][:, 2:3]), reads=[("sm", se)], writes=[("sm", se)])
                P.op("dve", lambda e: e.tensor_scalar(gohs[se][:], lgs[se][:, 0:4], sms[se][:, 0:1], None, ALU.is_equal), reads=[("lg", se), ("sm", se)], writes=[("goh", se)])
                P.op("dve", lambda e: e.tensor_scalar(esels[se][:], lgs[se][:, 4:12], gohs[se][:, 0:1], None, ALU.mult), reads=[("lg", se), ("goh", se)], writes=[("esel", se)])
                for g in range(1, 4):
                    P.op("dve", lambda e, g=g: e.scalar_tensor_tensor(esels[se][:], lgs[se][:, 4 + g * 8:12 + g * 8], gohs[se][:, g:g + 1], esels[se][:], ALU.mult, ALU.add),
                         reads=[("lg", se), ("goh", se), ("esel", se)], writes=[("esel", se)])
                P.op("dve", lambda e: e.reduce_max(sms[se][:, 4:5], esels[se][:], AX), reads=[("esel", se)], writes=[("sm", se)])
                P.op("dve", lambda e: e.tensor_scalar(eq1s[se][:], esels[se][:], sms[se][:, 4:5], None, ALU.is_equal), reads=[("esel", se), ("sm", se)], writes=[("eq1", se)])
                P.op("dve", lambda e: e.scalar_tensor_tensor(esel2s[se][:], eq1s[se][:], -1e30, esels[se][:], ALU.mult, ALU.add), reads=[("eq1", se), ("esel", se)], writes=[("esel2", se)])
                P.op("dve", lambda e: e.reduce_max(sms[se][:, 5:6], esel2s[se][:], AX), reads=[("esel2", se)], writes=[("sm", se)])
                P.op("dve", lambda e: e.tensor_scalar(eq2s[se][:], esel2s[se][:], sms[se][:, 5:6], None, ALU.is_equal), reads=[("esel2", se), ("sm", se)], writes=[("eq2", se)])
                P.op("dve", lambda e: e.tensor_tensor(sms[se][:, 6:7], sms[se][:, 4:5], sms[se][:, 5:6], ALU.subtract), reads=[("sm", se)], writes=[("sm", se)])
                P.op("act", lambda e: e.activation(out=sms[se][:, 7:8], in_=sms[se][:, 6:7], func=AF.Sigmoid), reads=[("sm", se)], writes=[("sm", se)])
                P.op("act", lambda e: e.activation(out=sms[se][:, 8:9], in_=sms[se][:, 6:7], func=AF.Sigmoid, scale=-1.0), reads=[("sm", se)], writes=[("sm", se)])
                P.op("dve", lambda e, li=li: e.tensor_scalar(gsc[:, li, :], sms[se][:, 7:9], sms[se][:, 3:4], None, ALU.mult), reads=[("sm", se)], writes=[("gsc", li)])
                P.op("dve", lambda e, li=li: e.tensor_tensor(oh0[:, li, :].rearrange("p (g x) -> p g x", g=4), bc_last(gohs[se][:], 8), bc_mid(eq1s[se][:], 4), ALU.mult),
                     reads=[("goh", se), ("eq1", se)], writes=[("oh0", li)])
                P.op("dve", lambda e, li=li: e.tensor_tensor(oh1[:, li, :].rearrange("p (g x) -> p g x", g=4), bc_last(gohs[se][:], 8), bc_mid(eq2s[se][:], 4), ALU.mult),
                     reads=[("goh", se), ("eq2", se)], writes=[("oh1", li)])
                P.op("pool", lambda e, li=li: e.tensor_tensor(cntb[:, li, :], oh0[:, li, :], oh1[:, li, :], ALU.add),
                     reads=[("oh0", li), ("oh1", li)], writes=[("cntb", li)])


            for li0 in range(0, nt2, 2):
                P.play(P.record(stageA, li0), P.record(stageA, li0 + 1) if li0 + 1 < nt2 else [])
            if stop == "p2a":
                P.flush()
                return nc
            call = [("cntb", i) for i in range(nt2)]
            nbank = (nt2 + 15) // 16
            for li in range(nt2):
                bk, off = 3 + li // 16, (li % 16) * 32

                def rk(e, li=li, bk=bk, off=off):
                    ins = None
                    for tp_ in range(li):
                        ins = e.matmul(B[bk][:, off:off + 32], lhsT=ones_b[:], rhs=cntb[:, tp_, :], start=(tp_ == 0), stop=False)
                    ins = e.matmul(B[bk][:, off:off + 32], lhsT=lt_b[:], rhs=cntb[:, li, :], start=(li == 0), stop=True)
                    return ins
                P.op("pe", rk, reads=call + ["ones_b", "lt_b"], writes=[("RK", li)])
            for bk in range(nbank):
                n_here = min(16, nt2 - bk * 16)
                P.op("act", lambda e, bk=bk, n_here=n_here: e.activation(
                    out=rank_s[:, bk * 16:bk * 16 + n_here, :], in_=B[3 + bk][:, 0:n_here * 32].rearrange("p (t x) -> p t x", x=32), func=AF.Copy),
                    reads=[("RK", i) for i in range(bk * 16, bk * 16 + n_here)], writes=[("rank_s", bk)])

            def tt(e):
                ins = None
                for tp_ in range(nt2):
                    ins = e.matmul(B[2][:, 64:96], lhsT=ones_b[:], rhs=cntb[:, tp_, :], start=(tp_ == 0), stop=(tp_ == nt2 - 1))
                return ins
            P.op("pe", tt, reads=call + ["ones_b"], writes=[("B", 2)])
            P.op("act", lambda e: e.activation(out=tot[:], in_=B[2][:, 64:96], func=AF.Copy), reads=[("B", 2)], writes=["tot"])
            P.op("dve", lambda e: e.tensor_scalar(nblk[:], tot[:], 0.0, None, ALU.is_gt), reads=["tot"], writes=["nblk"])
            for m in range(1, (2 * nt2 * 128) // BS + 1):
                P.op("dve", lambda e, m=m: e.scalar_tensor_tensor(nblk[:], tot[:], float(m * BS), nblk[:], ALU.is_gt, ALU.add),
                     reads=["tot", "nblk"], writes=["nblk"])
            P.op("dve", lambda e: e.tensor_copy(sc_a[:], nblk[:]), reads=["nblk"], writes=["sc_a"])
            cur, oth, cn, on = sc_a, sc_b, "sc_a", "sc_b"
            for dd in (1, 2, 4, 8, 16):
                P.op("dve", lambda e, cur=cur, oth=oth, dd=dd: e.tensor_copy(oth[:, 0:dd], cur[:, 0:dd]), reads=[cn], writes=[on])
                P.op("dve", lambda e, cur=cur, oth=oth, dd=dd: e.tensor_tensor(oth[:, dd:32], cur[:, dd:32], cur[:, 0:32 - dd], ALU.add),
                     reads=[cn, on], writes=[on])
                cur, oth, cn, on = oth, cur, on, cn
            pend, pendn = cur, cn
            P.op("dve", lambda e: e.tensor_tensor(pstart[:], pend[:], nblk[:], ALU.subtract), reads=[pendn, "nblk"], writes=["pstart"])
            P.op("dve", lambda e: e.tensor_scalar(pstart[:], pstart[:], float(BS), None, ALU.mult), reads=["pstart"], writes=["pstart"])
            rk_all = [("rank_s", i) for i in range(nbank)]
            P.op("dve", lambda e: e.tensor_tensor(rank_s[:], rank_s[:], bc_mid(pstart[:], nt2), ALU.add),
                 reads=rk_all + ["pstart"], writes=rk_all)
            for k, oh in enumerate((oh0, oh1)):
                P.op("dve", lambda e, oh=oh: e.tensor_tensor(dtmp[:], oh[:], rank_s[:], ALU.mult),
                     reads=rk_all + [("oh%d" % k, i) for i in range(nt2)], writes=["dtmp"])
                P.op("dve", lambda e, k=k: e.reduce_sum(destf[:, k, :], dtmp[:], AX), reads=["dtmp"], writes=[("destf", k)])
            P.op("dve", lambda e: e.tensor_copy(idx[:], destf[:]), reads=[("destf", 0), ("destf", 1)], writes=["idx"])
            P.op("dve", lambda e: e.tensor_scalar(bef[:], jvec[:], pend[:, 0:1], None, ALU.is_ge), reads=["jvec", pendn], writes=["bef"])
            for ee in range(1, 32):
                P.op("dve", lambda e, ee=ee: e.scalar_tensor_tensor(bef[:], jvec[:], pend[:, ee:ee + 1], bef[:], ALU.is_ge, ALU.add),
                     reads=["jvec", pendn, "bef"], writes=["bef"])
            P.op("dve", lambda e: e.tensor_scalar(bef[:], bef[:], 31.0, None, ALU.min), reads=["bef"], writes=["bef"])
            P.op("dve", lambda e: e.tensor_scalar(be1[:], bef[:], 128.0, float(l * NEXP * 128), ALU.mult, ALU.add), reads=["bef"], writes=["be1"])
            P.op("dve", lambda e: e.tensor_scalar(be1[:], be1[:], base1[:, 0:1], None, ALU.add), reads=["be1", "base1"], writes=["be1"])
            P.op("dve", lambda e: e.tensor_scalar(be1[:], be1[:], 4.0, None, ALU.mult), reads=["be1"], writes=["be1"])
            P.op("dve", lambda e: e.tensor_tensor(wif1[:], bc_last(be1[:], 4), bc_mid(base2[:], NBLK_MAX), ALU.add),
                 reads=["be1", "base2"], writes=["wif1"])
            P.op("dve", lambda e: e.tensor_copy(wi1[:], wif1[:]), reads=["wif1"], writes=["wi1"])
            if stop == "p2c":
                P.flush()
                return nc
            sres = []
            for li in range(nt2):
                hs_ = li % 2
                P.dma("sp", h2r[hs_][:], h2buf[li * 128:(li + 1) * 128, :], reads=[("h2buf", li)], writes=[("h2r", hs_)])
                for k in range(2):
                    P.idma(xs_dram[0:NBLK * BS, :], bass.IndirectOffsetOnAxis(ap=idx[:, k, li:li + 1], axis=0), h2r[hs_][:], None,
                           reads=[("h2r", hs_), "idx"] + zres, writes=[("xss", li, k)])
                    sres.append(("xss", li, k))

            if stop == "p2d":
                P.flush()
                return nc
            yres = []

            def blockA(j):
                ws = j % 2
                if not DBG.get("no_wgather"):
                    for a in range(4):
                        io = bass.IndirectOffsetOnAxis(ap=wi1[:, j, a:a + 1], axis=0)
                        P.idma(w1s[ws][:, 2 * a:2 * a + 2, :].rearrange("p k n -> p (k n)"), None, w1v, io, reads=["wi1"], writes=[("w1s", ws, a)])
                        P.idma(w3s[ws][:, 2 * a:2 * a + 2, :].rearrange("p k n -> p (k n)"), None, w3v, io, reads=["wi1"], writes=[("w3s", ws, a)])
                        P.idma(w2s[ws][:, a, :], None, w2v, io, reads=["wi1"], writes=[("w2s", ws, a)])
                P.dma("sp", xsb[ws][:], xs_dram[j * BS:(j + 1) * BS, :].rearrange("(a p) d -> p a d", p=128),
                      reads=sres + zres, writes=[("xsb", ws)])
                for a in range(4):
                    tb = a % 2

                    def tps_(e, a=a, tb=tb, ws=ws):
                        ins = None
                        for k in range(KC):
                            ins = e.transpose(TB[tb][:, k * 128:(k + 1) * 128], xsb[ws][:, a, k * 128:(k + 1) * 128], ident_b[:])
                        return ins
                    P.op("pe", tps_, reads=[("xsb", ws), "ident_b"], writes=[("TB", tb)])
                    P.op("act", lambda e, a=a, tb=tb, ws=ws: e.activation(out=hTb[ws][:, :, a * 128:(a + 1) * 128],
                                                                         in_=TB[tb][:, :].rearrange("p (k t) -> p k t", k=KC), func=AF.Copy),
                         reads=[("TB", tb)], writes=[("hTb", ws, a)])

            def blockB(j):
                ws = j % 2
                hres = [("hTb", ws, a) for a in range(4)]
                w13 = [("w1s", ws, a) for a in range(4)] + [("w3s", ws, a) for a in range(4)]
                for c in range(4):
                    pb = 2 + 2 * (c % 2)

                    def h13(e, c=c, pb=pb, ws=ws):
                        ins = None
                        for k in range(KC):
                            ins = e.matmul(B[pb][:, :], lhsT=w1s[ws][:, k, c * 128:(c + 1) * 128], rhs=hTb[ws][:, k, :],
                                           start=(k == 0), stop=(k == KC - 1))
                        for k in range(KC):
                            ins = e.matmul(B[pb + 1][:, :], lhsT=w3s[ws][:, k, c * 128:(c + 1) * 128], rhs=hTb[ws][:, k, :],
                                           start=(k == 0), stop=(k == KC - 1))
                        return ins
                    P.op("pe", h13, reads=w13 + hres, writes=[("B", pb), ("B", pb + 1)])
                    P.op("act", lambda e, c=c, pb=pb: e.activation(out=sg[c % 2][:], in_=B[pb][:, :], func=AF.Silu),
                         reads=[("B", pb)], writes=[("sg", c % 2)])
                    P.op("dve", lambda e, c=c, pb=pb, ws=ws: e.tensor_tensor(actT[ws][:, c, :], sg[c % 2][:], B[pb + 1][:, :], ALU.mult),
                         reads=[("sg", c % 2), ("B", pb + 1)], writes=[("actT", ws, c)])
                for a in range(4):
                    ys_ = a % 2
                    for hf in range(2):
                        yb = 6 + hf

                        def ymm(e, a=a, hf=hf, yb=yb, ws=ws):
                            ins = None
                            for c in range(4):
                                ins = e.matmul(B[yb][:, :], lhsT=actT[ws][:, c, a * 128:(a + 1) * 128],
                                               rhs=w2s[ws][:, c, hf * 512:(hf + 1) * 512], start=(c == 0), stop=(c == 3))
                            return ins
                        P.op("pe", ymm, reads=[("actT", ws, c) for c in range(4)] + [("w2s", ws, c) for c in range(4)], writes=[("B", yb)])
                        P.op("dve", lambda e, hf=hf, yb=yb, ys_=ys_: e.tensor_copy(ysb[ys_][:, hf * 512:(hf + 1) * 512], B[yb][:, :]),
                             reads=[("B", yb)], writes=[("ysb", ys_, hf)])
                    r0 = j * BS + a * 128
                    P.dma("pool", ys_dram[r0:r0 + 128, :], ysb[ys_][:], reads=[("ysb", ys_, 0), ("ysb", ys_, 1)], writes=[("ys", j, a)])
                    yres.append(("ys", j, a))


            P.play(P.record(blockA, 0))
            for j in range(NBLK):
                P.play(P.record(blockB, j), P.record(blockA, j + 1) if j + 1 < NBLK else [])
            if stop == "p2e":
                P.flush()
                return nc
            fA = [bufA, x1T[:].rearrange("p a b -> p (a b)")]
            fB = [bufB, dtmp_v]
            fY = [[ysb[0], ysb[1]], [rank_v, oh0_v]]

            def stageF(li):
                t = tiles[li]
                col = 0 if t < NB else 1
                xs = li % 2
                se = li % 2
                bA, bB, ygs = fA[se], fB[se], fY[se]
                nA = ("bufA", se)
                P.dma("sp", x1t[xs][:], x1buf[t * 128:(t + 1) * 128, :], writes=[("x1t", xs)])
                for k in range(2):
                    P.idma(ygs[k][:], None, ys_dram[0:NBLK * BS, :], bass.IndirectOffsetOnAxis(ap=idx[:, k, li:li + 1], axis=0),
                           reads=["idx"] + yres, writes=[("yg", se, k)])
                P.op("dve", lambda e: e.tensor_scalar(bA[:], ygs[0][:], gsc[:, li, 0:1], None, ALU.mult),
                     reads=[("yg", se, 0), ("gsc", li)], writes=[nA])
                P.op("dve", lambda e: e.scalar_tensor_tensor(bA[:], ygs[1][:], gsc[:, li, 1:2], bA[:], ALU.mult, ALU.add),
                     reads=[("yg", se, 1), ("gsc", li), nA], writes=[nA])
                P.op("pool", lambda e: e.tensor_tensor(bA[:], bA[:], g2b[col][:], ALU.mult),
                     reads=[nA, ("g2b", col)], writes=[nA])
                if last:
                    dst, dname = out_d[t * 128:(t + 1) * 128, :], ("out", t)
                else:
                    dst, dname = x2buf[t * 128:(t + 1) * 128, :], ("x2buf", t)
                ln_tail2(x1t[xs], ("x1t", xs), dst, dname, bA, bB, se)

            for li0 in range(0, nt2, 2):
                P.play(P.record(stageF, li0), P.record(stageF, li0 + 1) if li0 + 1 < nt2 else [])
            P.flush()
    glob.close()
    return nc


def _rope_tables(NB):
    NT = NB + NCB
    S = NB * 128
    m = 16
    freqs = (10000.0 ** (-np.arange(m, dtype=np.float32) / m)).astype(np.float32)
    tpos = np.arange(S)
    row = (tpos // GRID_W).astype(np.float32)
    colp = (tpos % GRID_W).astype(np.float32)
    cos = np.ones((128, NT * 128), np.float32)
    sin = np.zeros((128, NT * 128), np.float32)
    for p in range(128):
        d = p % 64
        pos = row if d < 32 else colp
        dd = d % 32
        f = freqs[dd % 16]
        ang = (pos * f).astype(np.float32)
        cos[p, :S] = np.cos(ang)
        sgn = -1.0 if dd < 16 else 1.0
        sin[p, :S] = sgn * np.sin(ang)
    cos_t = np.ascontiguousarray(cos.reshape(128, NT, 128).transpose(1, 0, 2))
    sin_t = np.ascontiguousarray(sin.reshape(128, NT, 128).transpose(1, 0, 2))
    return cos_t, sin_t


def make_in_maps(inputs, NB, ncores):
    f = lambda a: np.ascontiguousarray(np.asarray(a, dtype=np.float32))
    x = f(inputs["x"]); c = f(inputs["c"]); ctx = f(inputs["ctx"]); c_ctx = f(inputs["c_ctx"])
    depth = inputs["w_ada"].shape[0]
    cos_t, sin_t = _rope_tables(NB)
    j = np.arange(128)
    maskl = np.where(j[:, None] >= j[None, :], 0.0, NEG).astype(np.float32)
    maskr = np.where(j[:, None] <= j[None, :], 0.0, NEG).astype(np.float32)
    shared = {
        "w_ada": f(inputs["w_ada"]),
        "bada_p": f(np.asarray(inputs["b_ada"]).reshape(depth, 48, 128).transpose(0, 2, 1)),
        "b_ada": f(inputs["b_ada"]),
        "w_in": f(inputs["w_in"]),
        "convw": f(np.asarray(inputs["conv_w"]).reshape(depth, 3, 2, 128).transpose(0, 3, 2, 1)),
        "sink": f(inputs["attn_sink"]),
        "gm_wsT": f(np.asarray(inputs["gm_ws"]).transpose(0, 1, 3, 2)),
        "gm_bsp": f(np.asarray(inputs["gm_bs"]).transpose(0, 2, 1)),
        "w_out": f(inputs["w_out"]),
        "ln1_g": f(inputs["ln1_g"]), "ln1_b": f(inputs["ln1_b"]),
        "ln2_g": f(inputs["ln2_g"]), "ln2_b": f(inputs["ln2_b"]),
        "w_r": f(np.concatenate([np.asarray(inputs["w_rg"]), np.asarray(inputs["w_re"])], axis=-1)),
        "b_r": f(np.concatenate([np.asarray(inputs["b_rg"]), np.asarray(inputs["b_re"])], axis=-1)),
        "w1": f(np.asarray(inputs["w1"]).reshape(depth, NEXP, KC, 128, DEXP).transpose(0, 1, 3, 2, 4)),
        "w3": f(np.asarray(inputs["w3"]).reshape(depth, NEXP, KC, 128, DEXP).transpose(0, 1, 3, 2, 4)),
        "w2": f(np.asarray(inputs["w2"]).reshape(depth, NEXP, 4, 128, D).transpose(0, 1, 3, 2, 4)),
        "cos_t": cos_t, "sin_t": sin_t,
        "ident": np.eye(128, dtype=np.float32), "maskl": maskl, "maskr": maskr,
        "lt_d": (j[:, None] < j[None, :]).astype(np.float32),
        "jvec_d": np.tile(np.arange((2 * (NB + NCB) * 128) // 512 + NEXP, dtype=np.float32)[None, :], (128, 1)),
        "base1_d": (np.arange(8, dtype=np.float32)[None, :] * 128 + j[:, None]).astype(np.float32),
        "base2_d": np.tile(np.arange(4, dtype=np.float32)[None, :], (128, 1)),
    }
    maps = []
    for b in range(ncores):
        cv = np.stack([c[b].reshape(KC, 128).T, c_ctx.reshape(KC, 128).T], axis=-1)
        m = dict(shared)
        m["xin"] = np.ascontiguousarray(x[b])
        m["ctxin"] = np.ascontiguousarray(ctx[b])
        m["cvec"] = np.ascontiguousarray(cv.astype(np.float32))
        maps.append(m)
    return maps


_NC_CACHE = {}


def kernel(**inputs):
    x = np.asarray(inputs["x"])
    Bn, S, _ = x.shape
    NB = S // 128
    key = (NB,)
    if key not in _NC_CACHE:
        _NC_CACHE[key] = build_program(NB)
    nc = _NC_CACHE[key]
    maps = make_in_maps(inputs, NB, Bn)
    res = run_bass_kernel_spmd(nc, maps, core_ids=list(range(Bn)))
    return np.stack([np.asarray(r["out"], dtype=np.float32) for r in res.results], axis=0)
```

```python
from contextlib import ExitStack
import numpy as np
import ml_dtypes
import concourse.bass as bass
import concourse.mybir as mybir
from concourse.bass_utils import run_bass_kernel_spmd

F32 = mybir.dt.float32
BF16 = mybir.dt.bfloat16
AF = mybir.ActivationFunctionType
ALU = mybir.AluOpType

D = 1024
KC = 8
CTX = 256
NCB = CTX // 128
DEPTH = 2
NEXP = 32
DEXP = 512
ALPHA = (2 * DEPTH) ** 0.25
LN_EPS = 1e-6
GRID_W = 64
NEG = -30000.0
DBG = {}


class Prog:
    NDMA = 8

    def __init__(self, nc):
        self.nc = nc
        self.names = dict(pe="tensor", act="scalar", dve="vector", pool="gpsimd", sp="sync")
        self.sems = []
        self.semidx = {}
        for k in self.names:
            self.semidx[k] = len(self.sems)
            self.sems.append(nc.alloc_semaphore("s_" + k))
        self.cnt = {k: 0 for k in self.names}
        self.dq = {}
        for q in ("sp", "pool"):
            idx = []
            for i in range(self.NDMA):
                idx.append(len(self.sems))
                self.sems.append(nc.alloc_semaphore("d_%s%d" % (q, i)))
            self.dq[q] = dict(idx=idx, cnt=[0] * self.NDMA, nxt=0)
        self.seen = {k: {} for k in self.names}
        self.stream = {k: [] for k in self.names}
        self.res = {}
        self._cap = None

    def record(self, fn, *a, **kw):
        self._cap = []
        fn(*a, **kw)
        cap, self._cap = self._cap, None
        return cap

    def play(self, *lists):
        lists = [l for l in lists if l]
        pos = [0] * len(lists)
        while True:
            best, bf = None, None
            for i, l in enumerate(lists):
                if pos[i] < len(l):
                    f = pos[i] / len(l)
                    if bf is None or f < bf:
                        best, bf = i, f
            if best is None:
                break
            kind, a, kw = lists[best][pos[best]]
            pos[best] += 1
            getattr(self, kind)(*a, **kw)

    def _deps(self, reads, writes):
        deps = []
        for r in reads:
            e = self.res.get(r)
            if e and e["w"] is not None:
                deps.append(e["w"])
        for w in writes:
            e = self.res.get(w)
            if e:
                if e["w"] is not None:
                    deps.append(e["w"])
                deps.extend((si, v, src) for (si, src), v in e["r"].items())
        return deps

    def _waits(self, ek, deps):
        waits = {}
        for (si, val, src) in deps:
            if src == "pe" and ek == "pe":
                continue
            if self.seen[ek].get(si, 0) >= val:
                continue
            waits[si] = max(waits.get(si, 0), val)
        for si, v in waits.items():
            self.seen[ek][si] = v
        return list(waits.items())

    def _record(self, token, reads, writes):
        si, val, src = token
        for r in reads:
            e = self.res.setdefault(r, {"w": None, "r": {}})
            e["r"][(si, src)] = max(e["r"].get((si, src), 0), val)
        for w in writes:
            self.res[w] = {"w": token, "r": {}}

    def op(self, ek, fn, reads=(), writes=()):
        if self._cap is not None:
            self._cap.append(("op", (ek, fn), dict(reads=reads, writes=writes)))
            return None
        waits = self._waits(ek, self._deps(reads, writes))
        self.cnt[ek] += 1
        si = self.semidx[ek]
        token = (si, self.cnt[ek], ek)
        sems = self.sems

        def emit(eng):
            for (wsi, v) in waits:
                eng.wait_ge(sems[wsi], v)
            ins = fn(eng)
            ins.then_inc(sems[si], 1)

        self.stream[ek].append(emit)
        self._record(token, reads, writes)
        return token

    def dma(self, q, out, in_, reads=(), writes=()):
        if self._cap is not None:
            self._cap.append(("dma", (q, out, in_), dict(reads=reads, writes=writes)))
            return None
        d = self.dq[q]
        slot = d["nxt"]
        d["nxt"] = (slot + 1) % self.NDMA
        si = d["idx"][slot]
        deps = self._deps(reads, writes)
        if d["cnt"][slot] > 0:
            deps.append((si, 16 * d["cnt"][slot], "dma"))
        waits = self._waits(q, deps)
        d["cnt"][slot] += 1
        token = (si, 16 * d["cnt"][slot], "dma")
        sems = self.sems

        def emit(eng):
            for (wsi, v) in waits:
                eng.wait_ge(sems[wsi], v)
            eng.dma_start(out=out, in_=in_).then_inc(sems[si], 16)

        self.stream[q].append(emit)
        self._record(token, reads, writes)
        return token

    def idma(self, out, out_off, in_, in_off, reads=(), writes=()):
        if self._cap is not None:
            self._cap.append(("idma", (out, out_off, in_, in_off), dict(reads=reads, writes=writes)))
            return None
        q = "pool"
        d = self.dq[q]
        slot = d["nxt"]
        d["nxt"] = (slot + 1) % self.NDMA
        si = d["idx"][slot]
        deps = self._deps(reads, writes)
        if d["cnt"][slot] > 0:
            deps.append((si, 16 * d["cnt"][slot], "dma"))
        waits = self._waits(q, deps)
        d["cnt"][slot] += 1
        token = (si, 16 * d["cnt"][slot], "dma")
        sems = self.sems

        def emit(eng):
            for (wsi, v) in waits:
                eng.wait_ge(sems[wsi], v)
            eng.indirect_dma_start(out=out, out_offset=out_off, in_=in_, in_offset=in_off).then_inc(sems[si], 16)

        self.stream[q].append(emit)
        self._record(token, reads, writes)
        return token

    def flush(self):
        finals = []
        for k in self.names:
            if self.cnt[k] > 0:
                finals.append((self.semidx[k], self.cnt[k]))
        for q, d in self.dq.items():
            for i, si in enumerate(d["idx"]):
                if d["cnt"][i] > 0:
                    finals.append((si, 16 * d["cnt"][i]))
        with self.nc.Block() as block:
            for ek, nm in self.names.items():
                stream = self.stream[ek]
                seen = self.seen[ek]
                sems = self.sems

                def body(eng, stream=stream, seen=seen):
                    for emit in stream:
                        emit(eng)
                    for si, v in finals:
                        if seen.get(si, 0) < v:
                            eng.wait_ge(sems[si], v)
                            seen[si] = v

                getattr(block, nm)(body)
        self.stream = {k: [] for k in self.names}
        self.res = {}


def bc_mid(ap, n):
    p, f = ap.shape
    return ap.unsqueeze(1).to_broadcast([p, n, f])


def bc_last(ap, n):
    p, g = ap.shape
    return ap.unsqueeze(2).to_broadcast([p, g, n])


def build_program(NB, depth=DEPTH, tps=9, stop=None, nexp=NEXP, do_router=True, do_epi=True):
    S = NB * 128
    NT = NB + NCB
    T = NT * 128
    nc = bass.Bass("TRN2", target_bir_lowering=False)

    def din(name, shape, dt=F32):
        return nc.dram_tensor(name, list(shape), dt, kind="ExternalInput").ap()

    xin = din("xin", [S, D])
    ctxin = din("ctxin", [CTX, D])
    cvec = din("cvec", [128, KC, 2])
    w_ada = din("w_ada", [depth, D, 6 * D])
    bada_p = din("bada_p", [depth, 128, 48])
    b_ada = din("b_ada", [depth, 6 * D])
    w_in = din("w_in", [depth, D, 2048])
    convw = din("convw", [depth, 128, 2, 3])
    sink = din("sink", [depth, 8])
    gm_wsT = din("gm_wsT", [depth, 4, 128, 128])
    gm_bsp = din("gm_bsp", [depth, 128, 4])
    w_out = din("w_out", [depth, D, D])
    ln1_g = din("ln1_g", [depth, D])
    ln1_b = din("ln1_b", [depth, D])
    ln2_g = din("ln2_g", [depth, D])
    ln2_b = din("ln2_b", [depth, D])
    w_r = din("w_r", [depth, D, 36])
    b_r = din("b_r", [depth, 36])
    w1 = din("w1", [depth, NEXP, 128, KC, DEXP])
    w3 = din("w3", [depth, NEXP, 128, KC, DEXP])
    w2 = din("w2", [depth, NEXP, 128, 4, D])
    cos_t = din("cos_t", [NT, 128, 128])
    sin_t = din("sin_t", [NT, 128, 128])
    ident_d = din("ident", [128, 128])
    maskl_d = din("maskl", [128, 128])
    maskr_d = din("maskr", [128, 128])
    out_d = nc.dram_tensor("out", [S, D], F32, kind="ExternalOutput").ap()
    x1buf = nc.dram_tensor("x1buf", [T, D], F32).ap()
    x2buf = nc.dram_tensor("x2buf", [T, D], F32).ap()
    BS = 512
    NBLK_MAX = (2 * T) // BS + NEXP
    h2buf = nc.dram_tensor("h2buf", [T, D], BF16).ap()
    xs_dram = nc.dram_tensor("xs_dram", [NBLK_MAX * BS, D], BF16).ap()
    ys_dram = nc.dram_tensor("ys_dram", [NBLK_MAX * BS, D], F32).ap()
    modrows = nc.dram_tensor("modrows", [depth, 2, 4, D], F32).ap()
    lt_d = din("lt_d", [128, 128])
    jvec_d = din("jvec_d", [128, NBLK_MAX])
    base1_d = din("base1_d", [128, 8])
    base2_d = din("base2_d", [128, 4])

    P = Prog(nc)

    def tile_src(l, t):
        if l == 0:
            if t < NB:
                return xin[t * 128:(t + 1) * 128, :]
            return ctxin[(t - NB) * 128:(t - NB + 1) * 128, :]
        return x2buf[t * 128:(t + 1) * 128, :]

    glob = ExitStack()

    uid = [0]

    def alloc(st, name, shape, dt):
        uid[0] += 1
        return st.enter_context(nc.sbuf_tensor("sb%d_%s" % (uid[0], name), list(shape), dt))

    def palloc(st, name, shape, dt=F32):
        uid[0] += 1
        return st.enter_context(nc.psum_tensor("ps%d_%s" % (uid[0], name), list(shape), dt))

    ident_f = alloc(glob, "ident_f", [128, 128], F32)
    ident_b = alloc(glob, "ident_b", [128, 128], BF16)
    maskl = alloc(glob, "maskl", [128, 128], BF16)
    maskr = alloc(glob, "maskr", [128, 128], BF16)
    cact = alloc(glob, "cact", [128, KC, 2], F32)
    cact_rep = alloc(glob, "cact_rep", [128, KC, 2, 128], F32)
    ones_f = alloc(glob, "ones_f", [1, 128], F32)
    epsb = alloc(glob, "epsb", [128, 1], F32)
    modT = alloc(glob, "modT", [128, 48, 2], F32)

    P.dma("sp", ident_f[:], ident_d[:, :], writes=["ident_f"])
    P.dma("pool", ident_b[:], ident_d[:, :], writes=["ident_b"])
    P.dma("pool", maskl[:], maskl_d[:, :], writes=["maskl"])
    P.dma("pool", maskr[:], maskr_d[:, :], writes=["maskr"])
    P.dma("sp", cact[:], cvec[:, :, :], writes=["cact"])
    P.op("act", lambda e: e.activation(out=cact[:], in_=cact[:], func=AF.Silu), reads=["cact"], writes=["cact"])
    P.op("dve", lambda e: e.tensor_copy(cact_rep[:].rearrange("p k c m -> p (k c) m"),
                                        bc_last(cact[:].rearrange("p k c -> p (k c)"), 128)),
         reads=["cact"], writes=["cact_rep"])
    P.op("dve", lambda e: e.memset(ones_f[:], 1.0), writes=["ones_f"])
    P.op("dve", lambda e: e.memset(epsb[:], LN_EPS), writes=["epsb"])
    P.flush()
    if stop == "const":
        return nc

    for l in range(depth):
        last = (l == depth - 1)
        with ExitStack() as st:
            wa = [alloc(st, "wa%d" % i, [128, KC, D], F32) for i in range(2)]
            bada = alloc(st, "bada", [128, 48], F32)
            brow = [alloc(st, "brow%d" % i, [128, D], F32) for i in range(2)]
            grow = [alloc(st, "grow%d" % i, [128, D], F32) for i in range(2)]
            ps_mod = palloc(st, "ps_mod", [128, 96])
            ps_g = [palloc(st, "ps_g%d" % i, [128, 512]) for i in range(4)]
            P.dma("sp", bada[:], bada_p[l], writes=["bada"])
            for i in range(6):
                s = i % 2
                P.dma("sp", wa[s][:], w_ada[l][:, i * D:(i + 1) * D].rearrange("(k p) n -> p k n", p=128),
                      writes=[("wa", s)])

                def mm_mod(e, i=i, s=s):
                    ins = None
                    for j in range(KC):
                        for k in range(KC):
                            ins = e.matmul(ps_mod[:, (i * 8 + j) * 2:(i * 8 + j) * 2 + 2],
                                           lhsT=wa[s][:, k, j * 128:(j + 1) * 128], rhs=cact[:, k, :],
                                           start=(k == 0), stop=(k == KC - 1))
                    return ins
                P.op("pe", mm_mod, reads=[("wa", s), "cact"], writes=[("ps_mod", i)])
                if i >= 2:
                    gi = i - 2
                    P.dma("sp", brow[gi % 2][:], b_ada[l:l + 1, i * D:(i + 1) * D].to_broadcast([128, D]),
                          writes=[("brow", gi % 2)])
                    for c in range(2):
                        for h in range(2):
                            def mm_g(e, s=s, c=c, h=h):
                                ins = None
                                for k in range(KC):
                                    ins = e.matmul(ps_g[c * 2 + h][:, :], lhsT=cact_rep[:, k, c, :],
                                                   rhs=wa[s][:, k, h * 512:(h + 1) * 512],
                                                   start=(k == 0), stop=(k == KC - 1))
                                return ins
                            P.op("pe", mm_g, reads=[("wa", s), "cact_rep"], writes=[("ps_g", c * 2 + h)])
                            P.op("dve", lambda e, gi=gi, c=c, h=h: e.tensor_tensor(
                                grow[c][:, h * 512:(h + 1) * 512], ps_g[c * 2 + h][:, :],
                                brow[gi % 2][:, h * 512:(h + 1) * 512], ALU.add),
                                reads=[("ps_g", c * 2 + h), ("brow", gi % 2)], writes=[("grow", c, h)])
                        if i == 4:
                            P.op("dve", lambda e, c=c: e.tensor_scalar(grow[c][0:1, :], grow[c][0:1, :], 1.0, None, ALU.add),
                                 reads=[("grow", c, 0), ("grow", c, 1)], writes=[("grow", c, 0), ("grow", c, 1)])
                        P.dma("sp", modrows[l, c, gi:gi + 1, :], grow[c][0:1, :],
                              reads=[("grow", c, 0), ("grow", c, 1)], writes=[("modrows", c, gi)])
            P.op("dve", lambda e: e.tensor_tensor(modT[:], ps_mod[:].rearrange("p (j c) -> p j c", c=2),
                                                  bc_last(bada[:], 2), ALU.add),
                 reads=[("ps_mod", i) for i in range(6)] + ["bada"], writes=["modT"])
            P.op("dve", lambda e: e.tensor_scalar(modT[:, 8:16, :], modT[:, 8:16, :], 1.0, None, ALU.add),
                 reads=["modT"], writes=["modT"])
            P.op("dve", lambda e: e.tensor_scalar(modT[:, 32:40, :], modT[:, 32:40, :], 1.0, None, ALU.add),
                 reads=["modT"], writes=["modT"])
            P.flush()
        if stop == "p0":
            return nc

        with ExitStack() as st:
            NCOL = 18 * 128 + 640
            win = alloc(st, "win", [128, KC, NCOL], BF16)
            wout = alloc(st, "wout", [128, KC, D], BF16)
            kT = alloc(st, "kT", [128, 2, T], BF16)
            V = alloc(st, "V", [128, NT, 2, 65], BF16)
            convw_s = alloc(st, "convw_s", [128, 2, 3], F32)
            wsT = alloc(st, "wsT", [128, 4, 128], BF16)
            gmb = alloc(st, "gmb", [128, 4], F32)
            esink = alloc(st, "esink", [128, 8], F32)
            lng = alloc(st, "lng", [128, D], F32)
            lnb = alloc(st, "lnb", [128, D], F32)
            g1b = [alloc(st, "g1b%d" % c, [128, D], F32) for c in range(2)]
            for c in range(2):
                P.dma("sp", g1b[c][:], modrows[l, c, 0:1, :].to_broadcast([128, D]), writes=[("g1b", c)])
            R = 5
            xres = [alloc(st, "xres%d" % i, [128, D], F32) for i in range(5)]
            hT = [alloc(st, "hT%d" % i, [128, KC, 128], BF16) for i in range(2)]
            qrot = [alloc(st, "qrot%d" % i, [128, 4, 128], BF16) for i in range(R)]
            u = [alloc(st, "u%d" % i, [128, 2, 130], F32) for i in range(R)]
            cb = [alloc(st, "cb%d" % i, [128, 2, 128], F32) for i in range(R)]
            gut = [alloc(st, "gut%d" % i, [128, 256], F32) for i in range(R)]
            vln = [alloc(st, "vln%d" % i, [128, 256], BF16) for i in range(R)]
            cosb = [alloc(st, "cosb%d" % i, [128, 128], F32) for i in range(2)]
            sinb = [alloc(st, "sinb%d" % i, [128, 128], F32) for i in range(2)]
            t1 = alloc(st, "t1", [128, 4, 128], F32)
            t2 = alloc(st, "t2", [128, 4, 128], F32)
            cxs = alloc(st, "cxs", [128, 2, 128], F32)
            gvt = alloc(st, "gvt", [128, 256], F32)
            st6 = alloc(st, "st6", [128, 6], F32)
            mv = alloc(st, "mv", [128, 2], F32)
            rstd = alloc(st, "rstd", [128, 1], F32)
            nb_ = alloc(st, "nb_", [128, 1], F32)
            PT = [alloc(st, "PT%d" % i, [128, 5, 128], BF16) for i in range(4)]
            den = alloc(st, "den", [128, 4], F32)
            mixtok = [alloc(st, "mixtok%d" % i, [128, 768], BF16) for i in range(2)]
            mixT = [alloc(st, "mixT%d" % i, [128, KC, 128], BF16) for i in range(2)]
            ct = alloc(st, "ct", [128, 2, 128], F32)
            gtmp = alloc(st, "gtmp", [128, 256], F32)
            bufAs = [alloc(st, "bufA%d" % i, [128, D], F32) for i in range(2)]
            bufB = alloc(st, "bufB", [128, D], F32)
            st12 = alloc(st, "st12", [128, 12], F32)
            mv2 = alloc(st, "mv2", [128, 2], F32)
            rstd2 = alloc(st, "rstd2", [128, 1], F32)
            nb2 = alloc(st, "nb2", [128, 1], F32)
            B = [palloc(st, "B%d" % i, [128, 512]) for i in range(7)]
            Bbf = palloc(st, "Bbf", [128, 1024], BF16)
            Bo = B[4]

            wv = w_in[l].rearrange("(k p) n -> p k n", p=128)
            P.dma("pool", win[:, :, 0:512], wv[:, :, 0:512], writes=["win"])
            for kv in range(2):
                for dup in range(2):
                    c0 = 1024 + kv * 128 + dup * 64
                    P.dma("pool", win[:, :, c0:c0 + 64], wv[:, :, 512 + kv * 64:512 + (kv + 1) * 64], writes=["win"])
            P.dma("pool", win[:, :, 1536:2304], wv[:, :, 768:1536], writes=["win"])
            P.dma("pool", win[:, :, 2304:2432], wv[:, :, 640:768], writes=["win"])
            P.dma("pool", win[:, :, 2432:2688], wv[:, :, 1792:2048], writes=["win"])
            P.dma("pool", win[:, :, 2688:2944], wv[:, :, 1536:1792], writes=["win"])
            P.dma("pool", wout[:], w_out[l].rearrange("(k p) n -> p k n", p=128), writes=["wout"])
            for (src0, dst0, nblk) in ((0, 512, 16), (1024, 1280, 8)):
                for a in range(2):
                    def swp(e, src0=src0, dst0=dst0, nblk=nblk, a=a):
                        srcv = win[:, :, src0:src0 + nblk * 32].rearrange("p k (b a f) -> p k b a f", a=2, f=16)
                        dstv = win[:, :, dst0:dst0 + nblk * 32].rearrange("p k (b a f) -> p k b a f", a=2, f=16)
                        return e.tensor_copy(dstv[:, :, :, a, :], srcv[:, :, :, 1 - a, :])
                    P.op("dve", swp, reads=["win"], writes=["win"])
            P.dma("sp", convw_s[:], convw[l], writes=["convw_s"])
            P.dma("pool", wsT[:], gm_wsT[l].rearrange("g q p -> q g p"), writes=["wsT"])
            P.dma("sp", gmb[:], gm_bsp[l], writes=["gmb"])
            P.dma("sp", esink[:], sink[l:l + 1, :].to_broadcast([128, 8]), writes=["esink"])
            P.op("act", lambda e: e.activation(out=esink[:], in_=esink[:], func=AF.Exp), reads=["esink"], writes=["esink"])
            P.dma("sp", lng[:], ln1_g[l:l + 1, :].to_broadcast([128, D]), writes=["lng"])
            P.dma("sp", lnb[:], ln1_b[l:l + 1, :].to_broadcast([128, D]), writes=["lnb"])
            P.op("pool", lambda e: e.memset(V[:, :, :, 64:65], 1.0), writes=["Vones"])

            def seq_of(t):
                return (0, NB) if t < NB else (NB, NT)

            def inproj(t, kv_only=False):
                s = t % R
                xs = t % 5
                hs = t % 2
                cs = t % 2
                col = 0 if t < NB else 1
                lo, hi = seq_of(t)
                P.dma("sp", xres[xs][:], tile_src(l, t), writes=[("xres", xs)])
                P.dma("sp", cosb[cs][:], cos_t[t], writes=[("cosb", cs)])
                P.dma("sp", sinb[cs][:], sin_t[t], writes=[("sinb", cs)])
                for hb in range(2):
                    def tp(e, hb=hb):
                        ins = None
                        for i in range(4):
                            j = hb * 4 + i
                            ins = e.transpose(B[2 + hb][:, i * 128:(i + 1) * 128], xres[xs][:, j * 128:(j + 1) * 128], ident_f[:])
                        return ins
                    P.op("pe", tp, reads=[("xres", xs), "ident_f"], writes=[("B", 2 + hb)])

                    def ev(e, hb=hb):
                        ins = None
                        for i in range(4):
                            j = hb * 4 + i
                            ins = e.activation(out=hT[hs][:, j, :], in_=B[2 + hb][:, i * 128:(i + 1) * 128], func=AF.Identity,
                                               scale=modT[:, 8 + j, col:col + 1], bias=modT[:, j, col:col + 1])
                        return ins
                    P.op("act", ev, reads=[("B", 2 + hb), "modT"], writes=[("hT", hs, hb)])
                hres = [("hT", hs, 0), ("hT", hs, 1)]

                def fm_group(bank, off, chunks):
                    def f(e):
                        ins = None
                        for i, c in enumerate(chunks):
                            for k in range(KC):
                                ins = e.matmul(B[bank][:, off + i * 128:off + (i + 1) * 128],
                                               lhsT=win[:, k, c * 128:(c + 1) * 128], rhs=hT[hs][:, k, :],
                                               start=(k == 0), stop=(k == KC - 1))
                        return ins
                    return f

                if not kv_only:
                    P.op("pe", fm_group(2, 0, [0, 1, 2, 3]), reads=hres + ["win"], writes=[("B", 2)])
                    P.op("pe", fm_group(3, 0, [4, 5, 6, 7]), reads=hres + ["win"], writes=[("B", 3)])
                    P.op("dve", lambda e: e.tensor_tensor(t1[:], B[2][:, :].rearrange("p (c t) -> p c t", c=4),
                                                          bc_mid(cosb[cs][:], 4), ALU.mult),
                         reads=[("B", 2), ("cosb", cs)], writes=["t1"])
                    P.op("dve", lambda e: e.tensor_tensor(t2[:], B[3][:, :].rearrange("p (c t) -> p c t", c=4),
                                                          bc_mid(sinb[cs][:], 4), ALU.mult),
                         reads=[("B", 3), ("sinb", cs)], writes=["t2"])
                    P.op("pool", lambda e: e.tensor_tensor(qrot[s][:], t1[:], t2[:], ALU.add),
                         reads=["t1", "t2"], writes=[("qrot", s)])
                P.op("pe", fm_group(2, 0, [8, 9, 10, 11]), reads=hres + ["win"], writes=[("B", 2)])
                P.op("dve", lambda e: e.tensor_tensor(t1[:, 0:2, :], B[2][:, 0:256].rearrange("p (c t) -> p c t", c=2),
                                                      bc_mid(cosb[cs][:], 2), ALU.mult),
                     reads=[("B", 2), ("cosb", cs)], writes=["t1"])
                P.op("dve", lambda e: e.tensor_tensor(t2[:, 0:2, :], B[2][:, 256:512].rearrange("p (c t) -> p c t", c=2),
                                                      bc_mid(sinb[cs][:], 2), ALU.mult),
                     reads=[("B", 2), ("sinb", cs)], writes=["t2"])
                P.op("pool", lambda e: e.tensor_tensor(kT[:, :, t * 128:(t + 1) * 128], t1[:, 0:2, :], t2[:, 0:2, :], ALU.add),
                     reads=["t1", "t2"], writes=[("kT", t)])
                if not kv_only:
                    P.op("pe", fm_group(3, 0, [12, 13, 14, 15]), reads=hres + ["win"], writes=[("B", 3)])
                    P.op("pe", fm_group(2, 0, [16, 17]), reads=hres + ["win"], writes=[("B", 2)])
                    P.op("act", lambda e: e.activation(out=cb[s][:], in_=B[3][:, 0:256].rearrange("p (c t) -> p c t", c=2),
                                                       func=AF.Copy), reads=[("B", 3)], writes=[("cb", s)])
                    P.op("act", lambda e: e.activation(out=cxs[:], in_=B[2][:, 0:256].rearrange("p (c t) -> p c t", c=2),
                                                       func=AF.Copy), reads=[("B", 2)], writes=["cxs"])
                    P.op("dve", lambda e: e.tensor_tensor(u[s][:, :, 1:129], B[3][:, 256:512].rearrange("p (c t) -> p c t", c=2),
                                                          cxs[:], ALU.mult),
                         reads=[("B", 3), "cxs"], writes=[("u", s)])
                    if t > lo:
                        sp_ = (t - 1) % R
                        P.op("pool", lambda e: e.tensor_copy(u[sp_][:, :, 129:130], u[s][:, :, 1:2]),
                             reads=[("u", s)], writes=[("u", sp_)])
                    else:
                        P.op("pool", lambda e: e.memset(u[s][:, :, 0:1], 0.0), reads=[("u", s)], writes=[("u", s)])
                    if t < hi - 1:
                        sn_ = (t + 1) % R
                        P.op("pool", lambda e: e.tensor_copy(u[sn_][:, :, 0:1], u[s][:, :, 128:129]),
                             reads=[("u", s)], writes=[("u", sn_)])
                    else:
                        P.op("pool", lambda e: e.memset(u[s][:, :, 129:130], 0.0), reads=[("u", s)], writes=[("u", s)])

                def tm(e):
                    ins = None
                    for k in range(KC):
                        ins = e.matmul(B[3][:, 0:384], lhsT=hT[hs][:, k, :], rhs=win[:, k, 2304:2688],
                                       start=(k == 0), stop=(k == KC - 1))
                    return ins
                P.op("pe", tm, reads=hres + ["win"], writes=[("B", 3)])
                P.op("act", lambda e: e.activation(out=V[:, t, :, 0:64], in_=B[3][:, 0:128].rearrange("p (h d) -> p h d", h=2),
                                                   func=AF.Copy), reads=[("B", 3)], writes=[("V", t)])
                if not kv_only:
                    def tm2(e):
                        ins = None
                        for k in range(KC):
                            ins = e.matmul(B[2][:, 256:512], lhsT=hT[hs][:, k, :], rhs=win[:, k, 2688:2944],
                                           start=(k == 0), stop=(k == KC - 1))
                        return ins
                    P.op("pe", tm2, reads=hres + ["win"], writes=[("B", 2)])
                    P.op("act", lambda e: e.activation(out=gvt[:], in_=B[3][:, 128:384], func=AF.Gelu_apprx_tanh),
                         reads=[("B", 3)], writes=["gvt"])
                    P.op("act", lambda e: e.activation(out=gut[s][:], in_=B[2][:, 256:512], func=AF.Gelu_apprx_tanh),
                         reads=[("B", 2)], writes=[("gut", s)])
                    P.op("dve", lambda e: e.bn_stats(st6[:], gvt[:]), reads=["gvt"], writes=["st6"])
                    P.op("dve", lambda e: e.bn_aggr(mv[:], st6[:]), reads=["st6"], writes=["mv"])
                    P.op("act", lambda e: e.activation(out=rstd[:], in_=mv[:, 1:2], func=AF.Ln, bias=epsb[:, 0:1]),
                         reads=["mv", "epsb"], writes=["rstd"])
                    P.op("act", lambda e: e.activation(out=rstd[:], in_=rstd[:], func=AF.Exp, scale=-0.5), reads=["rstd"], writes=["rstd"])
                    P.op("dve", lambda e: e.scalar_tensor_tensor(nb_[:], mv[:, 0:1], -1.0, rstd[:], ALU.mult, ALU.mult),
                         reads=["mv", "rstd"], writes=["nb_"])
                    P.op("act", lambda e: e.activation(out=vln[s][:], in_=gvt[:], func=AF.Identity,
                                                       scale=rstd[:, 0:1], bias=nb_[:, 0:1]),
                         reads=["gvt", "rstd", "nb_"], writes=[("vln", s)])

            def attn(t):
                s = t % R
                xs = t % 5
                ms = t % 2
                col = 0 if t < NB else 1
                lo, hi = seq_of(t)
                if t < NB:
                    kcs = [(NB, None), (NB + 1, None)]
                    if t > 0:
                        kcs.append((t - 1, maskl))
                    kcs.append((t, None))
                    if t < NB - 1:
                        kcs.append((t + 1, maskr))
                else:
                    kcs = [(NB, None), (NB + 1, None)]
                nk = len(kcs)
                for h in range(8):
                    qc, po, kv, pslot = h // 2, (h % 2) * 64, h // 4, h % 4

                    if h % 2 == 0:
                        sbank, s5, wr5 = B[6], B[4][:, 384:512], ("B", 4)
                        wr = [("B", 6)] + ([wr5] if nk > 4 else [])
                    else:
                        sbank, s5, wr5 = B[5], B[1][:, 384:512], ("B", 1)
                        wr = [("B", 5)] + ([wr5] if nk > 4 else [])

                    def sc(e, qc=qc, po=po, kv=kv, sbank=sbank, s5=s5):
                        ins = None
                        for i, (c, msk) in enumerate(kcs):
                            dst = sbank[:, i * 128:(i + 1) * 128] if i < 4 else s5
                            ins = e.matmul(dst, lhsT=kT[po:po + 64, kv, c * 128:(c + 1) * 128],
                                           rhs=qrot[s][po:po + 64, qc, :], start=True, stop=(msk is None))
                            if msk is not None:
                                ins = e.matmul(dst, lhsT=ident_b[:], rhs=msk[:], start=False, stop=True)
                        return ins
                    P.op("pe", sc, reads=[("kT", c) for c, _ in kcs] + [("qrot", s), "ident_b", "maskl", "maskr"], writes=wr)

                    def ex(e, pslot=pslot, sbank=sbank, s5=s5):
                        n1 = min(nk, 4)
                        ins = e.activation(out=PT[pslot][:, 0:n1, :], in_=sbank[:, 0:n1 * 128].rearrange("p (c t) -> p c t", c=n1),
                                           func=AF.Exp, scale=0.125)
                        if nk > 4:
                            ins = e.activation(out=PT[pslot][:, 4, :], in_=s5, func=AF.Exp, scale=0.125)
                        return ins
                    P.op("act", ex, reads=wr, writes=[("PT", pslot)])

                    def pv(e, kv=kv, pslot=pslot):
                        ins = None
                        for i, (c, msk) in enumerate(kcs):
                            ins = e.matmul(Bo[:, pslot * 65:(pslot + 1) * 65],
                                           lhsT=PT[pslot][:, i, :], rhs=V[:, c, kv, :],
                                           start=(i == 0), stop=(i == nk - 1))
                        return ins
                    P.op("pe", pv, reads=[("PT", pslot), "Vones"] + [("V", c) for c, _ in kcs], writes=[("B", 4)])
                    if pslot == 3:
                        hg = h // 4
                        bov = Bo[:, 0:260].rearrange("p (h d) -> p h d", d=65)
                        P.op("dve", lambda e, hg=hg, bov=bov: e.tensor_tensor(den[:], bov[:, :, 64], esink[:, hg * 4:(hg + 1) * 4], ALU.add),
                             reads=[("B", 4), "esink"], writes=["den"])
                        P.op("dve", lambda e: e.reciprocal(den[:], den[:]), reads=["den"], writes=["den"])
                        P.op("dve", lambda e, hg=hg, bov=bov: e.tensor_tensor(
                            mixtok[ms][:, hg * 256:(hg + 1) * 256].rearrange("p (h d) -> p h d", d=64),
                            bov[:, :, 0:64], bc_last(den[:], 64), ALU.mult),
                            reads=[("B", 4), "den"], writes=[("mixtok", ms, hg)])

            def post(t):
                s = t % R
                ms = t % 2
                col = 0 if t < NB else 1
                for j in range(2):
                    P.op("pool", lambda e, j=j: e.tensor_scalar(ct[:, j, :], u[s][:, j, 0:128], convw_s[:, j, 0:1], None, ALU.mult),
                         reads=[("u", s), "convw_s"], writes=[("ct", j)])
                    P.op("dve", lambda e, j=j: e.scalar_tensor_tensor(ct[:, j, :], u[s][:, j, 1:129], convw_s[:, j, 1:2], ct[:, j, :], ALU.mult, ALU.add),
                         reads=[("u", s), "convw_s", ("ct", j)], writes=[("ct", j)])
                    P.op("dve", lambda e, j=j: e.scalar_tensor_tensor(ct[:, j, :], u[s][:, j, 2:130], convw_s[:, j, 2:3], ct[:, j, :], ALU.mult, ALU.add),
                         reads=[("u", s), "convw_s", ("ct", j)], writes=[("ct", j)])
                    P.op("pool", lambda e, j=j: e.tensor_tensor(mixT[ms][:, 4 + j, :], cb[s][:, j, :], ct[:, j, :], ALU.mult),
                         reads=[("cb", s), ("ct", j)], writes=[("mixT", ms, 4 + j)])
                def gm(e):
                    ins = None
                    for g in range(4):
                        ins = e.matmul(B[1][:, g * 64:(g + 1) * 64], lhsT=wsT[:, g, :], rhs=vln[s][:, g * 64:(g + 1) * 64],
                                       start=True, stop=True)
                    return ins
                P.op("pe", gm, reads=[("vln", s), "wsT"], writes=[("B", 1)])
                P.op("dve", lambda e: e.tensor_tensor(gtmp[:].rearrange("p (g c) -> p g c", g=4),
                                                      B[1][:, 0:256].rearrange("p (g c) -> p g c", g=4),
                                                      bc_last(gmb[:], 64), ALU.add),
                     reads=[("B", 1), "gmb"], writes=["gtmp"])
                P.op("dve", lambda e: e.tensor_tensor(mixtok[ms][:, 512:768], gtmp[:], gut[s][:], ALU.mult),
                     reads=["gtmp", ("gut", s)], writes=[("mixtok", ms, 2)])
                def tpm(e):
                    ins = None
                    for i in range(6):
                        ins = e.transpose(Bbf[:, i * 128:(i + 1) * 128], mixtok[ms][:, i * 128:(i + 1) * 128], ident_b[:])
                    return ins
                P.op("pe", tpm, reads=[("mixtok", ms, i) for i in range(3)] + ["ident_b"], writes=["Bbf"])
                P.op("act", lambda e: e.activation(out=mixT[ms][:, 0:4, :], in_=Bbf[:, 0:512].rearrange("p (c t) -> p c t", c=4), func=AF.Copy),
                     reads=["Bbf"], writes=[("mixT", ms, i) for i in range(4)])
                P.op("act", lambda e: e.activation(out=mixT[ms][:, 6:8, :], in_=Bbf[:, 512:768].rearrange("p (c t) -> p c t", c=2), func=AF.Copy),
                     reads=["Bbf"], writes=[("mixT", ms, 6), ("mixT", ms, 7)])
                for hf in range(2):
                    def op_(e, hf=hf):
                        ins = None
                        for k in range(KC):
                            ins = e.matmul(B[0][:, :], lhsT=mixT[ms][:, k, :], rhs=wout[:, k, hf * 512:(hf + 1) * 512],
                                           start=(k == 0), stop=(k == KC - 1))
                        return ins
                    P.op("pe", op_, reads=[("mixT", ms, i) for i in range(8)] + ["wout"], writes=[("B", 0)])
                    P.op("dve", lambda e, hf=hf: e.tensor_tensor(bufAs[t % 2][:, hf * 512:(hf + 1) * 512], B[0][:, :],
                                                                g1b[col][:, hf * 512:(hf + 1) * 512], ALU.mult),
                         reads=[("B", 0), ("g1b", col)], writes=[("bufA", t % 2, hf)])

            def mixB(t):
                xs = t % 5
                layer_norm_tail(xres[xs], ("xres", xs), lng, lnb, x1buf[t * 128:(t + 1) * 128, :], ("x1buf", t), t % 2)

            def layer_norm_tail(xt, xres_name, g_t, b_t, dst, dst_name, ab):
                bufA = bufAs[ab]
                P.op("dve", lambda e: e.scalar_tensor_tensor(bufA[:], xt[:], ALPHA, bufA[:], ALU.mult, ALU.add),
                     reads=[xres_name, ("bufA", ab, 0), ("bufA", ab, 1)], writes=[("bufA", ab, 0), ("bufA", ab, 1)])
                P.op("dve", lambda e: e.bn_stats(st12[:, 0:6], bufA[:, 0:512]), reads=[("bufA", ab, 0)], writes=[("st12", 0)])
                P.op("dve", lambda e: e.bn_stats(st12[:, 6:12], bufA[:, 512:1024]), reads=[("bufA", ab, 1)], writes=[("st12", 1)])
                P.op("dve", lambda e: e.bn_aggr(mv2[:], st12[:]), reads=[("st12", 0), ("st12", 1)], writes=["mv2"])
                P.op("act", lambda e: e.activation(out=rstd2[:], in_=mv2[:, 1:2], func=AF.Ln, bias=epsb[:, 0:1]),
                     reads=["mv2", "epsb"], writes=["rstd2"])
                P.op("act", lambda e: e.activation(out=rstd2[:], in_=rstd2[:], func=AF.Exp, scale=-0.5), reads=["rstd2"], writes=["rstd2"])
                P.op("dve", lambda e: e.scalar_tensor_tensor(nb2[:], mv2[:, 0:1], -1.0, rstd2[:], ALU.mult, ALU.mult),
                     reads=["mv2", "rstd2"], writes=["nb2"])
                P.op("act", lambda e: e.activation(out=bufB[:], in_=bufA[:], func=AF.Identity, scale=rstd2[:, 0:1], bias=nb2[:, 0:1]),
                     reads=[("bufA", ab, 0), ("bufA", ab, 1), "rstd2", "nb2"], writes=["bufB"])
                P.op("pool", lambda e: e.tensor_tensor(bufB[:], bufB[:], g_t[:], ALU.mult), reads=["bufB", "lng"], writes=["bufB"])
                P.op("pool", lambda e: e.tensor_tensor(bufB[:], bufB[:], b_t[:], ALU.add), reads=["bufB", "lnb"], writes=["bufB"])
                P.dma("pool", dst, bufB[:], reads=["bufB"], writes=[dst_name])

            for t in range(NB, NT):
                inproj(t, kv_only=last)
            if not last:
                for t in range(NB, NT):
                    attn(t)
                    post(t)
                    mixB(t)
            inproj(0)
            if NB > 1:
                inproj(1)
            for n in range(NB + 2):
                ls = []
                if n < NB:
                    ls.append(P.record(attn, n))
                if 0 <= n - 1 < NB:
                    ls.append(P.record(post, n - 1))
                if 0 <= n - 2 < NB:
                    ls.append(P.record(mixB, n - 2))
                if n + 2 < NB:
                    ls.append(P.record(inproj, n + 2))
                P.play(*ls)
            P.flush()
        if stop == "p1":
            return nc

        tiles = list(range(NB)) if last else list(range(NT))
        nt2 = len(tiles)
        NBLK = (2 * nt2 * 128) // BS + NEXP
        AX = mybir.AxisListType.X
        w1v = w1.rearrange("l e p (a k) n -> (l e p a) (k n)", a=4)
        w3v = w3.rearrange("l e p (a k) n -> (l e p a) (k n)", a=4)
        w2v = w2.rearrange("l e p (a k) n -> (l e p a) (k n)", a=4)
        with ExitStack() as st:
            wr_s = alloc(st, "wr_s", [128, KC, 36], F32)
            wr_m = [alloc(st, "wr_m%d" % c, [128, KC, 36], F32) for c in range(2)]
            sh2rep = [alloc(st, "sh2rep%d" % c, [128, KC, 128], F32) for c in range(2)]
            br_row = alloc(st, "br_row", [1, 36], F32)
            lng = alloc(st, "lng2", [128, D], F32)
            lnb = alloc(st, "lnb2", [128, D], F32)
            scb = [alloc(st, "scb%d" % c, [128, D], F32) for c in range(2)]
            shb = [alloc(st, "shb%d" % c, [128, D], F32) for c in range(2)]
            g2b = [alloc(st, "g2b%d" % c, [128, D], F32) for c in range(2)]
            lt_b = alloc(st, "lt_b", [128, 128], BF16)
            ones_b = alloc(st, "ones_b", [128, 128], BF16)
            jvec = alloc(st, "jvec", [128, NBLK_MAX], F32)
            base1 = alloc(st, "base1", [128, 8], F32)
            base2 = alloc(st, "base2", [128, 4], F32)
            x1t = [alloc(st, "x1t%d" % i, [128, D], F32) for i in range(2)]
            x1T = alloc(st, "x1T", [128, KC, 128], F32)
            h2r = [alloc(st, "h2r%d" % i, [128, D], BF16) for i in range(2)]
            lg = alloc(st, "lg", [128, 36], F32)
            sm = alloc(st, "sm", [128, 16], F32)
            goh = alloc(st, "goh", [128, 4], F32)
            gex = alloc(st, "gex", [128, 4], F32)
            esel = alloc(st, "esel", [128, 8], F32)
            esel2 = alloc(st, "esel2", [128, 8], F32)
            eq1 = alloc(st, "eq1", [128, 8], F32)
            eq2 = alloc(st, "eq2", [128, 8], F32)
            oh0 = alloc(st, "oh0", [128, nt2, 32], F32)
            oh1 = alloc(st, "oh1", [128, nt2, 32], F32)
            cntb = alloc(st, "cntb", [128, nt2, 32], BF16)
            gsc = alloc(st, "gsc", [128, nt2, 2], F32)
            rank_s = alloc(st, "rank_s", [128, nt2, 32], F32)
            dtmp = alloc(st, "dtmp", [128, nt2, 32], F32)
            tot = alloc(st, "tot", [128, 32], F32)
            nblk = alloc(st, "nblk", [128, 32], F32)
            sc_a = alloc(st, "sc_a", [128, 32], F32)
            sc_b = alloc(st, "sc_b", [128, 32], F32)
            pstart = alloc(st, "pstart", [128, 32], F32)
            destf = alloc(st, "destf", [128, 2, nt2], F32)
            idx = alloc(st, "idx", [128, 2, nt2], mybir.dt.int32)
            bef = alloc(st, "bef", [128, NBLK_MAX], F32)
            be1 = alloc(st, "be1", [128, NBLK_MAX], F32)
            wif1 = alloc(st, "wif1", [128, NBLK_MAX, 4], F32)
            wi1 = alloc(st, "wi1", [128, NBLK_MAX, 4], mybir.dt.int32)
            w1s = [alloc(st, "w1s%d" % i, [128, KC, DEXP], BF16) for i in range(2)]
            w3s = [alloc(st, "w3s%d" % i, [128, KC, DEXP], BF16) for i in range(2)]
            w2s = [alloc(st, "w2s%d" % i, [128, 4, D], BF16) for i in range(2)]
            xsb = [alloc(st, "xsb%d" % i, [128, 4, D], BF16) for i in range(2)]
            hTb = [alloc(st, "hTb%d" % i, [128, KC, 512], BF16) for i in range(2)]
            sg = [alloc(st, "sg%d" % i, [128, 512], F32) for i in range(2)]
            actT = [alloc(st, "actT%d" % i, [128, 4, 512], BF16) for i in range(2)]
            ysb = [alloc(st, "ysb%d" % i, [128, D], F32) for i in range(2)]
            yg = ysb
            bufA = alloc(st, "bufA2", [128, D], F32)
            bufB = alloc(st, "bufB2", [128, D], F32)
            st12 = alloc(st, "st12b", [128, 12], F32)
            mv2 = alloc(st, "mv2b", [128, 2], F32)
            rstd2 = alloc(st, "rstd2b", [128, 1], F32)
            nb2 = alloc(st, "nb2b", [128, 1], F32)
            B = [None, None] + [palloc(st, "M%d" % i, [128, 512]) for i in range(2, 8)]
            TB = [palloc(st, "TB%d" % i, [128, 1024], BF16) for i in range(2)]

            P.dma("sp", wr_s[:], w_r[l].rearrange("(k p) n -> p k n", p=128), writes=["wr_s"])
            P.dma("sp", br_row[:], b_r[l:l + 1, :], writes=["br_row"])
            P.dma("sp", lng[:], ln2_g[l:l + 1, :].to_broadcast([128, D]), writes=["lng"])
            P.dma("sp", lnb[:], ln2_b[l:l + 1, :].to_broadcast([128, D]), writes=["lnb"])
            for c in range(2):
                P.dma("sp", shb[c][:], modrows[l, c, 1:2, :].to_broadcast([128, D]), writes=[("shb", c)])
                P.dma("sp", scb[c][:], modrows[l, c, 2:3, :].to_broadcast([128, D]), writes=[("scb", c)])
                P.dma("sp", g2b[c][:], modrows[l, c, 3:4, :].to_broadcast([128, D]), writes=[("g2b", c)])
            P.dma("pool", lt_b[:], lt_d[:, :], writes=["lt_b"])
            P.dma("sp", jvec[:], jvec_d[:, :], writes=["jvec"])
            P.dma("sp", base1[:], base1_d[:, :], writes=["base1"])
            P.dma("sp", base2[:], base2_d[:, :], writes=["base2"])
            P.op("pool", lambda e: e.memset(ones_b[:], 1.0), writes=["ones_b"])
            P.op("pool", lambda e: e.memset(xsb[1][:], 0.0), writes=[("xsb", 1)])
            zres = []
            for i in range(NBLK):
                P.dma("pool", xs_dram[i * BS:(i + 1) * BS, :].rearrange("(a p) d -> p a d", p=128), xsb[1][:],
                      reads=[("xsb", 1)], writes=[("xsz", i)])
                zres.append(("xsz", i))
            for c in range(2):
                P.op("dve", lambda e, c=c: e.tensor_tensor(wr_m[c][:], wr_s[:], bc_last(modT[:, 32:40, c], 36), ALU.mult),
                     reads=["wr_s", "modT"], writes=[("wr_m", c)])
                P.op("dve", lambda e, c=c: e.tensor_copy(sh2rep[c][:], bc_last(modT[:, 24:32, c], 128)),
                     reads=["modT"], writes=[("sh2rep", c)])

            st12s = [st12, alloc(st, "st12c", [128, 12], F32)]
            mv2s = [mv2, alloc(st, "mv2c", [128, 2], F32)]
            rstd2s = [rstd2, alloc(st, "rstd2c", [128, 1], F32)]
            nb2s = [nb2, alloc(st, "nb2c", [128, 1], F32)]

            def ln_tail2(xt, xname, dst, dname, bA, bB, se):
                nA, nB = ("bufA", se), ("bufB", se)
                s12, m2, r2, n2 = st12s[se], mv2s[se], rstd2s[se], nb2s[se]
                P.op("dve", lambda e: e.scalar_tensor_tensor(bA[:], xt[:], ALPHA, bA[:], ALU.mult, ALU.add),
                     reads=[xname, nA], writes=[nA])
                P.op("dve", lambda e: e.bn_stats(s12[:, 0:6], bA[:, 0:512]), reads=[nA], writes=[("st12", se, 0)])
                P.op("dve", lambda e: e.bn_stats(s12[:, 6:12], bA[:, 512:1024]), reads=[nA], writes=[("st12", se, 1)])
                P.op("dve", lambda e: e.bn_aggr(m2[:], s12[:]), reads=[("st12", se, 0), ("st12", se, 1)], writes=[("mv2", se)])
                P.op("act", lambda e: e.activation(out=r2[:], in_=m2[:, 1:2], func=AF.Sqrt, bias=LN_EPS),
                     reads=[("mv2", se)], writes=[("rstd2", se)])
                P.op("dve", lambda e: e.reciprocal(r2[:], r2[:]), reads=[("rstd2", se)], writes=[("rstd2", se)])
                P.op("dve", lambda e: e.scalar_tensor_tensor(n2[:], m2[:, 0:1], -1.0, r2[:], ALU.mult, ALU.mult),
                     reads=[("mv2", se), ("rstd2", se)], writes=[("nb2", se)])
                P.op("act", lambda e: e.activation(out=bB[:], in_=bA[:], func=AF.Identity, scale=r2[:, 0:1], bias=n2[:, 0:1]),
                     reads=[nA, ("rstd2", se), ("nb2", se)], writes=[nB])
                P.op("pool", lambda e: e.tensor_tensor(bB[:], bB[:], lng[:], ALU.mult), reads=[nB, "lng"], writes=[nB])
                P.op("pool", lambda e: e.tensor_tensor(bB[:], bB[:], lnb[:], ALU.add), reads=[nB, "lnb"], writes=[nB])
                P.dma("pool", dst, bB[:], reads=[nB], writes=[dname])

            def v1024(tn, nm):
                if nt2 * 32 >= 1024:
                    return tn[:].rearrange("p a b -> p (a b)")[:, 0:1024]
                return alloc(st, nm, [128, D], F32)[:]
            rank_v, dtmp_v, oh0_v = v1024(rank_s, "rank_v"), v1024(dtmp, "dtmp_v"), v1024(oh0, "oh0_v")
            x1Ts = [x1T, rank_v.rearrange("p (k t) -> p k t", k=KC)]
            bufAs = [bufA, dtmp_v]
            lgs = [lg, alloc(st, "lg_b", [128, 36], F32)]
            sms = [sm, alloc(st, "sm_b", [128, 16], F32)]
            gohs = [goh, alloc(st, "goh_b", [128, 4], F32)]
            gexs = [gex, alloc(st, "gex_b", [128, 4], F32)]
            esels = [esel, alloc(st, "esel_b", [128, 8], F32)]
            esel2s = [esel2, alloc(st, "esel2_b", [128, 8], F32)]
            eq1s = [eq1, alloc(st, "eq1_b", [128, 8], F32)]
            eq2s = [eq2, alloc(st, "eq2_b", [128, 8], F32)]

            def stageA(li):
                t = tiles[li]
                col = 0 if t < NB else 1
                xs = li % 2
                se = li % 2
                tpb = 6 if se == 0 else 4
                rtb = 2 if se == 0 else 3
                P.dma("sp", x1t[xs][:], x1buf[t * 128:(t + 1) * 128, :], writes=[("x1t", xs)])
                P.op("dve", lambda e, xs=xs, col=col: e.tensor_tensor(bufAs[se][:], x1t[xs][:], scb[col][:], ALU.mult),
                     reads=[("x1t", xs), ("scb", col)], writes=[("bufA", se)])
                P.op("pool", lambda e, xs=xs, col=col: e.tensor_tensor(h2r[xs][:], bufAs[se][:], shb[col][:], ALU.add),
                     reads=[("bufA", se), ("shb", col)], writes=[("h2r", xs)])
                P.dma("sp", h2buf[li * 128:(li + 1) * 128, :], h2r[xs][:], reads=[("h2r", xs)], writes=[("h2buf", li)])
                for hb in range(2):
                    def tp(e, hb=hb, xs=xs):
                        ins = None
                        for i in range(4):
                            j = hb * 4 + i
                            ins = e.transpose(B[tpb + hb][:, i * 128:(i + 1) * 128], x1t[xs][:, j * 128:(j + 1) * 128], ident_f[:])
                        return ins
                    P.op("pe", tp, reads=[("x1t", xs), "ident_f"], writes=[("B", tpb + hb)])

                    def cpx(e, hb=hb):
                        ins = None
                        for i in range(4):
                            ins = e.activation(out=x1Ts[se][:, hb * 4 + i, :], in_=B[tpb + hb][:, i * 128:(i + 1) * 128], func=AF.Copy)
                        return ins
                    P.op("act", cpx, reads=[("B", tpb + hb)], writes=[("x1T", se, hb)])

                def rt(e, col=col):
                    ins = None
                    for k in range(KC):
                        ins = e.matmul(B[rtb][:, 0:36], lhsT=x1Ts[se][:, k, :], rhs=wr_m[col][:, k, :], start=(k == 0), stop=False)
                    for k in range(KC):
                        ins = e.matmul(B[rtb][:, 0:36], lhsT=sh2rep[col][:, k, :], rhs=wr_s[:, k, :], start=False, stop=False)
                    ins = e.matmul(B[rtb][:, 0:36], lhsT=ones_f[0:1, :], rhs=br_row[0:1, :], start=False, stop=True)
                    return ins
                P.op("pe", rt, reads=[("x1T", se, 0), ("x1T", se, 1), ("wr_m", col), ("sh2rep", col), "wr_s", "br_row", "ones_f"], writes=[("B", rtb)])
                P.op("act", lambda e: e.activation(out=lgs[se][:], in_=B[rtb][:, 0:36], func=AF.Copy), reads=[("B", rtb)], writes=[("lg", se)])
                P.op("dve", lambda e: e.reduce_max(sms[se][:, 0:1], lgs[se][:, 0:4], AX), reads=[("lg", se)], writes=[("sm", se)])
                P.op("dve", lambda e: e.tensor_scalar(sms[se][:, 1:2], sms[se][:, 0:1], -1.0, None, ALU.mult), reads=[("sm", se)], writes=[("sm", se)])
                P.op("act", lambda e: e.activation(out=gexs[se][:], in_=lgs[se][:, 0:4], func=AF.Exp, bias=sms[se][:, 1:2], accum_out=sms[se][:, 2:3]),
                     reads=[("lg", se), ("sm", se)], writes=[("gex", se), ("sm", se)])
                P.op("dve", lambda e: e.reciprocal(sms[se][:, 3:4], sms[se][:, 2:3]), reads=[("sm", se)], writes=[("sm", se)])
                P.op("dve", lambda e: e.tensor_scalar(gohs[se][:], lgs[se][:, 0:4], sms[se][:, 0:1], None, ALU.is_equal), reads=[("lg", se), ("sm", se)], writes=[("goh", se)])
                P.op("dve", lambda e: e.tensor_scalar(esels[se][:], lgs[se][:, 4:12], gohs[se][:, 0:1], None, ALU.mult), reads=[("lg", se), ("goh", se)], writes=[("esel", se)])
                for g in range(1, 4):
                    P.op("dve", lambda e, g=g: e.scalar_tensor_tensor(esels[se][:], lgs[se][:, 4 + g * 8:12 + g * 8], gohs[se][:, g:g + 1], esels[se][:], ALU.mult, ALU.add),
                         reads=[("lg", se), ("goh", se), ("esel", se)], writes=[("esel", se)])
                P.op("dve", lambda e: e.reduce_max(sms[se][:, 4:5], esels[se][:], AX), reads=[("esel", se)], writes=[("sm", se)])
                P.op("dve", lambda e: e.tensor_scalar(eq1s[se][:], esels[se][:], sms[se][:, 4:5], None, ALU.is_equal), reads=[("esel", se), ("sm", se)], writes=[("eq1", se)])
                P.op("dve", lambda e: e.scalar_tensor_tensor(esel2s[se][:], eq1s[se][:], -1e30, esels[se][:], ALU.mult, ALU.add), reads=[("eq1", se), ("esel", se)], writes=[("esel2", se)])
                P.op("dve", lambda e: e.reduce_max(sms[se][:, 5:6], esel2s[se][:], AX), reads=[("esel2", se)], writes=[("sm", se)])
                P.op("dve", lambda e: e.tensor_scalar(eq2s[se][:], esel2s[se][:], sms[se][:, 5:6], None, ALU.is_equal), reads=[("esel2", se), ("sm", se)], writes=[("eq2", se)])
                P.op("dve", lambda e: e.tensor_tensor(sms[se][:, 6:7], sms[se][:, 4:5], sms[se][:, 5:6], ALU.subtract), reads=[("sm", se)], writes=[("sm", se)])
                P.op("act", lambda e: e.activation(out=sms[se][:, 7:8], in_=sms[se][:, 6:7], func=AF.Sigmoid), reads=[("sm", se)], writes=[("sm", se)])
                P.op("act", lambda e: e.activation(out=sms[se][:, 8:9], in_=sms[se][:, 6:7], func=AF.Sigmoid, scale=-1.0), reads=[("sm", se)], writes=[("sm", se)])
                P.op("dve", lambda e, li=li: e.tensor_scalar(gsc[:, li, :], sms[se][:, 7:9], sms[se][:, 3:4], None, ALU.mult), reads=[("sm", se)], writes=[("gsc", li)])
                P.op("dve", lambda e, li=li: e.tensor_tensor(oh0[:, li, :].rearrange("p (g x) -> p g x", g=4), bc_last(gohs[se][:], 8), bc_mid(eq1s[se][:], 4), ALU.mult),
                     reads=[("goh", se), ("eq1", se)], writes=[("oh0", li)])
                P.op("dve", lambda e, li=li: e.tensor_tensor(oh1[:, li, :].rearrange("p (g x) -> p g x", g=4), bc_last(gohs[se][:], 8), bc_mid(eq2s[se][:], 4), ALU.mult),
                     reads=[("goh", se), ("eq2", se)], writes=[("oh1", li)])
                P.op("pool", lambda e, li=li: e.tensor_tensor(cntb[:, li, :], oh0[:, li, :], oh1[:, li, :], ALU.add),
                     reads=[("oh0", li), ("oh1", li)], writes=[("cntb", li)])


            for li0 in range(0, nt2, 2):
                P.play(P.record(stageA, li0), P.record(stageA, li0 + 1) if li0 + 1 < nt2 else [])
            if stop == "p2a":
                P.flush()
                return nc
            call = [("cntb", i) for i in range(nt2)]
            nbank = (nt2 + 15) // 16
            for li in range(nt2):
                bk, off = 3 + li // 16, (li % 16) * 32

                def rk(e, li=li, bk=bk, off=off):
                    ins = None
                    for tp_ in range(li):
                        ins = e.matmul(B[bk][:, off:off + 32], lhsT=ones_b[:], rhs=cntb[:, tp_, :], start=(tp_ == 0), stop=False)
                    ins = e.matmul(B[bk][:, off:off + 32], lhsT=lt_b[:], rhs=cntb[:, li, :], start=(li == 0), stop=True)
                    return ins
                P.op("pe", rk, reads=call + ["ones_b", "lt_b"], writes=[("RK", li)])
            for bk in range(nbank):
                n_here = min(16, nt2 - bk * 16)
                P.op("act", lambda e, bk=bk, n_here=n_here: e.activation(
                    out=rank_s[:, bk * 16:bk * 16 + n_here, :], in_=B[3 + bk][:, 0:n_here * 32].rearrange("p (t x) -> p t x", x=32), func=AF.Copy),
                    reads=[("RK", i) for i in range(bk * 16, bk * 16 + n_here)], writes=[("rank_s", bk)])

            def tt(e):
                ins = None
                for tp_ in range(nt2):
                    ins = e.matmul(B[2][:, 64:96], lhsT=ones_b[:], rhs=cntb[:, tp_, :], start=(tp_ == 0), stop=(tp_ == nt2 - 1))
                return ins
            P.op("pe", tt, reads=call + ["ones_b"], writes=[("B", 2)])
            P.op("act", lambda e: e.activation(out=tot[:], in_=B[2][:, 64:96], func=AF.Copy), reads=[("B", 2)], writes=["tot"])
            P.op("dve", lambda e: e.tensor_scalar(nblk[:], tot[:], 0.0, None, ALU.is_gt), reads=["tot"], writes=["nblk"])
            for m in range(1, (2 * nt2 * 128) // BS + 1):
                P.op("dve", lambda e, m=m: e.scalar_tensor_tensor(nblk[:], tot[:], float(m * BS), nblk[:], ALU.is_gt, ALU.add),
                     reads=["tot", "nblk"], writes=["nblk"])
            P.op("dve", lambda e: e.tensor_copy(sc_a[:], nblk[:]), reads=["nblk"], writes=["sc_a"])
            cur, oth, cn, on = sc_a, sc_b, "sc_a", "sc_b"
            for dd in (1, 2, 4, 8, 16):
                P.op("dve", lambda e, cur=cur, oth=oth, dd=dd: e.tensor_copy(oth[:, 0:dd], cur[:, 0:dd]), reads=[cn], writes=[on])
                P.op("dve", lambda e, cur=cur, oth=oth, dd=dd: e.tensor_tensor(oth[:, dd:32], cur[:, dd:32], cur[:, 0:32 - dd], ALU.add),
                     reads=[cn, on], writes=[on])
                cur, oth, cn, on = oth, cur, on, cn
            pend, pendn = cur, cn
            P.op("dve", lambda e: e.tensor_tensor(pstart[:], pend[:], nblk[:], ALU.subtract), reads=[pendn, "nblk"], writes=["pstart"])
            P.op("dve", lambda e: e.tensor_scalar(pstart[:], pstart[:], float(BS), None, ALU.mult), reads=["pstart"], writes=["pstart"])
            rk_all = [("rank_s", i) for i in range(nbank)]
            P.op("dve", lambda e: e.tensor_tensor(rank_s[:], rank_s[:], bc_mid(pstart[:], nt2), ALU.add),
                 reads=rk_all + ["pstart"], writes=rk_all)
            for k, oh in enumerate((oh0, oh1)):
                P.op("dve", lambda e, oh=oh: e.tensor_tensor(dtmp[:], oh[:], rank_s[:], ALU.mult),
                     reads=rk_all + [("oh%d" % k, i) for i in range(nt2)], writes=["dtmp"])
                P.op("dve", lambda e, k=k: e.reduce_sum(destf[:, k, :], dtmp[:], AX), reads=["dtmp"], writes=[("destf", k)])
            P.op("dve", lambda e: e.tensor_copy(idx[:], destf[:]), reads=[("destf", 0), ("destf", 1)], writes=["idx"])
            P.op("dve", lambda e: e.tensor_scalar(bef[:], jvec[:], pend[:, 0:1], None, ALU.is_ge), reads=["jvec", pendn], writes=["bef"])
            for ee in range(1, 32):
                P.op("dve", lambda e, ee=ee: e.scalar_tensor_tensor(bef[:], jvec[:], pend[:, ee:ee + 1], bef[:], ALU.is_ge, ALU.add),
                     reads=["jvec", pendn, "bef"], writes=["bef"])
            P.op("dve", lambda e: e.tensor_scalar(bef[:], bef[:], 31.0, None, ALU.min), reads=["bef"], writes=["bef"])
            P.op("dve", lambda e: e.tensor_scalar(be1[:], bef[:], 128.0, float(l * NEXP * 128), ALU.mult, ALU.add), reads=["bef"], writes=["be1"])
            P.op("dve", lambda e: e.tensor_scalar(be1[:], be1[:], base1[:, 0:1], None, ALU.add), reads=["be1", "base1"], writes=["be1"])
            P.op("dve", lambda e: e.tensor_scalar(be1[:], be1[:], 4.0, None, ALU.mult), reads=["be1"], writes=["be1"])
            P.op("dve", lambda e: e.tensor_tensor(wif1[:], bc_last(be1[:], 4), bc_mid(base2[:], NBLK_MAX), ALU.add),
                 reads=["be1", "base2"], writes=["wif1"])
            P.op("dve", lambda e: e.tensor_copy(wi1[:], wif1[:]), reads=["wif1"], writes=["wi1"])
            if stop == "p2c":
                P.flush()
                return nc
            sres = []
            for li in range(nt2):
                hs_ = li % 2
                P.dma("sp", h2r[hs_][:], h2buf[li * 128:(li + 1) * 128, :], reads=[("h2buf", li)], writes=[("h2r", hs_)])
                for k in range(2):
                    P.idma(xs_dram[0:NBLK * BS, :], bass.IndirectOffsetOnAxis(ap=idx[:, k, li:li + 1], axis=0), h2r[hs_][:], None,
                           reads=[("h2r", hs_), "idx"] + zres, writes=[("xss", li, k)])
                    sres.append(("xss", li, k))

            if stop == "p2d":
                P.flush()
                return nc
            yres = []

            def blockA(j):
                ws = j % 2
                if not DBG.get("no_wgather"):
                    for a in range(4):
                        io = bass.IndirectOffsetOnAxis(ap=wi1[:, j, a:a + 1], axis=0)
                        P.idma(w1s[ws][:, 2 * a:2 * a + 2, :].rearrange("p k n -> p (k n)"), None, w1v, io, reads=["wi1"], writes=[("w1s", ws, a)])
                        P.idma(w3s[ws][:, 2 * a:2 * a + 2, :].rearrange("p k n -> p (k n)"), None, w3v, io, reads=["wi1"], writes=[("w3s", ws, a)])
                        P.idma(w2s[ws][:, a, :], None, w2v, io, reads=["wi1"], writes=[("w2s", ws, a)])
                P.dma("sp", xsb[ws][:], xs_dram[j * BS:(j + 1) * BS, :].rearrange("(a p) d -> p a d", p=128),
                      reads=sres + zres, writes=[("xsb", ws)])
                for a in range(4):
                    tb = a % 2

                    def tps_(e, a=a, tb=tb, ws=ws):
                        ins = None
                        for k in range(KC):
                            ins = e.transpose(TB[tb][:, k * 128:(k + 1) * 128], xsb[ws][:, a, k * 128:(k + 1) * 128], ident_b[:])
                        return ins
                    P.op("pe", tps_, reads=[("xsb", ws), "ident_b"], writes=[("TB", tb)])
                    P.op("act", lambda e, a=a, tb=tb, ws=ws: e.activation(out=hTb[ws][:, :, a * 128:(a + 1) * 128],
                                                                         in_=TB[tb][:, :].rearrange("p (k t) -> p k t", k=KC), func=AF.Copy),
                         reads=[("TB", tb)], writes=[("hTb", ws, a)])

            def blockB(j):
                ws = j % 2
                hres = [("hTb", ws, a) for a in range(4)]
                w13 = [("w1s", ws, a) for a in range(4)] + [("w3s", ws, a) for a in range(4)]
                for c in range(4):
                    pb = 2 + 2 * (c % 2)

                    def h13(e, c=c, pb=pb, ws=ws):
                        ins = None
                        for k in range(KC):
                            ins = e.matmul(B[pb][:, :], lhsT=w1s[ws][:, k, c * 128:(c + 1) * 128], rhs=hTb[ws][:, k, :],
                                           start=(k == 0), stop=(k == KC - 1))
                        for k in range(KC):
                            ins = e.matmul(B[pb + 1][:, :], lhsT=w3s[ws][:, k, c * 128:(c + 1) * 128], rhs=hTb[ws][:, k, :],
                                           start=(k == 0), stop=(k == KC - 1))
                        return ins
                    P.op("pe", h13, reads=w13 + hres, writes=[("B", pb), ("B", pb + 1)])
                    P.op("act", lambda e, c=c, pb=pb: e.activation(out=sg[c % 2][:], in_=B[pb][:, :], func=AF.Silu),
                         reads=[("B", pb)], writes=[("sg", c % 2)])
                    P.op("dve", lambda e, c=c, pb=pb, ws=ws: e.tensor_tensor(actT[ws][:, c, :], sg[c % 2][:], B[pb + 1][:, :], ALU.mult),
                         reads=[("sg", c % 2), ("B", pb + 1)], writes=[("actT", ws, c)])
                for a in range(4):
                    ys_ = a % 2
                    for hf in range(2):
                        yb = 6 + hf

                        def ymm(e, a=a, hf=hf, yb=yb, ws=ws):
                            ins = None
                            for c in range(4):
                                ins = e.matmul(B[yb][:, :], lhsT=actT[ws][:, c, a * 128:(a + 1) * 128],
                                               rhs=w2s[ws][:, c, hf * 512:(hf + 1) * 512], start=(c == 0), stop=(c == 3))
                            return ins
                        P.op("pe", ymm, reads=[("actT", ws, c) for c in range(4)] + [("w2s", ws, c) for c in range(4)], writes=[("B", yb)])
                        P.op("dve", lambda e, hf=hf, yb=yb, ys_=ys_: e.tensor_copy(ysb[ys_][:, hf * 512:(hf + 1) * 512], B[yb][:, :]),
                             reads=[("B", yb)], writes=[("ysb", ys_, hf)])
                    r0 = j * BS + a * 128
                    P.dma("pool", ys_dram[r0:r0 + 128, :], ysb[ys_][:], reads=[("ysb", ys_, 0), ("ysb", ys_, 1)], writes=[("ys", j, a)])
                    yres.append(("ys", j, a))


            P.play(P.record(blockA, 0))
            for j in range(NBLK):
                P.play(P.record(blockB, j), P.record(blockA, j + 1) if j + 1 < NBLK else [])
            if stop == "p2e":
                P.flush()
                return nc
            fA = [bufA, x1T[:].rearrange("p a b -> p (a b)")]
            fB = [bufB, dtmp_v]
            fY = [[ysb[0], ysb[1]], [rank_v, oh0_v]]

            def stageF(li):
                t = tiles[li]
                col = 0 if t < NB else 1
                xs = li % 2
                se = li % 2
                bA, bB, ygs = fA[se], fB[se], fY[se]
                nA = ("bufA", se)
                P.dma("sp", x1t[xs][:], x1buf[t * 128:(t + 1) * 128, :], writes=[("x1t", xs)])
                for k in range(2):
                    P.idma(ygs[k][:], None, ys_dram[0:NBLK * BS, :], bass.IndirectOffsetOnAxis(ap=idx[:, k, li:li + 1], axis=0),
                           reads=["idx"] + yres, writes=[("yg", se, k)])
                P.op("dve", lambda e: e.tensor_scalar(bA[:], ygs[0][:], gsc[:, li, 0:1], None, ALU.mult),
                     reads=[("yg", se, 0), ("gsc", li)], writes=[nA])
                P.op("dve", lambda e: e.scalar_tensor_tensor(bA[:], ygs[1][:], gsc[:, li, 1:2], bA[:], ALU.mult, ALU.add),
                     reads=[("yg", se, 1), ("gsc", li), nA], writes=[nA])
                P.op("pool", lambda e: e.tensor_tensor(bA[:], bA[:], g2b[col][:], ALU.mult),
                     reads=[nA, ("g2b", col)], writes=[nA])
                if last:
                    dst, dname = out_d[t * 128:(t + 1) * 128, :], ("out", t)
                else:
                    dst, dname = x2buf[t * 128:(t + 1) * 128, :], ("x2buf", t)
                ln_tail2(x1t[xs], ("x1t", xs), dst, dname, bA, bB, se)

            for li0 in range(0, nt2, 2):
                P.play(P.record(stageF, li0), P.record(stageF, li0 + 1) if li0 + 1 < nt2 else [])
            P.flush()
    glob.close()
    return nc


def _rope_tables(NB):
    NT = NB + NCB
    S = NB * 128
    m = 16
    freqs = (10000.0 ** (-np.arange(m, dtype=np.float32) / m)).astype(np.float32)
    tpos = np.arange(S)
    row = (tpos // GRID_W).astype(np.float32)
    colp = (tpos % GRID_W).astype(np.float32)
    cos = np.ones((128, NT * 128), np.float32)
    sin = np.zeros((128, NT * 128), np.float32)
    for p in range(128):
        d = p % 64
        pos = row if d < 32 else colp
        dd = d % 32
        f = freqs[dd % 16]
        ang = (pos * f).astype(np.float32)
        cos[p, :S] = np.cos(ang)
        sgn = -1.0 if dd < 16 else 1.0
        sin[p, :S] = sgn * np.sin(ang)
    cos_t = np.ascontiguousarray(cos.reshape(128, NT, 128).transpose(1, 0, 2))
    sin_t = np.ascontiguousarray(sin.reshape(128, NT, 128).transpose(1, 0, 2))
    return cos_t, sin_t


def make_in_maps(inputs, NB, ncores):
    f = lambda a: np.ascontiguousarray(np.asarray(a, dtype=np.float32))
    x = f(inputs["x"]); c = f(inputs["c"]); ctx = f(inputs["ctx"]); c_ctx = f(inputs["c_ctx"])
    depth = inputs["w_ada"].shape[0]
    cos_t, sin_t = _rope_tables(NB)
    j = np.arange(128)
    maskl = np.where(j[:, None] >= j[None, :], 0.0, NEG).astype(np.float32)
    maskr = np.where(j[:, None] <= j[None, :], 0.0, NEG).astype(np.float32)
    shared = {
        "w_ada": f(inputs["w_ada"]),
        "bada_p": f(np.asarray(inputs["b_ada"]).reshape(depth, 48, 128).transpose(0, 2, 1)),
        "b_ada": f(inputs["b_ada"]),
        "w_in": f(inputs["w_in"]),
        "convw": f(np.asarray(inputs["conv_w"]).reshape(depth, 3, 2, 128).transpose(0, 3, 2, 1)),
        "sink": f(inputs["attn_sink"]),
        "gm_wsT": f(np.asarray(inputs["gm_ws"]).transpose(0, 1, 3, 2)),
        "gm_bsp": f(np.asarray(inputs["gm_bs"]).transpose(0, 2, 1)),
        "w_out": f(inputs["w_out"]),
        "ln1_g": f(inputs["ln1_g"]), "ln1_b": f(inputs["ln1_b"]),
        "ln2_g": f(inputs["ln2_g"]), "ln2_b": f(inputs["ln2_b"]),
        "w_r": f(np.concatenate([np.asarray(inputs["w_rg"]), np.asarray(inputs["w_re"])], axis=-1)),
        "b_r": f(np.concatenate([np.asarray(inputs["b_rg"]), np.asarray(inputs["b_re"])], axis=-1)),
        "w1": f(np.asarray(inputs["w1"]).reshape(depth, NEXP, KC, 128, DEXP).transpose(0, 1, 3, 2, 4)),
        "w3": f(np.asarray(inputs["w3"]).reshape(depth, NEXP, KC, 128, DEXP).transpose(0, 1, 3, 2, 4)),
        "w2": f(np.asarray(inputs["w2"]).reshape(depth, NEXP, 4, 128, D).transpose(0, 1, 3, 2, 4)),
        "cos_t": cos_t, "sin_t": sin_t,
        "ident": np.eye(128, dtype=np.float32), "maskl": maskl, "maskr": maskr,
        "lt_d": (j[:, None] < j[None, :]).astype(np.float32),
        "jvec_d": np.tile(np.arange((2 * (NB + NCB) * 128) // 512 + NEXP, dtype=np.float32)[None, :], (128, 1)),
        "base1_d": (np.arange(8, dtype=np.float32)[None, :] * 128 + j[:, None]).astype(np.float32),
        "base2_d": np.tile(np.arange(4, dtype=np.float32)[None, :], (128, 1)),
    }
    maps = []
    for b in range(ncores):
        cv = np.stack([c[b].reshape(KC, 128).T, c_ctx.reshape(KC, 128).T], axis=-1)
        m = dict(shared)
        m["xin"] = np.ascontiguousarray(x[b])
        m["ctxin"] = np.ascontiguousarray(ctx[b])
        m["cvec"] = np.ascontiguousarray(cv.astype(np.float32))
        maps.append(m)
    return maps


_NC_CACHE = {}


def kernel(**inputs):
    x = np.asarray(inputs["x"])
    Bn, S, _ = x.shape
    NB = S // 128
    key = (NB,)
    if key not in _NC_CACHE:
        _NC_CACHE[key] = build_program(NB)
    nc = _NC_CACHE[key]
    maps = make_in_maps(inputs, NB, Bn)
    res = run_bass_kernel_spmd(nc, maps, core_ids=list(range(Bn)))
    return np.stack([np.asarray(r["out"], dtype=np.float32) for r in res.results], axis=0)
```
